# Optimizing a Trainium2 kernel written in Bass

```python
import math
import jax, jax.numpy as jnp
from jax import lax
import numpy as np

D_MODEL = 1024
BATCH = 4
SEQ = 8192
DEPTH = 1

N_HEADS = 8
N_KV_HEADS = 2
HEAD_DIM = 64
ATTN_WIDTH = N_HEADS * HEAD_DIM
KV_WIDTH = N_KV_HEADS * HEAD_DIM
WINDOW = 128
BLOCK_Q = 128
HYENA_WIDTH = 512
HYENA_ORDER = 2
SHORT_CONV = 3
FILTER_EMB = 33
FILTER_BANDS = (FILTER_EMB - 1) // 2
FILTER_HIDDEN = 64
WINDOW_SHIFT = 0.05
FAST_DECAY_PCT = 0.3
SLOW_DECAY_PCT = 1.5
DECAY_TARGET = 1e-2
N_FILTERS = HYENA_ORDER * 2 * HYENA_WIDTH
N_GROUPS = 8
EXPERTS_PER_GROUP = 8
N_EXPERTS = N_GROUPS * EXPERTS_PER_GROUP
TOP_K = 2
D_EXPERT = 512
MOE_BLOCK = 256
LN_EPS = 1e-5
DN_ALPHA = (2.0 * DEPTH) ** 0.25
DN_BETA = (8.0 * DEPTH) ** -0.25
IN_WIDTH = ATTN_WIDTH + 2 * KV_WIDTH + (HYENA_ORDER + 1) * HYENA_WIDTH + 2 * D_MODEL

kernel_name = "hybrid_hyena_swa_hmoe_deepnorm_encoder"


def layer_norm(x, g, b):
    xf = x.astype(jnp.float32)
    mu = jnp.mean(xf, axis=-1, keepdims=True)
    var = jnp.mean(jnp.square(xf - mu), axis=-1, keepdims=True)
    y = (xf - mu) * lax.rsqrt(var + LN_EPS) * g.astype(jnp.float32) + b.astype(jnp.float32)
    return y.astype(x.dtype)


def windowed_attention(q, k, v, sink):
    B, S = q.shape[0], q.shape[1]
    nb = S // BLOCK_Q
    G = N_HEADS // N_KV_HEADS
    qb = q.reshape(B, nb, BLOCK_Q, N_KV_HEADS, G, HEAD_DIM)
    k = k.reshape(B, S, N_KV_HEADS, HEAD_DIM)
    v = v.reshape(B, S, N_KV_HEADS, HEAD_DIM)

    def band(t):
        tp = jnp.pad(t, ((0, 0), (BLOCK_Q, BLOCK_Q), (0, 0), (0, 0)))
        tp = tp.reshape(B, nb + 2, BLOCK_Q, N_KV_HEADS, HEAD_DIM)
        return jnp.concatenate([tp[:, :-2], tp[:, 1:-1], tp[:, 2:]], axis=2)

    kw, vw = band(k), band(v)
    s = jnp.einsum('bnqkgd,bnjkd->bnkgqj', qb, kw).astype(jnp.float32) * (HEAD_DIM ** -0.5)
    a = jnp.arange(BLOCK_Q)[:, None]
    j = jnp.arange(3 * BLOCK_Q)[None, :]
    rel = j - BLOCK_Q - a
    kpos = jnp.arange(nb)[:, None, None] * BLOCK_Q - BLOCK_Q + j[None]
    valid = (jnp.abs(rel) <= WINDOW)[None] & (kpos >= 0) & (kpos < S)
    slopes = (2.0 ** (-8.0 * jnp.arange(1, N_HEADS + 1, dtype=jnp.float32) / N_HEADS)).reshape(N_KV_HEADS, G)
    alibi = -slopes[:, :, None, None] * jnp.abs(rel).astype(jnp.float32)
    s = jnp.where(valid[None, :, None, None], s + alibi[None, None], -jnp.inf)
    snk = sink.astype(jnp.float32).reshape(N_KV_HEADS, G)[None, None, :, :, None, None]
    m = jnp.maximum(jnp.max(s, axis=-1, keepdims=True), snk)
    p = jnp.exp(s - m)
    p = p / (jnp.sum(p, axis=-1, keepdims=True) + jnp.exp(snk - m))
    o = jnp.einsum('bnkgqj,bnjkd->bnqkgd', p.astype(vw.dtype), vw)
    return o.reshape(B, S, ATTN_WIDTH)


def hyena_filters(L, w1, b1, fr1, w2, b2, fr2, w3, decay):
    f32 = jnp.float32
    t = jnp.linspace(0.0, 1.0, L, dtype=f32)[:, None]
    w = 2.0 * math.pi * jnp.arange(L, dtype=f32)[:, None] / L
    bands = jnp.linspace(1e-4, FILTER_BANDS - 1, FILTER_BANDS, dtype=f32)[None, :]
    z = jnp.concatenate([t, jnp.cos(bands * w), -jnp.sin(bands * w)], axis=-1)
    hdn = jnp.sin(fr1.astype(f32) * (z @ w1.astype(f32) + b1.astype(f32)))
    hdn = jnp.sin(fr2.astype(f32) * (hdn @ w2.astype(f32) + b2.astype(f32)))
    k = hdn @ w3.astype(f32)
    k = k * (jnp.exp(-t * jnp.abs(decay.astype(f32))) + WINDOW_SHIFT)
    k = k.reshape(L, HYENA_ORDER, 2, HYENA_WIDTH)
    k = k / jnp.sum(jnp.abs(k), axis=(0, 2), keepdims=True)
    fwd, bwd = k[:, :, 0], k[:, :, 1]
    return jnp.concatenate([fwd, jnp.zeros_like(fwd[:1]), bwd[:0:-1]], axis=0)


def fft_long_conv(z, kc, skip):
    L = z.shape[1]
    zf = z.astype(jnp.float32)
    y = jnp.fft.irfft(jnp.fft.rfft(zf, n=2 * L, axis=1) * jnp.fft.rfft(kc, axis=0)[None], n=2 * L, axis=1)[:, :L]
    return (y + zf * skip.astype(jnp.float32)).astype(z.dtype)


def hyena_branch(u, conv_w, conv_b, fw1, fb1, ff1, fw2, fb2, ff2, fw3, decay, skip):
    L = u.shape[1]
    u = lax.conv_general_dilated(u, conv_w[:, None, :].astype(u.dtype), (1,),
                                 [(SHORT_CONV // 2, SHORT_CONV // 2)],
                                 dimension_numbers=('NWC', 'WIO', 'NWC'),
                                 feature_group_count=u.shape[-1]) + conv_b
    v, x1, x2 = jnp.split(u, HYENA_ORDER + 1, axis=-1)
    kc = hyena_filters(L, fw1, fb1, ff1, fw2, fb2, ff2, fw3, decay)
    z = v
    for o, gate in enumerate((x1, x2)):
        z = gate * fft_long_conv(z, kc[:, o], skip[o])
    return z


def token_mixer(h, w_in, conv_w, conv_b, fw1, fb1, ff1, fw2, fb2, ff2, fw3, decay, skip,
                w_hy_o, w_attn_o, attn_sink, w_out):
    proj = h @ w_in
    cuts = np.cumsum([ATTN_WIDTH, KV_WIDTH, KV_WIDTH, (HYENA_ORDER + 1) * HYENA_WIDTH, D_MODEL]).tolist()
    q, k, v, hy_u, g_attn, g_hy = jnp.split(proj, cuts, axis=-1)
    attn = windowed_attention(q, k, v, attn_sink) @ w_attn_o
    hy = hyena_branch(hy_u, conv_w, conv_b, fw1, fb1, ff1, fw2, fb2, ff2, fw3, decay, skip) @ w_hy_o
    merged = jax.nn.sigmoid(g_attn) * attn + jax.nn.sigmoid(g_hy) * hy
    return merged @ w_out


def hierarchical_moe(h, wg, bg, we, be, w1, w3, w2):
    B, S, D = h.shape
    T = B * S
    hf = h.reshape(T, D)
    tid = jnp.arange(T)
    gl = (hf @ wg + bg).astype(jnp.float32)
    g = jnp.argmax(gl, axis=-1)
    pg = jax.nn.softmax(gl, axis=-1)[tid, g][:, None]
    el = (hf @ we + be).astype(jnp.float32).reshape(T, N_GROUPS, EXPERTS_PER_GROUP)[tid, g]
    topv, topi = lax.top_k(el, TOP_K)
    wts = (jax.nn.softmax(topv, axis=-1) * pg).reshape(-1)
    eid = (g[:, None] * EXPERTS_PER_GROUP + topi).reshape(-1)
    tok = jnp.repeat(tid, TOP_K)
    M = T * TOP_K
    order = jnp.argsort(eid)
    se, st, sw = eid[order], tok[order], wts[order]
    sizes = jnp.bincount(eid, length=N_EXPERTS)
    offs = jnp.cumsum(sizes) - sizes
    psizes = (sizes + MOE_BLOCK - 1) // MOE_BLOCK * MOE_BLOCK
    pends = jnp.cumsum(psizes)
    poffs = pends - psizes
    dest = poffs[se] + (jnp.arange(M) - offs[se])
    P = M + N_EXPERTS * MOE_BLOCK
    nblk = P // MOE_BLOCK
    buf_tok = jnp.full((P,), T, jnp.int32).at[dest].set(st.astype(jnp.int32))
    buf_w = jnp.zeros((P,), jnp.float32).at[dest].set(sw)
    blk_e = jnp.clip(jnp.searchsorted(pends, jnp.arange(nblk) * MOE_BLOCK, side='right'), 0, N_EXPERTS - 1)
    hpad = jnp.concatenate([hf, jnp.zeros((1, D), hf.dtype)], axis=0)
    xb = hpad[buf_tok].reshape(nblk, MOE_BLOCK, D)

    def expert_block(args):
        xi, e = args
        return (jax.nn.silu(xi @ w1[e]) * (xi @ w3[e])) @ w2[e]

    yb = lax.map(expert_block, (xb, blk_e)).reshape(P, D)
    y = jax.ops.segment_sum(yb * buf_w[:, None].astype(yb.dtype), buf_tok, num_segments=T + 1)[:T]
    return y.reshape(B, S, D)


def encoder_layer(x, c, w_ada, b_ada, w_in, conv_w, conv_b, fw1, fb1, ff1, fw2, fb2, ff2, fw3,
                  decay, skip, w_hy_o, w_attn_o, attn_sink, w_out, ln1_g, ln1_b,
                  rg_w, rg_b, re_w, re_b, ew1, ew3, ew2, ln2_g, ln2_b):
    mod = (jax.nn.silu(c) @ w_ada + b_ada)[:, None, :]
    shift1, scale1, gate1, shift2, scale2, gate2 = jnp.split(mod, 6, axis=-1)
    h = x * (1.0 + scale1) + shift1
    y = token_mixer(h, w_in, conv_w, conv_b, fw1, fb1, ff1, fw2, fb2, ff2, fw3, decay, skip,
                    w_hy_o, w_attn_o, attn_sink, w_out)
    x = layer_norm(DN_ALPHA * x + gate1 * y, ln1_g, ln1_b)
    h = x * (1.0 + scale2) + shift2
    y = hierarchical_moe(h, rg_w, rg_b, re_w, re_b, ew1, ew3, ew2)
    return layer_norm(DN_ALPHA * x + gate2 * y, ln2_g, ln2_b)


def setup_inputs(seed: int = 0) -> dict:
    key = jax.random.key(seed)
    ks = jax.random.split(key, 32)
    f32 = jnp.float32
    nrm = lambda k, shape, scale: jax.random.normal(k, shape, f32) * scale
    Dp, C, D = DEPTH, HYENA_WIDTH, D_MODEL
    col_scale = jnp.concatenate([jnp.ones((ATTN_WIDTH + KV_WIDTH,), f32),
                                 jnp.full((KV_WIDTH + C,), DN_BETA, f32),
                                 jnp.ones((2 * C + 2 * D,), f32)])
    d_min = -math.log(DECAY_TARGET) / SLOW_DECAY_PCT
    d_max = -math.log(DECAY_TARGET) / FAST_DECAY_PCT
    base_decay = jnp.tile(jnp.linspace(d_min, d_max, C, dtype=f32), HYENA_ORDER * 2)
    return {
        "x": nrm(ks[0], (BATCH, SEQ, D), 1.0),
        "c": nrm(ks[1], (BATCH, D), 1.0),
        "w_ada": nrm(ks[2], (Dp, D, 6 * D), D ** -0.5),
        "b_ada": nrm(ks[3], (Dp, 6 * D), 0.02),
        "w_in": nrm(ks[4], (Dp, D, IN_WIDTH), D ** -0.5) * col_scale,
        "conv_w": nrm(ks[5], (Dp, SHORT_CONV, (HYENA_ORDER + 1) * C), SHORT_CONV ** -0.5),
        "conv_b": nrm(ks[6], (Dp, (HYENA_ORDER + 1) * C), 0.02),
        "filt_w1": nrm(ks[7], (Dp, FILTER_EMB, FILTER_HIDDEN), FILTER_EMB ** -0.5),
        "filt_b1": nrm(ks[8], (Dp, FILTER_HIDDEN), 0.1),
        "filt_freq1": 1.0 + nrm(ks[9], (Dp, FILTER_HIDDEN), 0.1),
        "filt_w2": nrm(ks[10], (Dp, FILTER_HIDDEN, FILTER_HIDDEN), FILTER_HIDDEN ** -0.5),
        "filt_b2": nrm(ks[11], (Dp, FILTER_HIDDEN), 0.1),
        "filt_freq2": 1.0 + nrm(ks[12], (Dp, FILTER_HIDDEN), 0.1),
        "filt_w3": nrm(ks[13], (Dp, FILTER_HIDDEN, N_FILTERS), FILTER_HIDDEN ** -0.5),
        "filt_decay": base_decay[None] + nrm(ks[14], (Dp, N_FILTERS), 0.01),
        "hy_skip": nrm(ks[15], (Dp, HYENA_ORDER, C), 0.5),
        "w_hy_o": nrm(ks[16], (Dp, C, D), C ** -0.5 * DN_BETA),
        "w_attn_o": nrm(ks[17], (Dp, ATTN_WIDTH, D), ATTN_WIDTH ** -0.5 * DN_BETA),
        "attn_sink": nrm(ks[18], (Dp, N_HEADS), 0.5),
        "w_out": nrm(ks[19], (Dp, D, D), D ** -0.5 * DN_BETA),
        "ln1_g": 1.0 + nrm(ks[20], (Dp, D), 0.02),
        "ln1_b": nrm(ks[21], (Dp, D), 0.02),
        "router_group_w": nrm(ks[22], (Dp, D, N_GROUPS), D ** -0.5),
        "router_group_b": nrm(ks[23], (Dp, N_GROUPS), 0.01),
        "router_expert_w": nrm(ks[24], (Dp, D, N_EXPERTS), D ** -0.5),
        "router_expert_b": nrm(ks[25], (Dp, N_EXPERTS), 0.01),
        "exp_w1": nrm(ks[26], (Dp, N_EXPERTS, D, D_EXPERT), D ** -0.5),
        "exp_w3": nrm(ks[27], (Dp, N_EXPERTS, D, D_EXPERT), D ** -0.5),
        "exp_w2": nrm(ks[28], (Dp, N_EXPERTS, D_EXPERT, D), D_EXPERT ** -0.5 * DN_BETA),
        "ln2_g": 1.0 + nrm(ks[29], (Dp, D), 0.02),
        "ln2_b": nrm(ks[30], (Dp, D), 0.02),
    }


def reference(x, c, w_ada, b_ada, w_in, conv_w, conv_b, filt_w1, filt_b1, filt_freq1, filt_w2,
              filt_b2, filt_freq2, filt_w3, filt_decay, hy_skip, w_hy_o, w_attn_o, attn_sink,
              w_out, ln1_g, ln1_b, router_group_w, router_group_b, router_expert_w,
              router_expert_b, exp_w1, exp_w3, exp_w2, ln2_g, ln2_b):
    for l in range(DEPTH):
        x = encoder_layer(x, c, w_ada[l], b_ada[l], w_in[l], conv_w[l], conv_b[l], filt_w1[l],
                          filt_b1[l], filt_freq1[l], filt_w2[l], filt_b2[l], filt_freq2[l],
                          filt_w3[l], filt_decay[l], hy_skip[l], w_hy_o[l], w_attn_o[l],
                          attn_sink[l], w_out[l], ln1_g[l], ln1_b[l], router_group_w[l],
                          router_group_b[l], router_expert_w[l], router_expert_b[l],
                          exp_w1[l], exp_w3[l], exp_w2[l], ln2_g[l], ln2_b[l])
    return x
```

```python
from contextlib import ExitStack
import math
import numpy as np
import ml_dtypes
import concourse.bass as bass
import concourse.mybir as mybir
from concourse.bass_utils import run_bass_kernel_spmd

F32 = mybir.dt.float32
BF16 = mybir.dt.bfloat16
I32 = mybir.dt.int32
U32 = mybir.dt.uint32
AF = mybir.ActivationFunctionType
ALU = mybir.AluOpType
AX = mybir.AxisListType

NCORES = 8
D = 1024
SEQ = 8192
TOWN = 4096
L = 8192
NFFT = 16384
DN_ALPHA = 2.0 ** 0.25
LN_EPS = 1e-5
DBG_NSG = 2
NBLK = 128
PROWS = NBLK * 128


class Buf:
    __slots__ = ("w", "r", "name")

    def __init__(self, name=""):
        self.w = None
        self.r = {}
        self.name = name


class _Eng:
    def __init__(self, name, hname, sem, self_sync):
        self.name = name
        self.hname = hname
        self.sem = sem
        self.cnt = 0
        self.known = {}
        self.ops = []
        self.self_sync = self_sync


class Prog:
    ENG = {"pe": "tensor", "act": "scalar", "dve": "vector", "pool": "gpsimd", "sp": "sync"}

    def __init__(self, nc, stack, n_dma_sems=14):
        self.nc = nc
        self.sems = {}
        self.engs = {}
        for k, h in self.ENG.items():
            self.sems["e_" + k] = stack.enter_context(nc.semaphore("s_" + k))
            self.engs[k] = _Eng(k, h, "e_" + k, self_sync=(k in ("act", "dve", "pool")))
        self.dq = {}
        for q in ("sp", "act", "pool"):
            lst = []
            for i in range(n_dma_sems):
                key = "d_%s%d" % (q, i)
                self.sems[key] = stack.enter_context(nc.semaphore(key))
                lst.append([key, 0])
            self.dq[q] = [lst, 0]

    def _collect(self, E, reads, writes):
        waits = {}

        def need(tok):
            if tok is None:
                return
            k, v = tok
            if waits.get(k, 0) < v:
                waits[k] = v

        for b in reads:
            need(b.w)
        for b in writes:
            need(b.w)
            for k, v in b.r.items():
                need((k, v))
        wl = []
        for k, v in waits.items():
            if k == E.sem and not E.self_sync:
                continue
            if E.known.get(k, 0) >= v:
                continue
            E.known[k] = v
            wl.append((k, v))
        return wl

    def _commit(self, tok, reads, writes):
        k, v = tok
        for b in reads:
            if b.r.get(k, 0) < v:
                b.r[k] = v
        for b in writes:
            b.w = tok
            b.r = {}

    def op(self, eng, fn, reads=(), writes=()):
        E = self.engs[eng]
        wl = self._collect(E, reads, writes)
        E.cnt += 1
        tok = (E.sem, E.cnt)
        E.ops.append((wl, fn, (E.sem, 1)))
        self._commit(tok, reads, writes)
        return tok

    def group(self, eng, fns, reads=(), writes=()):
        E = self.engs[eng]
        wl = self._collect(E, reads, writes)
        E.cnt += 1
        tok = (E.sem, E.cnt)
        n = len(fns)
        for i, fn in enumerate(fns):
            E.ops.append((wl if i == 0 else [], fn, (E.sem, 1) if i == n - 1 else None))
        self._commit(tok, reads, writes)
        return tok

    def dma(self, q, fn, reads=(), writes=()):
        E = self.engs[q]
        lst, idx = self.dq[q]
        slot = lst[idx % len(lst)]
        self.dq[q][1] = idx + 1
        key, val = slot
        wl = self._collect(E, reads, writes)
        if val > 0 and E.known.get(key, 0) < val:
            E.known[key] = val
            wl.append((key, val))
        val += 16
        slot[1] = val
        tok = (key, val)
        E.ops.append((wl, fn, (key, 16)))
        self._commit(tok, reads, writes)
        return tok

    def _all_tokens(self):
        toks = []
        for E in self.engs.values():
            if E.cnt:
                toks.append((E.sem, E.cnt))
        for q, (lst, _) in self.dq.items():
            for key, val in lst:
                if val:
                    toks.append((key, val))
        return toks

    def barrier(self):
        toks = self._all_tokens()
        for E in self.engs.values():
            wl = []
            for k, v in toks:
                if E.known.get(k, 0) >= v:
                    continue
                E.known[k] = v
                wl.append((k, v))
            if wl:
                E.ops.append((wl, None, None))

    def emit(self):
        nc = self.nc
        sems = self.sems
        with nc.Block() as block:
            for k, E in self.engs.items():
                def body(h, E=E):
                    for wl, fn, inc in E.ops:
                        for sk, v in wl:
                            h.wait_ge(sems[sk], v)
                        if fn is None:
                            continue
                        ins = fn(h)
                        if inc is not None:
                            ins.then_inc(sems[inc[0]], inc[1])
                getattr(block, E.hname)(body)


def _bf(a):
    return np.ascontiguousarray(a.astype(ml_dtypes.bfloat16))


def host_constants(half):
    a = np.arange(128)
    ang = 2.0 * np.pi * np.outer(a, a) / 128.0
    Fr = np.cos(ang)
    Fi = -np.sin(ang)
    rowpos = np.concatenate([np.arange(32), (32 if half == 0 else 96) + np.arange(32)])
    tb = np.zeros((128, NTB), np.float64)
    tb[:, 0:128] = Fr
    tb[:, 128:256] = Fi
    tb[0:64, 256:384] = Fr[rowpos, :]
    tb[0:64, 384:512] = Fi[rowpos, :]
    tb[:, 512:640] = Fr
    tb[:, 640:768] = Fi
    tb[:, 768:896] = -Fi
    tb[:, 896:1024] = Fr
    tb[:, 1024:1152] = -Fi
    tb[:, 1152:1280] = Fi
    tb[:, 1280:1408] = Fr
    tb[:, 1408:1472] = Fr[:, rowpos] / NFFT
    tb[:, 1472:1536] = Fi[:, rowpos] / NFFT
    tb[:, 1536:1664] = np.eye(128)
    tb[:, 1664:1792] = (a[:, None] < a[None, :]).astype(np.float64)
    tb[:, 1792:1920] = 1.0
    j = a[:, None]
    q = a[None, :]
    for g in range(2):
        for ty, off in enumerate((-128, 0, 128)):
            rel = j + off - q
            val = (np.abs(rel) <= 128)
            blk = np.zeros((128, 4, 128))
            for hh in range(4):
                h = g * 4 + hh
                slope = 2.0 ** (-8.0 * (h + 1) / 8.0)
                blk[:, hh, :] = np.exp(-slope * np.abs(rel)) * val
            c0 = 1920 + (g * 3 + ty) * 512
            tb[:, c0:c0 + 512] = blk.reshape(128, 512)
    tw = 2.0 * np.pi * np.outer(a, a) / NFFT
    tb[:, CT_TC1:CT_TC1 + 128] = np.cos(tw); tb[:, CT_TC1 + 128:CT_TC1 + 256] = np.cos(tw)
    tb[:, CT_TC2:CT_TC2 + 128] = -np.sin(tw); tb[:, CT_TC2 + 128:CT_TC2 + 256] = -np.sin(tw)
    tb[:, CT_F2NR:CT_F2NR + 128] = -Fr
    tb[:, CT_G2NA:CT_G2NA + 128] = -Fr
    tb[:, CT_G2NA + 128:CT_G2NA + 256] = Fi
    tb[:, CT_G1NI:CT_G1NI + 64] = -Fi[:, rowpos] / NFFT
    tf = np.zeros((128, NTF), np.float32)
    tf[:, 0:128] = np.cos(tw)
    tf[:, 128:256] = -np.sin(tw)
    tf[:, 256:384] = np.eye(128)
    tf[:, 384] = a
    tf[:, 385] = 1.0 - half
    tf[:, 386] = float(half)
    tf[:, 387] = float(half)
    tf[:, 388] = 1.0 - half
    tf[:, 400:528] = 128.0 * a[None, :]
    cwn = 1.0 / (L - 1)
    tf[:64, 389] = -cwn
    tf[64:, 389] = cwn
    tf[:64, 390] = 128.0 * a[:64]
    tf[64:, 390] = -(8192.0 - 128.0 * (a[64:] - 64))
    tf[:, 528:656] = a[None, :]
    tf[:, 656:784] = 1.0
    return _bf(tb), tf


CT_F1FULL, CT_F1C, CT_F2R, CT_F2I, CT_F2NI, CT_G2A, CT_G2B, CT_G1R, CT_G1I = 0, 256, 512, 640, 768, 896, 1152, 1408, 1472
CT_ID, CT_TRI, CT_ONES, CT_EM = 1536, 1664, 1792, 1920
CT_TC1 = 1920 + 6 * 512
CT_TC2 = CT_TC1 + 256
CT_F2NR = CT_TC2 + 256
CT_G2NA = CT_F2NR + 128
CT_G1NI = CT_G2NA + 256
NTB = CT_G1NI + 64
NTF = 784


def q_perm():
    cols = []
    for c in range(4):
        cols += list(range(c * 64, c * 64 + 64))
        cols += list(range((4 + c) * 64, (4 + c) * 64 + 64))
    return np.array(cols + list(range(512, 4352)))


def build_program(debug=None, lite=False):
    nc = bass.Bass("TRN2", target_bir_lowering=False)
    dbg = debug is not None

    def din(name, shape, dt=F32):
        return nc.dram_tensor(name, list(shape), dt, kind="ExternalInput").ap()

    DBG_OUT = {"p0": ["mod_scr"], "p1a": ["mod_scr", "Uc", "Gs", "dbgq", "dbgk", "dbgv"], "p1b": ["AT"],
               "p2": ["AT", "Z2", "dbgkc", "dbgz1", "dbgkh"], "p3": ["X1", "H2", "dbgrt"], "p4": ["XB", "YB", "dbgrt"]}

    def dscr(name, shape, dt=F32):
        isout = dbg and name in DBG_OUT.get(debug, [])
        return nc.dram_tensor(name, list(shape), dt, kind=("ExternalOutput" if isout else "Internal")).ap()

    xcat = din("xcat", [SEQ, D])
    cb = din("cb", [128, 8])
    w_ada = din("w_ada", [D, 6 * D])
    b_ada = din("b_ada", [1, 6 * D])
    w_in = din("w_in", [D, 4352])
    conv_w = din("conv_w", [128, 3, 12])
    conv_b = din("conv_b", [128, 12])
    fw1 = din("fw1", [33, 64]); fb1 = din("fb1", [64, 1]); ff1 = din("ff1", [64, 1])
    fw2 = din("fw2", [64, 64]); fb2 = din("fb2", [64, 1]); ff2 = din("ff2", [64, 1])
    fw3 = din("fw3", [64, 2048]); fdec = din("fdec", [1, 2048])
    hskip = din("hskip", [1, 1024])
    w_hy_o = din("w_hy_o", [512, D]); w_attn_o = din("w_attn_o", [512, D])
    sink = din("sink", [1, 8])
    w_out = din("w_out", [D, D])
    ln1g = din("ln1g", [1, D]); ln1b = din("ln1b", [1, D]); ln2g = din("ln2g", [1, D]); ln2b = din("ln2b", [1, D])
    wr = din("wr", [D, 72]); br = din("br", [1, 72])
    ne = 256 if lite else 64 * 2 * 128
    ew1 = din("ew1", [ne, 2048]); ew3 = din("ew3", [ne, 2048]); ew2 = din("ew2", [ne, 2048])
    tabs_b = din("tabs_b", [128, NTB], BF16)
    tabs_f = din("tabs_f", [128, NTF])
    bands = din("bands", [33, 4])
    out = nc.dram_tensor("out", [TOWN, D], F32, kind="ExternalOutput").ap()

    mod_scr = dscr("mod_scr", [1, 6 * D])
    Uc = dscr("Uc", [1536, SEQ])
    Gs = dscr("Gs", [2048, TOWN], BF16)
    AT = dscr("AT", [512, TOWN], BF16)
    Z2 = dscr("Z2", [512, TOWN], BF16)
    X1 = dscr("X1", [TOWN, D])
    H2 = dscr("H2", [TOWN, D], BF16)
    XB = dscr("XB", [PROWS, D], BF16)
    YB = dscr("YB", [PROWS, D], BF16)
    EWB = [dscr("EWB%d" % i, [ne // 2, 4096], BF16) for i in range(3)]
    dbg1a = (debug == "p1a")
    dbgq = dscr("dbgq", [128, 4, TOWN], BF16) if dbg1a else None
    dbgk = dscr("dbgk", [128, 34 * 128], BF16) if dbg1a else None
    dbgv = dscr("dbgv", [128, 34, 2, 65], BF16) if dbg1a else None
    dbgkc = dscr("dbgkc", [128, 32, 128], BF16) if debug == "p2" else None
    dbgz1 = dscr("dbgz1", [64, 16, 128], BF16) if debug == "p2" else None
    dbgkh = dscr("dbgkh", [128, 2, 32, 128], BF16) if debug == "p2" else None

    with ExitStack() as gst:
        P = Prog(nc, gst)

        def SB(st, name, shape, dt):
            return st.enter_context(nc.sbuf_tensor(name, list(shape), dt))

        def PS(st, name, shape, dt=F32):
            return st.enter_context(nc.psum_tensor(name, list(shape), dt))

        tb = SB(gst, "tb", [128, NTB], BF16); b_tb = Buf("tb")
        tf = SB(gst, "tf", [128, NTF], F32); b_tf = Buf("tf")
        P.dma("sp", lambda e: e.dma_start(out=tb[:], in_=tabs_b[:, :]), writes=[b_tb])
        P.dma("sp", lambda e: e.dma_start(out=tf[:], in_=tabs_f[:, :]), writes=[b_tf])
        identf = tf[:, 256:384]
        identb = tb[:, CT_ID:CT_ID + 128]
        mA, mB, nmA, nmB = tf[:, 385:386], tf[:, 386:387], tf[:, 387:388], tf[:, 388:389]
        m1 = SB(gst, "m1", [128, 16], F32); b_m1 = Buf()

        with ExitStack() as st:
            cbt = SB(st, "cbt", [128, 8], F32); b_cbt = Buf()
            sig = SB(st, "sig", [128, 8], F32)
            modrow = SB(st, "modrow", [1, 6 * D], F32); b_mod = Buf()
            badar = SB(st, "badar", [1, 6 * D], F32); b_bada = Buf()
            wa = [SB(st, "wa%d" % i, [128, 8, 512], F32) for i in range(2)]
            b_wa = [Buf(), Buf()]
            pm = [PS(st, "pm%d" % i, [1, 512]) for i in range(2)]
            b_pm = [Buf(), Buf()]
            P.dma("act", lambda e: e.dma_start(out=cbt[:], in_=cb[:, :]), writes=[b_cbt])
            P.dma("act", lambda e: e.dma_start(out=badar[:], in_=b_ada[:, :]), writes=[b_bada])
            P.op("act", lambda e: e.activation(out=sig[:], in_=cbt[:], func=AF.Sigmoid), reads=[b_cbt], writes=[b_cbt])
            P.op("dve", lambda e: e.tensor_tensor(out=cbt[:], in0=cbt[:], in1=sig[:], op=ALU.mult), reads=[b_cbt], writes=[b_cbt])
            for blk in range(12):
                s = blk % 2
                P.dma("sp", lambda e, s=s, blk=blk: e.dma_start(
                    out=wa[s][:], in_=w_ada[:, blk * 512:(blk + 1) * 512].rearrange("(c p) n -> p c n", p=128)),
                    writes=[b_wa[s]])
                P.group("pe", [lambda e, s=s, kc=kc: e.matmul(pm[s][:], lhsT=cbt[:, kc:kc + 1], rhs=wa[s][:, kc, :],
                                                               start=(kc == 0), stop=(kc == 7)) for kc in range(8)],
                        reads=[b_cbt, b_wa[s]], writes=[b_pm[s]])
                P.op("dve", lambda e, s=s, blk=blk: e.tensor_tensor(out=modrow[0:1, blk * 512:(blk + 1) * 512], in0=pm[s][:],
                                                                   in1=badar[0:1, blk * 512:(blk + 1) * 512], op=ALU.add),
                     reads=[b_pm[s], b_bada], writes=[b_mod])
            for off in (1024, 4096):
                P.op("dve", lambda e, off=off: e.tensor_scalar(out=modrow[0:1, off:off + 1024], in0=modrow[0:1, off:off + 1024],
                                                               scalar1=1.0, scalar2=None, op0=ALU.add), reads=[b_mod], writes=[b_mod])
            b_modscr = Buf()
            P.dma("sp", lambda e: e.dma_start(out=mod_scr[:, :], in_=modrow[:]), reads=[b_mod], writes=[b_modscr])
            pTm = PS(st, "pTm", [128, 16]); b_pTm = Buf()
            P.group("pe", [lambda e, c=c: e.transpose(out=pTm[:, c:c + 1], in_=modrow[0:1, c * 128:(c + 1) * 128],
                                                      identity=identf[0:1, 0:1]) for c in range(16)],
                    reads=[b_mod, b_tf], writes=[b_pTm])
            P.op("dve", lambda e: e.tensor_copy(out=m1[:], in_=pTm[:]), reads=[b_pTm], writes=[b_m1])
            P.barrier()

        if debug == "p0":
            return _finish(nc, P, out, gst)

        with ExitStack() as st1:
            qT = SB(st1, "qT", [128, 4, TOWN], BF16); b_qT = Buf()
            kT = SB(st1, "kT", [128, 34 * 128], BF16); b_kT = Buf()
            Va = SB(st1, "Va", [128, 34, 2, 65], BF16); b_Va = Buf()
            P.op("pool", lambda e: e.memset(Va[:], 1.0), writes=[b_Va])
            with ExitStack() as st:
                win = SB(st, "win", [128, 8, 4352], BF16); b_win = Buf()
                for kc in range(8):
                    for (c0, c1) in ((0, 2048), (2048, 4096), (4096, 4352)):
                        P.dma("pool", lambda e, kc=kc, c0=c0, c1=c1: e.dma_start(
                            out=win[:, kc, c0:c1], in_=w_in[kc * 128:(kc + 1) * 128, c0:c1]), writes=[b_win])
                cw = SB(st, "cw", [128, 3, 12], F32); cbias = SB(st, "cbias", [128, 12], F32); b_cw = Buf()
                P.dma("act", lambda e: e.dma_start(out=cw[:], in_=conv_w[:, :, :]), writes=[b_cw])
                P.dma("act", lambda e: e.dma_start(out=cbias[:], in_=conv_b[:, :]), writes=[b_cw])
                NXS = 3
                xs = [SB(st, "xs%d" % i, [128, D], F32) for i in range(NXS)]; b_xs = [Buf() for _ in range(NXS)]
                hT = [SB(st, "hT%d" % i, [128, 8, 512], BF16) for i in range(2)]; b_hT = [Buf(), Buf()]
                carry = SB(st, "carry", [128, 12, 2], F32); b_carry = Buf()
                NUB = 3
                ub = [SB(st, "ub%d" % i, [128, 514], F32) for i in range(NUB)]; b_ub = [Buf() for _ in range(NUB)]
                ua = [SB(st, "ua%d" % i, [128, 512], F32) for i in range(NUB)]; b_ua = [Buf() for _ in range(NUB)]
                gsb = [SB(st, "gsb%d" % i, [128, 512], BF16) for i in range(2)]; b_gsb = [Buf(), Buf()]
                fix = SB(st, "fix", [128, 2], F32); b_fix = Buf()
                pT = [PS(st, "pT%d" % i, [128, 8, 128]) for i in range(2)]; b_pT = [Buf(), Buf()]
                pO = [PS(st, "pO%d" % i, [128, 512]) for i in range(3)]; b_pO = [Buf() for _ in range(3)]
                pV = PS(st, "pV", [128, 128]); b_pV = Buf()
                cnt = {"x": 0, "pT": 0, "pO": 0, "ub": 0, "g": 0}
                b_Uc = Buf(); b_Gs = Buf()

                def load_transpose(tile_idx, hslot, col0, ncols=128):
                    xi = cnt["x"] % NXS; cnt["x"] += 1
                    P.dma("sp", lambda e: e.dma_start(out=xs[xi][:], in_=xcat[tile_idx * 128:(tile_idx + 1) * 128, :]),
                          writes=[b_xs[xi]])
                    pi = cnt["pT"] % 2; cnt["pT"] += 1
                    P.group("pe", [lambda e, kc=kc: e.transpose(out=pT[pi][:, kc, :], in_=xs[xi][:, kc * 128:(kc + 1) * 128],
                                                               identity=identf) for kc in range(8)],
                            reads=[b_xs[xi], b_tf], writes=[b_pT[pi]])
                    for kc in range(8):
                        P.op("act", lambda e, kc=kc: e.activation(out=hT[hslot][:, kc, col0:col0 + 128], in_=pT[pi][:, kc, :],
                                                                 func=AF.Identity, scale=m1[:, 8 + kc:9 + kc], bias=m1[:, kc:kc + 1]),
                             reads=[b_pT[pi], b_m1], writes=[b_hT[hslot]])

                def proj_chunk(hslot, oc, ncols=512):
                    pi = cnt["pO"] % 3; cnt["pO"] += 1
                    P.group("pe", [lambda e, kc=kc: e.matmul(pO[pi][:, 0:ncols], lhsT=win[:, kc, oc * 128:(oc + 1) * 128],
                                                             rhs=hT[hslot][:, kc, 0:ncols], start=(kc == 0), stop=(kc == 7))
                                   for kc in range(8)],
                            reads=[b_win, b_hT[hslot]], writes=[b_pO[pi]])
                    return pi

                def proj_v(hslot, t, vtile):
                    P.group("pe", [lambda e, kc=kc: e.matmul(pV[:], lhsT=hT[hslot][:, kc, t * 128:(t + 1) * 128],
                                                             rhs=win[:, kc, 640:768], start=(kc == 0), stop=(kc == 7))
                                   for kc in range(8)],
                            reads=[b_win, b_hT[hslot]], writes=[b_pV])
                    P.op("dve", lambda e: e.tensor_copy(out=Va[:, vtile, :, 0:64], in_=pV[:].rearrange("p (g d) -> p g d", g=2)),
                         reads=[b_pV], writes=[b_Va])

                load_transpose(63, 0, 0)
                for j in range(12):
                    pi = proj_chunk(0, 6 + j, ncols=128)
                    P.op("act", lambda e, j=j, pi=pi: e.copy(out=carry[:, j, :], in_=pO[pi][:, 126:128]),
                         reads=[b_pO[pi]], writes=[b_carry])

                def do_supertile(st_i):
                    hs = st_i % 2
                    own = st_i < 8
                    if st_i == 0:
                        for t in range(4):
                            load_transpose(st_i * 4 + t, hs, t * 128)
                    chunks = list(range(0, 5)) + list(range(6, 34)) if own else list(range(6, 18))
                    if st_i in (8, 15):
                        chunks = [4] + chunks
                    pre_at = {}
                    if st_i + 1 < 16:
                        step = max(1, (len(chunks) - 2) // 4)
                        for t in range(4):
                            pre_at[1 + t * step] = t
                    for ci_, oc in enumerate(chunks):
                        if ci_ in pre_at:
                            load_transpose((st_i + 1) * 4 + pre_at[ci_], 1 - hs, pre_at[ci_] * 128)
                        pi = proj_chunk(hs, oc)
                        if oc < 4:
                            P.op("dve", lambda e, oc=oc, pi=pi: e.tensor_copy(out=qT[:, oc, st_i * 512:(st_i + 1) * 512], in_=pO[pi][:]),
                                 reads=[b_pO[pi]], writes=[b_qT])
                        elif oc == 4:
                            if own:
                                P.op("dve", lambda e, pi=pi: e.tensor_copy(out=kT[:, 128 + st_i * 512:128 + (st_i + 1) * 512], in_=pO[pi][:]),
                                     reads=[b_pO[pi]], writes=[b_kT])
                            elif st_i == 8:
                                P.op("dve", lambda e, pi=pi: e.tensor_copy(out=kT[:, 33 * 128:34 * 128], in_=pO[pi][:, 0:128]),
                                     reads=[b_pO[pi]], writes=[b_kT])
                            else:
                                P.op("dve", lambda e, pi=pi: e.tensor_copy(out=kT[:, 0:128], in_=pO[pi][:, 384:512]),
                                     reads=[b_pO[pi]], writes=[b_kT])
                        elif oc < 18:
                            j = oc - 6
                            ui = cnt["ub"] % NUB; cnt["ub"] += 1
                            P.op("act", lambda e, pi=pi, ui=ui: e.copy(out=ub[ui][:, 2:514], in_=pO[pi][:]),
                                 reads=[b_pO[pi]], writes=[b_ub[ui]])
                            P.op("act", lambda e, j=j, ui=ui: e.copy(out=ub[ui][:, 0:2], in_=carry[:, j, :]),
                                 reads=[b_carry], writes=[b_ub[ui]])
                            P.op("act", lambda e, j=j, ui=ui: e.copy(out=carry[:, j, :], in_=ub[ui][:, 512:514]),
                                 reads=[b_ub[ui]], writes=[b_carry])
                            P.op("act", lambda e, j=j, ui=ui: e.activation(out=ua[ui][:], in_=ub[ui][:, 1:513], func=AF.Identity,
                                                                          scale=cw[:, 1, j:j + 1], bias=cbias[:, j:j + 1]),
                                 reads=[b_ub[ui], b_cw], writes=[b_ua[ui]])
                            P.op("dve", lambda e, j=j, ui=ui: e.scalar_tensor_tensor(out=ua[ui][:], in0=ub[ui][:, 0:512], scalar=cw[:, 0, j:j + 1],
                                                                                    in1=ua[ui][:], op0=ALU.mult, op1=ALU.add),
                                 reads=[b_ub[ui], b_cw], writes=[b_ua[ui]])
                            P.op("dve", lambda e, j=j, ui=ui: e.scalar_tensor_tensor(out=ua[ui][:], in0=ub[ui][:, 2:514], scalar=cw[:, 2, j:j + 1],
                                                                                    in1=ua[ui][:], op0=ALU.mult, op1=ALU.add),
                                 reads=[b_ub[ui], b_cw], writes=[b_ua[ui]])
                            if st_i in (0, 8):
                                nm = nmB if st_i == 0 else nmA
                                P.op("dve", lambda e, j=j, ui=ui, nm=nm: e.tensor_scalar(out=fix[:, 0:1], in0=ub[ui][:, 2:3], scalar1=cw[:, 2, j:j + 1],
                                                                                        scalar2=nm, op0=ALU.mult, op1=ALU.mult),
                                     reads=[b_ub[ui], b_cw, b_tf], writes=[b_fix])
                                P.op("dve", lambda e, j=j, ui=ui, nm=nm: e.tensor_scalar(out=fix[:, 1:2], in0=ub[ui][:, 1:2], scalar1=cw[:, 0, j:j + 1],
                                                                                        scalar2=nm, op0=ALU.mult, op1=ALU.mult),
                                     reads=[b_ub[ui], b_cw, b_tf], writes=[b_fix])
                                P.op("dve", lambda e, ui=ui: e.tensor_tensor(out=ua[ui][:, 0:2], in0=ua[ui][:, 0:2], in1=fix[:, 0:2], op=ALU.subtract),
                                     reads=[b_fix], writes=[b_ua[ui]])
                            s0 = st_i * 512
                            r0 = j * 128
                            P.dma("pool", lambda e, ui=ui, r0=r0, s0=s0: e.dma_start(out=Uc[r0:r0 + 128, (s0 - 1) % SEQ:(s0 - 1) % SEQ + 1], in_=ua[ui][:, 0:1], allow_slow_non_contiguous=True),
                                  reads=[b_ua[ui]])
                            P.dma("pool", lambda e, ui=ui, r0=r0, s0=s0: e.dma_start(out=Uc[r0:r0 + 128, s0:s0 + 511], in_=ua[ui][:, 1:512]),
                                  reads=[b_ua[ui]])
                        else:
                            gi = cnt["g"] % 2; cnt["g"] += 1
                            P.op("act", lambda e, pi=pi, gi=gi: e.activation(out=gsb[gi][:], in_=pO[pi][:], func=AF.Sigmoid),
                                 reads=[b_pO[pi]], writes=[b_gsb[gi]])
                            r0 = (oc - 18) * 128
                            P.dma("pool", lambda e, gi=gi, r0=r0: e.dma_start(out=Gs[r0:r0 + 128, st_i * 512:(st_i + 1) * 512], in_=gsb[gi][:]),
                                  reads=[b_gsb[gi]])
                    if own:
                        for t in range(4):
                            proj_v(hs, t, 1 + st_i * 4 + t)
                    elif st_i == 8:
                        proj_v(hs, 0, 33)
                    elif st_i == 15:
                        proj_v(hs, 3, 0)
                for st_i in range(16):
                    do_supertile(st_i)
                if dbg1a:
                    P.dma("sp", lambda e: e.dma_start(out=dbgq[:, :, :], in_=qT[:]), reads=[b_qT])
                    P.dma("sp", lambda e: e.dma_start(out=dbgk[:, :], in_=kT[:]), reads=[b_kT])
                    P.dma("sp", lambda e: e.dma_start(out=dbgv[:, :, :, :], in_=Va[:]), reads=[b_Va])
                P.barrier()
            if debug == "p1a":
                return _finish(nc, P, out, gst)

            with ExitStack() as st:
                esink = SB(st, "esink", [128, 8], F32); b_es = Buf()
                P.dma("sp", lambda e: e.dma_start(out=esink[:], in_=sink[0:1, :].partition_broadcast(128)), writes=[b_es])
                P.op("act", lambda e: e.activation(out=esink[:], in_=esink[:], func=AF.Exp), reads=[b_es], writes=[b_es])
                emJ = SB(st, "emJ", [128, 2, 2, 512], BF16); b_emJ = Buf()
                for g in range(2):
                    P.op("dve", lambda e, g=g: e.tensor_scalar(out=emJ[:, g, 0, :], in0=tb[:, CT_EM + (g * 3 + 0) * 512:CT_EM + (g * 3 + 1) * 512],
                                                               scalar1=mB, scalar2=None, op0=ALU.mult), reads=[b_tb, b_tf], writes=[b_emJ])
                    P.op("dve", lambda e, g=g: e.tensor_scalar(out=emJ[:, g, 1, :], in0=tb[:, CT_EM + (g * 3 + 2) * 512:CT_EM + (g * 3 + 3) * 512],
                                                               scalar1=mA, scalar2=None, op0=ALU.mult), reads=[b_tb, b_tf], writes=[b_emJ])
                pS = [PS(st, "pS%d" % i, [128, 512]) for i in range(3)]; b_pS = [Buf() for _ in range(3)]
                pPVb = [[PS(st, "pPV%d_%d" % (s, g), [128, 512]) for g in range(2)] for s in range(2)]
                pPV = [[pPVb[s][g][:, 0:260].rearrange("p (h d) -> p h d", h=4) for g in range(2)] for s in range(2)]
                b_pPV = [[Buf(), Buf()], [Buf(), Buf()]]
                pTab = PS(st, "pTa", [128, 1024], BF16); b_pTa = Buf()
                pTa = pTab[:, 0:512].rearrange("p (c q) -> p c q", c=4)
                pex = [SB(st, "pex%d" % i, [128, 512], BF16) for i in range(3)]; b_pex = [Buf() for _ in range(3)]
                pmk = [[[SB(st, "pmk%d_%d_%d" % (s, g, kb), [128, 512], BF16) for kb in range(3)] for g in range(2)] for s in range(2)]
                b_pmk = [[[Buf() for kb in range(3)] for g in range(2)] for s in range(2)]
                den = SB(st, "den", [128, 2, 4], F32); b_den = Buf()
                att = [SB(st, "att%d" % i, [128, 8, 64], BF16) for i in range(2)]; b_att = [Buf(), Buf()]
                atT = [SB(st, "atT%d" % i, [128, 4, 128], BF16) for i in range(2)]; b_atT = [Buf(), Buf()]
                cn = {"s": 0}

                def attn_part1(i):
                    s2 = i % 2
                    for g in range(2):
                        for kb in range(3):
                            si = cn["s"] % 3; cn["s"] += 1
                            P.group("pe", [lambda e, g=g, kb=kb, si=si: e.matmul(pS[si][:].rearrange("p (h q) -> p h q", h=4),
                                                              lhsT=kT[64 * g:64 * g + 64, (i + kb) * 128:(i + kb + 1) * 128],
                                                              rhs=qT[64 * g:64 * g + 64, :, i * 128:(i + 1) * 128], start=True, stop=True)],
                                    reads=[b_kT, b_qT], writes=[b_pS[si]])
                            P.op("act", lambda e, si=si: e.activation(out=pex[si][:], in_=pS[si][:], func=AF.Exp, scale=0.125),
                                 reads=[b_pS[si]], writes=[b_pex[si]])
                            if kb == 0 and i == 0:
                                em = emJ[:, g, 0, :]
                            elif kb == 2 and i == 31:
                                em = emJ[:, g, 1, :]
                            else:
                                em = tb[:, CT_EM + (g * 3 + kb) * 512:CT_EM + (g * 3 + kb + 1) * 512]
                            eng = "dve"
                            P.op(eng, lambda e, em=em, g=g, kb=kb, si=si: e.tensor_tensor(out=pmk[s2][g][kb][:], in0=pex[si][:], in1=em, op=ALU.mult),
                                 reads=[b_pex[si], b_tb, b_emJ], writes=[b_pmk[s2][g][kb]])
                def attn_part2(i):
                    s2 = i % 2
                    for g in range(2):
                        fns = []
                        for hh in range(4):
                            for kb in range(3):
                                fns.append(lambda e, hh=hh, kb=kb, g=g: e.matmul(pPV[s2][g][:, hh, :], lhsT=pmk[s2][g][kb][:, hh * 128:(hh + 1) * 128],
                                                                          rhs=Va[:, i + kb, g, :], start=(kb == 0), stop=(kb == 2)))
                        P.group("pe", fns, reads=[b_pmk[s2][g][0], b_pmk[s2][g][1], b_pmk[s2][g][2], b_Va], writes=[b_pPV[s2][g]])
                        P.op("dve", lambda e, g=g: e.tensor_tensor(out=den[:, g, :], in0=pPV[s2][g][:, :, 64], in1=esink[:, 4 * g:4 * g + 4], op=ALU.add),
                             reads=[b_pPV[s2][g], b_es], writes=[b_den])
                        P.op("dve", lambda e, g=g: e.reciprocal(out=den[:, g, :], in_=den[:, g, :]), reads=[b_den], writes=[b_den])
                        P.op("dve", lambda e, g=g: e.tensor_tensor(out=att[s2][:, 4 * g:4 * g + 4, :], in0=pPV[s2][g][:, :, 0:64],
                                                                   in1=den[:, g, :].unsqueeze(2).to_broadcast([128, 4, 64]), op=ALU.mult),
                             reads=[b_pPV[s2][g], b_den], writes=[b_att[s2]])
                    P.group("pe", [lambda e, c=c: e.transpose(out=pTa[:, c, :], in_=att[s2][:].rearrange("p h d -> p (h d)")[:, c * 128:(c + 1) * 128],
                                                               identity=identb) for c in range(4)],
                            reads=[b_att[s2], b_tb], writes=[b_pTa])
                    P.op("act", lambda e: e.copy(out=atT[s2][:], in_=pTa), reads=[b_pTa], writes=[b_atT[s2]])
                    P.dma("sp", lambda e: e.dma_start(out=AT[:, i * 128:(i + 1) * 128].rearrange("(c p) q -> p c q", p=128), in_=atT[s2][:]),
                          reads=[b_atT[s2]])

                attn_part1(0)
                for i in range(32):
                    if i + 1 < 32:
                        attn_part1(i + 1)
                    attn_part2(i)
                P.barrier()
        if debug == "p1b":
            return _finish(nc, P, out, gst)

        TWO_PI = 2.0 * math.pi
        with ExitStack() as st2:
            hdn2T = SB(st2, "hdn2T", [64, NFFT], BF16); b_h2 = Buf()
            w3b = SB(st2, "w3b", [64, 2048], BF16); b_w3 = Buf()
            P.dma("pool", lambda e: e.dma_start(out=w3b[:, :], in_=fw3[:, :]), writes=[b_w3])
            dec = SB(st2, "dec", [128, 2, 512], F32); b_dec = Buf()

            def load_dec(o):
                P.dma("sp", lambda e: e.dma_start(out=dec[0:64, o, :], in_=fdec[0:1, o * 1024:o * 1024 + 512].partition_broadcast(64)), writes=[b_dec])
                P.dma("sp", lambda e: e.dma_start(out=dec[64:128, o, :], in_=fdec[0:1, o * 1024 + 512:(o + 1) * 1024].partition_broadcast(64)), writes=[b_dec])
            load_dec(0); load_dec(1)
            P.op("act", lambda e: e.activation(out=dec[:], in_=dec[:], func=AF.Abs), reads=[b_dec], writes=[b_dec])
            P.op("dve", lambda e: e.tensor_scalar(out=dec[:], in0=dec[:], scalar1=tf[:, 389:390], scalar2=None, op0=ALU.mult),
                 reads=[b_dec, b_tf], writes=[b_dec])
            skA = SB(st2, "skA", [128, 1024], F32); b_sk = Buf()
            P.dma("sp", lambda e: e.dma_start(out=skA[:], in_=hskip[0:1, :].partition_broadcast(128)), writes=[b_sk])

            with ExitStack() as st:
                w1t = SB(st, "w1t", [33, 64], F32); w2t = SB(st, "w2t", [64, 64], F32); b_fw = Buf()
                fsc = SB(st, "fsc", [64, 8], F32); b_fsc = Buf()
                bnd = SB(st, "bnd", [33, 4], F32)
                P.dma("sp", lambda e: e.dma_start(out=w1t[:], in_=fw1[:, :]), writes=[b_fw])
                P.dma("sp", lambda e: e.dma_start(out=w2t[:], in_=fw2[:, :]), writes=[b_fw])
                P.dma("sp", lambda e: e.dma_start(out=bnd[:], in_=bands[:, :]), writes=[b_fw])
                for ci, srcap in enumerate((ff1, fb1, ff2, fb2)):
                    P.dma("sp", lambda e, ci=ci, srcap=srcap: e.dma_start(out=fsc[:, ci:ci + 1], in_=srcap[:, :]), writes=[b_fsc])
                for (a, b, o1, o2) in ((0, 1, 4, 5), (2, 3, 6, 7)):
                    P.op("dve", lambda e, a=a, b=b, o2=o2: e.tensor_tensor(out=fsc[:, o2:o2 + 1], in0=fsc[:, a:a + 1], in1=fsc[:, b:b + 1], op=ALU.mult),
                         reads=[b_fsc], writes=[b_fsc])
                    P.op("dve", lambda e, o2=o2: e.tensor_scalar(out=fsc[:, o2:o2 + 1], in0=fsc[:, o2:o2 + 1], scalar1=1.0 / TWO_PI, scalar2=8.5,
                                                                 op0=ALU.mult, op1=ALU.add), reads=[b_fsc], writes=[b_fsc])
                    P.op("dve", lambda e, a=a, o1=o1: e.tensor_scalar(out=fsc[:, o1:o1 + 1], in0=fsc[:, a:a + 1], scalar1=1.0 / TWO_PI, scalar2=None,
                                                                      op0=ALU.mult), reads=[b_fsc], writes=[b_fsc])
                idx = SB(st, "idx", [33, 16, 128], I32); b_idx = Buf()
                idxf = SB(st, "idxf", [33, 2048], F32); b_idxf = Buf()
                uu = SB(st, "uu", [64, 2048], F32); b_uu = Buf()
                ki = SB(st, "ki", [64, 2048], I32); b_ki = Buf()
                zT = SB(st, "zT", [33, 2048], F32); b_zT = Buf()
                h1 = SB(st, "h1", [64, 512], F32); b_h1 = Buf()
                pH = [PS(st, "pH%d" % i, [64, 512]) for i in range(2)]; b_pH = [Buf(), Buf()]

                def sin_reduce(np_, ncols, src, b_src, sc_mul, sc_add, dst, b_dst, extra_reads=()):
                    P.op("dve", lambda e: e.tensor_scalar(out=uu[0:np_, 0:ncols], in0=src, scalar1=sc_mul, scalar2=sc_add, op0=ALU.mult, op1=ALU.add),
                         reads=[b_src] + list(extra_reads), writes=[b_uu])
                    P.op("dve", lambda e: e.tensor_copy(out=ki[0:np_, 0:ncols], in_=uu[0:np_, 0:ncols]), reads=[b_uu], writes=[b_ki])
                    P.op("dve", lambda e: e.tensor_tensor(out=uu[0:np_, 0:ncols], in0=uu[0:np_, 0:ncols], in1=ki[0:np_, 0:ncols], op=ALU.subtract),
                         reads=[b_uu, b_ki], writes=[b_uu])
                    P.op("dve", lambda e: e.scalar_tensor_tensor(out=uu[0:np_, 0:ncols], in0=uu[0:np_, 0:ncols], scalar=0.0, in1=uu[0:np_, 0:ncols],
                                                                 op0=ALU.is_lt, op1=ALU.add), reads=[b_uu], writes=[b_uu])
                    P.op("act", lambda e: e.activation(out=dst, in_=uu[0:np_, 0:ncols], func=AF.Sin, bias=-math.pi, scale=TWO_PI),
                         reads=[b_uu], writes=[b_dst])

                def mlp_chunk(c):
                    P.op("pool", lambda e: e.iota(idx[:, :, 0:64], pattern=[[1, 16], [128, 64]], base=16 * c, channel_multiplier=0), writes=[b_idx])
                    P.op("pool", lambda e: e.iota(idx[:, :, 64:128], pattern=[[-1, 16], [-128, 64]], base=8192 - 16 * c, channel_multiplier=0), writes=[b_idx])
                    P.op("dve", lambda e: e.tensor_single_scalar(out=idx[:, :, 64:128], in_=idx[:, :, 64:128], scalar=8191, op=ALU.bitwise_and),
                         reads=[b_idx], writes=[b_idx])
                    P.op("dve", lambda e: e.tensor_copy(out=idxf[:], in_=idx[:].rearrange("p a b -> p (a b)")), reads=[b_idx], writes=[b_idxf])
                    sin_reduce(33, 2048, idxf[:], b_idxf, bnd[:, 0:1], bnd[:, 1:2], zT[:], b_zT, extra_reads=[b_fw])
                    P.op("dve", lambda e: e.tensor_scalar(out=zT[0:1, :], in0=idxf[0:1, :], scalar1=1.0 / (L - 1), scalar2=None, op0=ALU.mult),
                         reads=[b_idxf], writes=[b_zT])

                    def quarter(q):
                        s = q % 2
                        P.group("pe", [lambda e: e.matmul(pH[s][:], lhsT=w1t[:], rhs=zT[:, q * 512:(q + 1) * 512], start=True, stop=True)],
                                reads=[b_fw, b_zT], writes=[b_pH[s]])
                        sin_reduce(64, 512, pH[s][:], b_pH[s], fsc[:, 4:5], fsc[:, 5:6], h1[:], b_h1, extra_reads=[b_fsc])
                        P.group("pe", [lambda e: e.matmul(pH[s][:], lhsT=w2t[:], rhs=h1[:], start=True, stop=True)],
                                reads=[b_fw, b_h1], writes=[b_pH[s]])
                        sin_reduce(64, 512, pH[s][:], b_pH[s], fsc[:, 6:7], fsc[:, 7:8], hdn2T[:, c * 2048 + q * 512:c * 2048 + (q + 1) * 512], b_h2,
                                   extra_reads=[b_fsc])
                    for q in range(4):
                        quarter(q)
                for c in range(8):
                    mlp_chunk(c)
                P.barrier()

            kraw = SB(st2, "kraw", [128, 32, 128], F32); b_kraw = Buf()
            wtmp = SB(st2, "wtmp", [128, 32, 128], F32); b_wtmp = Buf()
            kcs = SB(st2, "kcs", [128, 32, 128], BF16); b_kcs = Buf()
            Khr = SB(st2, "Khr", [128, 32, 128], BF16); Khi = SB(st2, "Khi", [128, 32, 128], BF16); b_Kh = Buf()
            e1 = SB(st2, "e1", [128, 32], F32); b_e1 = Buf()
            ksum = SB(st2, "ksum", [128, 32], F32); b_ksum = Buf()
            r64 = SB(st2, "r64", [128, 32], F32); b_r64 = Buf()
            ut = [[SB(st2, "ut%d_%d" % (s, k), [64, 16, 128], BF16) for k in range(3)] for s in range(2)]
            b_ut = [[Buf() for k in range(3)] for s in range(2)]
            z1t = SB(st2, "z1t", [64, 16, 128], BF16); b_z1 = Buf()
            z2t = [SB(st2, "z2t%d" % s, [64, 16, 128], BF16) for s in range(2)]; b_z2 = [Buf(), Buf()]
            pK = [PS(st2, "pK%d" % i, [128, 512]) for i in range(2)]; b_pK = [Buf(), Buf()]
            pN = pK[0]; b_pN = b_pK[0]
            NL = 3
            lanes = []
            for li in range(NL):
                ln = {"pL": PS(st2, "pL%d" % li, [128, 1024]), "b_pL": Buf()}
                ln["S"] = SB(st2, "S%d" % li, [128, 1024], BF16); ln["b_S"] = Buf()
                ln["P1"] = [SB(st2, "P1_%d_%d" % (li, k), [128, 1024], BF16) for k in range(2)]; ln["b_P1"] = [Buf(), Buf()]
                ln["P2"] = [SB(st2, "P2_%d_%d" % (li, k), [128, 1024], BF16) for k in range(2)]; ln["b_P2"] = [Buf(), Buf()]
                ln["pk"] = 0
                lanes.append(ln)
            TC1 = tb[:, CT_TC1:CT_TC1 + 256].unsqueeze(1).to_broadcast([128, 4, 256])
            TC2 = tb[:, CT_TC2:CT_TC2 + 256].unsqueeze(1).to_broadcast([128, 4, 256])
            F1FULL = tb[:, CT_F1FULL:CT_F1FULL + 256]
            F1C = tb[0:64, CT_F1C:CT_F1C + 256]
            F2R = tb[:, CT_F2R:CT_F2R + 128]; F2I = tb[:, CT_F2I:CT_F2I + 128]; F2NI = tb[:, CT_F2NI:CT_F2NI + 128]
            G2A = tb[:, CT_G2A:CT_G2A + 256]; G2B = tb[:, CT_G2B:CT_G2B + 256]
            G1R = tb[:, CT_G1R:CT_G1R + 64]; G1I = tb[:, CT_G1I:CT_G1I + 64]
            w3v = w3b[:, :].rearrange("p (o d c) -> p o d c", o=2, d=2)

            F2NR = tb[:, CT_F2NR:CT_F2NR + 128]; G2NA = tb[:, CT_G2NA:CT_G2NA + 256]; G1NI = tb[:, CT_G1NI:CT_G1NI + 64]

            def cmul_ci(ln):
                k = ln["pk"]; ln["pk"] = 1 - k; ln["cur"] = k
                S4 = ln["S"][:, :].rearrange("p (c x) -> p c x", c=4)
                P1 = ln["P1"][k][:, :].rearrange("p (c x) -> p c x", c=4); P2 = ln["P2"][k][:, :].rearrange("p (c x) -> p c x", c=4)
                P.op("act", lambda e: e.copy(out=ln["S"][:, :], in_=ln["pL"][:, :]), reads=[ln["b_pL"]], writes=[ln["b_S"]])
                P.op("dve", lambda e: e.tensor_tensor(out=P1, in0=S4, in1=TC1, op=ALU.mult), reads=[ln["b_S"], b_tb], writes=[ln["b_P1"][k]])
                P.op("dve", lambda e: e.tensor_tensor(out=P2, in0=S4, in1=TC2, op=ALU.mult), reads=[ln["b_S"], b_tb], writes=[ln["b_P2"][k]])

            def cmul_ic(ln, f0):
                k = ln["pk"]; ln["pk"] = 1 - k; ln["cur"] = k
                S4 = ln["S"][:, :].rearrange("p (r c x) -> p r c x", r=2, c=4)
                P1 = ln["P1"][k][:, :].rearrange("p (r c x) -> p r c x", r=2, c=4); P2 = ln["P2"][k][:, :].rearrange("p (r c x) -> p r c x", r=2, c=4)
                kr = Khr[:, f0:f0 + 4, :].unsqueeze(1).to_broadcast([128, 2, 4, 128]); ki_ = Khi[:, f0:f0 + 4, :].unsqueeze(1).to_broadcast([128, 2, 4, 128])
                P.op("act", lambda e: e.copy(out=ln["S"][:, :], in_=ln["pL"][:, :]), reads=[ln["b_pL"]], writes=[ln["b_S"]])
                P.op("dve", lambda e: e.tensor_tensor(out=P1, in0=S4, in1=kr, op=ALU.mult), reads=[ln["b_S"], b_Kh], writes=[ln["b_P1"][k]])
                P.op("dve", lambda e: e.tensor_tensor(out=P2, in0=S4, in1=ki_, op=ALU.mult), reads=[ln["b_S"], b_Kh], writes=[ln["b_P2"][k]])

            def st_S1(ln, src, b_src, rhs):
                pA = ln["pL"][:, :].rearrange("p (c x) -> p c x", c=4)
                P.group("pe", [lambda e, cl=cl: e.matmul(pA[:, cl, :], lhsT=src[:, cl, :], rhs=rhs, start=True, stop=True) for cl in range(4)],
                        reads=[b_src, b_tb], writes=[ln["b_pL"]])

            def st_TW(ln):
                cmul_ci(ln)

            def st_S2(ln):
                k = ln["cur"]
                P1 = ln["P1"][k][:, :].rearrange("p (c x) -> p c x", c=4); P2 = ln["P2"][k][:, :].rearrange("p (c x) -> p c x", c=4)
                m0, m3, m2, m1 = P1[:, :, 0:128], P1[:, :, 128:256], P2[:, :, 0:128], P2[:, :, 128:256]
                pXr = ln["pL"][:, 0:512].rearrange("p (c x) -> p c x", c=4); pXi = ln["pL"][:, 512:1024].rearrange("p (c x) -> p c x", c=4)
                P.group("pe", [lambda e: e.matmul(pXr, lhsT=F2R, rhs=m0, start=True, stop=False),
                               lambda e: e.matmul(pXr, lhsT=F2NR, rhs=m1, start=False, stop=False),
                               lambda e: e.matmul(pXr, lhsT=F2NI, rhs=m2, start=False, stop=False),
                               lambda e: e.matmul(pXr, lhsT=F2NI, rhs=m3, start=False, stop=True),
                               lambda e: e.matmul(pXi, lhsT=F2R, rhs=m2, start=True, stop=False),
                               lambda e: e.matmul(pXi, lhsT=F2R, rhs=m3, start=False, stop=False),
                               lambda e: e.matmul(pXi, lhsT=F2I, rhs=m0, start=False, stop=False),
                               lambda e: e.matmul(pXi, lhsT=F2NI, rhs=m1, start=False, stop=True)],
                        reads=[ln["b_P1"][k], ln["b_P2"][k], b_tb], writes=[ln["b_pL"]])

            def st_SPEC(ln, f0):
                cmul_ic(ln, f0)

            def st_IS1(ln):
                k = ln["cur"]
                P1 = ln["P1"][k][:, :].rearrange("p (r c x) -> p r c x", r=2, c=4); P2 = ln["P2"][k][:, :].rearrange("p (r c x) -> p r c x", r=2, c=4)
                pB = ln["pL"][:, :].rearrange("p (c x) -> p c x", c=4)
                fns = []
                for cl in range(4):
                    fns.append(lambda e, cl=cl: e.matmul(pB[:, cl, :], lhsT=P1[:, 0, cl, :], rhs=G2A, start=True, stop=False))
                    fns.append(lambda e, cl=cl: e.matmul(pB[:, cl, :], lhsT=P2[:, 1, cl, :], rhs=G2NA, start=False, stop=False))
                    fns.append(lambda e, cl=cl: e.matmul(pB[:, cl, :], lhsT=P2[:, 0, cl, :], rhs=G2B, start=False, stop=False))
                    fns.append(lambda e, cl=cl: e.matmul(pB[:, cl, :], lhsT=P1[:, 1, cl, :], rhs=G2B, start=False, stop=True))
                P.group("pe", fns, reads=[ln["b_P1"][k], ln["b_P2"][k], b_tb], writes=[ln["b_pL"]])

            def st_ITW(ln):
                cmul_ci(ln)

            def st_IS2(ln):
                k = ln["cur"]
                P1 = ln["P1"][k][:, :].rearrange("p (c x) -> p c x", c=4); P2 = ln["P2"][k][:, :].rearrange("p (c x) -> p c x", c=4)
                n0, n3, n2, n1 = P1[:, :, 0:128], P1[:, :, 128:256], P2[:, :, 0:128], P2[:, :, 128:256]
                pY = ln["pL"][0:64, 0:512].rearrange("p (c x) -> p c x", c=4)
                P.group("pe", [lambda e: e.matmul(pY, lhsT=G1R, rhs=n0, start=True, stop=False),
                               lambda e: e.matmul(pY, lhsT=G1R, rhs=n1, start=False, stop=False),
                               lambda e: e.matmul(pY, lhsT=G1I, rhs=n3, start=False, stop=False),
                               lambda e: e.matmul(pY, lhsT=G1NI, rhs=n2, start=False, stop=True)],
                        reads=[ln["b_P1"][k], ln["b_P2"][k], b_tb], writes=[ln["b_pL"]])

            def st_gate(ln, cg, gin, b_gin, zout, b_zout):
                pY = ln["pL"][0:64, 0:512].rearrange("p (c x) -> p c x", c=4)
                P.op("dve", lambda e: e.tensor_tensor(out=zout, in0=pY, in1=gin, op=ALU.mult), reads=[ln["b_pL"], b_gin], writes=[b_zout])

            def sg_load(sg):
                us = sg % 2
                c0 = 16 * sg
                for k in range(3):
                    P.dma("pool", lambda e, k=k: e.dma_start(out=ut[us][k][:], in_=Uc[k * 512 + c0:k * 512 + c0 + 16, :].rearrange("c (a b) -> a c b", b=128)),
                          reads=[], writes=[b_ut[us][k]])

            def sg_kbatch(sg, bi):
                c0 = 16 * sg
                s = bi % 2
                pKv = pK[s][:, :].rearrange("p (n f) -> p n f", n=16)
                fns = []
                for nl in range(16):
                    n2 = bi * 16 + nl
                    fns.append(lambda e, nl=nl, n2=n2: e.matmul(pKv[0:64, nl, :].rearrange("p (o c) -> p o c", o=2),
                                                                lhsT=hdn2T[:, n2 * 128:n2 * 128 + 64], rhs=w3v[:, :, 0, c0:c0 + 16], start=True, stop=True))
                    fns.append(lambda e, nl=nl, n2=n2: e.matmul(pKv[64:128, nl, :].rearrange("p (o c) -> p o c", o=2),
                                                                lhsT=hdn2T[:, n2 * 128 + 64:n2 * 128 + 128], rhs=w3v[:, :, 1, c0:c0 + 16], start=True, stop=True))
                P.group("pe", fns, reads=[b_h2, b_w3], writes=[b_pK[s]])
                P.op("act", lambda e: e.copy(out=kraw[:, :, bi * 16:(bi + 1) * 16].rearrange("p f n -> p n f"), in_=pKv), reads=[b_pK[s]], writes=[b_kraw])

            def sg_window(sg):
                c0 = 16 * sg
                decv = dec[:, :, c0:c0 + 16]
                e1v = e1[:, :].rearrange("p (o c) -> p o c", o=2)
                wt4 = wtmp[:, :, :].rearrange("p (o c) n -> p o c n", o=2)

                def w0():
                    P.op("act", lambda e: e.activation(out=e1v, in_=decv, func=AF.Exp, scale=tf[:, 390:391]), reads=[b_dec, b_tf], writes=[b_e1])
                    P.op("pool", lambda e: e.tensor_tensor(out=wt4, in0=decv.unsqueeze(3).to_broadcast([128, 2, 16, 128]),
                                                           in1=tf[:, 528:656].unsqueeze(1).unsqueeze(1).to_broadcast([128, 2, 16, 128]), op=ALU.mult),
                         reads=[b_dec, b_tf], writes=[b_wtmp])

                def w1():
                    P.op("act", lambda e: e.activation(out=wtmp[:], in_=wtmp[:], func=AF.Exp), reads=[b_wtmp], writes=[b_wtmp])

                def w2():
                    P.op("pool", lambda e: e.tensor_tensor(out=wtmp[:], in0=wtmp[:], in1=e1[:, :].unsqueeze(2).to_broadcast([128, 32, 128]), op=ALU.mult),
                         reads=[b_wtmp, b_e1], writes=[b_wtmp])
                    P.op("pool", lambda e: e.tensor_scalar(out=r64[64:65, :], in0=kraw[64:65, :, 0], scalar1=1.05, scalar2=None, op0=ALU.mult),
                         reads=[b_kraw], writes=[b_r64])

                def w3():
                    P.op("dve", lambda e: e.scalar_tensor_tensor(out=kraw[:], in0=wtmp[:], scalar=0.05, in1=kraw[:], op0=ALU.add, op1=ALU.mult),
                         reads=[b_wtmp, b_kraw, b_r64], writes=[b_kraw])
                    P.op("pool", lambda e: e.tensor_copy(out=kraw[64:65, :, 0], in_=r64[64:65, :]), reads=[b_r64], writes=[b_kraw])

                def w4():
                    P.op("act", lambda e: e.activation(out=wtmp[:], in_=kraw[:], func=AF.Abs), reads=[b_kraw], writes=[b_wtmp])

                def w5():
                    P.op("dve", lambda e: e.tensor_reduce(out=ksum[:], in_=wtmp[:], axis=AX.X, op=ALU.add), reads=[b_wtmp], writes=[b_ksum])
                    P.group("pe", [lambda e: e.matmul(pN[:, 0:32], lhsT=tf[:, 656:784], rhs=ksum[:], start=True, stop=True)],
                            reads=[b_ksum, b_tf], writes=[b_pN])

                def w6():
                    P.op("dve", lambda e: e.reciprocal(out=ksum[:], in_=pN[:, 0:32]), reads=[b_pN], writes=[b_ksum])
                    P.op("pool", lambda e: e.memset(kraw[64:65, :, 0], 0.0), reads=[b_wtmp], writes=[b_kraw])

                def w7():
                    P.op("dve", lambda e: e.tensor_tensor(out=kcs[:], in0=kraw[:], in1=ksum[:, :].unsqueeze(2).to_broadcast([128, 32, 128]), op=ALU.mult),
                         reads=[b_kraw, b_ksum], writes=[b_kcs])
                    P.op("dve", lambda e: e.tensor_tensor(out=kcs[0:1, :, 0].rearrange("p (o c) -> p o c", o=2), in0=kcs[0:1, :, 0].rearrange("p (o c) -> p o c", o=2),
                                                          in1=skA[0:1, :].rearrange("p (o c) -> p o c", o=2)[:, :, c0:c0 + 16], op=ALU.add),
                         reads=[b_kcs, b_sk], writes=[b_kcs])
                    if debug == "p2" and sg == 0:
                        P.dma("sp", lambda e: e.dma_start(out=dbgkc[:, :, :], in_=kcs[:]), reads=[b_kcs])
                return [w0, w1, w2, w3, w4, w5, w6, w7]

            def sg_filtfft(sg):
                c0 = 16 * sg

                def filt_batch(f0s):
                    grp = [(lanes[li], f0) for li, f0 in enumerate(f0s)]
                    for ln, f0 in grp:
                        st_S1(ln, kcs[:, f0:f0 + 4, :], b_kcs, F1FULL)
                    for ln, f0 in grp:
                        st_TW(ln)
                    for ln, f0 in grp:
                        st_S2(ln)
                    for ln, f0 in grp:
                        pXr = ln["pL"][:, 0:512].rearrange("p (c x) -> p c x", c=4); pXi = ln["pL"][:, 512:1024].rearrange("p (c x) -> p c x", c=4)
                        P.op("act", lambda e, f0=f0, pXr=pXr: e.copy(out=Khr[:, f0:f0 + 4, :], in_=pXr), reads=[ln["b_pL"]], writes=[b_Kh])
                        P.op("act", lambda e, f0=f0, pXi=pXi: e.copy(out=Khi[:, f0:f0 + 4, :], in_=pXi), reads=[ln["b_pL"]], writes=[b_Kh])
                for f0s in ((0, 4, 8), (12, 16, 20), (24, 28)):
                    filt_batch(f0s)
                if debug == "p2" and sg == 0:
                    P.dma("sp", lambda e: e.dma_start(out=dbgkh[:, 0, :, :], in_=Khr[:]), reads=[b_Kh])
                    P.dma("sp", lambda e: e.dma_start(out=dbgkh[:, 1, :, :], in_=Khi[:]), reads=[b_Kh])

            def sg_conv_batch(sg, tasks, b_z1g, hooks=None):
                hk = (lambda k: hooks[k]()) if hooks else (lambda k: None)
                us = sg % 2
                grp = []
                for li, (o, cg) in enumerate(tasks):
                    if o == 0:
                        zin, b_zin = ut[us][0][:, 4 * cg:4 * cg + 4, :], b_ut[us][0]
                        gin, b_gin = ut[us][1][:, 4 * cg:4 * cg + 4, :], b_ut[us][1]
                        zout, b_zout = z1t[:, 4 * cg:4 * cg + 4, :], b_z1g[cg]
                    else:
                        zin, b_zin = z1t[:, 4 * cg:4 * cg + 4, :], b_z1g[cg]
                        gin, b_gin = ut[us][2][:, 4 * cg:4 * cg + 4, :], b_ut[us][2]
                        zout, b_zout = z2t[us][:, 4 * cg:4 * cg + 4, :], b_z2[us]
                    grp.append((lanes[li], o, cg, zin, b_zin, gin, b_gin, zout, b_zout))
                for (ln, o, cg, zin, b_zin, gin, b_gin, zout, b_zout) in grp:
                    st_S1(ln, zin, b_zin, F1C)
                hk(0)
                for g_ in grp:
                    st_TW(g_[0])
                hk(1)
                for g_ in grp:
                    st_S2(g_[0])
                hk(2)
                for g_ in grp:
                    st_SPEC(g_[0], g_[1] * 16 + 4 * g_[2])
                hk(3)
                for g_ in grp:
                    st_IS1(g_[0])
                hk(4)
                for g_ in grp:
                    st_ITW(g_[0])
                hk(5)
                for g_ in grp:
                    st_IS2(g_[0])
                hk(6)
                for (ln, o, cg, zin, b_zin, gin, b_gin, zout, b_zout) in grp:
                    st_gate(ln, cg, gin, b_gin, zout, b_zout)
                hk(7)

            def sg_store(sg):
                us = sg % 2
                c0 = 16 * sg
                P.dma("sp", lambda e: e.dma_start(out=Z2[c0:c0 + 16, :].rearrange("c (a b) -> a c b", b=128), in_=z2t[us][0:32, :, :]), reads=[b_z2[us]])

            cvs = [SB(st2, "cvs%d" % i, [128, 2048], BF16) for i in range(3)]; b_cvs = [Buf() for _ in range(3)]
            cv_tasks = [(ti, rb) for rb in range(ne // 128) for ti in range(3)]
            cv_state = {"i": 0}
            ew_src = (ew1, ew3, ew2)

            def cv():
                i = cv_state["i"]
                if i >= len(cv_tasks):
                    return
                cv_state["i"] = i + 1
                ti, rb = cv_tasks[i]
                s = i % 3
                P.dma("pool", lambda e: e.dma_start(out=cvs[s][:], in_=ew_src[ti][rb * 128:(rb + 1) * 128, :]), writes=[b_cvs[s]])
                ee, hh_ = rb // 2, rb % 2
                P.dma("sp", lambda e: e.dma_start(out=EWB[ti][ee * 128:(ee + 1) * 128, hh_ * 2048:(hh_ + 1) * 2048], in_=cvs[s][:]), reads=[b_cvs[s]])
            CONV_TASKS = (((0, 0), (0, 1), (0, 2)), ((0, 3), (1, 0), (1, 1)), ((1, 2), (1, 3)))
            nsg = 32 if debug != "p2" else int(DBG_NSG)
            b_z1g = [Buf() for _ in range(4)]
            sg_load(0)
            for bi in range(8):
                sg_kbatch(0, bi)
            for w_ in sg_window(0):
                w_()
            for sg in range(nsg):
                sg_filtfft(sg)
                nxt = sg + 1 < nsg
                if nxt:
                    sg_load(sg + 1)
                sg_conv_batch(sg, CONV_TASKS[0], b_z1g, hooks=[cv, cv, cv, cv, cv, cv, (lambda: None), (lambda: None)])
                if nxt:
                    for bi in range(0, 4):
                        sg_kbatch(sg + 1, bi)
                sg_conv_batch(sg, CONV_TASKS[1], b_z1g, hooks=[cv, cv, cv, cv, cv, cv, (lambda: None), (lambda: None)])
                if debug == "p2" and sg == 0:
                    P.dma("sp", lambda e: e.dma_start(out=dbgz1[:, :, :], in_=z1t[:]), reads=b_z1g)
                if nxt:
                    for bi in range(4, 8):
                        sg_kbatch(sg + 1, bi)
                sg_conv_batch(sg, CONV_TASKS[2], b_z1g, hooks=(sg_window(sg + 1) if nxt else None))
                sg_store(sg)
            while cv_state["i"] < len(cv_tasks):
                cv()
            P.barrier()
        if debug == "p2":
            return _finish(nc, P, out, gst)

        def layer_norm(st_tiles, r, b_r, g_b, b_b, b_gb, dst, b_dst, eng2="pool"):
            stats, mv, b_stat = st_tiles
            P.op("dve", lambda e: e.bn_stats(out=stats[:, 0, :], in_=r[:, 0:512]), reads=[b_r], writes=[b_stat])
            P.op("dve", lambda e: e.bn_stats(out=stats[:, 1, :], in_=r[:, 512:1024]), reads=[b_r], writes=[b_stat])
            P.op("dve", lambda e: e.bn_aggr(out=mv[:, 0:2], in_=stats[:].rearrange("p a b -> p (a b)")), reads=[b_stat], writes=[b_stat])
            P.op("dve", lambda e: e.tensor_scalar(out=mv[:, 2:3], in0=mv[:, 1:2], scalar1=LN_EPS, scalar2=None, op0=ALU.add), reads=[b_stat], writes=[b_stat])
            P.op("act", lambda e: e.activation(out=mv[:, 2:3], in_=mv[:, 2:3], func=AF.Sqrt), reads=[b_stat], writes=[b_stat])
            P.op("dve", lambda e: e.reciprocal(out=mv[:, 2:3], in_=mv[:, 2:3]), reads=[b_stat], writes=[b_stat])
            P.op("dve", lambda e: e.scalar_tensor_tensor(out=mv[:, 3:4], in0=mv[:, 0:1], scalar=-1.0, in1=mv[:, 2:3], op0=ALU.mult, op1=ALU.mult),
                 reads=[b_stat], writes=[b_stat])
            P.op("act", lambda e: e.activation(out=r[:], in_=r[:], func=AF.Identity, scale=mv[:, 2:3], bias=mv[:, 3:4]), reads=[b_r, b_stat], writes=[b_r])
            P.op(eng2, lambda e: e.tensor_tensor(out=r[:], in0=r[:], in1=g_b, op=ALU.mult), reads=[b_r, b_gb], writes=[b_r])
            P.op("dve", lambda e: e.tensor_tensor(out=dst, in0=r[:], in1=b_b, op=ALU.add), reads=[b_r, b_gb], writes=[b_dst])

        with ExitStack() as st34:
            dest = SB(st34, "dest", [128, 32, 2], I32); b_dest = Buf()
            wts = SB(st34, "wts", [128, 32, 2], F32); b_wts = Buf()
            widx = SB(st34, "widx", [128, 128, 2], I32); b_widx = Buf()
            with ExitStack() as st:
                wao = SB(st, "wao", [128, 4, D], BF16); who = SB(st, "who", [128, 4, D], BF16); wout = SB(st, "wout", [128, 8, D], BF16); b_w3p = Buf()

                def ldw(dst, srcw, nk):
                    for kc in range(nk):
                        P.dma("pool", lambda e, kc=kc: e.dma_start(out=dst[:, kc, :], in_=srcw[kc * 128:(kc + 1) * 128, :]), writes=[b_w3p])
                ldw(wao, w_attn_o, 4); ldw(who, w_hy_o, 4); ldw(wout, w_out, 8)
                bc = SB(st, "bc", [128, 5, D], F32); b_bc = Buf()
                for k, srcap in enumerate((mod_scr[0:1, 2048:3072], ln1g[0:1, :], ln1b[0:1, :], mod_scr[0:1, 4096:5120], mod_scr[0:1, 3072:4096])):
                    P.dma("sp", lambda e, k=k, srcap=srcap: e.dma_start(out=bc[:, k, :], in_=srcap.partition_broadcast(128)), writes=[b_bc])
                wrt = SB(st, "wrt", [128, 8, 72], F32); brb = SB(st, "brb", [128, 72], F32); b_wr = Buf()
                P.dma("sp", lambda e: e.dma_start(out=wrt[:], in_=wr[:, :].rearrange("(c p) n -> p c n", p=128)), writes=[b_wr])
                P.dma("sp", lambda e: e.dma_start(out=brb[:], in_=br[0:1, :].partition_broadcast(128)), writes=[b_wr])
                rcarry = SB(st, "rcarry", [128, 64], F32); b_rcarry = Buf()
                P.op("pool", lambda e: e.memset(rcarry[:], 0.0), writes=[b_rcarry])
                rnk = SB(st, "rnk", [128, 32, 2], F32); b_rnk = Buf()
                ohall = SB(st, "ohall", [128, 32, 2, 64], BF16); b_oh = Buf()
                atM = SB(st, "atTm", [128, 4, 512], BF16); z2T = SB(st, "z2T", [128, 4, 512], BF16)
                sga = SB(st, "sga", [128, 8, 512], BF16); sgh = SB(st, "sgh", [128, 8, 512], BF16)
                b_at = Buf(); b_z2T = Buf(); b_sga = Buf(); b_sgh = Buf()
                mrg = [SB(st, "mrg%d" % i, [128, 8, 512], BF16) for i in range(2)]; b_mrg = [Buf(), Buf()]
                t1 = [SB(st, "t1_%d" % i, [128, 512], F32) for i in range(2)]; t2 = [SB(st, "t2_%d" % i, [128, 512], F32) for i in range(2)]
                b_t1 = [Buf(), Buf()]; b_t2 = [Buf(), Buf()]
                xt = [SB(st, "xt%d" % i, [128, D], F32) for i in range(2)]; b_xt = [Buf(), Buf()]
                rr = [SB(st, "rr%d" % i, [128, D], F32) for i in range(2)]; b_rr = [Buf(), Buf()]
                x1s = [SB(st, "x1s%d" % i, [128, D], F32) for i in range(2)]; b_x1s = [Buf(), Buf()]
                h2 = SB(st, "h2", [128, D], F32); b_h2t = Buf()
                h2b = [SB(st, "h2b%d" % i, [128, D], BF16) for i in range(2)]; b_h2b = [Buf(), Buf()]
                h2T = SB(st, "h2T", [128, 8, 128], F32); b_h2T = Buf()
                stats = SB(st, "stats", [128, 2, 6], F32); mv = SB(st, "mv", [128, 4], F32); b_stat = Buf()
                lg = SB(st, "lg", [128, 72], F32); b_lg = Buf()
                sm = SB(st, "sm", [128, 64], F32); b_sm = Buf()
                elm = SB(st, "elm", [128, 64], F32); b_elm = Buf()
                m8 = SB(st, "m8", [128, 16], F32); b_m8 = Buf()
                ohs = SB(st, "ohs", [128, 64], BF16); b_ohs = Buf()
                basef = SB(st, "basef", [128, 64], F32); b_base = Buf()
                junk = SB(st, "junk", [128, 64], F32); b_junk = Buf()
                pMa = PS(st, "pMa", [128, 512]); pMh = PS(st, "pMh", [128, 512]); b_pMa = Buf(); b_pMh = Buf()
                pYt = PS(st, "pYt", [128, 1024]); b_pYt = Buf()
                pHT = PS(st, "pHT", [128, 1024]); b_pHT = Buf()
                pR = PS(st, "pR", [128, 512]); b_pR = Buf()
                pLg = PS(st, "pLg", [128, 512]); b_pLg = Buf()

                def route_tile(i, x1tile, b_x1tile, hs):
                    P.op("dve", lambda e: e.tensor_tensor(out=h2[:], in0=x1tile[:], in1=bc[:, 3, :], op=ALU.mult), reads=[b_x1tile, b_bc], writes=[b_h2t])
                    P.op("dve", lambda e: e.tensor_tensor(out=h2[:], in0=h2[:], in1=bc[:, 4, :], op=ALU.add), reads=[b_h2t, b_bc], writes=[b_h2t])
                    P.op("act", lambda e: e.copy(out=h2b[hs][:], in_=h2[:]), reads=[b_h2t], writes=[b_h2b[hs]])
                    P.dma("sp", lambda e: e.dma_start(out=H2[i * 128:(i + 1) * 128, :], in_=h2b[hs][:]), reads=[b_h2b[hs]])
                    pHTv = pHT[:, :].rearrange("p (c q) -> p c q", c=8)
                    P.group("pe", [lambda e, kc=kc: e.transpose(out=pHTv[:, kc, :], in_=h2[:, kc * 128:(kc + 1) * 128], identity=identf) for kc in range(8)],
                            reads=[b_h2t, b_tf], writes=[b_pHT])
                    P.op("act", lambda e: e.copy(out=h2T[:], in_=pHTv), reads=[b_pHT], writes=[b_h2T])
                    P.group("pe", [lambda e, kc=kc: e.matmul(pLg[:, 0:72], lhsT=h2T[:, kc, :], rhs=wrt[:, kc, :], start=(kc == 0), stop=(kc == 7)) for kc in range(8)],
                            reads=[b_h2T, b_wr], writes=[b_pLg])
                    P.op("dve", lambda e: e.tensor_tensor(out=lg[:], in0=pLg[:, 0:72], in1=brb[:], op=ALU.add), reads=[b_pLg, b_wr], writes=[b_lg])
                    P.op("dve", lambda e: e.max(out=m8[:, 0:8], in_=lg[:, 0:8]), reads=[b_lg], writes=[b_m8])
                    P.op("dve", lambda e: e.tensor_scalar(out=sm[:, 0:8], in0=lg[:, 0:8], scalar1=m8[:, 0:1], scalar2=None, op0=ALU.is_equal),
                         reads=[b_lg, b_m8], writes=[b_sm])
                    P.op("dve", lambda e: e.tensor_scalar(out=sm[:, 8:9], in0=m8[:, 0:1], scalar1=-1.0, scalar2=None, op0=ALU.mult), reads=[b_m8], writes=[b_sm])
                    P.op("act", lambda e: e.activation(out=sm[:, 16:24], in_=lg[:, 0:8], func=AF.Exp, bias=sm[:, 8:9], scale=1.0, accum_out=sm[:, 9:10]),
                         reads=[b_lg, b_sm], writes=[b_sm])
                    P.op("dve", lambda e: e.reciprocal(out=sm[:, 10:11], in_=sm[:, 9:10]), reads=[b_sm], writes=[b_sm])
                    P.op("dve", lambda e: e.tensor_scalar(out=sm[:, 24:32], in0=sm[:, 0:8], scalar1=1e30, scalar2=-1e30, op0=ALU.mult, op1=ALU.add),
                         reads=[b_sm], writes=[b_sm])
                    P.op("dve", lambda e: e.tensor_tensor(out=elm[:, :].rearrange("p (g e) -> p g e", g=8), in0=lg[:, 8:72].rearrange("p (g e) -> p g e", g=8),
                                                          in1=sm[:, 24:32].unsqueeze(2).to_broadcast([128, 8, 8]), op=ALU.add),
                         reads=[b_lg, b_sm], writes=[b_elm])
                    P.op("dve", lambda e: e.max(out=m8[:, 8:16], in_=elm[:]), reads=[b_elm], writes=[b_m8])
                    P.op("dve", lambda e: e.tensor_scalar(out=ohall[:, i, 0, :], in0=elm[:], scalar1=m8[:, 8:9], scalar2=None, op0=ALU.is_equal),
                         reads=[b_elm, b_m8], writes=[b_oh])
                    P.op("dve", lambda e: e.tensor_scalar(out=ohall[:, i, 1, :], in0=elm[:], scalar1=m8[:, 9:10], scalar2=None, op0=ALU.is_equal),
                         reads=[b_elm, b_m8], writes=[b_oh])
                    P.op("dve", lambda e: e.tensor_tensor(out=sm[:, 11:12], in0=m8[:, 9:10], in1=m8[:, 8:9], op=ALU.subtract), reads=[b_m8], writes=[b_sm])
                    P.op("act", lambda e: e.activation(out=sm[:, 12:13], in_=sm[:, 11:12], func=AF.Exp), reads=[b_sm], writes=[b_sm])
                    P.op("dve", lambda e: e.tensor_scalar(out=sm[:, 12:13], in0=sm[:, 12:13], scalar1=1.0, scalar2=None, op0=ALU.add), reads=[b_sm], writes=[b_sm])
                    P.op("dve", lambda e: e.reciprocal(out=sm[:, 13:14], in_=sm[:, 12:13]), reads=[b_sm], writes=[b_sm])
                    P.op("dve", lambda e: e.tensor_tensor(out=wts[:, i, 0:1], in0=sm[:, 13:14], in1=sm[:, 10:11], op=ALU.mult), reads=[b_sm], writes=[b_wts])
                    P.op("dve", lambda e: e.tensor_tensor(out=wts[:, i, 1:2], in0=sm[:, 10:11], in1=wts[:, i, 0:1], op=ALU.subtract), reads=[b_sm, b_wts], writes=[b_wts])
                    P.op("dve", lambda e: e.tensor_tensor(out=ohs[:], in0=ohall[:, i, 0, :], in1=ohall[:, i, 1, :], op=ALU.add), reads=[b_oh], writes=[b_ohs])
                    P.group("pe", [lambda e: e.matmul(pR[:, 0:64], lhsT=tb[:, CT_TRI:CT_TRI + 128], rhs=ohs[:], start=True, stop=True),
                                   lambda e: e.matmul(pR[:, 64:128], lhsT=tb[:, CT_ONES:CT_ONES + 128], rhs=ohs[:], start=True, stop=True)],
                            reads=[b_ohs, b_tb], writes=[b_pR])
                    P.op("dve", lambda e: e.tensor_tensor(out=basef[:], in0=pR[:, 0:64], in1=rcarry[:], op=ALU.add), reads=[b_pR, b_rcarry], writes=[b_base])
                    for k in range(2):
                        P.op("dve", lambda e, k=k: e.tensor_tensor(out=junk[:], in0=ohall[:, i, k, :], in1=basef[:], op=ALU.mult),
                             reads=[b_oh, b_base], writes=[b_junk])
                        P.op("dve", lambda e, k=k: e.tensor_reduce(out=rnk[:, i, k:k + 1], in_=junk[:], axis=AX.X, op=ALU.add),
                             reads=[b_junk], writes=[b_rnk])
                    P.op("dve", lambda e: e.tensor_tensor(out=rcarry[:], in0=rcarry[:], in1=pR[:, 64:128], op=ALU.add), reads=[b_pR, b_rcarry], writes=[b_rcarry])

                def merge_loads(s_i):
                    c0 = s_i * 512
                    P.dma("sp", lambda e: e.dma_start(out=atM[:], in_=AT[:, c0:c0 + 512].rearrange("(c p) q -> p c q", p=128)), writes=[b_at])
                    P.dma("sp", lambda e: e.dma_start(out=z2T[:], in_=Z2[:, c0:c0 + 512].rearrange("(c p) q -> p c q", p=128)), writes=[b_z2T])
                    P.dma("sp", lambda e: e.dma_start(out=sga[:], in_=Gs[0:1024, c0:c0 + 512].rearrange("(c p) q -> p c q", p=128)), writes=[b_sga])
                    P.dma("sp", lambda e: e.dma_start(out=sgh[:], in_=Gs[1024:2048, c0:c0 + 512].rearrange("(c p) q -> p c q", p=128)), writes=[b_sgh])

                def merge_compute(s_i):
                    ms = s_i % 2

                    def fchunk(fc):
                        ts = fc % 2
                        P.group("pe", [lambda e, kc=kc: e.matmul(pMa[:], lhsT=wao[:, kc, fc * 128:(fc + 1) * 128], rhs=atM[:, kc, :], start=(kc == 0), stop=(kc == 3))
                                       for kc in range(4)], reads=[b_w3p, b_at], writes=[b_pMa])
                        P.group("pe", [lambda e, kc=kc: e.matmul(pMh[:], lhsT=who[:, kc, fc * 128:(fc + 1) * 128], rhs=z2T[:, kc, :], start=(kc == 0), stop=(kc == 3))
                                       for kc in range(4)], reads=[b_w3p, b_z2T], writes=[b_pMh])
                        P.op("dve", lambda e: e.tensor_tensor(out=t1[ts][:], in0=pMa[:], in1=sga[:, fc, :], op=ALU.mult), reads=[b_pMa, b_sga], writes=[b_t1[ts]])
                        P.op("dve", lambda e: e.tensor_tensor(out=t2[ts][:], in0=pMh[:], in1=sgh[:, fc, :], op=ALU.mult), reads=[b_pMh, b_sgh], writes=[b_t2[ts]])
                        P.op("dve", lambda e: e.tensor_tensor(out=mrg[ms][:, fc, :], in0=t1[ts][:], in1=t2[ts][:], op=ALU.add),
                             reads=[b_t1[ts], b_t2[ts]], writes=[b_mrg[ms]])
                    for fc in range(8):
                        fchunk(fc)

                def tile_A(s_i, t):
                    ms = s_i % 2
                    i = s_i * 4 + t
                    xs_ = i % 2
                    P.dma("sp", lambda e: e.dma_start(out=xt[xs_][:], in_=xcat[i * 128:(i + 1) * 128, :]), writes=[b_xt[xs_]])
                    fns = []
                    for nh in range(2):
                        for kc in range(8):
                            fns.append(lambda e, nh=nh, kc=kc: e.matmul(pYt[:, nh * 512:(nh + 1) * 512], lhsT=mrg[ms][:, kc, t * 128:(t + 1) * 128],
                                                                      rhs=wout[:, kc, nh * 512:(nh + 1) * 512], start=(kc == 0), stop=(kc == 7)))
                    P.group("pe", fns, reads=[b_mrg[ms], b_w3p], writes=[b_pYt])
                    P.op("dve", lambda e: e.tensor_tensor(out=rr[xs_][:], in0=pYt[:], in1=bc[:, 0, :], op=ALU.mult), reads=[b_pYt, b_bc], writes=[b_rr[xs_]])
                    P.op("dve", lambda e: e.scalar_tensor_tensor(out=rr[xs_][:], in0=xt[xs_][:], scalar=DN_ALPHA, in1=rr[xs_][:], op0=ALU.mult, op1=ALU.add),
                         reads=[b_xt[xs_], b_rr[xs_]], writes=[b_rr[xs_]])
                    layer_norm((stats, mv, b_stat), rr[xs_], b_rr[xs_], bc[:, 1, :], bc[:, 2, :], b_bc, x1s[xs_][:], b_x1s[xs_], eng2="dve")
                    P.dma("act", lambda e: e.dma_start(out=X1[i * 128:(i + 1) * 128, :], in_=x1s[xs_][:]), reads=[b_x1s[xs_]])

                def tile_B(s_i, t):
                    i = s_i * 4 + t
                    xs_ = i % 2
                    route_tile(i, x1s[xs_], b_x1s[xs_], xs_)

                merge_loads(0)
                merge_compute(0)
                for s_i in range(8):
                    nx = s_i + 1 < 8
                    if nx:
                        merge_loads(s_i + 1)
                    tile_A(s_i, 0)
                    tile_A(s_i, 1)
                    tile_B(s_i, 0)
                    if nx:
                        merge_compute(s_i + 1)
                    tile_A(s_i, 2)
                    tile_B(s_i, 1)
                    tile_A(s_i, 3)
                    tile_B(s_i, 2)
                    tile_B(s_i, 3)

                ci = SB(st, "cnt_i", [128, 64], I32); b_ci = Buf()
                psz = SB(st, "psz", [128, 64], F32); pends = SB(st, "pends", [128, 64], F32); poffs = SB(st, "poffs", [128, 64], F32)
                zer = SB(st, "zer", [128, 64], F32); b_pz = Buf()
                P.op("dve", lambda e: e.tensor_scalar(out=psz[:], in0=rcarry[:], scalar1=127.0, scalar2=None, op0=ALU.add), reads=[b_rcarry], writes=[b_pz])
                P.op("dve", lambda e: e.tensor_copy(out=ci[:], in_=psz[:]), reads=[b_pz], writes=[b_ci])
                P.op("dve", lambda e: e.tensor_scalar(out=ci[:], in0=ci[:], scalar1=7, scalar2=7, op0=ALU.arith_shift_right, op1=ALU.logical_shift_left),
                     reads=[b_ci], writes=[b_ci])
                P.op("dve", lambda e: e.tensor_copy(out=psz[:], in_=ci[:]), reads=[b_ci], writes=[b_pz])
                P.op("dve", lambda e: e.memset(zer[:], 0.0), writes=[b_pz])
                P.op("dve", lambda e: e.tensor_tensor_scan(out=pends[:], data0=psz[:], data1=zer[:], initial=0.0, op0=ALU.add, op1=ALU.add), reads=[b_pz], writes=[b_pz])
                P.op("dve", lambda e: e.tensor_tensor(out=poffs[:], in0=pends[:], in1=psz[:], op=ALU.subtract), reads=[b_pz], writes=[b_pz])
                big = SB(st, "big", [128, 64, 64], F32); b_big = Buf()
                dsf = SB(st, "dsf", [128, 64], F32); b_dsf = Buf()
                P.op("dve", lambda e: e.tensor_tensor(out=big[:], in0=ohall[:].rearrange("p i k e -> p (i k) e"),
                                                      in1=poffs[:, :].unsqueeze(1).to_broadcast([128, 64, 64]), op=ALU.mult), reads=[b_oh, b_pz], writes=[b_big])
                P.op("dve", lambda e: e.tensor_reduce(out=dsf[:], in_=big[:], axis=AX.X, op=ALU.add), reads=[b_big], writes=[b_dsf])
                P.op("dve", lambda e: e.tensor_tensor(out=dsf[:], in0=dsf[:], in1=rnk[:].rearrange("p i k -> p (i k)"), op=ALU.add), reads=[b_dsf, b_rnk], writes=[b_dsf])
                P.op("dve", lambda e: e.tensor_copy(out=dest[:].rearrange("p i k -> p (i k)"), in_=dsf[:]), reads=[b_dsf], writes=[b_dest])
                blke = SB(st, "blke", [128, 128], F32); b_blke = Buf()

                def blk_chunk(jc):
                    bigv = big[:, 0:32, :]
                    P.op("dve", lambda e: e.tensor_tensor(out=bigv, in0=pends[:, :].unsqueeze(1).to_broadcast([128, 32, 64]),
                                                          in1=tf[:, 400 + jc * 32:400 + (jc + 1) * 32].unsqueeze(2).to_broadcast([128, 32, 64]), op=ALU.is_le),
                         reads=[b_pz, b_tf, b_dsf], writes=[b_big])
                    P.op("dve", lambda e: e.tensor_reduce(out=blke[:, jc * 32:(jc + 1) * 32], in_=bigv, axis=AX.X, op=ALU.add), reads=[b_big], writes=[b_blke])
                for jc in range(4):
                    blk_chunk(jc)
                skipf = SB(st, "skipf", [128, 128], F32); b_skipf = Buf()
                P.op("dve", lambda e: e.memset(skipf[:, 0:3], 0.0), writes=[b_skipf])
                P.op("dve", lambda e: e.tensor_scalar(out=blke[:], in0=blke[:], scalar1=63.0, scalar2=None, op0=ALU.min), reads=[b_blke], writes=[b_blke])
                P.op("dve", lambda e: e.tensor_tensor(out=skipf[:, 3:128], in0=blke[:, 3:128], in1=blke[:, 0:125], op=ALU.is_equal), reads=[b_blke, b_skipf], writes=[b_skipf])
                P.op("dve", lambda e: e.tensor_scalar(out=blke[:], in0=blke[:], scalar1=128.0, scalar2=None, op0=ALU.mult), reads=[b_blke, b_skipf], writes=[b_blke])
                P.op("dve", lambda e: e.scalar_tensor_tensor(out=blke[:], in0=skipf[:], scalar=1000000.0, in1=blke[:], op0=ALU.mult, op1=ALU.add),
                     reads=[b_blke, b_skipf], writes=[b_blke])
                P.op("dve", lambda e: e.tensor_scalar(out=blke[:], in0=blke[:], scalar1=tf[:, 384:385], scalar2=None, op0=ALU.add), reads=[b_blke, b_tf], writes=[b_blke])
                P.op("dve", lambda e: e.tensor_copy(out=widx[:, :, 0], in_=blke[:]), reads=[b_blke], writes=[b_widx])
                P.op("dve", lambda e: e.tensor_scalar(out=blke[:], in0=blke[:], scalar1=128.0, scalar2=None, op0=ALU.add), reads=[b_blke, b_widx], writes=[b_blke])
                P.op("dve", lambda e: e.tensor_copy(out=widx[:, :, 1], in_=blke[:]), reads=[b_blke], writes=[b_widx])
                if debug in ("p3", "p4"):
                    dbgrt = dscr("dbgrt", [128, 64 + 64 + 256], F32)
                    dbt = SB(st, "dbt", [128, 384], F32); b_dbt = Buf()
                    P.op("dve", lambda e: e.tensor_copy(out=dbt[:, 0:64], in_=dest[:].rearrange("p i k -> p (i k)")), reads=[b_dest], writes=[b_dbt])
                    P.op("dve", lambda e: e.tensor_copy(out=dbt[:, 64:128], in_=wts[:].rearrange("p i k -> p (i k)")), reads=[b_wts], writes=[b_dbt])
                    P.op("dve", lambda e: e.tensor_copy(out=dbt[:, 128:384], in_=widx[:].rearrange("p j h -> p (j h)")), reads=[b_widx], writes=[b_dbt])
                    P.dma("sp", lambda e: e.dma_start(out=dbgrt[:, :], in_=dbt[:]), reads=[b_dbt])
                P.barrier()
            if debug == "p3":
                return _finish(nc, P, out, gst)

            with ExitStack() as st:
                hrow = [SB(st, "hrow%d" % i, [128, D], BF16) for i in range(3)]; b_hrow = [Buf() for _ in range(3)]

                def scat(i):
                    s = i % 3
                    P.dma("sp", lambda e: e.dma_start(out=hrow[s][:], in_=H2[i * 128:(i + 1) * 128, :]), writes=[b_hrow[s]])
                    for k in range(2):
                        P.dma("pool", lambda e, k=k: e.indirect_dma_start(out=XB[:, :], out_offset=bass.IndirectOffsetOnAxis(ap=dest[:, i, k:k + 1], axis=0),
                                                                          in_=hrow[s][:], in_offset=None),
                              reads=[b_hrow[s], b_dest])
                for i in range(32):
                    scat(i)
                P.barrier()

            with ExitStack() as st:
                w1s = [SB(st, "w1s%d" % i, [128, 2, 2048], BF16) for i in range(3)]
                w3s = [SB(st, "w3s%d" % i, [128, 2, 2048], BF16) for i in range(3)]
                w2s = [SB(st, "w2s%d" % i, [128, 2, 2048], BF16) for i in range(3)]
                b_w1s = [Buf() for _ in range(3)]; b_w3s = [Buf() for _ in range(3)]; b_w2s = [Buf() for _ in range(3)]
                xbt = [SB(st, "xbt%d" % i, [128, D], BF16) for i in range(3)]; b_xbt = [Buf() for _ in range(3)]
                xbT = [SB(st, "xbT%d" % i, [128, 8, 128], BF16) for i in range(2)]; b_xbT = [Buf(), Buf()]
                slu = SB(st, "slu", [128, 512], F32); b_slu = Buf()
                ggb = SB(st, "ggb", [128, 512], BF16); b_ggb = Buf()
                gTb = SB(st, "gTb", [128, 4, 128], BF16); b_gTb = Buf()
                ybt = [SB(st, "ybt%d" % i, [128, D], BF16) for i in range(2)]; b_ybt = [Buf(), Buf()]
                pXT = PS(st, "pXT", [128, 1024], BF16); b_pXT = Buf()
                pH1 = PS(st, "pE1", [128, 512]); pH3 = PS(st, "pE3", [128, 512]); b_pH1 = Buf(); b_pH3 = Buf()
                pGT = PS(st, "pGT", [128, 1024], BF16); b_pGT = Buf()
                pO2 = PS(st, "pE2", [128, 1024]); b_pO2 = Buf()

                _bcr = {}

                def _bc_reg(e):
                    if "r" not in _bcr:
                        _bcr["r"] = e.to_reg(ne // 2 - 1)
                    return _bcr["r"]

                NW = 3

                def stG(j):
                    s = j % NW
                    for (wsb, wdram, bw) in ((w1s, EWB[0], b_w1s), (w3s, EWB[1], b_w3s), (w2s, EWB[2], b_w2s)):
                        P.dma("pool", lambda e, wsb=wsb, wdram=wdram: e.indirect_dma_start(
                            out=wsb[s][:, :, :].rearrange("p h n -> p (h n)"), out_offset=None, in_=wdram[:, :],
                            in_offset=bass.IndirectOffsetOnAxis(ap=widx[:, j, 0:1], axis=0), bounds_check=_bc_reg(e), oob_is_err=False),
                            reads=[b_widx], writes=[bw[s]])
                    x3 = j % 3
                    P.dma("sp", lambda e: e.dma_start(out=xbt[x3][:], in_=XB[j * 128:(j + 1) * 128, :]), writes=[b_xbt[x3]])

                def stAT(j):
                    x2 = j % 2
                    x3 = j % 3
                    pXTv = pXT[:, :].rearrange("p (c q) -> p c q", c=8)
                    P.group("pe", [lambda e, kc=kc: e.transpose(out=pXTv[:, kc, :], in_=xbt[x3][:, kc * 128:(kc + 1) * 128], identity=identb) for kc in range(8)],
                            reads=[b_xbt[x3], b_tb], writes=[b_pXT])
                    P.op("dve", lambda e: e.tensor_copy(out=xbT[x2][:], in_=pXTv), reads=[b_pXT], writes=[b_xbT[x2]])

                def stBM(j):
                    s = j % NW
                    x2 = j % 2
                    P.group("pe", [lambda e, kc=kc: e.matmul(pH1[:], lhsT=xbT[x2][:, kc, :], rhs=w1s[s][:, kc // 4, (kc % 4) * 512:(kc % 4 + 1) * 512],
                                                             start=(kc == 0), stop=(kc == 7)) for kc in range(8)],
                            reads=[b_xbT[x2], b_w1s[s]], writes=[b_pH1])
                    P.group("pe", [lambda e, kc=kc: e.matmul(pH3[:], lhsT=xbT[x2][:, kc, :], rhs=w3s[s][:, kc // 4, (kc % 4) * 512:(kc % 4 + 1) * 512],
                                                             start=(kc == 0), stop=(kc == 7)) for kc in range(8)],
                            reads=[b_xbT[x2], b_w3s[s]], writes=[b_pH3])
                    P.op("act", lambda e: e.activation(out=slu[:], in_=pH1[:], func=AF.Silu), reads=[b_pH1], writes=[b_slu])
                    P.op("dve", lambda e: e.tensor_tensor(out=ggb[:], in0=pH3[:], in1=slu[:], op=ALU.mult), reads=[b_pH3, b_slu], writes=[b_ggb])

                def stCT(j):
                    pGTv = pGT[:, 0:512].rearrange("p (c q) -> p c q", c=4)
                    P.group("pe", [lambda e, fc=fc: e.transpose(out=pGTv[:, fc, :], in_=ggb[:, fc * 128:(fc + 1) * 128], identity=identb) for fc in range(4)],
                            reads=[b_ggb, b_tb], writes=[b_pGT])
                    P.op("act", lambda e: e.copy(out=gTb[:], in_=pGTv), reads=[b_pGT], writes=[b_gTb])

                def stDM(j):
                    s = j % NW
                    y2 = j % 2
                    fns = []
                    for nh in range(2):
                        for fc in range(4):
                            fns.append(lambda e, nh=nh, fc=fc: e.matmul(pO2[:, nh * 512:(nh + 1) * 512], lhsT=gTb[:, fc, :],
                                                                      rhs=w2s[s][:, fc // 2, (fc % 2) * 1024 + nh * 512:(fc % 2) * 1024 + (nh + 1) * 512],
                                                                      start=(fc == 0), stop=(fc == 3)))
                    P.group("pe", fns, reads=[b_gTb, b_w2s[s]], writes=[b_pO2])
                    P.op("act", lambda e: e.copy(out=ybt[y2][:, 0:512], in_=pO2[:, 0:512]), reads=[b_pO2], writes=[b_ybt[y2]])
                    P.op("dve", lambda e: e.tensor_copy(out=ybt[y2][:, 512:1024], in_=pO2[:, 512:1024]), reads=[b_pO2], writes=[b_ybt[y2]])
                    P.dma("act", lambda e: e.dma_start(out=YB[j * 128:(j + 1) * 128, :], in_=ybt[y2][:]), reads=[b_ybt[y2]])

                for j in range(3):
                    stG(j)
                stAT(0); stAT(1); stBM(0)
                for j in range(NBLK):
                    stCT(j)
                    if j + 1 < NBLK:
                        stBM(j + 1)
                    if j + 2 < NBLK:
                        stAT(j + 2)
                    stDM(j)
                    if j + 3 < NBLK:
                        stG(j + 3)
                P.barrier()
            if debug == "p4":
                return _finish(nc, P, out, gst)

            with ExitStack() as st:
                bc2 = SB(st, "bc2", [128, 3, D], F32); b_bc2 = Buf()
                for k, srcap in enumerate((mod_scr[0:1, 5120:6144], ln2g[0:1, :], ln2b[0:1, :])):
                    P.dma("sp", lambda e, k=k, srcap=srcap: e.dma_start(out=bc2[:, k, :], in_=srcap.partition_broadcast(128)), writes=[b_bc2])
                NR = 3
                ra = [SB(st, "ra%d" % i, [128, D], F32) for i in range(NR)]; rb = [SB(st, "rb%d" % i, [128, D], BF16) for i in range(NR)]
                rab = [SB(st, "rab%d" % i, [128, D], BF16) for i in range(NR)]
                b_ra = [Buf() for _ in range(NR)]; b_rb = [Buf() for _ in range(NR)]
                x1c = [SB(st, "x1c%d" % i, [128, D], F32) for i in range(NR)]; b_x1c = [Buf() for _ in range(NR)]
                oc_ = [SB(st, "oc%d" % i, [128, D], F32) for i in range(2)]; b_oc = [Buf(), Buf()]
                stats2 = SB(st, "stats2", [128, 2, 6], F32); mv2 = SB(st, "mv2", [128, 4], F32); b_stat2 = Buf()

                def comb_load(i):
                    s = i % NR
                    P.dma("pool", lambda e: e.indirect_dma_start(out=rab[s][:], out_offset=None, in_=YB[:, :],
                                                                 in_offset=bass.IndirectOffsetOnAxis(ap=dest[:, i, 0:1], axis=0)), reads=[b_dest], writes=[b_ra[s]])
                    P.dma("pool", lambda e: e.indirect_dma_start(out=rb[s][:], out_offset=None, in_=YB[:, :],
                                                                 in_offset=bass.IndirectOffsetOnAxis(ap=dest[:, i, 1:2], axis=0)), reads=[b_dest], writes=[b_rb[s]])
                    P.dma("sp", lambda e: e.dma_start(out=x1c[s][:], in_=X1[i * 128:(i + 1) * 128, :]), writes=[b_x1c[s]])

                def comb(i):
                    s = i % NR
                    so = i % 2
                    P.op("act", lambda e: e.activation(out=ra[s][:], in_=rab[s][:], func=AF.Identity, scale=wts[:, i, 0:1]), reads=[b_ra[s], b_wts], writes=[b_ra[s]])
                    P.op("dve", lambda e: e.scalar_tensor_tensor(out=ra[s][:], in0=rb[s][:], scalar=wts[:, i, 1:2], in1=ra[s][:], op0=ALU.mult, op1=ALU.add),
                         reads=[b_rb[s], b_ra[s], b_wts], writes=[b_ra[s]])
                    P.op("dve", lambda e: e.tensor_tensor(out=ra[s][:], in0=ra[s][:], in1=bc2[:, 0, :], op=ALU.mult), reads=[b_ra[s], b_bc2], writes=[b_ra[s]])
                    P.op("dve", lambda e: e.scalar_tensor_tensor(out=ra[s][:], in0=x1c[s][:], scalar=DN_ALPHA, in1=ra[s][:], op0=ALU.mult, op1=ALU.add),
                         reads=[b_x1c[s], b_ra[s]], writes=[b_ra[s]])
                    layer_norm((stats2, mv2, b_stat2), ra[s], b_ra[s], bc2[:, 1, :], bc2[:, 2, :], b_bc2, oc_[so][:], b_oc[so], eng2="dve")
                    P.dma("sp", lambda e: e.dma_start(out=out[i * 128:(i + 1) * 128, :], in_=oc_[so][:]), reads=[b_oc[so]])
                comb_load(0); comb_load(1)
                for i in range(32):
                    if i + 2 < 32:
                        comb_load(i + 2)
                    comb(i)
    return _finish(nc, P, out, gst)


def _finish(nc, P, out, gst):
    P.barrier()
    P.emit()
    return nc


def make_in_maps(inp):
    f32 = np.float32
    x = np.asarray(inp["x"], f32)
    c = np.asarray(inp["c"], f32)
    perm = q_perm()
    w_in = np.ascontiguousarray(np.asarray(inp["w_in"], f32)[0][:, perm])
    conv_w = np.asarray(inp["conv_w"], f32)[0]
    conv_b = np.asarray(inp["conv_b"], f32)[0]
    cwl = np.ascontiguousarray(conv_w.reshape(3, 12, 128).transpose(2, 0, 1))
    cbl = np.ascontiguousarray(conv_b.reshape(12, 128).T)

    def ewl(w, kchunks):
        E, K, N = w.shape
        return np.ascontiguousarray(w.reshape(E, 2, kchunks, 128, N).transpose(0, 1, 3, 2, 4).reshape(E * 2 * 128, kchunks * N))

    ew1 = ewl(np.asarray(inp["exp_w1"], f32)[0], 4)
    ew3 = ewl(np.asarray(inp["exp_w3"], f32)[0], 4)
    ew2 = ewl(np.asarray(inp["exp_w2"], f32)[0], 2)
    wrr = np.ascontiguousarray(np.concatenate([np.asarray(inp["router_group_w"], f32)[0], np.asarray(inp["router_expert_w"], f32)[0]], axis=1))
    brr = np.ascontiguousarray(np.concatenate([np.asarray(inp["router_group_b"], f32)[0], np.asarray(inp["router_expert_b"], f32)[0]])[None, :])
    shared = {
        "w_ada": np.asarray(inp["w_ada"], f32)[0], "b_ada": np.asarray(inp["b_ada"], f32)[0][None, :],
        "w_in": w_in, "conv_w": cwl, "conv_b": cbl,
        "fw1": np.asarray(inp["filt_w1"], f32)[0], "fb1": np.asarray(inp["filt_b1"], f32)[0][:, None],
        "ff1": np.asarray(inp["filt_freq1"], f32)[0][:, None],
        "fw2": np.asarray(inp["filt_w2"], f32)[0], "fb2": np.asarray(inp["filt_b2"], f32)[0][:, None],
        "ff2": np.asarray(inp["filt_freq2"], f32)[0][:, None],
        "fw3": np.asarray(inp["filt_w3"], f32)[0], "fdec": np.asarray(inp["filt_decay"], f32)[0][None, :],
        "hskip": np.asarray(inp["hy_skip"], f32)[0].reshape(1, 1024),
        "w_hy_o": np.asarray(inp["w_hy_o"], f32)[0], "w_attn_o": np.asarray(inp["w_attn_o"], f32)[0],
        "sink": np.asarray(inp["attn_sink"], f32)[0][None, :], "w_out": np.asarray(inp["w_out"], f32)[0],
        "ln1g": np.asarray(inp["ln1_g"], f32)[0][None, :], "ln1b": np.asarray(inp["ln1_b"], f32)[0][None, :],
        "ln2g": np.asarray(inp["ln2_g"], f32)[0][None, :], "ln2b": np.asarray(inp["ln2_b"], f32)[0][None, :],
        "wr": wrr, "br": brr, "ew1": ew1, "ew3": ew3, "ew2": ew2,
    }
    bnd = np.zeros((33, 4), f32)
    bv = np.linspace(1e-4, 15.0, 16, dtype=f32)
    bnd[0, 0] = 0.0; bnd[0, 1] = 0.5
    bnd[1:17, 0] = bv / L; bnd[1:17, 1] = 0.25 + 0.5
    bnd[17:33, 0] = bv / L; bnd[17:33, 1] = 0.5 + 0.5
    shared["bands"] = bnd
    shared = {k: np.ascontiguousarray(v) for k, v in shared.items()}
    consts = [host_constants(0), host_constants(1)]
    maps = []
    for i in range(NCORES):
        b, half = i // 2, i % 2
        own = x[b, half * TOWN:(half + 1) * TOWN]
        oth = x[b, (1 - half) * TOWN:(2 - half) * TOWN]
        m = dict(shared)
        m["xcat"] = np.ascontiguousarray(np.concatenate([own, oth], axis=0))
        m["cb"] = np.ascontiguousarray(c[b].reshape(8, 128).T)
        m["tabs_b"], m["tabs_f"] = consts[half]
        maps.append(m)
    return maps


_NC_CACHE = {}


def kernel(**inputs):
    if "nc" not in _NC_CACHE:
        _NC_CACHE["nc"] = build_program()
    nc = _NC_CACHE["nc"]
    maps = make_in_maps(inputs)
    res = run_bass_kernel_spmd(nc, maps, core_ids=list(range(NCORES)))
    outp = np.zeros((4, SEQ, D), np.float32)
    for i in range(NCORES):
        b, half = i // 2, i % 2
        outp[b, half * TOWN:(half + 1) * TOWN] = res.results[i]["out"]
    return outp
```

```python
from contextlib import ExitStack
import math
import numpy as np
import ml_dtypes
import concourse.bass as bass
import concourse.mybir as mybir
from concourse.bass_utils import run_bass_kernel_spmd

F32 = mybir.dt.float32
BF16 = mybir.dt.bfloat16
I32 = mybir.dt.int32
U32 = mybir.dt.uint32
AF = mybir.ActivationFunctionType
ALU = mybir.AluOpType
AX = mybir.AxisListType

NCORES = 8
D = 1024
SEQ = 8192
TOWN = 4096
L = 8192
NFFT = 16384
DN_ALPHA = 2.0 ** 0.25
LN_EPS = 1e-5
DBG_NSG = 2
NBLK = 128
PROWS = NBLK * 128


class Buf:
    __slots__ = ("w", "r", "name")

    def __init__(self, name=""):
        self.w = None
        self.r = {}
        self.name = name


class _Eng:
    def __init__(self, name, hname, sem, self_sync):
        self.name = name
        self.hname = hname
        self.sem = sem
        self.cnt = 0
        self.known = {}
        self.ops = []
        self.self_sync = self_sync


class Prog:
    ENG = {"pe": "tensor", "act": "scalar", "dve": "vector", "pool": "gpsimd", "sp": "sync"}

    def __init__(self, nc, stack, n_dma_sems=8):
        self.nc = nc
        self.sems = {}
        self.engs = {}
        for k, h in self.ENG.items():
            self.sems["e_" + k] = stack.enter_context(nc.semaphore("s_" + k))
            self.engs[k] = _Eng(k, h, "e_" + k, self_sync=(k in ("act", "dve", "pool")))
        self.dq = {}
        for q in ("sp", "act", "pool"):
            lst = []
            for i in range(n_dma_sems):
                key = "d_%s%d" % (q, i)
                self.sems[key] = stack.enter_context(nc.semaphore(key))
                lst.append([key, 0])
            self.dq[q] = [lst, 0]

    def _collect(self, E, reads, writes):
        waits = {}

        def need(tok):
            if tok is None:
                return
            k, v = tok
            if waits.get(k, 0) < v:
                waits[k] = v

        for b in reads:
            need(b.w)
        for b in writes:
            need(b.w)
            for k, v in b.r.items():
                need((k, v))
        wl = []
        for k, v in waits.items():
            if k == E.sem and not E.self_sync:
                continue
            if E.known.get(k, 0) >= v:
                continue
            E.known[k] = v
            wl.append((k, v))
        return wl

    def _commit(self, tok, reads, writes):
        k, v = tok
        for b in reads:
            if b.r.get(k, 0) < v:
                b.r[k] = v
        for b in writes:
            b.w = tok
            b.r = {}

    def op(self, eng, fn, reads=(), writes=()):
        E = self.engs[eng]
        wl = self._collect(E, reads, writes)
        E.cnt += 1
        tok = (E.sem, E.cnt)
        E.ops.append((wl, fn, (E.sem, 1)))
        self._commit(tok, reads, writes)
        return tok

    def group(self, eng, fns, reads=(), writes=()):
        E = self.engs[eng]
        wl = self._collect(E, reads, writes)
        E.cnt += 1
        tok = (E.sem, E.cnt)
        n = len(fns)
        for i, fn in enumerate(fns):
            E.ops.append((wl if i == 0 else [], fn, (E.sem, 1) if i == n - 1 else None))
        self._commit(tok, reads, writes)
        return tok

    def dma(self, q, fn, reads=(), writes=()):
        E = self.engs[q]
        lst, idx = self.dq[q]
        slot = lst[idx % len(lst)]
        self.dq[q][1] = idx + 1
        key, val = slot
        wl = self._collect(E, reads, writes)
        if val > 0 and E.known.get(key, 0) < val:
            E.known[key] = val
            wl.append((key, val))
        val += 16
        slot[1] = val
        tok = (key, val)
        E.ops.append((wl, fn, (key, 16)))
        self._commit(tok, reads, writes)
        return tok

    def _all_tokens(self):
        toks = []
        for E in self.engs.values():
            if E.cnt:
                toks.append((E.sem, E.cnt))
        for q, (lst, _) in self.dq.items():
            for key, val in lst:
                if val:
                    toks.append((key, val))
        return toks

    def barrier(self):
        toks = self._all_tokens()
        for E in self.engs.values():
            wl = []
            for k, v in toks:
                if E.known.get(k, 0) >= v:
                    continue
                E.known[k] = v
                wl.append((k, v))
            if wl:
                E.ops.append((wl, None, None))

    def emit(self):
        nc = self.nc
        sems = self.sems
        with nc.Block() as block:
            for k, E in self.engs.items():
                def body(h, E=E):
                    for wl, fn, inc in E.ops:
                        for sk, v in wl:
                            h.wait_ge(sems[sk], v)
                        if fn is None:
                            continue
                        ins = fn(h)
                        if inc is not None:
                            ins.then_inc(sems[inc[0]], inc[1])
                getattr(block, E.hname)(body)


def _bf(a):
    return np.ascontiguousarray(a.astype(ml_dtypes.bfloat16))


def host_constants(half):
    a = np.arange(128)
    ang = 2.0 * np.pi * np.outer(a, a) / 128.0
    Fr = np.cos(ang)
    Fi = -np.sin(ang)
    rowpos = np.concatenate([np.arange(32), (32 if half == 0 else 96) + np.arange(32)])
    tb = np.zeros((128, NTB), np.float64)
    tb[:, 0:128] = Fr
    tb[:, 128:256] = Fi
    tb[0:64, 256:384] = Fr[rowpos, :]
    tb[0:64, 384:512] = Fi[rowpos, :]
    tb[:, 512:640] = Fr
    tb[:, 640:768] = Fi
    tb[:, 768:896] = -Fi
    tb[:, 896:1024] = Fr
    tb[:, 1024:1152] = -Fi
    tb[:, 1152:1280] = Fi
    tb[:, 1280:1408] = Fr
    tb[:, 1408:1472] = Fr[:, rowpos] / NFFT
    tb[:, 1472:1536] = Fi[:, rowpos] / NFFT
    tb[:, 1536:1664] = np.eye(128)
    tb[:, 1664:1792] = (a[:, None] < a[None, :]).astype(np.float64)
    tb[:, 1792:1920] = 1.0
    j = a[:, None]
    q = a[None, :]
    for g in range(2):
        for ty, off in enumerate((-128, 0, 128)):
            rel = j + off - q
            val = (np.abs(rel) <= 128)
            blk = np.zeros((128, 4, 128))
            for hh in range(4):
                h = g * 4 + hh
                slope = 2.0 ** (-8.0 * (h + 1) / 8.0)
                blk[:, hh, :] = np.exp(-slope * np.abs(rel)) * val
            c0 = 1920 + (g * 3 + ty) * 512
            tb[:, c0:c0 + 512] = blk.reshape(128, 512)
    tw = 2.0 * np.pi * np.outer(a, a) / NFFT
    tb[:, CT_TC1:CT_TC1 + 128] = np.cos(tw); tb[:, CT_TC1 + 128:CT_TC1 + 256] = np.cos(tw)
    tb[:, CT_TC2:CT_TC2 + 128] = -np.sin(tw); tb[:, CT_TC2 + 128:CT_TC2 + 256] = -np.sin(tw)
    tb[:, CT_F2NR:CT_F2NR + 128] = -Fr
    tb[:, CT_G2NA:CT_G2NA + 128] = -Fr
    tb[:, CT_G2NA + 128:CT_G2NA + 256] = Fi
    tb[:, CT_G1NI:CT_G1NI + 64] = -Fi[:, rowpos] / NFFT
    tf = np.zeros((128, NTF), np.float32)
    tf[:, 0:128] = np.cos(tw)
    tf[:, 128:256] = -np.sin(tw)
    tf[:, 256:384] = np.eye(128)
    tf[:, 384] = a
    tf[:, 385] = 1.0 - half
    tf[:, 386] = float(half)
    tf[:, 387] = float(half)
    tf[:, 388] = 1.0 - half
    tf[:, 400:528] = 128.0 * a[None, :]
    cwn = 1.0 / (L - 1)
    tf[:64, 389] = -cwn
    tf[64:, 389] = cwn
    tf[:64, 390] = 128.0 * a[:64]
    tf[64:, 390] = -(8192.0 - 128.0 * (a[64:] - 64))
    tf[:, 528:656] = a[None, :]
    tf[:, 656:784] = 1.0
    return _bf(tb), tf


CT_F1FULL, CT_F1C, CT_F2R, CT_F2I, CT_F2NI, CT_G2A, CT_G2B, CT_G1R, CT_G1I = 0, 256, 512, 640, 768, 896, 1152, 1408, 1472
CT_ID, CT_TRI, CT_ONES, CT_EM = 1536, 1664, 1792, 1920
CT_TC1 = 1920 + 6 * 512
CT_TC2 = CT_TC1 + 256
CT_F2NR = CT_TC2 + 256
CT_G2NA = CT_F2NR + 128
CT_G1NI = CT_G2NA + 256
NTB = CT_G1NI + 64
NTF = 784


def q_perm():
    cols = []
    for c in range(4):
        cols += list(range(c * 64, c * 64 + 64))
        cols += list(range((4 + c) * 64, (4 + c) * 64 + 64))
    return np.array(cols + list(range(512, 4352)))


def build_program(debug=None, lite=False):
    nc = bass.Bass("TRN2", target_bir_lowering=False)
    dbg = debug is not None

    def din(name, shape, dt=F32):
        return nc.dram_tensor(name, list(shape), dt, kind="ExternalInput").ap()

    DBG_OUT = {"p0": ["mod_scr"], "p1a": ["mod_scr", "Uc", "Gs", "dbgq", "dbgk", "dbgv"], "p1b": ["AT"],
               "p2": ["AT", "Z2", "dbgkc", "dbgz1", "dbgkh"], "p3": ["X1", "H2", "dbgrt"], "p4": ["XB", "YB", "dbgrt"]}

    def dscr(name, shape, dt=F32):
        isout = dbg and name in DBG_OUT.get(debug, [])
        return nc.dram_tensor(name, list(shape), dt, kind=("ExternalOutput" if isout else "Internal")).ap()

    xcat = din("xcat", [SEQ, D])
    cb = din("cb", [128, 8])
    w_ada = din("w_ada", [D, 6 * D])
    b_ada = din("b_ada", [1, 6 * D])
    w_in = din("w_in", [D, 4352])
    conv_w = din("conv_w", [128, 3, 12])
    conv_b = din("conv_b", [128, 12])
    fw1 = din("fw1", [33, 64]); fb1 = din("fb1", [64, 1]); ff1 = din("ff1", [64, 1])
    fw2 = din("fw2", [64, 64]); fb2 = din("fb2", [64, 1]); ff2 = din("ff2", [64, 1])
    fw3 = din("fw3", [64, 2048]); fdec = din("fdec", [1, 2048])
    hskip = din("hskip", [1, 1024])
    w_hy_o = din("w_hy_o", [512, D]); w_attn_o = din("w_attn_o", [512, D])
    sink = din("sink", [1, 8])
    w_out = din("w_out", [D, D])
    ln1g = din("ln1g", [1, D]); ln1b = din("ln1b", [1, D]); ln2g = din("ln2g", [1, D]); ln2b = din("ln2b", [1, D])
    wr = din("wr", [D, 72]); br = din("br", [1, 72])
    ne = 256 if lite else 64 * 2 * 128
    ew1 = din("ew1", [ne, 2048]); ew3 = din("ew3", [ne, 2048]); ew2 = din("ew2", [ne, 2048])
    tabs_b = din("tabs_b", [128, NTB], BF16)
    tabs_f = din("tabs_f", [128, NTF])
    bands = din("bands", [33, 4])
    out = nc.dram_tensor("out", [TOWN, D], F32, kind="ExternalOutput").ap()

    mod_scr = dscr("mod_scr", [1, 6 * D])
    Uc = dscr("Uc", [1536, SEQ])
    Gs = dscr("Gs", [2048, TOWN], BF16)
    AT = dscr("AT", [512, TOWN], BF16)
    Z2 = dscr("Z2", [512, TOWN], BF16)
    X1 = dscr("X1", [TOWN, D])
    H2 = dscr("H2", [TOWN, D], BF16)
    XB = dscr("XB", [PROWS, D], BF16)
    YB = dscr("YB", [PROWS, D], BF16)
    EWB = [dscr("EWB%d" % i, [ne // 2, 4096], BF16) for i in range(3)]
    dbg1a = (debug == "p1a")
    dbgq = dscr("dbgq", [128, 4, TOWN], BF16) if dbg1a else None
    dbgk = dscr("dbgk", [128, 34 * 128], BF16) if dbg1a else None
    dbgv = dscr("dbgv", [128, 34, 2, 65], BF16) if dbg1a else None
    dbgkc = dscr("dbgkc", [128, 32, 128], BF16) if debug == "p2" else None
    dbgz1 = dscr("dbgz1", [64, 16, 128], BF16) if debug == "p2" else None
    dbgkh = dscr("dbgkh", [128, 2, 32, 128], BF16) if debug == "p2" else None

    with ExitStack() as gst:
        P = Prog(nc, gst)

        def SB(st, name, shape, dt):
            return st.enter_context(nc.sbuf_tensor(name, list(shape), dt))

        def PS(st, name, shape, dt=F32):
            return st.enter_context(nc.psum_tensor(name, list(shape), dt))

        tb = SB(gst, "tb", [128, NTB], BF16); b_tb = Buf("tb")
        tf = SB(gst, "tf", [128, NTF], F32); b_tf = Buf("tf")
        P.dma("sp", lambda e: e.dma_start(out=tb[:], in_=tabs_b[:, :]), writes=[b_tb])
        P.dma("sp", lambda e: e.dma_start(out=tf[:], in_=tabs_f[:, :]), writes=[b_tf])
        identf = tf[:, 256:384]
        identb = tb[:, CT_ID:CT_ID + 128]
        mA, mB, nmA, nmB = tf[:, 385:386], tf[:, 386:387], tf[:, 387:388], tf[:, 388:389]
        m1 = SB(gst, "m1", [128, 16], F32); b_m1 = Buf()

        with ExitStack() as st:
            cbt = SB(st, "cbt", [128, 8], F32); b_cbt = Buf()
            sig = SB(st, "sig", [128, 8], F32)
            modrow = SB(st, "modrow", [1, 6 * D], F32); b_mod = Buf()
            badar = SB(st, "badar", [1, 6 * D], F32); b_bada = Buf()
            wa = [SB(st, "wa%d" % i, [128, 8, 512], BF16) for i in range(2)]
            cbb = SB(st, "cbb", [128, 8], BF16)
            b_wa = [Buf(), Buf()]
            pm = [PS(st, "pm%d" % i, [1, 512]) for i in range(2)]
            b_pm = [Buf(), Buf()]
            P.dma("act", lambda e: e.dma_start(out=cbt[:], in_=cb[:, :]), writes=[b_cbt])
            P.dma("act", lambda e: e.dma_start(out=badar[:], in_=b_ada[:, :]), writes=[b_bada])
            P.op("act", lambda e: e.activation(out=sig[:], in_=cbt[:], func=AF.Sigmoid), reads=[b_cbt], writes=[b_cbt])
            P.op("dve", lambda e: e.tensor_tensor(out=cbt[:], in0=cbt[:], in1=sig[:], op=ALU.mult), reads=[b_cbt], writes=[b_cbt])
            P.op("dve", lambda e: e.tensor_copy(out=cbb[:], in_=cbt[:]), reads=[b_cbt], writes=[b_cbt])
            for blk in range(12):
                s = blk % 2
                P.dma("pool", lambda e, s=s, blk=blk: e.dma_start(
                    out=wa[s][:], in_=w_ada[:, blk * 512:(blk + 1) * 512].rearrange("(c p) n -> p c n", p=128)),
                    writes=[b_wa[s]])
                P.group("pe", [lambda e, s=s, kc=kc: e.matmul(pm[s][:], lhsT=cbb[:, kc:kc + 1], rhs=wa[s][:, kc, :],
                                                               start=(kc == 0), stop=(kc == 7)) for kc in range(8)],
                        reads=[b_cbt, b_wa[s]], writes=[b_pm[s]])
                P.op("dve", lambda e, s=s, blk=blk: e.tensor_tensor(out=modrow[0:1, blk * 512:(blk + 1) * 512], in0=pm[s][:],
                                                                   in1=badar[0:1, blk * 512:(blk + 1) * 512], op=ALU.add),
                     reads=[b_pm[s], b_bada], writes=[b_mod])
            for off in (1024, 4096):
                P.op("dve", lambda e, off=off: e.tensor_scalar(out=modrow[0:1, off:off + 1024], in0=modrow[0:1, off:off + 1024],
                                                               scalar1=1.0, scalar2=None, op0=ALU.add), reads=[b_mod], writes=[b_mod])
            b_modscr = Buf()
            P.dma("sp", lambda e: e.dma_start(out=mod_scr[:, :], in_=modrow[:]), reads=[b_mod], writes=[b_modscr])
            pTm = PS(st, "pTm", [128, 16]); b_pTm = Buf()
            P.group("pe", [lambda e, c=c: e.transpose(out=pTm[:, c:c + 1], in_=modrow[0:1, c * 128:(c + 1) * 128],
                                                      identity=identf[0:1, 0:1]) for c in range(16)],
                    reads=[b_mod, b_tf], writes=[b_pTm])
            P.op("dve", lambda e: e.tensor_copy(out=m1[:], in_=pTm[:]), reads=[b_pTm], writes=[b_m1])
            P.barrier()

        if debug == "p0":
            return _finish(nc, P, out, gst)

        with ExitStack() as st1:
            qT = SB(st1, "qT", [128, 4, TOWN], BF16); b_qT = Buf()
            kT = SB(st1, "kT", [128, 34 * 128], BF16); b_kT = Buf()
            Va = SB(st1, "Va", [128, 34, 2, 65], BF16); b_Va = Buf()
            P.op("pool", lambda e: e.memset(Va[:], 1.0), writes=[b_Va])
            with ExitStack() as st:
                win = SB(st, "win", [128, 8, 4352], BF16); b_win = Buf()
                for kc in range(8):
                    for (c0, c1) in ((0, 2048), (2048, 4096), (4096, 4352)):
                        P.dma("pool", lambda e, kc=kc, c0=c0, c1=c1: e.dma_start(
                            out=win[:, kc, c0:c1], in_=w_in[kc * 128:(kc + 1) * 128, c0:c1]), writes=[b_win])
                cw = SB(st, "cw", [128, 3, 12], F32); cbias = SB(st, "cbias", [128, 12], F32); b_cw = Buf()
                P.dma("act", lambda e: e.dma_start(out=cw[:], in_=conv_w[:, :, :]), writes=[b_cw])
                P.dma("act", lambda e: e.dma_start(out=cbias[:], in_=conv_b[:, :]), writes=[b_cw])
                NXS = 3
                xs = [SB(st, "xs%d" % i, [128, D], F32) for i in range(NXS)]; b_xs = [Buf() for _ in range(NXS)]
                hT = [SB(st, "hT%d" % i, [128, 8, 512], BF16) for i in range(2)]; b_hT = [Buf(), Buf()]
                carry = SB(st, "carry", [128, 12, 2], F32); b_carry = Buf()
                NUB = 3
                ub = [SB(st, "ub%d" % i, [128, 514], F32) for i in range(NUB)]; b_ub = [Buf() for _ in range(NUB)]
                ua = [SB(st, "ua%d" % i, [128, 512], F32) for i in range(NUB)]; b_ua = [Buf() for _ in range(NUB)]
                gsb = [SB(st, "gsb%d" % i, [128, 512], BF16) for i in range(2)]; b_gsb = [Buf(), Buf()]
                fix = SB(st, "fix", [128, 2], F32); b_fix = Buf()
                pT = [PS(st, "pT%d" % i, [128, 8, 128]) for i in range(2)]; b_pT = [Buf(), Buf()]
                pO = [PS(st, "pO%d" % i, [128, 512]) for i in range(3)]; b_pO = [Buf() for _ in range(3)]
                pV = PS(st, "pV", [128, 128]); b_pV = Buf()
                cnt = {"x": 0, "pT": 0, "pO": 0, "ub": 0, "g": 0}
                b_Uc = Buf(); b_Gs = Buf()

                def load_transpose(tile_idx, hslot, col0, ncols=128):
                    xi = cnt["x"] % NXS; cnt["x"] += 1
                    P.dma("sp", lambda e: e.dma_start(out=xs[xi][:], in_=xcat[tile_idx * 128:(tile_idx + 1) * 128, :]),
                          writes=[b_xs[xi]])
                    pi = cnt["pT"] % 2; cnt["pT"] += 1
                    P.group("pe", [lambda e, kc=kc: e.transpose(out=pT[pi][:, kc, :], in_=xs[xi][:, kc * 128:(kc + 1) * 128],
                                                               identity=identf) for kc in range(8)],
                            reads=[b_xs[xi], b_tf], writes=[b_pT[pi]])
                    for kc in range(8):
                        P.op("act", lambda e, kc=kc: e.activation(out=hT[hslot][:, kc, col0:col0 + 128], in_=pT[pi][:, kc, :],
                                                                 func=AF.Identity, scale=m1[:, 8 + kc:9 + kc], bias=m1[:, kc:kc + 1]),
                             reads=[b_pT[pi], b_m1], writes=[b_hT[hslot]])

                def proj_chunk(hslot, oc, ncols=512):
                    pi = cnt["pO"] % 3; cnt["pO"] += 1
                    P.group("pe", [lambda e, kc=kc: e.matmul(pO[pi][:, 0:ncols], lhsT=win[:, kc, oc * 128:(oc + 1) * 128],
                                                             rhs=hT[hslot][:, kc, 0:ncols], start=(kc == 0), stop=(kc == 7))
                                   for kc in range(8)],
                            reads=[b_win, b_hT[hslot]], writes=[b_pO[pi]])
                    return pi

                def proj_v(hslot, t, vtile):
                    P.group("pe", [lambda e, kc=kc: e.matmul(pV[:], lhsT=hT[hslot][:, kc, t * 128:(t + 1) * 128],
                                                             rhs=win[:, kc, 640:768], start=(kc == 0), stop=(kc == 7))
                                   for kc in range(8)],
                            reads=[b_win, b_hT[hslot]], writes=[b_pV])
                    P.op("dve", lambda e: e.tensor_copy(out=Va[:, vtile, :, 0:64], in_=pV[:].rearrange("p (g d) -> p g d", g=2)),
                         reads=[b_pV], writes=[b_Va])

                load_transpose(63, 0, 0)
                for j in range(12):
                    pi = proj_chunk(0, 6 + j, ncols=128)
                    P.op("act", lambda e, j=j, pi=pi: e.copy(out=carry[:, j, :], in_=pO[pi][:, 126:128]),
                         reads=[b_pO[pi]], writes=[b_carry])

                def do_supertile(st_i):
                    hs = st_i % 2
                    own = st_i < 8
                    if st_i == 0:
                        for t in range(4):
                            load_transpose(st_i * 4 + t, hs, t * 128)
                    chunks = list(range(0, 5)) + list(range(6, 34)) if own else list(range(6, 18))
                    if st_i in (8, 15):
                        chunks = [4] + chunks
                    pre_at = {}
                    if st_i + 1 < 16:
                        step = max(1, (len(chunks) - 2) // 4)
                        for t in range(4):
                            pre_at[1 + t * step] = t
                    for ci_, oc in enumerate(chunks):
                        if ci_ in pre_at:
                            load_transpose((st_i + 1) * 4 + pre_at[ci_], 1 - hs, pre_at[ci_] * 128)
                        pi = proj_chunk(hs, oc)
                        if oc < 4:
                            P.op("dve", lambda e, oc=oc, pi=pi: e.tensor_copy(out=qT[:, oc, st_i * 512:(st_i + 1) * 512], in_=pO[pi][:]),
                                 reads=[b_pO[pi]], writes=[b_qT])
                        elif oc == 4:
                            if own:
                                P.op("dve", lambda e, pi=pi: e.tensor_copy(out=kT[:, 128 + st_i * 512:128 + (st_i + 1) * 512], in_=pO[pi][:]),
                                     reads=[b_pO[pi]], writes=[b_kT])
                            elif st_i == 8:
                                P.op("dve", lambda e, pi=pi: e.tensor_copy(out=kT[:, 33 * 128:34 * 128], in_=pO[pi][:, 0:128]),
                                     reads=[b_pO[pi]], writes=[b_kT])
                            else:
                                P.op("dve", lambda e, pi=pi: e.tensor_copy(out=kT[:, 0:128], in_=pO[pi][:, 384:512]),
                                     reads=[b_pO[pi]], writes=[b_kT])
                        elif oc < 18:
                            j = oc - 6
                            ui = cnt["ub"] % NUB; cnt["ub"] += 1
                            P.op("act", lambda e, pi=pi, ui=ui: e.copy(out=ub[ui][:, 2:514], in_=pO[pi][:]),
                                 reads=[b_pO[pi]], writes=[b_ub[ui]])
                            P.op("act", lambda e, j=j, ui=ui: e.copy(out=ub[ui][:, 0:2], in_=carry[:, j, :]),
                                 reads=[b_carry], writes=[b_ub[ui]])
                            P.op("act", lambda e, j=j, ui=ui: e.copy(out=carry[:, j, :], in_=ub[ui][:, 512:514]),
                                 reads=[b_ub[ui]], writes=[b_carry])
                            P.op("act", lambda e, j=j, ui=ui: e.activation(out=ua[ui][:], in_=ub[ui][:, 1:513], func=AF.Identity,
                                                                          scale=cw[:, 1, j:j + 1], bias=cbias[:, j:j + 1]),
                                 reads=[b_ub[ui], b_cw], writes=[b_ua[ui]])
                            P.op("dve", lambda e, j=j, ui=ui: e.scalar_tensor_tensor(out=ua[ui][:], in0=ub[ui][:, 0:512], scalar=cw[:, 0, j:j + 1],
                                                                                    in1=ua[ui][:], op0=ALU.mult, op1=ALU.add),
                                 reads=[b_ub[ui], b_cw], writes=[b_ua[ui]])
                            P.op("dve", lambda e, j=j, ui=ui: e.scalar_tensor_tensor(out=ua[ui][:], in0=ub[ui][:, 2:514], scalar=cw[:, 2, j:j + 1],
                                                                                    in1=ua[ui][:], op0=ALU.mult, op1=ALU.add),
                                 reads=[b_ub[ui], b_cw], writes=[b_ua[ui]])
                            if st_i in (0, 8):
                                nm = nmB if st_i == 0 else nmA
                                P.op("dve", lambda e, j=j, ui=ui, nm=nm: e.tensor_scalar(out=fix[:, 0:1], in0=ub[ui][:, 2:3], scalar1=cw[:, 2, j:j + 1],
                                                                                        scalar2=nm, op0=ALU.mult, op1=ALU.mult),
                                     reads=[b_ub[ui], b_cw, b_tf], writes=[b_fix])
                                P.op("dve", lambda e, j=j, ui=ui, nm=nm: e.tensor_scalar(out=fix[:, 1:2], in0=ub[ui][:, 1:2], scalar1=cw[:, 0, j:j + 1],
                                                                                        scalar2=nm, op0=ALU.mult, op1=ALU.mult),
                                     reads=[b_ub[ui], b_cw, b_tf], writes=[b_fix])
                                P.op("dve", lambda e, ui=ui: e.tensor_tensor(out=ua[ui][:, 0:2], in0=ua[ui][:, 0:2], in1=fix[:, 0:2], op=ALU.subtract),
                                     reads=[b_fix], writes=[b_ua[ui]])
                            s0 = st_i * 512
                            r0 = j * 128
                            P.dma("pool", lambda e, ui=ui, r0=r0, s0=s0: e.dma_start(out=Uc[r0:r0 + 128, (s0 - 1) % SEQ:(s0 - 1) % SEQ + 1], in_=ua[ui][:, 0:1], allow_slow_non_contiguous=True),
                                  reads=[b_ua[ui]])
                            P.dma("pool", lambda e, ui=ui, r0=r0, s0=s0: e.dma_start(out=Uc[r0:r0 + 128, s0:s0 + 511], in_=ua[ui][:, 1:512]),
                                  reads=[b_ua[ui]])
                        else:
                            gi = cnt["g"] % 2; cnt["g"] += 1
                            P.op("act", lambda e, pi=pi, gi=gi: e.activation(out=gsb[gi][:], in_=pO[pi][:], func=AF.Sigmoid),
                                 reads=[b_pO[pi]], writes=[b_gsb[gi]])
                            r0 = (oc - 18) * 128
                            P.dma("pool", lambda e, gi=gi, r0=r0: e.dma_start(out=Gs[r0:r0 + 128, st_i * 512:(st_i + 1) * 512], in_=gsb[gi][:]),
                                  reads=[b_gsb[gi]])
                    if own:
                        for t in range(4):
                            proj_v(hs, t, 1 + st_i * 4 + t)
                    elif st_i == 8:
                        proj_v(hs, 0, 33)
                    elif st_i == 15:
                        proj_v(hs, 3, 0)
                for st_i in range(16):
                    do_supertile(st_i)
                if dbg1a:
                    P.dma("sp", lambda e: e.dma_start(out=dbgq[:, :, :], in_=qT[:]), reads=[b_qT])
                    P.dma("sp", lambda e: e.dma_start(out=dbgk[:, :], in_=kT[:]), reads=[b_kT])
                    P.dma("sp", lambda e: e.dma_start(out=dbgv[:, :, :, :], in_=Va[:]), reads=[b_Va])
                P.barrier()
            if debug == "p1a":
                return _finish(nc, P, out, gst)

            with ExitStack() as st:
                esink = SB(st, "esink", [128, 8], F32); b_es = Buf()
                P.dma("sp", lambda e: e.dma_start(out=esink[:], in_=sink[0:1, :].partition_broadcast(128)), writes=[b_es])
                P.op("act", lambda e: e.activation(out=esink[:], in_=esink[:], func=AF.Exp), reads=[b_es], writes=[b_es])
                emJ = SB(st, "emJ", [128, 2, 2, 512], BF16); b_emJ = Buf()
                for g in range(2):
                    P.op("dve", lambda e, g=g: e.tensor_scalar(out=emJ[:, g, 0, :], in0=tb[:, CT_EM + (g * 3 + 0) * 512:CT_EM + (g * 3 + 1) * 512],
                                                               scalar1=mB, scalar2=None, op0=ALU.mult), reads=[b_tb, b_tf], writes=[b_emJ])
                    P.op("dve", lambda e, g=g: e.tensor_scalar(out=emJ[:, g, 1, :], in0=tb[:, CT_EM + (g * 3 + 2) * 512:CT_EM + (g * 3 + 3) * 512],
                                                               scalar1=mA, scalar2=None, op0=ALU.mult), reads=[b_tb, b_tf], writes=[b_emJ])
                pS = [PS(st, "pS%d" % i, [128, 512]) for i in range(3)]; b_pS = [Buf() for _ in range(3)]
                pPVb = [[PS(st, "pPV%d_%d" % (s, g), [128, 512]) for g in range(2)] for s in range(2)]
                pPV = [[pPVb[s][g][:, 0:260].rearrange("p (h d) -> p h d", h=4) for g in range(2)] for s in range(2)]
                b_pPV = [[Buf(), Buf()], [Buf(), Buf()]]
                pTab = PS(st, "pTa", [128, 1024], BF16); b_pTa = Buf()
                pTa = pTab[:, 0:512].rearrange("p (c q) -> p c q", c=4)
                pex = [SB(st, "pex%d" % i, [128, 512], BF16) for i in range(3)]; b_pex = [Buf() for _ in range(3)]
                pmk = [[[SB(st, "pmk%d_%d_%d" % (s, g, kb), [128, 512], BF16) for kb in range(3)] for g in range(2)] for s in range(2)]
                b_pmk = [[[Buf() for kb in range(3)] for g in range(2)] for s in range(2)]
                den = SB(st, "den", [128, 2, 4], F32); b_den = Buf()
                att = [SB(st, "att%d" % i, [128, 8, 64], BF16) for i in range(2)]; b_att = [Buf(), Buf()]
                atT = [SB(st, "atT%d" % i, [128, 4, 128], BF16) for i in range(2)]; b_atT = [Buf(), Buf()]
                cn = {"s": 0}

                def attn_part1(i):
                    s2 = i % 2
                    for g in range(2):
                        for kb in range(3):
                            si = cn["s"] % 3; cn["s"] += 1
                            P.group("pe", [lambda e, g=g, kb=kb, si=si: e.matmul(pS[si][:].rearrange("p (h q) -> p h q", h=4),
                                                              lhsT=kT[64 * g:64 * g + 64, (i + kb) * 128:(i + kb + 1) * 128],
                                                              rhs=qT[64 * g:64 * g + 64, :, i * 128:(i + 1) * 128], start=True, stop=True)],
                                    reads=[b_kT, b_qT], writes=[b_pS[si]])
                            P.op("act", lambda e, si=si: e.activation(out=pex[si][:], in_=pS[si][:], func=AF.Exp, scale=0.125),
                                 reads=[b_pS[si]], writes=[b_pex[si]])
                            if kb == 0 and i == 0:
                                em = emJ[:, g, 0, :]
                            elif kb == 2 and i == 31:
                                em = emJ[:, g, 1, :]
                            else:
                                em = tb[:, CT_EM + (g * 3 + kb) * 512:CT_EM + (g * 3 + kb + 1) * 512]
                            eng = "dve"
                            P.op(eng, lambda e, em=em, g=g, kb=kb, si=si: e.tensor_tensor(out=pmk[s2][g][kb][:], in0=pex[si][:], in1=em, op=ALU.mult),
                                 reads=[b_pex[si], b_tb, b_emJ], writes=[b_pmk[s2][g][kb]])
                def attn_part2(i):
                    s2 = i % 2
                    for g in range(2):
                        fns = []
                        for hh in range(4):
                            for kb in range(3):
                                fns.append(lambda e, hh=hh, kb=kb, g=g: e.matmul(pPV[s2][g][:, hh, :], lhsT=pmk[s2][g][kb][:, hh * 128:(hh + 1) * 128],
                                                                          rhs=Va[:, i + kb, g, :], start=(kb == 0), stop=(kb == 2)))
                        P.group("pe", fns, reads=[b_pmk[s2][g][0], b_pmk[s2][g][1], b_pmk[s2][g][2], b_Va], writes=[b_pPV[s2][g]])
                        P.op("dve", lambda e, g=g: e.tensor_tensor(out=den[:, g, :], in0=pPV[s2][g][:, :, 64], in1=esink[:, 4 * g:4 * g + 4], op=ALU.add),
                             reads=[b_pPV[s2][g], b_es], writes=[b_den])
                        P.op("dve", lambda e, g=g: e.reciprocal(out=den[:, g, :], in_=den[:, g, :]), reads=[b_den], writes=[b_den])
                        P.op("dve", lambda e, g=g: e.tensor_tensor(out=att[s2][:, 4 * g:4 * g + 4, :], in0=pPV[s2][g][:, :, 0:64],
                                                                   in1=den[:, g, :].unsqueeze(2).to_broadcast([128, 4, 64]), op=ALU.mult),
                             reads=[b_pPV[s2][g], b_den], writes=[b_att[s2]])
                    P.group("pe", [lambda e, c=c: e.transpose(out=pTa[:, c, :], in_=att[s2][:].rearrange("p h d -> p (h d)")[:, c * 128:(c + 1) * 128],
                                                               identity=identb) for c in range(4)],
                            reads=[b_att[s2], b_tb], writes=[b_pTa])
                    P.op("act", lambda e: e.copy(out=atT[s2][:], in_=pTa), reads=[b_pTa], writes=[b_atT[s2]])
                    P.dma("sp", lambda e: e.dma_start(out=AT[:, i * 128:(i + 1) * 128].rearrange("(c p) q -> p c q", p=128), in_=atT[s2][:]),
                          reads=[b_atT[s2]])

                attn_part1(0)
                for i in range(32):
                    if i + 1 < 32:
                        attn_part1(i + 1)
                    attn_part2(i)
                P.barrier()
        if debug == "p1b":
            return _finish(nc, P, out, gst)

        TWO_PI = 2.0 * math.pi
        with ExitStack() as st2:
            hdn2T = SB(st2, "hdn2T", [64, NFFT], BF16); b_h2 = Buf()
            w3b = SB(st2, "w3b", [64, 2048], BF16); b_w3 = Buf()
            P.dma("pool", lambda e: e.dma_start(out=w3b[:, :], in_=fw3[:, :]), writes=[b_w3])
            dec = SB(st2, "dec", [128, 2, 512], F32); b_dec = Buf()

            def load_dec(o):
                P.dma("sp", lambda e: e.dma_start(out=dec[0:64, o, :], in_=fdec[0:1, o * 1024:o * 1024 + 512].partition_broadcast(64)), writes=[b_dec])
                P.dma("sp", lambda e: e.dma_start(out=dec[64:128, o, :], in_=fdec[0:1, o * 1024 + 512:(o + 1) * 1024].partition_broadcast(64)), writes=[b_dec])
            load_dec(0); load_dec(1)
            P.op("act", lambda e: e.activation(out=dec[:], in_=dec[:], func=AF.Abs), reads=[b_dec], writes=[b_dec])
            P.op("dve", lambda e: e.tensor_scalar(out=dec[:], in0=dec[:], scalar1=tf[:, 389:390], scalar2=None, op0=ALU.mult),
                 reads=[b_dec, b_tf], writes=[b_dec])
            skA = SB(st2, "skA", [128, 1024], F32); b_sk = Buf()
            P.dma("sp", lambda e: e.dma_start(out=skA[:], in_=hskip[0:1, :].partition_broadcast(128)), writes=[b_sk])

            with ExitStack() as st:
                w1t = SB(st, "w1t", [33, 64], F32); w2t = SB(st, "w2t", [64, 64], F32); b_fw = Buf()
                fsc = SB(st, "fsc", [64, 8], F32); b_fsc = Buf()
                bnd = SB(st, "bnd", [33, 4], F32)
                P.dma("sp", lambda e: e.dma_start(out=w1t[:], in_=fw1[:, :]), writes=[b_fw])
                P.dma("sp", lambda e: e.dma_start(out=w2t[:], in_=fw2[:, :]), writes=[b_fw])
                P.dma("sp", lambda e: e.dma_start(out=bnd[:], in_=bands[:, :]), writes=[b_fw])
                for ci, srcap in enumerate((ff1, fb1, ff2, fb2)):
                    P.dma("sp", lambda e, ci=ci, srcap=srcap: e.dma_start(out=fsc[:, ci:ci + 1], in_=srcap[:, :]), writes=[b_fsc])
                for (a, b, o1, o2) in ((0, 1, 4, 5), (2, 3, 6, 7)):
                    P.op("dve", lambda e, a=a, b=b, o2=o2: e.tensor_tensor(out=fsc[:, o2:o2 + 1], in0=fsc[:, a:a + 1], in1=fsc[:, b:b + 1], op=ALU.mult),
                         reads=[b_fsc], writes=[b_fsc])
                    P.op("dve", lambda e, o2=o2: e.tensor_scalar(out=fsc[:, o2:o2 + 1], in0=fsc[:, o2:o2 + 1], scalar1=1.0 / TWO_PI, scalar2=8.5,
                                                                 op0=ALU.mult, op1=ALU.add), reads=[b_fsc], writes=[b_fsc])
                    P.op("dve", lambda e, a=a, o1=o1: e.tensor_scalar(out=fsc[:, o1:o1 + 1], in0=fsc[:, a:a + 1], scalar1=1.0 / TWO_PI, scalar2=None,
                                                                      op0=ALU.mult), reads=[b_fsc], writes=[b_fsc])
                idx = SB(st, "idx", [33, 16, 128], I32); b_idx = Buf()
                idxf = SB(st, "idxf", [33, 2048], F32); b_idxf = Buf()
                uu = SB(st, "uu", [64, 2048], F32); b_uu = Buf()
                ki = SB(st, "ki", [64, 2048], I32); b_ki = Buf()
                zT = SB(st, "zT", [33, 2048], F32); b_zT = Buf()
                h1 = SB(st, "h1", [64, 512], F32); b_h1 = Buf()
                pH = [PS(st, "pH%d" % i, [64, 512]) for i in range(2)]; b_pH = [Buf(), Buf()]

                def sin_reduce(np_, ncols, src, b_src, sc_mul, sc_add, dst, b_dst, extra_reads=()):
                    P.op("dve", lambda e: e.tensor_scalar(out=uu[0:np_, 0:ncols], in0=src, scalar1=sc_mul, scalar2=sc_add, op0=ALU.mult, op1=ALU.add),
                         reads=[b_src] + list(extra_reads), writes=[b_uu])
                    P.op("dve", lambda e: e.tensor_copy(out=ki[0:np_, 0:ncols], in_=uu[0:np_, 0:ncols]), reads=[b_uu], writes=[b_ki])
                    P.op("dve", lambda e: e.tensor_tensor(out=uu[0:np_, 0:ncols], in0=uu[0:np_, 0:ncols], in1=ki[0:np_, 0:ncols], op=ALU.subtract),
                         reads=[b_uu, b_ki], writes=[b_uu])
                    P.op("dve", lambda e: e.scalar_tensor_tensor(out=uu[0:np_, 0:ncols], in0=uu[0:np_, 0:ncols], scalar=0.0, in1=uu[0:np_, 0:ncols],
                                                                 op0=ALU.is_lt, op1=ALU.add), reads=[b_uu], writes=[b_uu])
                    P.op("act", lambda e: e.activation(out=dst, in_=uu[0:np_, 0:ncols], func=AF.Sin, bias=-math.pi, scale=TWO_PI),
                         reads=[b_uu], writes=[b_dst])

                def mlp_chunk(c):
                    P.op("pool", lambda e: e.iota(idx[:, :, 0:64], pattern=[[1, 16], [128, 64]], base=16 * c, channel_multiplier=0), writes=[b_idx])
                    P.op("pool", lambda e: e.iota(idx[:, :, 64:128], pattern=[[-1, 16], [-128, 64]], base=8192 - 16 * c, channel_multiplier=0), writes=[b_idx])
                    P.op("dve", lambda e: e.tensor_single_scalar(out=idx[:, :, 64:128], in_=idx[:, :, 64:128], scalar=8191, op=ALU.bitwise_and),
                         reads=[b_idx], writes=[b_idx])
                    P.op("dve", lambda e: e.tensor_copy(out=idxf[:], in_=idx[:].rearrange("p a b -> p (a b)")), reads=[b_idx], writes=[b_idxf])
                    sin_reduce(33, 2048, idxf[:], b_idxf, bnd[:, 0:1], bnd[:, 1:2], zT[:], b_zT, extra_reads=[b_fw])
                    P.op("dve", lambda e: e.tensor_scalar(out=zT[0:1, :], in0=idxf[0:1, :], scalar1=1.0 / (L - 1), scalar2=None, op0=ALU.mult),
                         reads=[b_idxf], writes=[b_zT])

                    def quarter(q):
                        s = q % 2
                        P.group("pe", [lambda e: e.matmul(pH[s][:], lhsT=w1t[:], rhs=zT[:, q * 512:(q + 1) * 512], start=True, stop=True)],
                                reads=[b_fw, b_zT], writes=[b_pH[s]])
                        sin_reduce(64, 512, pH[s][:], b_pH[s], fsc[:, 4:5], fsc[:, 5:6], h1[:], b_h1, extra_reads=[b_fsc])
                        P.group("pe", [lambda e: e.matmul(pH[s][:], lhsT=w2t[:], rhs=h1[:], start=True, stop=True)],
                                reads=[b_fw, b_h1], writes=[b_pH[s]])
                        sin_reduce(64, 512, pH[s][:], b_pH[s], fsc[:, 6:7], fsc[:, 7:8], hdn2T[:, c * 2048 + q * 512:c * 2048 + (q + 1) * 512], b_h2,
                                   extra_reads=[b_fsc])
                    for q in range(4):
                        quarter(q)
                for c in range(8):
                    mlp_chunk(c)
                P.barrier()

            kraw = SB(st2, "kraw", [128, 32, 128], F32); b_kraw = Buf()
            wtmp = SB(st2, "wtmp", [128, 32, 128], F32); b_wtmp = Buf()
            kcs = SB(st2, "kcs", [128, 32, 128], BF16); b_kcs = Buf()
            Khr = SB(st2, "Khr", [128, 32, 128], BF16); Khi = SB(st2, "Khi", [128, 32, 128], BF16); b_Kh = Buf()
            e1 = SB(st2, "e1", [128, 32], F32); b_e1 = Buf()
            ksum = SB(st2, "ksum", [128, 32], F32); b_ksum = Buf()
            r64 = SB(st2, "r64", [128, 32], F32); b_r64 = Buf()
            ut = [[SB(st2, "ut%d_%d" % (s, k), [64, 16, 128], BF16) for k in range(3)] for s in range(2)]
            b_ut = [[Buf() for k in range(3)] for s in range(2)]
            z1t = SB(st2, "z1t", [64, 16, 128], BF16); b_z1 = Buf()
            z2t = [SB(st2, "z2t%d" % s, [64, 16, 128], BF16) for s in range(2)]; b_z2 = [Buf(), Buf()]
            pK = [PS(st2, "pK%d" % i, [128, 512]) for i in range(2)]; b_pK = [Buf(), Buf()]
            pN = pK[0]; b_pN = b_pK[0]
            NL = 3
            lanes = []
            for li in range(NL):
                ln = {"pL": PS(st2, "pL%d" % li, [128, 1024]), "b_pL": Buf()}
                ln["S"] = SB(st2, "S%d" % li, [128, 1024], BF16); ln["b_S"] = Buf()
                ln["P1"] = [SB(st2, "P1_%d_%d" % (li, k), [128, 1024], BF16) for k in range(2)]; ln["b_P1"] = [Buf(), Buf()]
                ln["P2"] = [SB(st2, "P2_%d_%d" % (li, k), [128, 1024], BF16) for k in range(2)]; ln["b_P2"] = [Buf(), Buf()]
                ln["pk"] = 0
                lanes.append(ln)
            TC1 = tb[:, CT_TC1:CT_TC1 + 256].unsqueeze(1).to_broadcast([128, 4, 256])
            TC2 = tb[:, CT_TC2:CT_TC2 + 256].unsqueeze(1).to_broadcast([128, 4, 256])
            F1FULL = tb[:, CT_F1FULL:CT_F1FULL + 256]
            F1C = tb[0:64, CT_F1C:CT_F1C + 256]
            F2R = tb[:, CT_F2R:CT_F2R + 128]; F2I = tb[:, CT_F2I:CT_F2I + 128]; F2NI = tb[:, CT_F2NI:CT_F2NI + 128]
            G2A = tb[:, CT_G2A:CT_G2A + 256]; G2B = tb[:, CT_G2B:CT_G2B + 256]
            G1R = tb[:, CT_G1R:CT_G1R + 64]; G1I = tb[:, CT_G1I:CT_G1I + 64]
            w3v = w3b[:, :].rearrange("p (o d c) -> p o d c", o=2, d=2)

            F2NR = tb[:, CT_F2NR:CT_F2NR + 128]; G2NA = tb[:, CT_G2NA:CT_G2NA + 256]; G1NI = tb[:, CT_G1NI:CT_G1NI + 64]

            def cmul_ci(ln):
                k = ln["pk"]; ln["pk"] = 1 - k; ln["cur"] = k
                S4 = ln["S"][:, :].rearrange("p (c x) -> p c x", c=4)
                P1 = ln["P1"][k][:, :].rearrange("p (c x) -> p c x", c=4); P2 = ln["P2"][k][:, :].rearrange("p (c x) -> p c x", c=4)
                P.op("act", lambda e: e.copy(out=ln["S"][:, :], in_=ln["pL"][:, :]), reads=[ln["b_pL"]], writes=[ln["b_S"]])
                P.op("dve", lambda e: e.tensor_tensor(out=P1, in0=S4, in1=TC1, op=ALU.mult), reads=[ln["b_S"], b_tb], writes=[ln["b_P1"][k]])
                P.op("dve", lambda e: e.tensor_tensor(out=P2, in0=S4, in1=TC2, op=ALU.mult), reads=[ln["b_S"], b_tb], writes=[ln["b_P2"][k]])

            def cmul_ic(ln, f0):
                k = ln["pk"]; ln["pk"] = 1 - k; ln["cur"] = k
                S4 = ln["S"][:, :].rearrange("p (r c x) -> p r c x", r=2, c=4)
                P1 = ln["P1"][k][:, :].rearrange("p (r c x) -> p r c x", r=2, c=4); P2 = ln["P2"][k][:, :].rearrange("p (r c x) -> p r c x", r=2, c=4)
                kr = Khr[:, f0:f0 + 4, :].unsqueeze(1).to_broadcast([128, 2, 4, 128]); ki_ = Khi[:, f0:f0 + 4, :].unsqueeze(1).to_broadcast([128, 2, 4, 128])
                P.op("act", lambda e: e.copy(out=ln["S"][:, :], in_=ln["pL"][:, :]), reads=[ln["b_pL"]], writes=[ln["b_S"]])
                P.op("dve", lambda e: e.tensor_tensor(out=P1, in0=S4, in1=kr, op=ALU.mult), reads=[ln["b_S"], b_Kh], writes=[ln["b_P1"][k]])
                P.op("dve", lambda e: e.tensor_tensor(out=P2, in0=S4, in1=ki_, op=ALU.mult), reads=[ln["b_S"], b_Kh], writes=[ln["b_P2"][k]])

            def st_S1(ln, src, b_src, rhs):
                pA = ln["pL"][:, :].rearrange("p (c x) -> p c x", c=4)
                P.group("pe", [lambda e, cl=cl: e.matmul(pA[:, cl, :], lhsT=src[:, cl, :], rhs=rhs, start=True, stop=True) for cl in range(4)],
                        reads=[b_src, b_tb], writes=[ln["b_pL"]])

            def st_TW(ln):
                cmul_ci(ln)

            def st_S2(ln):
                k = ln["cur"]
                P1 = ln["P1"][k][:, :].rearrange("p (c x) -> p c x", c=4); P2 = ln["P2"][k][:, :].rearrange("p (c x) -> p c x", c=4)
                m0, m3, m2, m1 = P1[:, :, 0:128], P1[:, :, 128:256], P2[:, :, 0:128], P2[:, :, 128:256]
                pXr = ln["pL"][:, 0:512].rearrange("p (c x) -> p c x", c=4); pXi = ln["pL"][:, 512:1024].rearrange("p (c x) -> p c x", c=4)
                P.group("pe", [lambda e: e.matmul(pXr, lhsT=F2R, rhs=m0, start=True, stop=False),
                               lambda e: e.matmul(pXr, lhsT=F2NR, rhs=m1, start=False, stop=False),
                               lambda e: e.matmul(pXr, lhsT=F2NI, rhs=m2, start=False, stop=False),
                               lambda e: e.matmul(pXr, lhsT=F2NI, rhs=m3, start=False, stop=True),
                               lambda e: e.matmul(pXi, lhsT=F2R, rhs=m2, start=True, stop=False),
                               lambda e: e.matmul(pXi, lhsT=F2R, rhs=m3, start=False, stop=False),
                               lambda e: e.matmul(pXi, lhsT=F2I, rhs=m0, start=False, stop=False),
                               lambda e: e.matmul(pXi, lhsT=F2NI, rhs=m1, start=False, stop=True)],
                        reads=[ln["b_P1"][k], ln["b_P2"][k], b_tb], writes=[ln["b_pL"]])

            def st_SPEC(ln, f0):
                cmul_ic(ln, f0)

            def st_IS1(ln):
                k = ln["cur"]
                P1 = ln["P1"][k][:, :].rearrange("p (r c x) -> p r c x", r=2, c=4); P2 = ln["P2"][k][:, :].rearrange("p (r c x) -> p r c x", r=2, c=4)
                pB = ln["pL"][:, :].rearrange("p (c x) -> p c x", c=4)
                fns = []
                for cl in range(4):
                    fns.append(lambda e, cl=cl: e.matmul(pB[:, cl, :], lhsT=P1[:, 0, cl, :], rhs=G2A, start=True, stop=False))
                    fns.append(lambda e, cl=cl: e.matmul(pB[:, cl, :], lhsT=P2[:, 1, cl, :], rhs=G2NA, start=False, stop=False))
                    fns.append(lambda e, cl=cl: e.matmul(pB[:, cl, :], lhsT=P2[:, 0, cl, :], rhs=G2B, start=False, stop=False))
                    fns.append(lambda e, cl=cl: e.matmul(pB[:, cl, :], lhsT=P1[:, 1, cl, :], rhs=G2B, start=False, stop=True))
                P.group("pe", fns, reads=[ln["b_P1"][k], ln["b_P2"][k], b_tb], writes=[ln["b_pL"]])

            def st_ITW(ln):
                cmul_ci(ln)

            def st_IS2(ln):
                k = ln["cur"]
                P1 = ln["P1"][k][:, :].rearrange("p (c x) -> p c x", c=4); P2 = ln["P2"][k][:, :].rearrange("p (c x) -> p c x", c=4)
                n0, n3, n2, n1 = P1[:, :, 0:128], P1[:, :, 128:256], P2[:, :, 0:128], P2[:, :, 128:256]
                pY = ln["pL"][0:64, 0:512].rearrange("p (c x) -> p c x", c=4)
                P.group("pe", [lambda e: e.matmul(pY, lhsT=G1R, rhs=n0, start=True, stop=False),
                               lambda e: e.matmul(pY, lhsT=G1R, rhs=n1, start=False, stop=False),
                               lambda e: e.matmul(pY, lhsT=G1I, rhs=n3, start=False, stop=False),
                               lambda e: e.matmul(pY, lhsT=G1NI, rhs=n2, start=False, stop=True)],
                        reads=[ln["b_P1"][k], ln["b_P2"][k], b_tb], writes=[ln["b_pL"]])

            def st_gate(ln, cg, gin, b_gin, zout, b_zout):
                pY = ln["pL"][0:64, 0:512].rearrange("p (c x) -> p c x", c=4)
                P.op("dve", lambda e: e.tensor_tensor(out=zout, in0=pY, in1=gin, op=ALU.mult), reads=[ln["b_pL"], b_gin], writes=[b_zout])

            def sg_load(sg):
                us = sg % 2
                c0 = 16 * sg
                for k in range(3):
                    P.dma("pool", lambda e, k=k: e.dma_start(out=ut[us][k][:], in_=Uc[k * 512 + c0:k * 512 + c0 + 16, :].rearrange("c (a b) -> a c b", b=128)),
                          reads=[], writes=[b_ut[us][k]])

            def sg_kbatch(sg, bi):
                c0 = 16 * sg
                s = bi % 2
                pKv = pK[s][:, :].rearrange("p (n f) -> p n f", n=16)
                fns = []
                for nl in range(16):
                    n2 = bi * 16 + nl
                    fns.append(lambda e, nl=nl, n2=n2: e.matmul(pKv[0:64, nl, :].rearrange("p (o c) -> p o c", o=2),
                                                                lhsT=hdn2T[:, n2 * 128:n2 * 128 + 64], rhs=w3v[:, :, 0, c0:c0 + 16], start=True, stop=True))
                    fns.append(lambda e, nl=nl, n2=n2: e.matmul(pKv[64:128, nl, :].rearrange("p (o c) -> p o c", o=2),
                                                                lhsT=hdn2T[:, n2 * 128 + 64:n2 * 128 + 128], rhs=w3v[:, :, 1, c0:c0 + 16], start=True, stop=True))
                P.group("pe", fns, reads=[b_h2, b_w3], writes=[b_pK[s]])
                P.op("act", lambda e: e.copy(out=kraw[:, :, bi * 16:(bi + 1) * 16].rearrange("p f n -> p n f"), in_=pKv), reads=[b_pK[s]], writes=[b_kraw])

            def sg_window(sg):
                c0 = 16 * sg
                decv = dec[:, :, c0:c0 + 16]
                e1v = e1[:, :].rearrange("p (o c) -> p o c", o=2)
                wt4 = wtmp[:, :, :].rearrange("p (o c) n -> p o c n", o=2)

                def w0():
                    P.op("act", lambda e: e.activation(out=e1v, in_=decv, func=AF.Exp, scale=tf[:, 390:391]), reads=[b_dec, b_tf], writes=[b_e1])
                    P.op("pool", lambda e: e.tensor_tensor(out=wt4, in0=decv.unsqueeze(3).to_broadcast([128, 2, 16, 128]),
                                                           in1=tf[:, 528:656].unsqueeze(1).unsqueeze(1).to_broadcast([128, 2, 16, 128]), op=ALU.mult),
                         reads=[b_dec, b_tf], writes=[b_wtmp])

                def w1():
                    P.op("act", lambda e: e.activation(out=wtmp[:], in_=wtmp[:], func=AF.Exp), reads=[b_wtmp], writes=[b_wtmp])

                def w2():
                    P.op("pool", lambda e: e.tensor_tensor(out=wtmp[:], in0=wtmp[:], in1=e1[:, :].unsqueeze(2).to_broadcast([128, 32, 128]), op=ALU.mult),
                         reads=[b_wtmp, b_e1], writes=[b_wtmp])
                    P.op("pool", lambda e: e.tensor_scalar(out=r64[64:65, :], in0=kraw[64:65, :, 0], scalar1=1.05, scalar2=None, op0=ALU.mult),
                         reads=[b_kraw], writes=[b_r64])

                def w3():
                    P.op("dve", lambda e: e.scalar_tensor_tensor(out=kraw[:], in0=wtmp[:], scalar=0.05, in1=kraw[:], op0=ALU.add, op1=ALU.mult),
                         reads=[b_wtmp, b_kraw, b_r64], writes=[b_kraw])
                    P.op("pool", lambda e: e.tensor_copy(out=kraw[64:65, :, 0], in_=r64[64:65, :]), reads=[b_r64], writes=[b_kraw])

                def w4():
                    P.op("act", lambda e: e.activation(out=wtmp[:], in_=kraw[:], func=AF.Abs), reads=[b_kraw], writes=[b_wtmp])

                def w5():
                    P.op("dve", lambda e: e.tensor_reduce(out=ksum[:], in_=wtmp[:], axis=AX.X, op=ALU.add), reads=[b_wtmp], writes=[b_ksum])
                    P.group("pe", [lambda e: e.matmul(pN[:, 0:32], lhsT=tf[:, 656:784], rhs=ksum[:], start=True, stop=True)],
                            reads=[b_ksum, b_tf], writes=[b_pN])

                def w6():
                    P.op("dve", lambda e: e.reciprocal(out=ksum[:], in_=pN[:, 0:32]), reads=[b_pN], writes=[b_ksum])
                    P.op("pool", lambda e: e.memset(kraw[64:65, :, 0], 0.0), reads=[b_wtmp], writes=[b_kraw])

                def w7():
                    P.op("dve", lambda e: e.tensor_tensor(out=kcs[:], in0=kraw[:], in1=ksum[:, :].unsqueeze(2).to_broadcast([128, 32, 128]), op=ALU.mult),
                         reads=[b_kraw, b_ksum], writes=[b_kcs])
                    P.op("dve", lambda e: e.tensor_tensor(out=kcs[0:1, :, 0].rearrange("p (o c) -> p o c", o=2), in0=kcs[0:1, :, 0].rearrange("p (o c) -> p o c", o=2),
                                                          in1=skA[0:1, :].rearrange("p (o c) -> p o c", o=2)[:, :, c0:c0 + 16], op=ALU.add),
                         reads=[b_kcs, b_sk], writes=[b_kcs])
                    if debug == "p2" and sg == 0:
                        P.dma("sp", lambda e: e.dma_start(out=dbgkc[:, :, :], in_=kcs[:]), reads=[b_kcs])
                return [w0, w1, w2, w3, w4, w5, w6, w7]

            def sg_filtfft(sg):
                c0 = 16 * sg

                def filt_batch(f0s):
                    grp = [(lanes[li], f0) for li, f0 in enumerate(f0s)]
                    for ln, f0 in grp:
                        st_S1(ln, kcs[:, f0:f0 + 4, :], b_kcs, F1FULL)
                    for ln, f0 in grp:
                        st_TW(ln)
                    for ln, f0 in grp:
                        st_S2(ln)
                    for ln, f0 in grp:
                        pXr = ln["pL"][:, 0:512].rearrange("p (c x) -> p c x", c=4); pXi = ln["pL"][:, 512:1024].rearrange("p (c x) -> p c x", c=4)
                        P.op("act", lambda e, f0=f0, pXr=pXr: e.copy(out=Khr[:, f0:f0 + 4, :], in_=pXr), reads=[ln["b_pL"]], writes=[b_Kh])
                        P.op("act", lambda e, f0=f0, pXi=pXi: e.copy(out=Khi[:, f0:f0 + 4, :], in_=pXi), reads=[ln["b_pL"]], writes=[b_Kh])
                for f0s in ((0, 4, 8), (12, 16, 20), (24, 28)):
                    filt_batch(f0s)
                if debug == "p2" and sg == 0:
                    P.dma("sp", lambda e: e.dma_start(out=dbgkh[:, 0, :, :], in_=Khr[:]), reads=[b_Kh])
                    P.dma("sp", lambda e: e.dma_start(out=dbgkh[:, 1, :, :], in_=Khi[:]), reads=[b_Kh])

            def sg_conv_batch(sg, tasks, b_z1g, hooks=None):
                hk = (lambda k: hooks[k]()) if hooks else (lambda k: None)
                us = sg % 2
                grp = []
                for li, (o, cg) in enumerate(tasks):
                    if o == 0:
                        zin, b_zin = ut[us][0][:, 4 * cg:4 * cg + 4, :], b_ut[us][0]
                        gin, b_gin = ut[us][1][:, 4 * cg:4 * cg + 4, :], b_ut[us][1]
                        zout, b_zout = z1t[:, 4 * cg:4 * cg + 4, :], b_z1g[cg]
                    else:
                        zin, b_zin = z1t[:, 4 * cg:4 * cg + 4, :], b_z1g[cg]
                        gin, b_gin = ut[us][2][:, 4 * cg:4 * cg + 4, :], b_ut[us][2]
                        zout, b_zout = z2t[us][:, 4 * cg:4 * cg + 4, :], b_z2[us]
                    grp.append((lanes[li], o, cg, zin, b_zin, gin, b_gin, zout, b_zout))
                for (ln, o, cg, zin, b_zin, gin, b_gin, zout, b_zout) in grp:
                    st_S1(ln, zin, b_zin, F1C)
                hk(0)
                for g_ in grp:
                    st_TW(g_[0])
                hk(1)
                for g_ in grp:
                    st_S2(g_[0])
                hk(2)
                for g_ in grp:
                    st_SPEC(g_[0], g_[1] * 16 + 4 * g_[2])
                hk(3)
                for g_ in grp:
                    st_IS1(g_[0])
                hk(4)
                for g_ in grp:
                    st_ITW(g_[0])
                hk(5)
                for g_ in grp:
                    st_IS2(g_[0])
                hk(6)
                for (ln, o, cg, zin, b_zin, gin, b_gin, zout, b_zout) in grp:
                    st_gate(ln, cg, gin, b_gin, zout, b_zout)
                hk(7)

            def sg_store(sg):
                us = sg % 2
                c0 = 16 * sg
                P.dma("sp", lambda e: e.dma_start(out=Z2[c0:c0 + 16, :].rearrange("c (a b) -> a c b", b=128), in_=z2t[us][0:32, :, :]), reads=[b_z2[us]])

            cvs = [SB(st2, "cvs%d" % i, [128, 2048], BF16) for i in range(3)]; b_cvs = [Buf() for _ in range(3)]
            cv_tasks = [(ti, rb) for rb in range(ne // 128) for ti in range(3)]
            cv_state = {"i": 0}
            ew_src = (ew1, ew3, ew2)

            def cv():
                i = cv_state["i"]
                if i >= len(cv_tasks):
                    return
                cv_state["i"] = i + 1
                ti, rb = cv_tasks[i]
                s = i % 3
                P.dma("pool", lambda e: e.dma_start(out=cvs[s][:], in_=ew_src[ti][rb * 128:(rb + 1) * 128, :]), writes=[b_cvs[s]])
                ee, hh_ = rb // 2, rb % 2
                P.dma("sp", lambda e: e.dma_start(out=EWB[ti][ee * 128:(ee + 1) * 128, hh_ * 2048:(hh_ + 1) * 2048], in_=cvs[s][:]), reads=[b_cvs[s]])
            CONV_TASKS = (((0, 0), (0, 1), (0, 2)), ((0, 3), (1, 0), (1, 1)), ((1, 2), (1, 3)))
            nsg = 32 if debug != "p2" else int(DBG_NSG)
            b_z1g = [Buf() for _ in range(4)]
            sg_load(0)
            for bi in range(8):
                sg_kbatch(0, bi)
            for w_ in sg_window(0):
                w_()
            for sg in range(nsg):
                sg_filtfft(sg)
                nxt = sg + 1 < nsg
                if nxt:
                    sg_load(sg + 1)
                sg_conv_batch(sg, CONV_TASKS[0], b_z1g, hooks=[cv, cv, cv, cv, cv, cv, (lambda: None), (lambda: None)])
                if nxt:
                    for bi in range(0, 4):
                        sg_kbatch(sg + 1, bi)
                sg_conv_batch(sg, CONV_TASKS[1], b_z1g, hooks=[cv, cv, cv, cv, cv, cv, (lambda: None), (lambda: None)])
                if debug == "p2" and sg == 0:
                    P.dma("sp", lambda e: e.dma_start(out=dbgz1[:, :, :], in_=z1t[:]), reads=b_z1g)
                if nxt:
                    for bi in range(4, 8):
                        sg_kbatch(sg + 1, bi)
                sg_conv_batch(sg, CONV_TASKS[2], b_z1g, hooks=(sg_window(sg + 1) if nxt else None))
                sg_store(sg)
            while cv_state["i"] < len(cv_tasks):
                cv()
            P.barrier()
        if debug == "p2":
            return _finish(nc, P, out, gst)

        def layer_norm(st_tiles, r, b_r, g_b, b_b, b_gb, dst, b_dst, eng2="pool", lnexp=False):
            stats, mv, b_stat = st_tiles
            P.op("dve", lambda e: e.bn_stats(out=stats[:, 0, :], in_=r[:, 0:512]), reads=[b_r], writes=[b_stat])
            P.op("dve", lambda e: e.bn_stats(out=stats[:, 1, :], in_=r[:, 512:1024]), reads=[b_r], writes=[b_stat])
            P.op("dve", lambda e: e.bn_aggr(out=mv[:, 0:2], in_=stats[:].rearrange("p a b -> p (a b)")), reads=[b_stat], writes=[b_stat])
            P.op("dve", lambda e: e.tensor_scalar(out=mv[:, 2:3], in0=mv[:, 1:2], scalar1=LN_EPS, scalar2=None, op0=ALU.add), reads=[b_stat], writes=[b_stat])
            if lnexp:
                P.op("act", lambda e: e.activation(out=mv[:, 2:3], in_=mv[:, 2:3], func=AF.Ln), reads=[b_stat], writes=[b_stat])
                P.op("act", lambda e: e.activation(out=mv[:, 2:3], in_=mv[:, 2:3], func=AF.Exp, scale=-0.5), reads=[b_stat], writes=[b_stat])
            else:
                P.op("act", lambda e: e.activation(out=mv[:, 2:3], in_=mv[:, 2:3], func=AF.Sqrt), reads=[b_stat], writes=[b_stat])
                P.op("dve", lambda e: e.reciprocal(out=mv[:, 2:3], in_=mv[:, 2:3]), reads=[b_stat], writes=[b_stat])
            P.op("dve", lambda e: e.scalar_tensor_tensor(out=mv[:, 3:4], in0=mv[:, 0:1], scalar=-1.0, in1=mv[:, 2:3], op0=ALU.mult, op1=ALU.mult),
                 reads=[b_stat], writes=[b_stat])
            P.op("act", lambda e: e.activation(out=r[:], in_=r[:], func=AF.Identity, scale=mv[:, 2:3], bias=mv[:, 3:4]), reads=[b_r, b_stat], writes=[b_r])
            P.op(eng2, lambda e: e.tensor_tensor(out=r[:], in0=r[:], in1=g_b, op=ALU.mult), reads=[b_r, b_gb], writes=[b_r])
            P.op("dve", lambda e: e.tensor_tensor(out=dst, in0=r[:], in1=b_b, op=ALU.add), reads=[b_r, b_gb], writes=[b_dst])

        with ExitStack() as st34:
            dest = SB(st34, "dest", [128, 32, 2], I32); b_dest = Buf()
            wts = SB(st34, "wts", [128, 32, 2], F32); b_wts = Buf()
            widx = SB(st34, "widx", [128, 128, 2], I32); b_widx = Buf()
            with ExitStack() as st:
                wao = SB(st, "wao", [128, 4, D], BF16); who = SB(st, "who", [128, 4, D], BF16); wout = SB(st, "wout", [128, 8, D], BF16); b_w3p = Buf()

                def ldw(dst, srcw, nk):
                    for kc in range(nk):
                        P.dma("pool", lambda e, kc=kc: e.dma_start(out=dst[:, kc, :], in_=srcw[kc * 128:(kc + 1) * 128, :]), writes=[b_w3p])
                ldw(wao, w_attn_o, 4); ldw(who, w_hy_o, 4); ldw(wout, w_out, 8)
                bc = SB(st, "bc", [128, 5, D], F32); b_bc = Buf()
                for k, srcap in enumerate((mod_scr[0:1, 2048:3072], ln1g[0:1, :], ln1b[0:1, :], mod_scr[0:1, 4096:5120], mod_scr[0:1, 3072:4096])):
                    P.dma("sp", lambda e, k=k, srcap=srcap: e.dma_start(out=bc[:, k, :], in_=srcap.partition_broadcast(128)), writes=[b_bc])
                wrt = SB(st, "wrt", [128, 8, 72], F32); brb = SB(st, "brb", [128, 72], F32); b_wr = Buf()
                P.dma("sp", lambda e: e.dma_start(out=wrt[:], in_=wr[:, :].rearrange("(c p) n -> p c n", p=128)), writes=[b_wr])
                P.dma("sp", lambda e: e.dma_start(out=brb[:], in_=br[0:1, :].partition_broadcast(128)), writes=[b_wr])
                rcarry = SB(st, "rcarry", [128, 64], F32); b_rcarry = Buf()
                P.op("pool", lambda e: e.memset(rcarry[:], 0.0), writes=[b_rcarry])
                rnk = SB(st, "rnk", [128, 32, 2], F32); b_rnk = Buf()
                ohall = SB(st, "ohall", [128, 32, 2, 64], BF16); b_oh = Buf()
                atM = SB(st, "atTm", [128, 4, 512], BF16); z2T = SB(st, "z2T", [128, 4, 512], BF16)
                sga = SB(st, "sga", [128, 8, 512], BF16); sgh = SB(st, "sgh", [128, 8, 512], BF16)
                b_at = Buf(); b_z2T = Buf(); b_sga = Buf(); b_sgh = Buf()
                mrg = [SB(st, "mrg%d" % i, [128, 8, 512], BF16) for i in range(2)]; b_mrg = [Buf(), Buf()]
                t1 = [SB(st, "t1_%d" % i, [128, 512], F32) for i in range(2)]; t2 = [SB(st, "t2_%d" % i, [128, 512], F32) for i in range(2)]
                b_t1 = [Buf(), Buf()]; b_t2 = [Buf(), Buf()]
                xt = [SB(st, "xt%d" % i, [128, D], F32) for i in range(2)]; b_xt = [Buf(), Buf()]
                rr = [SB(st, "rr%d" % i, [128, D], F32) for i in range(2)]; b_rr = [Buf(), Buf()]
                x1s = [SB(st, "x1s%d" % i, [128, D], F32) for i in range(2)]; b_x1s = [Buf(), Buf()]
                h2 = SB(st, "h2", [128, D], F32); b_h2t = Buf()
                h2b = [SB(st, "h2b%d" % i, [128, D], BF16) for i in range(2)]; b_h2b = [Buf(), Buf()]
                h2T = SB(st, "h2T", [128, 8, 128], F32); b_h2T = Buf()
                stats = SB(st, "stats", [128, 2, 6], F32); mv = SB(st, "mv", [128, 4], F32); b_stat = Buf()
                lg = SB(st, "lg", [128, 72], F32); b_lg = Buf()
                sm = SB(st, "sm", [128, 64], F32); b_sm = Buf()
                elm = SB(st, "elm", [128, 64], F32); b_elm = Buf()
                m8 = SB(st, "m8", [128, 16], F32); b_m8 = Buf()
                ohs = SB(st, "ohs", [128, 64], BF16); b_ohs = Buf()
                basef = SB(st, "basef", [128, 64], F32); b_base = Buf()
                junk = SB(st, "junk", [128, 64], F32); b_junk = Buf()
                pMa = PS(st, "pMa", [128, 512]); pMh = PS(st, "pMh", [128, 512]); b_pMa = Buf(); b_pMh = Buf()
                pYt = PS(st, "pYt", [128, 1024]); b_pYt = Buf()
                pHT = PS(st, "pHT", [128, 1024]); b_pHT = Buf()
                pR = PS(st, "pR", [128, 512]); b_pR = Buf()
                pLg = PS(st, "pLg", [128, 512]); b_pLg = Buf()

                def route_tile(i, x1tile, b_x1tile, hs):
                    P.op("dve", lambda e: e.tensor_tensor(out=h2[:], in0=x1tile[:], in1=bc[:, 3, :], op=ALU.mult), reads=[b_x1tile, b_bc], writes=[b_h2t])
                    P.op("dve", lambda e: e.tensor_tensor(out=h2[:], in0=h2[:], in1=bc[:, 4, :], op=ALU.add), reads=[b_h2t, b_bc], writes=[b_h2t])
                    P.op("act", lambda e: e.copy(out=h2b[hs][:], in_=h2[:]), reads=[b_h2t], writes=[b_h2b[hs]])
                    P.dma("sp", lambda e: e.dma_start(out=H2[i * 128:(i + 1) * 128, :], in_=h2b[hs][:]), reads=[b_h2b[hs]])
                    pHTv = pHT[:, :].rearrange("p (c q) -> p c q", c=8)
                    P.group("pe", [lambda e, kc=kc: e.transpose(out=pHTv[:, kc, :], in_=h2[:, kc * 128:(kc + 1) * 128], identity=identf) for kc in range(8)],
                            reads=[b_h2t, b_tf], writes=[b_pHT])
                    P.op("act", lambda e: e.copy(out=h2T[:], in_=pHTv), reads=[b_pHT], writes=[b_h2T])
                    P.group("pe", [lambda e, kc=kc: e.matmul(pLg[:, 0:72], lhsT=h2T[:, kc, :], rhs=wrt[:, kc, :], start=(kc == 0), stop=(kc == 7)) for kc in range(8)],
                            reads=[b_h2T, b_wr], writes=[b_pLg])
                    P.op("dve", lambda e: e.tensor_tensor(out=lg[:], in0=pLg[:, 0:72], in1=brb[:], op=ALU.add), reads=[b_pLg, b_wr], writes=[b_lg])
                    P.op("dve", lambda e: e.max(out=m8[:, 0:8], in_=lg[:, 0:8]), reads=[b_lg], writes=[b_m8])
                    P.op("dve", lambda e: e.tensor_scalar(out=sm[:, 0:8], in0=lg[:, 0:8], scalar1=m8[:, 0:1], scalar2=None, op0=ALU.is_equal),
                         reads=[b_lg, b_m8], writes=[b_sm])
                    P.op("dve", lambda e: e.tensor_scalar(out=sm[:, 8:9], in0=m8[:, 0:1], scalar1=-1.0, scalar2=None, op0=ALU.mult), reads=[b_m8], writes=[b_sm])
                    P.op("act", lambda e: e.activation(out=sm[:, 16:24], in_=lg[:, 0:8], func=AF.Exp, bias=sm[:, 8:9], scale=1.0, accum_out=sm[:, 9:10]),
                         reads=[b_lg, b_sm], writes=[b_sm])
                    P.op("dve", lambda e: e.reciprocal(out=sm[:, 10:11], in_=sm[:, 9:10]), reads=[b_sm], writes=[b_sm])
                    P.op("dve", lambda e: e.tensor_scalar(out=sm[:, 24:32], in0=sm[:, 0:8], scalar1=1e30, scalar2=-1e30, op0=ALU.mult, op1=ALU.add),
                         reads=[b_sm], writes=[b_sm])
                    P.op("dve", lambda e: e.tensor_tensor(out=elm[:, :].rearrange("p (g e) -> p g e", g=8), in0=lg[:, 8:72].rearrange("p (g e) -> p g e", g=8),
                                                          in1=sm[:, 24:32].unsqueeze(2).to_broadcast([128, 8, 8]), op=ALU.add),
                         reads=[b_lg, b_sm], writes=[b_elm])
                    P.op("dve", lambda e: e.max(out=m8[:, 8:16], in_=elm[:]), reads=[b_elm], writes=[b_m8])
                    P.op("dve", lambda e: e.tensor_scalar(out=ohall[:, i, 0, :], in0=elm[:], scalar1=m8[:, 8:9], scalar2=None, op0=ALU.is_equal),
                         reads=[b_elm, b_m8], writes=[b_oh])
                    P.op("dve", lambda e: e.tensor_scalar(out=ohall[:, i, 1, :], in0=elm[:], scalar1=m8[:, 9:10], scalar2=None, op0=ALU.is_equal),
                         reads=[b_elm, b_m8], writes=[b_oh])
                    P.op("dve", lambda e: e.tensor_tensor(out=sm[:, 11:12], in0=m8[:, 9:10], in1=m8[:, 8:9], op=ALU.subtract), reads=[b_m8], writes=[b_sm])
                    P.op("act", lambda e: e.activation(out=sm[:, 12:13], in_=sm[:, 11:12], func=AF.Exp), reads=[b_sm], writes=[b_sm])
                    P.op("dve", lambda e: e.tensor_scalar(out=sm[:, 12:13], in0=sm[:, 12:13], scalar1=1.0, scalar2=None, op0=ALU.add), reads=[b_sm], writes=[b_sm])
                    P.op("dve", lambda e: e.reciprocal(out=sm[:, 13:14], in_=sm[:, 12:13]), reads=[b_sm], writes=[b_sm])
                    P.op("dve", lambda e: e.tensor_tensor(out=wts[:, i, 0:1], in0=sm[:, 13:14], in1=sm[:, 10:11], op=ALU.mult), reads=[b_sm], writes=[b_wts])
                    P.op("dve", lambda e: e.tensor_tensor(out=wts[:, i, 1:2], in0=sm[:, 10:11], in1=wts[:, i, 0:1], op=ALU.subtract), reads=[b_sm, b_wts], writes=[b_wts])
                    P.op("dve", lambda e: e.tensor_tensor(out=ohs[:], in0=ohall[:, i, 0, :], in1=ohall[:, i, 1, :], op=ALU.add), reads=[b_oh], writes=[b_ohs])
                    P.group("pe", [lambda e: e.matmul(pR[:, 0:64], lhsT=tb[:, CT_TRI:CT_TRI + 128], rhs=ohs[:], start=True, stop=True),
                                   lambda e: e.matmul(pR[:, 64:128], lhsT=tb[:, CT_ONES:CT_ONES + 128], rhs=ohs[:], start=True, stop=True)],
                            reads=[b_ohs, b_tb], writes=[b_pR])
                    P.op("dve", lambda e: e.tensor_tensor(out=basef[:], in0=pR[:, 0:64], in1=rcarry[:], op=ALU.add), reads=[b_pR, b_rcarry], writes=[b_base])
                    for k in range(2):
                        P.op("dve", lambda e, k=k: e.tensor_tensor(out=junk[:], in0=ohall[:, i, k, :], in1=basef[:], op=ALU.mult),
                             reads=[b_oh, b_base], writes=[b_junk])
                        P.op("dve", lambda e, k=k: e.tensor_reduce(out=rnk[:, i, k:k + 1], in_=junk[:], axis=AX.X, op=ALU.add),
                             reads=[b_junk], writes=[b_rnk])
                    P.op("dve", lambda e: e.tensor_tensor(out=rcarry[:], in0=rcarry[:], in1=pR[:, 64:128], op=ALU.add), reads=[b_pR, b_rcarry], writes=[b_rcarry])

                def merge_loads(s_i):
                    c0 = s_i * 512
                    P.dma("sp", lambda e: e.dma_start(out=atM[:], in_=AT[:, c0:c0 + 512].rearrange("(c p) q -> p c q", p=128)), writes=[b_at])
                    P.dma("sp", lambda e: e.dma_start(out=z2T[:], in_=Z2[:, c0:c0 + 512].rearrange("(c p) q -> p c q", p=128)), writes=[b_z2T])
                    P.dma("sp", lambda e: e.dma_start(out=sga[:], in_=Gs[0:1024, c0:c0 + 512].rearrange("(c p) q -> p c q", p=128)), writes=[b_sga])
                    P.dma("sp", lambda e: e.dma_start(out=sgh[:], in_=Gs[1024:2048, c0:c0 + 512].rearrange("(c p) q -> p c q", p=128)), writes=[b_sgh])

                def merge_compute(s_i):
                    ms = s_i % 2

                    def fchunk(fc):
                        ts = fc % 2
                        P.group("pe", [lambda e, kc=kc: e.matmul(pMa[:], lhsT=wao[:, kc, fc * 128:(fc + 1) * 128], rhs=atM[:, kc, :], start=(kc == 0), stop=(kc == 3))
                                       for kc in range(4)], reads=[b_w3p, b_at], writes=[b_pMa])
                        P.group("pe", [lambda e, kc=kc: e.matmul(pMh[:], lhsT=who[:, kc, fc * 128:(fc + 1) * 128], rhs=z2T[:, kc, :], start=(kc == 0), stop=(kc == 3))
                                       for kc in range(4)], reads=[b_w3p, b_z2T], writes=[b_pMh])
                        P.op("dve", lambda e: e.tensor_tensor(out=t1[ts][:], in0=pMa[:], in1=sga[:, fc, :], op=ALU.mult), reads=[b_pMa, b_sga], writes=[b_t1[ts]])
                        P.op("dve", lambda e: e.tensor_tensor(out=t2[ts][:], in0=pMh[:], in1=sgh[:, fc, :], op=ALU.mult), reads=[b_pMh, b_sgh], writes=[b_t2[ts]])
                        P.op("dve", lambda e: e.tensor_tensor(out=mrg[ms][:, fc, :], in0=t1[ts][:], in1=t2[ts][:], op=ALU.add),
                             reads=[b_t1[ts], b_t2[ts]], writes=[b_mrg[ms]])
                    for fc in range(8):
                        fchunk(fc)

                def tile_A(s_i, t):
                    ms = s_i % 2
                    i = s_i * 4 + t
                    xs_ = i % 2
                    P.dma("sp", lambda e: e.dma_start(out=xt[xs_][:], in_=xcat[i * 128:(i + 1) * 128, :]), writes=[b_xt[xs_]])
                    fns = []
                    for nh in range(2):
                        for kc in range(8):
                            fns.append(lambda e, nh=nh, kc=kc: e.matmul(pYt[:, nh * 512:(nh + 1) * 512], lhsT=mrg[ms][:, kc, t * 128:(t + 1) * 128],
                                                                      rhs=wout[:, kc, nh * 512:(nh + 1) * 512], start=(kc == 0), stop=(kc == 7)))
                    P.group("pe", fns, reads=[b_mrg[ms], b_w3p], writes=[b_pYt])
                    P.op("dve", lambda e: e.tensor_tensor(out=rr[xs_][:], in0=pYt[:], in1=bc[:, 0, :], op=ALU.mult), reads=[b_pYt, b_bc], writes=[b_rr[xs_]])
                    P.op("dve", lambda e: e.scalar_tensor_tensor(out=rr[xs_][:], in0=xt[xs_][:], scalar=DN_ALPHA, in1=rr[xs_][:], op0=ALU.mult, op1=ALU.add),
                         reads=[b_xt[xs_], b_rr[xs_]], writes=[b_rr[xs_]])
                    layer_norm((stats, mv, b_stat), rr[xs_], b_rr[xs_], bc[:, 1, :], bc[:, 2, :], b_bc, x1s[xs_][:], b_x1s[xs_], eng2="dve", lnexp=True)
                    P.dma("act", lambda e: e.dma_start(out=X1[i * 128:(i + 1) * 128, :], in_=x1s[xs_][:]), reads=[b_x1s[xs_]])

                def tile_B(s_i, t):
                    i = s_i * 4 + t
                    xs_ = i % 2
                    route_tile(i, x1s[xs_], b_x1s[xs_], xs_)

                merge_loads(0)
                merge_compute(0)
                for s_i in range(8):
                    nx = s_i + 1 < 8
                    if nx:
                        merge_loads(s_i + 1)
                    tile_A(s_i, 0)
                    tile_A(s_i, 1)
                    tile_B(s_i, 0)
                    if nx:
                        merge_compute(s_i + 1)
                    tile_A(s_i, 2)
                    tile_B(s_i, 1)
                    tile_A(s_i, 3)
                    tile_B(s_i, 2)
                    tile_B(s_i, 3)

                ci = SB(st, "cnt_i", [128, 64], I32); b_ci = Buf()
                psz = SB(st, "psz", [128, 64], F32); pends = SB(st, "pends", [128, 64], F32); poffs = SB(st, "poffs", [128, 64], F32)
                zer = SB(st, "zer", [128, 64], F32); b_pz = Buf()
                P.op("dve", lambda e: e.tensor_scalar(out=psz[:], in0=rcarry[:], scalar1=127.0, scalar2=None, op0=ALU.add), reads=[b_rcarry], writes=[b_pz])
                P.op("dve", lambda e: e.tensor_copy(out=ci[:], in_=psz[:]), reads=[b_pz], writes=[b_ci])
                P.op("dve", lambda e: e.tensor_scalar(out=ci[:], in0=ci[:], scalar1=7, scalar2=7, op0=ALU.arith_shift_right, op1=ALU.logical_shift_left),
                     reads=[b_ci], writes=[b_ci])
                P.op("dve", lambda e: e.tensor_copy(out=psz[:], in_=ci[:]), reads=[b_ci], writes=[b_pz])
                P.op("dve", lambda e: e.memset(zer[:], 0.0), writes=[b_pz])
                P.op("dve", lambda e: e.tensor_tensor_scan(out=pends[:], data0=psz[:], data1=zer[:], initial=0.0, op0=ALU.add, op1=ALU.add), reads=[b_pz], writes=[b_pz])
                P.op("dve", lambda e: e.tensor_tensor(out=poffs[:], in0=pends[:], in1=psz[:], op=ALU.subtract), reads=[b_pz], writes=[b_pz])
                big = SB(st, "big", [128, 64, 64], F32); b_big = Buf()
                dsf = SB(st, "dsf", [128, 64], F32); b_dsf = Buf()
                P.op("dve", lambda e: e.tensor_tensor(out=big[:], in0=ohall[:].rearrange("p i k e -> p (i k) e"),
                                                      in1=poffs[:, :].unsqueeze(1).to_broadcast([128, 64, 64]), op=ALU.mult), reads=[b_oh, b_pz], writes=[b_big])
                P.op("dve", lambda e: e.tensor_reduce(out=dsf[:], in_=big[:], axis=AX.X, op=ALU.add), reads=[b_big], writes=[b_dsf])
                P.op("dve", lambda e: e.tensor_tensor(out=dsf[:], in0=dsf[:], in1=rnk[:].rearrange("p i k -> p (i k)"), op=ALU.add), reads=[b_dsf, b_rnk], writes=[b_dsf])
                P.op("dve", lambda e: e.tensor_copy(out=dest[:].rearrange("p i k -> p (i k)"), in_=dsf[:]), reads=[b_dsf], writes=[b_dest])
                blke = SB(st, "blke", [128, 128], F32); b_blke = Buf()

                def blk_chunk(jc):
                    bigv = big[:, 0:32, :]
                    P.op("dve", lambda e: e.tensor_tensor(out=bigv, in0=pends[:, :].unsqueeze(1).to_broadcast([128, 32, 64]),
                                                          in1=tf[:, 400 + jc * 32:400 + (jc + 1) * 32].unsqueeze(2).to_broadcast([128, 32, 64]), op=ALU.is_le),
                         reads=[b_pz, b_tf, b_dsf], writes=[b_big])
                    P.op("dve", lambda e: e.tensor_reduce(out=blke[:, jc * 32:(jc + 1) * 32], in_=bigv, axis=AX.X, op=ALU.add), reads=[b_big], writes=[b_blke])
                for jc in range(4):
                    blk_chunk(jc)
                skipf = SB(st, "skipf", [128, 128], F32); b_skipf = Buf()
                P.op("dve", lambda e: e.memset(skipf[:, 0:3], 0.0), writes=[b_skipf])
                P.op("dve", lambda e: e.tensor_scalar(out=blke[:], in0=blke[:], scalar1=63.0, scalar2=None, op0=ALU.min), reads=[b_blke], writes=[b_blke])
                P.op("dve", lambda e: e.tensor_tensor(out=skipf[:, 3:128], in0=blke[:, 3:128], in1=blke[:, 0:125], op=ALU.is_equal), reads=[b_blke, b_skipf], writes=[b_skipf])
                P.op("dve", lambda e: e.tensor_scalar(out=blke[:], in0=blke[:], scalar1=128.0, scalar2=None, op0=ALU.mult), reads=[b_blke, b_skipf], writes=[b_blke])
                P.op("dve", lambda e: e.scalar_tensor_tensor(out=blke[:], in0=skipf[:], scalar=1000000.0, in1=blke[:], op0=ALU.mult, op1=ALU.add),
                     reads=[b_blke, b_skipf], writes=[b_blke])
                P.op("dve", lambda e: e.tensor_scalar(out=blke[:], in0=blke[:], scalar1=tf[:, 384:385], scalar2=None, op0=ALU.add), reads=[b_blke, b_tf], writes=[b_blke])
                P.op("dve", lambda e: e.tensor_copy(out=widx[:, :, 0], in_=blke[:]), reads=[b_blke], writes=[b_widx])
                P.op("dve", lambda e: e.tensor_scalar(out=blke[:], in0=blke[:], scalar1=128.0, scalar2=None, op0=ALU.add), reads=[b_blke, b_widx], writes=[b_blke])
                P.op("dve", lambda e: e.tensor_copy(out=widx[:, :, 1], in_=blke[:]), reads=[b_blke], writes=[b_widx])
                if debug in ("p3", "p4"):
                    dbgrt = dscr("dbgrt", [128, 64 + 64 + 256], F32)
                    dbt = SB(st, "dbt", [128, 384], F32); b_dbt = Buf()
                    P.op("dve", lambda e: e.tensor_copy(out=dbt[:, 0:64], in_=dest[:].rearrange("p i k -> p (i k)")), reads=[b_dest], writes=[b_dbt])
                    P.op("dve", lambda e: e.tensor_copy(out=dbt[:, 64:128], in_=wts[:].rearrange("p i k -> p (i k)")), reads=[b_wts], writes=[b_dbt])
                    P.op("dve", lambda e: e.tensor_copy(out=dbt[:, 128:384], in_=widx[:].rearrange("p j h -> p (j h)")), reads=[b_widx], writes=[b_dbt])
                    P.dma("sp", lambda e: e.dma_start(out=dbgrt[:, :], in_=dbt[:]), reads=[b_dbt])
                P.barrier()
            if debug == "p3":
                return _finish(nc, P, out, gst)

            with ExitStack() as st:
                hrow = [SB(st, "hrow%d" % i, [128, D], BF16) for i in range(3)]; b_hrow = [Buf() for _ in range(3)]

                def scat(i):
                    s = i % 3
                    P.dma("sp", lambda e: e.dma_start(out=hrow[s][:], in_=H2[i * 128:(i + 1) * 128, :]), writes=[b_hrow[s]])
                    for k in range(2):
                        P.dma("pool", lambda e, k=k: e.indirect_dma_start(out=XB[:, :], out_offset=bass.IndirectOffsetOnAxis(ap=dest[:, i, k:k + 1], axis=0),
                                                                          in_=hrow[s][:], in_offset=None),
                              reads=[b_hrow[s], b_dest])
                for i in range(32):
                    scat(i)
                P.barrier()

            with ExitStack() as st:
                w1s = [SB(st, "w1s%d" % i, [128, 2, 2048], BF16) for i in range(3)]
                w3s = [SB(st, "w3s%d" % i, [128, 2, 2048], BF16) for i in range(3)]
                w2s = [SB(st, "w2s%d" % i, [128, 2, 2048], BF16) for i in range(3)]
                b_w1s = [Buf() for _ in range(3)]; b_w3s = [Buf() for _ in range(3)]; b_w2s = [Buf() for _ in range(3)]
                xbt = [SB(st, "xbt%d" % i, [128, D], BF16) for i in range(3)]; b_xbt = [Buf() for _ in range(3)]
                xbT = [SB(st, "xbT%d" % i, [128, 8, 128], BF16) for i in range(2)]; b_xbT = [Buf(), Buf()]
                slu = SB(st, "slu", [128, 512], F32); b_slu = Buf()
                ggb = SB(st, "ggb", [128, 512], BF16); b_ggb = Buf()
                gTb = SB(st, "gTb", [128, 4, 128], BF16); b_gTb = Buf()
                ybt = [SB(st, "ybt%d" % i, [128, D], BF16) for i in range(2)]; b_ybt = [Buf(), Buf()]
                pXT = PS(st, "pXT", [128, 1024], BF16); b_pXT = Buf()
                pH1 = PS(st, "pE1", [128, 512]); pH3 = PS(st, "pE3", [128, 512]); b_pH1 = Buf(); b_pH3 = Buf()
                pGT = PS(st, "pGT", [128, 1024], BF16); b_pGT = Buf()
                pO2 = PS(st, "pE2", [128, 1024]); b_pO2 = Buf()

                _bcr = {}

                def _bc_reg(e):
                    if "r" not in _bcr:
                        _bcr["r"] = e.to_reg(ne // 2 - 1)
                    return _bcr["r"]

                NW = 3

                def stG(j):
                    s = j % NW
                    for (wsb, wdram, bw) in ((w1s, EWB[0], b_w1s), (w3s, EWB[1], b_w3s), (w2s, EWB[2], b_w2s)):
                        P.dma("pool", lambda e, wsb=wsb, wdram=wdram: e.indirect_dma_start(
                            out=wsb[s][:, :, :].rearrange("p h n -> p (h n)"), out_offset=None, in_=wdram[:, :],
                            in_offset=bass.IndirectOffsetOnAxis(ap=widx[:, j, 0:1], axis=0), bounds_check=_bc_reg(e), oob_is_err=False),
                            reads=[b_widx], writes=[bw[s]])
                    x3 = j % 3
                    P.dma("sp", lambda e: e.dma_start(out=xbt[x3][:], in_=XB[j * 128:(j + 1) * 128, :]), writes=[b_xbt[x3]])

                def stAT(j):
                    x2 = j % 2
                    x3 = j % 3
                    pXTv = pXT[:, :].rearrange("p (c q) -> p c q", c=8)
                    P.group("pe", [lambda e, kc=kc: e.transpose(out=pXTv[:, kc, :], in_=xbt[x3][:, kc * 128:(kc + 1) * 128], identity=identb) for kc in range(8)],
                            reads=[b_xbt[x3], b_tb], writes=[b_pXT])
                    P.op("dve", lambda e: e.tensor_copy(out=xbT[x2][:], in_=pXTv), reads=[b_pXT], writes=[b_xbT[x2]])

                def stBM(j):
                    s = j % NW
                    x2 = j % 2
                    P.group("pe", [lambda e, kc=kc: e.matmul(pH1[:], lhsT=xbT[x2][:, kc, :], rhs=w1s[s][:, kc // 4, (kc % 4) * 512:(kc % 4 + 1) * 512],
                                                             start=(kc == 0), stop=(kc == 7)) for kc in range(8)],
                            reads=[b_xbT[x2], b_w1s[s]], writes=[b_pH1])
                    P.group("pe", [lambda e, kc=kc: e.matmul(pH3[:], lhsT=xbT[x2][:, kc, :], rhs=w3s[s][:, kc // 4, (kc % 4) * 512:(kc % 4 + 1) * 512],
                                                             start=(kc == 0), stop=(kc == 7)) for kc in range(8)],
                            reads=[b_xbT[x2], b_w3s[s]], writes=[b_pH3])
                    P.op("act", lambda e: e.activation(out=slu[:], in_=pH1[:], func=AF.Silu), reads=[b_pH1], writes=[b_slu])
                    P.op("dve", lambda e: e.tensor_tensor(out=ggb[:], in0=pH3[:], in1=slu[:], op=ALU.mult), reads=[b_pH3, b_slu], writes=[b_ggb])

                def stCT(j):
                    pGTv = pGT[:, 0:512].rearrange("p (c q) -> p c q", c=4)
                    P.group("pe", [lambda e, fc=fc: e.transpose(out=pGTv[:, fc, :], in_=ggb[:, fc * 128:(fc + 1) * 128], identity=identb) for fc in range(4)],
                            reads=[b_ggb, b_tb], writes=[b_pGT])
                    P.op("act", lambda e: e.copy(out=gTb[:], in_=pGTv), reads=[b_pGT], writes=[b_gTb])

                def stDM(j):
                    s = j % NW
                    y2 = j % 2
                    fns = []
                    for nh in range(2):
                        for fc in range(4):
                            fns.append(lambda e, nh=nh, fc=fc: e.matmul(pO2[:, nh * 512:(nh + 1) * 512], lhsT=gTb[:, fc, :],
                                                                      rhs=w2s[s][:, fc // 2, (fc % 2) * 1024 + nh * 512:(fc % 2) * 1024 + (nh + 1) * 512],
                                                                      start=(fc == 0), stop=(fc == 3)))
                    P.group("pe", fns, reads=[b_gTb, b_w2s[s]], writes=[b_pO2])
                    P.op("act", lambda e: e.copy(out=ybt[y2][:, 0:512], in_=pO2[:, 0:512]), reads=[b_pO2], writes=[b_ybt[y2]])
                    P.op("dve", lambda e: e.tensor_copy(out=ybt[y2][:, 512:1024], in_=pO2[:, 512:1024]), reads=[b_pO2], writes=[b_ybt[y2]])
                    P.dma("act", lambda e: e.dma_start(out=YB[j * 128:(j + 1) * 128, :], in_=ybt[y2][:]), reads=[b_ybt[y2]])

                for j in range(3):
                    stG(j)
                stAT(0); stAT(1); stBM(0)
                for j in range(NBLK):
                    stCT(j)
                    if j + 1 < NBLK:
                        stBM(j + 1)
                    if j + 2 < NBLK:
                        stAT(j + 2)
                    stDM(j)
                    if j + 3 < NBLK:
                        stG(j + 3)
                P.barrier()
            if debug == "p4":
                return _finish(nc, P, out, gst)

            with ExitStack() as st:
                bc2 = SB(st, "bc2", [128, 3, D], F32); b_bc2 = Buf()
                for k, srcap in enumerate((mod_scr[0:1, 5120:6144], ln2g[0:1, :], ln2b[0:1, :])):
                    P.dma("sp", lambda e, k=k, srcap=srcap: e.dma_start(out=bc2[:, k, :], in_=srcap.partition_broadcast(128)), writes=[b_bc2])
                NR = 3
                ra = [SB(st, "ra%d" % i, [128, D], F32) for i in range(NR)]; rb = [SB(st, "rb%d" % i, [128, D], BF16) for i in range(NR)]
                rab = [SB(st, "rab%d" % i, [128, D], BF16) for i in range(NR)]
                b_ra = [Buf() for _ in range(NR)]; b_rb = [Buf() for _ in range(NR)]
                x1c = [SB(st, "x1c%d" % i, [128, D], F32) for i in range(NR)]; b_x1c = [Buf() for _ in range(NR)]
                oc_ = [SB(st, "oc%d" % i, [128, D], F32) for i in range(2)]; b_oc = [Buf(), Buf()]
                stats2 = SB(st, "stats2", [128, 2, 6], F32); mv2 = SB(st, "mv2", [128, 4], F32); b_stat2 = Buf()

                def comb_load(i):
                    s = i % NR
                    P.dma("pool", lambda e: e.indirect_dma_start(out=rab[s][:], out_offset=None, in_=YB[:, :],
                                                                 in_offset=bass.IndirectOffsetOnAxis(ap=dest[:, i, 0:1], axis=0)), reads=[b_dest], writes=[b_ra[s]])
                    P.dma("pool", lambda e: e.indirect_dma_start(out=rb[s][:], out_offset=None, in_=YB[:, :],
                                                                 in_offset=bass.IndirectOffsetOnAxis(ap=dest[:, i, 1:2], axis=0)), reads=[b_dest], writes=[b_rb[s]])
                    P.dma("sp", lambda e: e.dma_start(out=x1c[s][:], in_=X1[i * 128:(i + 1) * 128, :]), writes=[b_x1c[s]])

                def comb(i):
                    s = i % NR
                    so = i % 2
                    P.op("act", lambda e: e.activation(out=ra[s][:], in_=rab[s][:], func=AF.Identity, scale=wts[:, i, 0:1]), reads=[b_ra[s], b_wts], writes=[b_ra[s]])
                    P.op("dve", lambda e: e.scalar_tensor_tensor(out=ra[s][:], in0=rb[s][:], scalar=wts[:, i, 1:2], in1=ra[s][:], op0=ALU.mult, op1=ALU.add),
                         reads=[b_rb[s], b_ra[s], b_wts], writes=[b_ra[s]])
                    P.op("dve", lambda e: e.tensor_tensor(out=ra[s][:], in0=ra[s][:], in1=bc2[:, 0, :], op=ALU.mult), reads=[b_ra[s], b_bc2], writes=[b_ra[s]])
                    P.op("dve", lambda e: e.scalar_tensor_tensor(out=ra[s][:], in0=x1c[s][:], scalar=DN_ALPHA, in1=ra[s][:], op0=ALU.mult, op1=ALU.add),
                         reads=[b_x1c[s], b_ra[s]], writes=[b_ra[s]])
                    layer_norm((stats2, mv2, b_stat2), ra[s], b_ra[s], bc2[:, 1, :], bc2[:, 2, :], b_bc2, oc_[so][:], b_oc[so], eng2="dve")
                    P.dma("sp", lambda e: e.dma_start(out=out[i * 128:(i + 1) * 128, :], in_=oc_[so][:]), reads=[b_oc[so]])
                comb_load(0); comb_load(1)
                for i in range(32):
                    if i + 2 < 32:
                        comb_load(i + 2)
                    comb(i)
    return _finish(nc, P, out, gst)


def _finish(nc, P, out, gst):
    P.barrier()
    P.emit()
    return nc


def make_in_maps(inp):
    f32 = np.float32
    x = np.asarray(inp["x"], f32)
    c = np.asarray(inp["c"], f32)
    perm = q_perm()
    w_in = np.ascontiguousarray(np.asarray(inp["w_in"], f32)[0][:, perm])
    conv_w = np.asarray(inp["conv_w"], f32)[0]
    conv_b = np.asarray(inp["conv_b"], f32)[0]
    cwl = np.ascontiguousarray(conv_w.reshape(3, 12, 128).transpose(2, 0, 1))
    cbl = np.ascontiguousarray(conv_b.reshape(12, 128).T)

    def ewl(w, kchunks):
        E, K, N = w.shape
        return np.ascontiguousarray(w.reshape(E, 2, kchunks, 128, N).transpose(0, 1, 3, 2, 4).reshape(E * 2 * 128, kchunks * N))

    ew1 = ewl(np.asarray(inp["exp_w1"], f32)[0], 4)
    ew3 = ewl(np.asarray(inp["exp_w3"], f32)[0], 4)
    ew2 = ewl(np.asarray(inp["exp_w2"], f32)[0], 2)
    wrr = np.ascontiguousarray(np.concatenate([np.asarray(inp["router_group_w"], f32)[0], np.asarray(inp["router_expert_w"], f32)[0]], axis=1))
    brr = np.ascontiguousarray(np.concatenate([np.asarray(inp["router_group_b"], f32)[0], np.asarray(inp["router_expert_b"], f32)[0]])[None, :])
    shared = {
        "w_ada": np.asarray(inp["w_ada"], f32)[0], "b_ada": np.asarray(inp["b_ada"], f32)[0][None, :],
        "w_in": w_in, "conv_w": cwl, "conv_b": cbl,
        "fw1": np.asarray(inp["filt_w1"], f32)[0], "fb1": np.asarray(inp["filt_b1"], f32)[0][:, None],
        "ff1": np.asarray(inp["filt_freq1"], f32)[0][:, None],
        "fw2": np.asarray(inp["filt_w2"], f32)[0], "fb2": np.asarray(inp["filt_b2"], f32)[0][:, None],
        "ff2": np.asarray(inp["filt_freq2"], f32)[0][:, None],
        "fw3": np.asarray(inp["filt_w3"], f32)[0], "fdec": np.asarray(inp["filt_decay"], f32)[0][None, :],
        "hskip": np.asarray(inp["hy_skip"], f32)[0].reshape(1, 1024),
        "w_hy_o": np.asarray(inp["w_hy_o"], f32)[0], "w_attn_o": np.asarray(inp["w_attn_o"], f32)[0],
        "sink": np.asarray(inp["attn_sink"], f32)[0][None, :], "w_out": np.asarray(inp["w_out"], f32)[0],
        "ln1g": np.asarray(inp["ln1_g"], f32)[0][None, :], "ln1b": np.asarray(inp["ln1_b"], f32)[0][None, :],
        "ln2g": np.asarray(inp["ln2_g"], f32)[0][None, :], "ln2b": np.asarray(inp["ln2_b"], f32)[0][None, :],
        "wr": wrr, "br": brr, "ew1": ew1, "ew3": ew3, "ew2": ew2,
    }
    bnd = np.zeros((33, 4), f32)
    bv = np.linspace(1e-4, 15.0, 16, dtype=f32)
    bnd[0, 0] = 0.0; bnd[0, 1] = 0.5
    bnd[1:17, 0] = bv / L; bnd[1:17, 1] = 0.25 + 0.5
    bnd[17:33, 0] = bv / L; bnd[17:33, 1] = 0.5 + 0.5
    shared["bands"] = bnd
    shared = {k: np.ascontiguousarray(v) for k, v in shared.items()}
    consts = [host_constants(0), host_constants(1)]
    maps = []
    for i in range(NCORES):
        b, half = i // 2, i % 2
        own = x[b, half * TOWN:(half + 1) * TOWN]
        oth = x[b, (1 - half) * TOWN:(2 - half) * TOWN]
        m = dict(shared)
        m["xcat"] = np.ascontiguousarray(np.concatenate([own, oth], axis=0))
        m["cb"] = np.ascontiguousarray(c[b].reshape(8, 128).T)
        m["tabs_b"], m["tabs_f"] = consts[half]
        maps.append(m)
    return maps


_NC_CACHE = {}


def kernel(**inputs):
    if "nc" not in _NC_CACHE:
        _NC_CACHE["nc"] = build_program()
    nc = _NC_CACHE["nc"]
    maps = make_in_maps(inputs)
    res = run_bass_kernel_spmd(nc, maps, core_ids=list(range(NCORES)))
    outp = np.zeros((4, SEQ, D), np.float32)
    for i in range(NCORES):
        b, half = i // 2, i % 2
        outp[b, half * TOWN:(half + 1) * TOWN] = res.results[i]["out"]
    return outp
```

```python
from contextlib import ExitStack
import math
import numpy as np
import ml_dtypes
import concourse.bass as bass
import concourse.mybir as mybir
from concourse.bass_utils import run_bass_kernel_spmd

F32 = mybir.dt.float32
BF16 = mybir.dt.bfloat16
I32 = mybir.dt.int32
U32 = mybir.dt.uint32
AF = mybir.ActivationFunctionType
ALU = mybir.AluOpType
AX = mybir.AxisListType

NCORES = 8
D = 1024
SEQ = 8192
TOWN = 4096
L = 8192
NFFT = 16384
DN_ALPHA = 2.0 ** 0.25
LN_EPS = 1e-5
DBG_NSG = 2
NBLK = 128
PROWS = NBLK * 128


class Buf:
    __slots__ = ("w", "r", "name")

    def __init__(self, name=""):
        self.w = None
        self.r = {}
        self.name = name


class _Eng:
    def __init__(self, name, hname, sem, self_sync):
        self.name = name
        self.hname = hname
        self.sem = sem
        self.cnt = 0
        self.known = {}
        self.ops = []
        self.self_sync = self_sync


class Prog:
    ENG = {"pe": "tensor", "act": "scalar", "dve": "vector", "pool": "gpsimd", "sp": "sync"}

    def __init__(self, nc, stack, n_dma_sems=8):
        self.nc = nc
        self.sems = {}
        self.engs = {}
        for k, h in self.ENG.items():
            self.sems["e_" + k] = stack.enter_context(nc.semaphore("s_" + k))
            self.engs[k] = _Eng(k, h, "e_" + k, self_sync=(k in ("act", "dve", "pool")))
        self.dq = {}
        for q in ("sp", "act", "pool"):
            lst = []
            for i in range(n_dma_sems):
                key = "d_%s%d" % (q, i)
                self.sems[key] = stack.enter_context(nc.semaphore(key))
                lst.append([key, 0])
            self.dq[q] = [lst, 0]

    def _collect(self, E, reads, writes):
        waits = {}

        def need(tok):
            if tok is None:
                return
            k, v = tok
            if waits.get(k, 0) < v:
                waits[k] = v

        for b in reads:
            need(b.w)
        for b in writes:
            need(b.w)
            for k, v in b.r.items():
                need((k, v))
        wl = []
        for k, v in waits.items():
            if k == E.sem and not E.self_sync:
                continue
            if E.known.get(k, 0) >= v:
                continue
            E.known[k] = v
            wl.append((k, v))
        return wl

    def _commit(self, tok, reads, writes):
        k, v = tok
        for b in reads:
            if b.r.get(k, 0) < v:
                b.r[k] = v
        for b in writes:
            b.w = tok
            b.r = {}

    def op(self, eng, fn, reads=(), writes=()):
        E = self.engs[eng]
        wl = self._collect(E, reads, writes)
        E.cnt += 1
        tok = (E.sem, E.cnt)
        E.ops.append((wl, fn, (E.sem, 1)))
        self._commit(tok, reads, writes)
        return tok

    def group(self, eng, fns, reads=(), writes=()):
        E = self.engs[eng]
        wl = self._collect(E, reads, writes)
        E.cnt += 1
        tok = (E.sem, E.cnt)
        n = len(fns)
        for i, fn in enumerate(fns):
            E.ops.append((wl if i == 0 else [], fn, (E.sem, 1) if i == n - 1 else None))
        self._commit(tok, reads, writes)
        return tok

    def dma(self, q, fn, reads=(), writes=()):
        E = self.engs[q]
        lst, idx = self.dq[q]
        slot = lst[idx % len(lst)]
        self.dq[q][1] = idx + 1
        key, val = slot
        wl = self._collect(E, reads, writes)
        if val > 0 and E.known.get(key, 0) < val:
            E.known[key] = val
            wl.append((key, val))
        val += 16
        slot[1] = val
        tok = (key, val)
        E.ops.append((wl, fn, (key, 16)))
        self._commit(tok, reads, writes)
        return tok

    def _all_tokens(self):
        toks = []
        for E in self.engs.values():
            if E.cnt:
                toks.append((E.sem, E.cnt))
        for q, (lst, _) in self.dq.items():
            for key, val in lst:
                if val:
                    toks.append((key, val))
        return toks

    def barrier(self):
        toks = self._all_tokens()
        for E in self.engs.values():
            wl = []
            for k, v in toks:
                if E.known.get(k, 0) >= v:
                    continue
                E.known[k] = v
                wl.append((k, v))
            if wl:
                E.ops.append((wl, None, None))

    def emit(self):
        nc = self.nc
        sems = self.sems
        with nc.Block() as block:
            for k, E in self.engs.items():
                def body(h, E=E):
                    for wl, fn, inc in E.ops:
                        for sk, v in wl:
                            h.wait_ge(sems[sk], v)
                        if fn is None:
                            continue
                        ins = fn(h)
                        if inc is not None:
                            ins.then_inc(sems[inc[0]], inc[1])
                getattr(block, E.hname)(body)


def _bf(a):
    return np.ascontiguousarray(a.astype(ml_dtypes.bfloat16))


def host_constants(half):
    a = np.arange(128)
    ang = 2.0 * np.pi * np.outer(a, a) / 128.0
    Fr = np.cos(ang)
    Fi = -np.sin(ang)
    rowpos = np.concatenate([np.arange(32), (32 if half == 0 else 96) + np.arange(32)])
    tb = np.zeros((128, NTB), np.float64)
    tb[:, 0:128] = Fr
    tb[:, 128:256] = Fi
    tb[0:64, 256:384] = Fr[rowpos, :]
    tb[0:64, 384:512] = Fi[rowpos, :]
    tb[:, 512:640] = Fr
    tb[:, 640:768] = Fi
    tb[:, 768:896] = -Fi
    tb[:, 896:1024] = Fr
    tb[:, 1024:1152] = -Fi
    tb[:, 1152:1280] = Fi
    tb[:, 1280:1408] = Fr
    tb[:, 1408:1472] = Fr[:, rowpos] / NFFT
    tb[:, 1472:1536] = Fi[:, rowpos] / NFFT
    tb[:, 1536:1664] = np.eye(128)
    tb[:, 1664:1792] = (a[:, None] < a[None, :]).astype(np.float64)
    tb[:, 1792:1920] = 1.0
    j = a[:, None]
    q = a[None, :]
    for g in range(2):
        for ty, off in enumerate((-128, 0, 128)):
            rel = j + off - q
            val = (np.abs(rel) <= 128)
            blk = np.zeros((128, 4, 128))
            for hh in range(4):
                h = g * 4 + hh
                slope = 2.0 ** (-8.0 * (h + 1) / 8.0)
                blk[:, hh, :] = np.exp(-slope * np.abs(rel)) * val
            c0 = 1920 + (g * 3 + ty) * 512
            tb[:, c0:c0 + 512] = blk.reshape(128, 512)
    tw = 2.0 * np.pi * np.outer(a, a) / NFFT
    tb[:, CT_TC1:CT_TC1 + 128] = np.cos(tw); tb[:, CT_TC1 + 128:CT_TC1 + 256] = np.cos(tw)
    tb[:, CT_TC2:CT_TC2 + 128] = -np.sin(tw); tb[:, CT_TC2 + 128:CT_TC2 + 256] = -np.sin(tw)
    tb[:, CT_F2NR:CT_F2NR + 128] = -Fr
    tb[:, CT_G2NA:CT_G2NA + 128] = -Fr
    tb[:, CT_G2NA + 128:CT_G2NA + 256] = Fi
    tb[:, CT_G1NI:CT_G1NI + 64] = -Fi[:, rowpos] / NFFT
    tf = np.zeros((128, NTF), np.float32)
    tf[:, 0:128] = np.cos(tw)
    tf[:, 128:256] = -np.sin(tw)
    tf[:, 256:384] = np.eye(128)
    tf[:, 384] = a
    tf[:, 385] = 1.0 - half
    tf[:, 386] = float(half)
    tf[:, 387] = float(half)
    tf[:, 388] = 1.0 - half
    tf[:, 400:528] = 128.0 * a[None, :]
    cwn = 1.0 / (L - 1)
    tf[:64, 389] = -cwn
    tf[64:, 389] = cwn
    tf[:64, 390] = 128.0 * a[:64]
    tf[64:, 390] = -(8192.0 - 128.0 * (a[64:] - 64))
    tf[:, 528:656] = a[None, :]
    tf[:, 656:784] = 1.0
    return _bf(tb), tf


CT_F1FULL, CT_F1C, CT_F2R, CT_F2I, CT_F2NI, CT_G2A, CT_G2B, CT_G1R, CT_G1I = 0, 256, 512, 640, 768, 896, 1152, 1408, 1472
CT_ID, CT_TRI, CT_ONES, CT_EM = 1536, 1664, 1792, 1920
CT_TC1 = 1920 + 6 * 512
CT_TC2 = CT_TC1 + 256
CT_F2NR = CT_TC2 + 256
CT_G2NA = CT_F2NR + 128
CT_G1NI = CT_G2NA + 256
NTB = CT_G1NI + 64
NTF = 784


def q_perm():
    cols = []
    for c in range(4):
        cols += list(range(c * 64, c * 64 + 64))
        cols += list(range((4 + c) * 64, (4 + c) * 64 + 64))
    return np.array(cols + list(range(512, 4352)))


def build_program(debug=None, lite=False):
    nc = bass.Bass("TRN2", target_bir_lowering=False)
    dbg = debug is not None

    def din(name, shape, dt=F32):
        return nc.dram_tensor(name, list(shape), dt, kind="ExternalInput").ap()

    DBG_OUT = {"p0": ["mod_scr"], "p1a": ["mod_scr", "Uc", "Gs", "dbgq", "dbgk", "dbgv"], "p1b": ["AT"],
               "p2": ["AT", "Z2", "dbgkc", "dbgz1", "dbgkh"], "p3": ["X1", "H2", "dbgrt"], "p4": ["XB", "YB", "dbgrt"]}

    def dscr(name, shape, dt=F32):
        isout = dbg and name in DBG_OUT.get(debug, [])
        return nc.dram_tensor(name, list(shape), dt, kind=("ExternalOutput" if isout else "Internal")).ap()

    xcat = din("xcat", [SEQ, D])
    cb = din("cb", [128, 8])
    w_ada = din("w_ada", [D, 6 * D])
    b_ada = din("b_ada", [1, 6 * D])
    w_in = din("w_in", [D, 4352])
    conv_w = din("conv_w", [128, 3, 12])
    conv_b = din("conv_b", [128, 12])
    fw1 = din("fw1", [33, 64]); fb1 = din("fb1", [64, 1]); ff1 = din("ff1", [64, 1])
    fw2 = din("fw2", [64, 64]); fb2 = din("fb2", [64, 1]); ff2 = din("ff2", [64, 1])
    fw3 = din("fw3", [64, 2048]); fdec = din("fdec", [1, 2048])
    hskip = din("hskip", [1, 1024])
    w_hy_o = din("w_hy_o", [512, D]); w_attn_o = din("w_attn_o", [512, D])
    sink = din("sink", [1, 8])
    w_out = din("w_out", [D, D])
    ln1g = din("ln1g", [1, D]); ln1b = din("ln1b", [1, D]); ln2g = din("ln2g", [1, D]); ln2b = din("ln2b", [1, D])
    wr = din("wr", [D, 72]); br = din("br", [1, 72])
    ne = 256 if lite else 64 * 2 * 128
    ew1 = din("ew1", [ne, 2048]); ew3 = din("ew3", [ne, 2048]); ew2 = din("ew2", [ne, 2048])
    tabs_b = din("tabs_b", [128, NTB], BF16)
    tabs_f = din("tabs_f", [128, NTF])
    bands = din("bands", [33, 4])
    out = nc.dram_tensor("out", [TOWN, D], F32, kind="ExternalOutput").ap()

    mod_scr = dscr("mod_scr", [1, 6 * D])
    Uc = dscr("Uc", [1536, SEQ])
    Gs = dscr("Gs", [2048, TOWN], BF16)
    AT = dscr("AT", [512, TOWN], BF16)
    Z2 = dscr("Z2", [512, TOWN], BF16)
    X1 = dscr("X1", [TOWN, D])
    H2 = dscr("H2", [TOWN, D], BF16)
    XB = dscr("XB", [PROWS, D], BF16)
    YB = dscr("YB", [PROWS, D], BF16)
    EWB = [dscr("EWB%d" % i, [ne // 2, 4096], BF16) for i in range(3)]
    dbg1a = (debug == "p1a")
    dbgq = dscr("dbgq", [128, 4, TOWN], BF16) if dbg1a else None
    dbgk = dscr("dbgk", [128, 34 * 128], BF16) if dbg1a else None
    dbgv = dscr("dbgv", [128, 34, 2, 65], BF16) if dbg1a else None
    dbgkc = dscr("dbgkc", [128, 32, 128], BF16) if debug == "p2" else None
    dbgz1 = dscr("dbgz1", [64, 16, 128], BF16) if debug == "p2" else None
    dbgkh = dscr("dbgkh", [128, 2, 32, 128], BF16) if debug == "p2" else None

    with ExitStack() as gst:
        P = Prog(nc, gst)

        def SB(st, name, shape, dt):
            return st.enter_context(nc.sbuf_tensor(name, list(shape), dt))

        def PS(st, name, shape, dt=F32):
            return st.enter_context(nc.psum_tensor(name, list(shape), dt))

        tb = SB(gst, "tb", [128, NTB], BF16); b_tb = Buf("tb")
        tf = SB(gst, "tf", [128, NTF], F32); b_tf = Buf("tf")
        P.dma("sp", lambda e: e.dma_start(out=tb[:], in_=tabs_b[:, :]), writes=[b_tb])
        P.dma("sp", lambda e: e.dma_start(out=tf[:], in_=tabs_f[:, :]), writes=[b_tf])
        identf = tf[:, 256:384]
        identb = tb[:, CT_ID:CT_ID + 128]
        mA, mB, nmA, nmB = tf[:, 385:386], tf[:, 386:387], tf[:, 387:388], tf[:, 388:389]
        m1 = SB(gst, "m1", [128, 16], F32); b_m1 = Buf()

        with ExitStack() as st:
            cbt = SB(st, "cbt", [128, 8], F32); b_cbt = Buf()
            sig = SB(st, "sig", [128, 8], F32)
            modrow = SB(st, "modrow", [1, 6 * D], F32); b_mod = Buf()
            badar = SB(st, "badar", [1, 6 * D], F32); b_bada = Buf()
            wa = [SB(st, "wa%d" % i, [128, 8, 512], BF16) for i in range(2)]
            cbb = SB(st, "cbb", [128, 8], BF16)
            b_wa = [Buf(), Buf()]
            pm = [PS(st, "pm%d" % i, [1, 512]) for i in range(2)]
            b_pm = [Buf(), Buf()]
            P.dma("act", lambda e: e.dma_start(out=cbt[:], in_=cb[:, :]), writes=[b_cbt])
            P.dma("act", lambda e: e.dma_start(out=badar[:], in_=b_ada[:, :]), writes=[b_bada])
            P.op("act", lambda e: e.activation(out=sig[:], in_=cbt[:], func=AF.Sigmoid), reads=[b_cbt], writes=[b_cbt])
            P.op("dve", lambda e: e.tensor_tensor(out=cbt[:], in0=cbt[:], in1=sig[:], op=ALU.mult), reads=[b_cbt], writes=[b_cbt])
            P.op("dve", lambda e: e.tensor_copy(out=cbb[:], in_=cbt[:]), reads=[b_cbt], writes=[b_cbt])
            for blk in range(12):
                s = blk % 2
                P.dma("pool", lambda e, s=s, blk=blk: e.dma_start(
                    out=wa[s][:], in_=w_ada[:, blk * 512:(blk + 1) * 512].rearrange("(c p) n -> p c n", p=128)),
                    writes=[b_wa[s]])
                P.group("pe", [lambda e, s=s, kc=kc: e.matmul(pm[s][:], lhsT=cbb[:, kc:kc + 1], rhs=wa[s][:, kc, :],
                                                               start=(kc == 0), stop=(kc == 7)) for kc in range(8)],
                        reads=[b_cbt, b_wa[s]], writes=[b_pm[s]])
                P.op("dve", lambda e, s=s, blk=blk: e.tensor_tensor(out=modrow[0:1, blk * 512:(blk + 1) * 512], in0=pm[s][:],
                                                                   in1=badar[0:1, blk * 512:(blk + 1) * 512], op=ALU.add),
                     reads=[b_pm[s], b_bada], writes=[b_mod])
            for off in (1024, 4096):
                P.op("dve", lambda e, off=off: e.tensor_scalar(out=modrow[0:1, off:off + 1024], in0=modrow[0:1, off:off + 1024],
                                                               scalar1=1.0, scalar2=None, op0=ALU.add), reads=[b_mod], writes=[b_mod])
            b_modscr = Buf()
            P.dma("sp", lambda e: e.dma_start(out=mod_scr[:, :], in_=modrow[:]), reads=[b_mod], writes=[b_modscr])
            pTm = PS(st, "pTm", [128, 16]); b_pTm = Buf()
            P.group("pe", [lambda e, c=c: e.transpose(out=pTm[:, c:c + 1], in_=modrow[0:1, c * 128:(c + 1) * 128],
                                                      identity=identf[0:1, 0:1]) for c in range(16)],
                    reads=[b_mod, b_tf], writes=[b_pTm])
            P.op("dve", lambda e: e.tensor_copy(out=m1[:], in_=pTm[:]), reads=[b_pTm], writes=[b_m1])
            P.barrier()

        if debug == "p0":
            return _finish(nc, P, out, gst)

        with ExitStack() as st1:
            qT = SB(st1, "qT", [128, 4, TOWN], BF16); b_qT = Buf()
            kT = SB(st1, "kT", [128, 34 * 128], BF16); b_kT = Buf()
            Va = SB(st1, "Va", [128, 34, 2, 65], BF16); b_Va = Buf()
            P.op("pool", lambda e: e.memset(Va[:], 1.0), writes=[b_Va])
            with ExitStack() as st:
                win = SB(st, "win", [128, 8, 4352], BF16); b_win = Buf()
                for kc in range(8):
                    for (c0, c1) in ((0, 2048), (2048, 4096), (4096, 4352)):
                        P.dma("pool", lambda e, kc=kc, c0=c0, c1=c1: e.dma_start(
                            out=win[:, kc, c0:c1], in_=w_in[kc * 128:(kc + 1) * 128, c0:c1]), writes=[b_win])
                cw = SB(st, "cw", [128, 3, 12], F32); cbias = SB(st, "cbias", [128, 12], F32); b_cw = Buf()
                P.dma("act", lambda e: e.dma_start(out=cw[:], in_=conv_w[:, :, :]), writes=[b_cw])
                P.dma("act", lambda e: e.dma_start(out=cbias[:], in_=conv_b[:, :]), writes=[b_cw])
                NXS = 3
                xs = [SB(st, "xs%d" % i, [128, D], F32) for i in range(NXS)]; b_xs = [Buf() for _ in range(NXS)]
                hT = [SB(st, "hT%d" % i, [128, 8, 512], BF16) for i in range(2)]; b_hT = [Buf(), Buf()]
                carry = SB(st, "carry", [128, 12, 2], F32); b_carry = Buf()
                NUB = 3
                ub = [SB(st, "ub%d" % i, [128, 514], F32) for i in range(NUB)]; b_ub = [Buf() for _ in range(NUB)]
                ua = [SB(st, "ua%d" % i, [128, 512], F32) for i in range(NUB)]; b_ua = [Buf() for _ in range(NUB)]
                gsb = [SB(st, "gsb%d" % i, [128, 512], BF16) for i in range(2)]; b_gsb = [Buf(), Buf()]
                fix = SB(st, "fix", [128, 2], F32); b_fix = Buf()
                pT = [PS(st, "pT%d" % i, [128, 8, 128]) for i in range(2)]; b_pT = [Buf(), Buf()]
                pO = [PS(st, "pO%d" % i, [128, 512]) for i in range(3)]; b_pO = [Buf() for _ in range(3)]
                pV = PS(st, "pV", [128, 128]); b_pV = Buf()
                cnt = {"x": 0, "pT": 0, "pO": 0, "ub": 0, "g": 0}
                b_Uc = Buf(); b_Gs = Buf()

                def load_transpose(tile_idx, hslot, col0, ncols=128):
                    xi = cnt["x"] % NXS; cnt["x"] += 1
                    P.dma("sp", lambda e: e.dma_start(out=xs[xi][:], in_=xcat[tile_idx * 128:(tile_idx + 1) * 128, :]),
                          writes=[b_xs[xi]])
                    pi = cnt["pT"] % 2; cnt["pT"] += 1
                    P.group("pe", [lambda e, kc=kc: e.transpose(out=pT[pi][:, kc, :], in_=xs[xi][:, kc * 128:(kc + 1) * 128],
                                                               identity=identf) for kc in range(8)],
                            reads=[b_xs[xi], b_tf], writes=[b_pT[pi]])
                    for kc in range(8):
                        P.op("act", lambda e, kc=kc: e.activation(out=hT[hslot][:, kc, col0:col0 + 128], in_=pT[pi][:, kc, :],
                                                                 func=AF.Identity, scale=m1[:, 8 + kc:9 + kc], bias=m1[:, kc:kc + 1]),
                             reads=[b_pT[pi], b_m1], writes=[b_hT[hslot]])

                def proj_chunk(hslot, oc, ncols=512):
                    pi = cnt["pO"] % 3; cnt["pO"] += 1
                    P.group("pe", [lambda e, kc=kc: e.matmul(pO[pi][:, 0:ncols], lhsT=win[:, kc, oc * 128:(oc + 1) * 128],
                                                             rhs=hT[hslot][:, kc, 0:ncols], start=(kc == 0), stop=(kc == 7))
                                   for kc in range(8)],
                            reads=[b_win, b_hT[hslot]], writes=[b_pO[pi]])
                    return pi

                def proj_v(hslot, t, vtile):
                    P.group("pe", [lambda e, kc=kc: e.matmul(pV[:], lhsT=hT[hslot][:, kc, t * 128:(t + 1) * 128],
                                                             rhs=win[:, kc, 640:768], start=(kc == 0), stop=(kc == 7))
                                   for kc in range(8)],
                            reads=[b_win, b_hT[hslot]], writes=[b_pV])
                    P.op("dve", lambda e: e.tensor_copy(out=Va[:, vtile, :, 0:64], in_=pV[:].rearrange("p (g d) -> p g d", g=2)),
                         reads=[b_pV], writes=[b_Va])

                load_transpose(63, 0, 0)
                for j in range(12):
                    pi = proj_chunk(0, 6 + j, ncols=128)
                    P.op("act", lambda e, j=j, pi=pi: e.copy(out=carry[:, j, :], in_=pO[pi][:, 126:128]),
                         reads=[b_pO[pi]], writes=[b_carry])

                def do_supertile(st_i):
                    hs = st_i % 2
                    own = st_i < 8
                    if st_i == 0:
                        for t in range(4):
                            load_transpose(st_i * 4 + t, hs, t * 128)
                    chunks = list(range(0, 5)) + list(range(6, 34)) if own else list(range(6, 18))
                    if st_i in (8, 15):
                        chunks = [4] + chunks
                    pre_at = {}
                    if st_i + 1 < 16:
                        step = max(1, (len(chunks) - 2) // 4)
                        for t in range(4):
                            pre_at[1 + t * step] = t
                    for ci_, oc in enumerate(chunks):
                        if ci_ in pre_at:
                            load_transpose((st_i + 1) * 4 + pre_at[ci_], 1 - hs, pre_at[ci_] * 128)
                        pi = proj_chunk(hs, oc)
                        if oc < 4:
                            P.op("dve", lambda e, oc=oc, pi=pi: e.tensor_copy(out=qT[:, oc, st_i * 512:(st_i + 1) * 512], in_=pO[pi][:]),
                                 reads=[b_pO[pi]], writes=[b_qT])
                        elif oc == 4:
                            if own:
                                P.op("dve", lambda e, pi=pi: e.tensor_copy(out=kT[:, 128 + st_i * 512:128 + (st_i + 1) * 512], in_=pO[pi][:]),
                                     reads=[b_pO[pi]], writes=[b_kT])
                            elif st_i == 8:
                                P.op("dve", lambda e, pi=pi: e.tensor_copy(out=kT[:, 33 * 128:34 * 128], in_=pO[pi][:, 0:128]),
                                     reads=[b_pO[pi]], writes=[b_kT])
                            else:
                                P.op("dve", lambda e, pi=pi: e.tensor_copy(out=kT[:, 0:128], in_=pO[pi][:, 384:512]),
                                     reads=[b_pO[pi]], writes=[b_kT])
                        elif oc < 18:
                            j = oc - 6
                            ui = cnt["ub"] % NUB; cnt["ub"] += 1
                            P.op("act", lambda e, pi=pi, ui=ui: e.copy(out=ub[ui][:, 2:514], in_=pO[pi][:]),
                                 reads=[b_pO[pi]], writes=[b_ub[ui]])
                            P.op("act", lambda e, j=j, ui=ui: e.copy(out=ub[ui][:, 0:2], in_=carry[:, j, :]),
                                 reads=[b_carry], writes=[b_ub[ui]])
                            P.op("act", lambda e, j=j, ui=ui: e.copy(out=carry[:, j, :], in_=ub[ui][:, 512:514]),
                                 reads=[b_ub[ui]], writes=[b_carry])
                            P.op("act", lambda e, j=j, ui=ui: e.activation(out=ua[ui][:], in_=ub[ui][:, 1:513], func=AF.Identity,
                                                                          scale=cw[:, 1, j:j + 1], bias=cbias[:, j:j + 1]),
                                 reads=[b_ub[ui], b_cw], writes=[b_ua[ui]])
                            P.op("dve", lambda e, j=j, ui=ui: e.scalar_tensor_tensor(out=ua[ui][:], in0=ub[ui][:, 0:512], scalar=cw[:, 0, j:j + 1],
                                                                                    in1=ua[ui][:], op0=ALU.mult, op1=ALU.add),
                                 reads=[b_ub[ui], b_cw], writes=[b_ua[ui]])
                            P.op("dve", lambda e, j=j, ui=ui: e.scalar_tensor_tensor(out=ua[ui][:], in0=ub[ui][:, 2:514], scalar=cw[:, 2, j:j + 1],
                                                                                    in1=ua[ui][:], op0=ALU.mult, op1=ALU.add),
                                 reads=[b_ub[ui], b_cw], writes=[b_ua[ui]])
                            if st_i in (0, 8):
                                nm = nmB if st_i == 0 else nmA
                                P.op("dve", lambda e, j=j, ui=ui, nm=nm: e.tensor_scalar(out=fix[:, 0:1], in0=ub[ui][:, 2:3], scalar1=cw[:, 2, j:j + 1],
                                                                                        scalar2=nm, op0=ALU.mult, op1=ALU.mult),
                                     reads=[b_ub[ui], b_cw, b_tf], writes=[b_fix])
                                P.op("dve", lambda e, j=j, ui=ui, nm=nm: e.tensor_scalar(out=fix[:, 1:2], in0=ub[ui][:, 1:2], scalar1=cw[:, 0, j:j + 1],
                                                                                        scalar2=nm, op0=ALU.mult, op1=ALU.mult),
                                     reads=[b_ub[ui], b_cw, b_tf], writes=[b_fix])
                                P.op("dve", lambda e, ui=ui: e.tensor_tensor(out=ua[ui][:, 0:2], in0=ua[ui][:, 0:2], in1=fix[:, 0:2], op=ALU.subtract),
                                     reads=[b_fix], writes=[b_ua[ui]])
                            s0 = st_i * 512
                            r0 = j * 128
                            P.dma("pool", lambda e, ui=ui, r0=r0, s0=s0: e.dma_start(out=Uc[r0:r0 + 128, (s0 - 1) % SEQ:(s0 - 1) % SEQ + 1], in_=ua[ui][:, 0:1], allow_slow_non_contiguous=True),
                                  reads=[b_ua[ui]])
                            P.dma("pool", lambda e, ui=ui, r0=r0, s0=s0: e.dma_start(out=Uc[r0:r0 + 128, s0:s0 + 511], in_=ua[ui][:, 1:512]),
                                  reads=[b_ua[ui]])
                        else:
                            gi = cnt["g"] % 2; cnt["g"] += 1
                            P.op("act", lambda e, pi=pi, gi=gi: e.activation(out=gsb[gi][:], in_=pO[pi][:], func=AF.Sigmoid),
                                 reads=[b_pO[pi]], writes=[b_gsb[gi]])
                            r0 = (oc - 18) * 128
                            P.dma("pool", lambda e, gi=gi, r0=r0: e.dma_start(out=Gs[r0:r0 + 128, st_i * 512:(st_i + 1) * 512], in_=gsb[gi][:]),
                                  reads=[b_gsb[gi]])
                    if own:
                        for t in range(4):
                            proj_v(hs, t, 1 + st_i * 4 + t)
                    elif st_i == 8:
                        proj_v(hs, 0, 33)
                    elif st_i == 15:
                        proj_v(hs, 3, 0)
                for st_i in range(16):
                    do_supertile(st_i)
                if dbg1a:
                    P.dma("sp", lambda e: e.dma_start(out=dbgq[:, :, :], in_=qT[:]), reads=[b_qT])
                    P.dma("sp", lambda e: e.dma_start(out=dbgk[:, :], in_=kT[:]), reads=[b_kT])
                    P.dma("sp", lambda e: e.dma_start(out=dbgv[:, :, :, :], in_=Va[:]), reads=[b_Va])
                P.barrier()
            if debug == "p1a":
                return _finish(nc, P, out, gst)

            with ExitStack() as st:
                esink = SB(st, "esink", [128, 8], F32); b_es = Buf()
                P.dma("sp", lambda e: e.dma_start(out=esink[:], in_=sink[0:1, :].partition_broadcast(128)), writes=[b_es])
                P.op("act", lambda e: e.activation(out=esink[:], in_=esink[:], func=AF.Exp), reads=[b_es], writes=[b_es])
                emJ = SB(st, "emJ", [128, 2, 2, 512], BF16); b_emJ = Buf()
                for g in range(2):
                    P.op("dve", lambda e, g=g: e.tensor_scalar(out=emJ[:, g, 0, :], in0=tb[:, CT_EM + (g * 3 + 0) * 512:CT_EM + (g * 3 + 1) * 512],
                                                               scalar1=mB, scalar2=None, op0=ALU.mult), reads=[b_tb, b_tf], writes=[b_emJ])
                    P.op("dve", lambda e, g=g: e.tensor_scalar(out=emJ[:, g, 1, :], in0=tb[:, CT_EM + (g * 3 + 2) * 512:CT_EM + (g * 3 + 3) * 512],
                                                               scalar1=mA, scalar2=None, op0=ALU.mult), reads=[b_tb, b_tf], writes=[b_emJ])
                pS = [PS(st, "pS%d" % i, [128, 512]) for i in range(3)]; b_pS = [Buf() for _ in range(3)]
                pPVb = [[PS(st, "pPV%d_%d" % (s, g), [128, 512]) for g in range(2)] for s in range(2)]
                pPV = [[pPVb[s][g][:, 0:260].rearrange("p (h d) -> p h d", h=4) for g in range(2)] for s in range(2)]
                b_pPV = [[Buf(), Buf()], [Buf(), Buf()]]
                pTab = PS(st, "pTa", [128, 1024], BF16); b_pTa = Buf()
                pTa = pTab[:, 0:512].rearrange("p (c q) -> p c q", c=4)
                pex = [SB(st, "pex%d" % i, [128, 512], BF16) for i in range(3)]; b_pex = [Buf() for _ in range(3)]
                pmk = [[[SB(st, "pmk%d_%d_%d" % (s, g, kb), [128, 512], BF16) for kb in range(3)] for g in range(2)] for s in range(2)]
                b_pmk = [[[Buf() for kb in range(3)] for g in range(2)] for s in range(2)]
                den = SB(st, "den", [128, 2, 4], F32); b_den = Buf()
                att = [SB(st, "att%d" % i, [128, 8, 64], BF16) for i in range(2)]; b_att = [Buf(), Buf()]
                atT = [SB(st, "atT%d" % i, [128, 4, 128], BF16) for i in range(2)]; b_atT = [Buf(), Buf()]
                cn = {"s": 0}

                def attn_part1(i):
                    s2 = i % 2
                    for g in range(2):
                        for kb in range(3):
                            si = cn["s"] % 3; cn["s"] += 1
                            P.group("pe", [lambda e, g=g, kb=kb, si=si: e.matmul(pS[si][:].rearrange("p (h q) -> p h q", h=4),
                                                              lhsT=kT[64 * g:64 * g + 64, (i + kb) * 128:(i + kb + 1) * 128],
                                                              rhs=qT[64 * g:64 * g + 64, :, i * 128:(i + 1) * 128], start=True, stop=True)],
                                    reads=[b_kT, b_qT], writes=[b_pS[si]])
                            P.op("act", lambda e, si=si: e.activation(out=pex[si][:], in_=pS[si][:], func=AF.Exp, scale=0.125),
                                 reads=[b_pS[si]], writes=[b_pex[si]])
                            if kb == 0 and i == 0:
                                em = emJ[:, g, 0, :]
                            elif kb == 2 and i == 31:
                                em = emJ[:, g, 1, :]
                            else:
                                em = tb[:, CT_EM + (g * 3 + kb) * 512:CT_EM + (g * 3 + kb + 1) * 512]
                            eng = "dve"
                            P.op(eng, lambda e, em=em, g=g, kb=kb, si=si: e.tensor_tensor(out=pmk[s2][g][kb][:], in0=pex[si][:], in1=em, op=ALU.mult),
                                 reads=[b_pex[si], b_tb, b_emJ], writes=[b_pmk[s2][g][kb]])
                def attn_part2(i):
                    s2 = i % 2
                    for g in range(2):
                        fns = []
                        for hh in range(4):
                            for kb in range(3):
                                fns.append(lambda e, hh=hh, kb=kb, g=g: e.matmul(pPV[s2][g][:, hh, :], lhsT=pmk[s2][g][kb][:, hh * 128:(hh + 1) * 128],
                                                                          rhs=Va[:, i + kb, g, :], start=(kb == 0), stop=(kb == 2)))
                        P.group("pe", fns, reads=[b_pmk[s2][g][0], b_pmk[s2][g][1], b_pmk[s2][g][2], b_Va], writes=[b_pPV[s2][g]])
                        P.op("dve", lambda e, g=g: e.tensor_tensor(out=den[:, g, :], in0=pPV[s2][g][:, :, 64], in1=esink[:, 4 * g:4 * g + 4], op=ALU.add),
                             reads=[b_pPV[s2][g], b_es], writes=[b_den])
                        P.op("dve", lambda e, g=g: e.reciprocal(out=den[:, g, :], in_=den[:, g, :]), reads=[b_den], writes=[b_den])
                        P.op("dve", lambda e, g=g: e.tensor_tensor(out=att[s2][:, 4 * g:4 * g + 4, :], in0=pPV[s2][g][:, :, 0:64],
                                                                   in1=den[:, g, :].unsqueeze(2).to_broadcast([128, 4, 64]), op=ALU.mult),
                             reads=[b_pPV[s2][g], b_den], writes=[b_att[s2]])
                    P.group("pe", [lambda e, c=c: e.transpose(out=pTa[:, c, :], in_=att[s2][:].rearrange("p h d -> p (h d)")[:, c * 128:(c + 1) * 128],
                                                               identity=identb) for c in range(4)],
                            reads=[b_att[s2], b_tb], writes=[b_pTa])
                    P.op("act", lambda e: e.copy(out=atT[s2][:], in_=pTa), reads=[b_pTa], writes=[b_atT[s2]])
                    P.dma("sp", lambda e: e.dma_start(out=AT[:, i * 128:(i + 1) * 128].rearrange("(c p) q -> p c q", p=128), in_=atT[s2][:]),
                          reads=[b_atT[s2]])

                attn_part1(0)
                for i in range(32):
                    if i + 1 < 32:
                        attn_part1(i + 1)
                    attn_part2(i)
                P.barrier()
        if debug == "p1b":
            return _finish(nc, P, out, gst)

        TWO_PI = 2.0 * math.pi
        with ExitStack() as st2:
            hdn2T = SB(st2, "hdn2T", [64, NFFT], BF16); b_h2 = Buf()
            w3b = SB(st2, "w3b", [64, 2048], BF16); b_w3 = Buf()
            P.dma("pool", lambda e: e.dma_start(out=w3b[:, :], in_=fw3[:, :]), writes=[b_w3])
            dec = SB(st2, "dec", [128, 2, 512], F32); b_dec = Buf()

            def load_dec(o):
                P.dma("sp", lambda e: e.dma_start(out=dec[0:64, o, :], in_=fdec[0:1, o * 1024:o * 1024 + 512].partition_broadcast(64)), writes=[b_dec])
                P.dma("sp", lambda e: e.dma_start(out=dec[64:128, o, :], in_=fdec[0:1, o * 1024 + 512:(o + 1) * 1024].partition_broadcast(64)), writes=[b_dec])
            load_dec(0); load_dec(1)
            P.op("act", lambda e: e.activation(out=dec[:], in_=dec[:], func=AF.Abs), reads=[b_dec], writes=[b_dec])
            P.op("dve", lambda e: e.tensor_scalar(out=dec[:], in0=dec[:], scalar1=tf[:, 389:390], scalar2=None, op0=ALU.mult),
                 reads=[b_dec, b_tf], writes=[b_dec])
            skA = SB(st2, "skA", [128, 1024], F32); b_sk = Buf()
            P.dma("sp", lambda e: e.dma_start(out=skA[:], in_=hskip[0:1, :].partition_broadcast(128)), writes=[b_sk])

            with ExitStack() as st:
                w1t = SB(st, "w1t", [33, 64], F32); w2t = SB(st, "w2t", [64, 64], F32); b_fw = Buf()
                fsc = SB(st, "fsc", [64, 8], F32); b_fsc = Buf()
                bnd = SB(st, "bnd", [33, 4], F32)
                P.dma("sp", lambda e: e.dma_start(out=w1t[:], in_=fw1[:, :]), writes=[b_fw])
                P.dma("sp", lambda e: e.dma_start(out=w2t[:], in_=fw2[:, :]), writes=[b_fw])
                P.dma("sp", lambda e: e.dma_start(out=bnd[:], in_=bands[:, :]), writes=[b_fw])
                for ci, srcap in enumerate((ff1, fb1, ff2, fb2)):
                    P.dma("sp", lambda e, ci=ci, srcap=srcap: e.dma_start(out=fsc[:, ci:ci + 1], in_=srcap[:, :]), writes=[b_fsc])
                for (a, b, o1, o2) in ((0, 1, 4, 5), (2, 3, 6, 7)):
                    P.op("dve", lambda e, a=a, b=b, o2=o2: e.tensor_tensor(out=fsc[:, o2:o2 + 1], in0=fsc[:, a:a + 1], in1=fsc[:, b:b + 1], op=ALU.mult),
                         reads=[b_fsc], writes=[b_fsc])
                    P.op("dve", lambda e, o2=o2: e.tensor_scalar(out=fsc[:, o2:o2 + 1], in0=fsc[:, o2:o2 + 1], scalar1=1.0 / TWO_PI, scalar2=8.5,
                                                                 op0=ALU.mult, op1=ALU.add), reads=[b_fsc], writes=[b_fsc])
                    P.op("dve", lambda e, a=a, o1=o1: e.tensor_scalar(out=fsc[:, o1:o1 + 1], in0=fsc[:, a:a + 1], scalar1=1.0 / TWO_PI, scalar2=None,
                                                                      op0=ALU.mult), reads=[b_fsc], writes=[b_fsc])
                idx = SB(st, "idx", [33, 16, 128], I32); b_idx = Buf()
                idxf = SB(st, "idxf", [33, 2048], F32); b_idxf = Buf()
                uu = SB(st, "uu", [64, 2048], F32); b_uu = Buf()
                ki = SB(st, "ki", [64, 2048], I32); b_ki = Buf()
                zT = SB(st, "zT", [33, 2048], F32); b_zT = Buf()
                h1 = SB(st, "h1", [64, 512], F32); b_h1 = Buf()
                pH = [PS(st, "pH%d" % i, [64, 512]) for i in range(2)]; b_pH = [Buf(), Buf()]

                def sin_reduce(np_, ncols, src, b_src, sc_mul, sc_add, dst, b_dst, extra_reads=()):
                    P.op("dve", lambda e: e.tensor_scalar(out=uu[0:np_, 0:ncols], in0=src, scalar1=sc_mul, scalar2=sc_add, op0=ALU.mult, op1=ALU.add),
                         reads=[b_src] + list(extra_reads), writes=[b_uu])
                    P.op("dve", lambda e: e.tensor_copy(out=ki[0:np_, 0:ncols], in_=uu[0:np_, 0:ncols]), reads=[b_uu], writes=[b_ki])
                    P.op("dve", lambda e: e.tensor_tensor(out=uu[0:np_, 0:ncols], in0=uu[0:np_, 0:ncols], in1=ki[0:np_, 0:ncols], op=ALU.subtract),
                         reads=[b_uu, b_ki], writes=[b_uu])
                    P.op("dve", lambda e: e.scalar_tensor_tensor(out=uu[0:np_, 0:ncols], in0=uu[0:np_, 0:ncols], scalar=0.0, in1=uu[0:np_, 0:ncols],
                                                                 op0=ALU.is_lt, op1=ALU.add), reads=[b_uu], writes=[b_uu])
                    P.op("act", lambda e: e.activation(out=dst, in_=uu[0:np_, 0:ncols], func=AF.Sin, bias=-math.pi, scale=TWO_PI),
                         reads=[b_uu], writes=[b_dst])

                def mlp_chunk(c):
                    P.op("pool", lambda e: e.iota(idx[:, :, 0:64], pattern=[[1, 16], [128, 64]], base=16 * c, channel_multiplier=0), writes=[b_idx])
                    P.op("pool", lambda e: e.iota(idx[:, :, 64:128], pattern=[[-1, 16], [-128, 64]], base=8192 - 16 * c, channel_multiplier=0), writes=[b_idx])
                    P.op("dve", lambda e: e.tensor_single_scalar(out=idx[:, :, 64:128], in_=idx[:, :, 64:128], scalar=8191, op=ALU.bitwise_and),
                         reads=[b_idx], writes=[b_idx])
                    P.op("dve", lambda e: e.tensor_copy(out=idxf[:], in_=idx[:].rearrange("p a b -> p (a b)")), reads=[b_idx], writes=[b_idxf])
                    sin_reduce(33, 2048, idxf[:], b_idxf, bnd[:, 0:1], bnd[:, 1:2], zT[:], b_zT, extra_reads=[b_fw])
                    P.op("dve", lambda e: e.tensor_scalar(out=zT[0:1, :], in0=idxf[0:1, :], scalar1=1.0 / (L - 1), scalar2=None, op0=ALU.mult),
                         reads=[b_idxf], writes=[b_zT])

                    def quarter(q):
                        s = q % 2
                        P.group("pe", [lambda e: e.matmul(pH[s][:], lhsT=w1t[:], rhs=zT[:, q * 512:(q + 1) * 512], start=True, stop=True)],
                                reads=[b_fw, b_zT], writes=[b_pH[s]])
                        sin_reduce(64, 512, pH[s][:], b_pH[s], fsc[:, 4:5], fsc[:, 5:6], h1[:], b_h1, extra_reads=[b_fsc])
                        P.group("pe", [lambda e: e.matmul(pH[s][:], lhsT=w2t[:], rhs=h1[:], start=True, stop=True)],
                                reads=[b_fw, b_h1], writes=[b_pH[s]])
                        sin_reduce(64, 512, pH[s][:], b_pH[s], fsc[:, 6:7], fsc[:, 7:8], hdn2T[:, c * 2048 + q * 512:c * 2048 + (q + 1) * 512], b_h2,
                                   extra_reads=[b_fsc])
                    for q in range(4):
                        quarter(q)
                for c in range(8):
                    mlp_chunk(c)
                P.barrier()

            kraw = SB(st2, "kraw", [128, 32, 128], F32); b_kraw = Buf()
            wtmp = SB(st2, "wtmp", [128, 32, 128], F32); b_wtmp = Buf()
            kcs = SB(st2, "kcs", [128, 32, 128], BF16); b_kcs = Buf()
            Khr = SB(st2, "Khr", [128, 32, 128], BF16); Khi = SB(st2, "Khi", [128, 32, 128], BF16); b_Kh = Buf()
            e1 = SB(st2, "e1", [128, 32], F32); b_e1 = Buf()
            ksum = SB(st2, "ksum", [128, 32], F32); b_ksum = Buf()
            r64 = SB(st2, "r64", [128, 32], F32); b_r64 = Buf()
            ut = [[SB(st2, "ut%d_%d" % (s, k), [64, 16, 128], BF16) for k in range(3)] for s in range(2)]
            b_ut = [[Buf() for k in range(3)] for s in range(2)]
            z1t = SB(st2, "z1t", [64, 16, 128], BF16); b_z1 = Buf()
            z2t = [SB(st2, "z2t%d" % s, [64, 16, 128], BF16) for s in range(2)]; b_z2 = [Buf(), Buf()]
            pK = [PS(st2, "pK%d" % i, [128, 512]) for i in range(2)]; b_pK = [Buf(), Buf()]
            pN = pK[0]; b_pN = b_pK[0]
            NL = 3
            lanes = []
            for li in range(NL):
                ln = {"pL": PS(st2, "pL%d" % li, [128, 1024]), "b_pL": Buf()}
                ln["S"] = SB(st2, "S%d" % li, [128, 1024], BF16); ln["b_S"] = Buf()
                ln["P1"] = [SB(st2, "P1_%d_%d" % (li, k), [128, 1024], BF16) for k in range(2)]; ln["b_P1"] = [Buf(), Buf()]
                ln["P2"] = [SB(st2, "P2_%d_%d" % (li, k), [128, 1024], BF16) for k in range(2)]; ln["b_P2"] = [Buf(), Buf()]
                ln["pk"] = 0
                lanes.append(ln)
            TC1 = tb[:, CT_TC1:CT_TC1 + 256].unsqueeze(1).to_broadcast([128, 4, 256])
            TC2 = tb[:, CT_TC2:CT_TC2 + 256].unsqueeze(1).to_broadcast([128, 4, 256])
            F1FULL = tb[:, CT_F1FULL:CT_F1FULL + 256]
            F1C = tb[0:64, CT_F1C:CT_F1C + 256]
            F2R = tb[:, CT_F2R:CT_F2R + 128]; F2I = tb[:, CT_F2I:CT_F2I + 128]; F2NI = tb[:, CT_F2NI:CT_F2NI + 128]
            G2A = tb[:, CT_G2A:CT_G2A + 256]; G2B = tb[:, CT_G2B:CT_G2B + 256]
            G1R = tb[:, CT_G1R:CT_G1R + 64]; G1I = tb[:, CT_G1I:CT_G1I + 64]
            w3v = w3b[:, :].rearrange("p (o d c) -> p o d c", o=2, d=2)

            F2NR = tb[:, CT_F2NR:CT_F2NR + 128]; G2NA = tb[:, CT_G2NA:CT_G2NA + 256]; G1NI = tb[:, CT_G1NI:CT_G1NI + 64]

            def cmul_ci(ln):
                k = ln["pk"]; ln["pk"] = 1 - k; ln["cur"] = k
                S4 = ln["S"][:, :].rearrange("p (c x) -> p c x", c=4)
                P1 = ln["P1"][k][:, :].rearrange("p (c x) -> p c x", c=4); P2 = ln["P2"][k][:, :].rearrange("p (c x) -> p c x", c=4)
                P.op("act", lambda e: e.copy(out=ln["S"][:, :], in_=ln["pL"][:, :]), reads=[ln["b_pL"]], writes=[ln["b_S"]])
                P.op("dve", lambda e: e.tensor_tensor(out=P1, in0=S4, in1=TC1, op=ALU.mult), reads=[ln["b_S"], b_tb], writes=[ln["b_P1"][k]])
                P.op("dve", lambda e: e.tensor_tensor(out=P2, in0=S4, in1=TC2, op=ALU.mult), reads=[ln["b_S"], b_tb], writes=[ln["b_P2"][k]])

            def cmul_ic(ln, f0):
                k = ln["pk"]; ln["pk"] = 1 - k; ln["cur"] = k
                S4 = ln["S"][:, :].rearrange("p (r c x) -> p r c x", r=2, c=4)
                P1 = ln["P1"][k][:, :].rearrange("p (r c x) -> p r c x", r=2, c=4); P2 = ln["P2"][k][:, :].rearrange("p (r c x) -> p r c x", r=2, c=4)
                kr = Khr[:, f0:f0 + 4, :].unsqueeze(1).to_broadcast([128, 2, 4, 128]); ki_ = Khi[:, f0:f0 + 4, :].unsqueeze(1).to_broadcast([128, 2, 4, 128])
                P.op("act", lambda e: e.copy(out=ln["S"][:, :], in_=ln["pL"][:, :]), reads=[ln["b_pL"]], writes=[ln["b_S"]])
                P.op("dve", lambda e: e.tensor_tensor(out=P1, in0=S4, in1=kr, op=ALU.mult), reads=[ln["b_S"], b_Kh], writes=[ln["b_P1"][k]])
                P.op("dve", lambda e: e.tensor_tensor(out=P2, in0=S4, in1=ki_, op=ALU.mult), reads=[ln["b_S"], b_Kh], writes=[ln["b_P2"][k]])

            def st_S1(ln, src, b_src, rhs):
                pA = ln["pL"][:, :].rearrange("p (c x) -> p c x", c=4)
                P.group("pe", [lambda e, cl=cl: e.matmul(pA[:, cl, :], lhsT=src[:, cl, :], rhs=rhs, start=True, stop=True) for cl in range(4)],
                        reads=[b_src, b_tb], writes=[ln["b_pL"]])

            def st_TW(ln):
                cmul_ci(ln)

            def st_S2(ln):
                k = ln["cur"]
                P1 = ln["P1"][k][:, :].rearrange("p (c x) -> p c x", c=4); P2 = ln["P2"][k][:, :].rearrange("p (c x) -> p c x", c=4)
                m0, m3, m2, m1 = P1[:, :, 0:128], P1[:, :, 128:256], P2[:, :, 0:128], P2[:, :, 128:256]
                pXr = ln["pL"][:, 0:512].rearrange("p (c x) -> p c x", c=4); pXi = ln["pL"][:, 512:1024].rearrange("p (c x) -> p c x", c=4)
                P.group("pe", [lambda e: e.matmul(pXr, lhsT=F2R, rhs=m0, start=True, stop=False),
                               lambda e: e.matmul(pXr, lhsT=F2NR, rhs=m1, start=False, stop=False),
                               lambda e: e.matmul(pXr, lhsT=F2NI, rhs=m2, start=False, stop=False),
                               lambda e: e.matmul(pXr, lhsT=F2NI, rhs=m3, start=False, stop=True),
                               lambda e: e.matmul(pXi, lhsT=F2R, rhs=m2, start=True, stop=False),
                               lambda e: e.matmul(pXi, lhsT=F2R, rhs=m3, start=False, stop=False),
                               lambda e: e.matmul(pXi, lhsT=F2I, rhs=m0, start=False, stop=False),
                               lambda e: e.matmul(pXi, lhsT=F2NI, rhs=m1, start=False, stop=True)],
                        reads=[ln["b_P1"][k], ln["b_P2"][k], b_tb], writes=[ln["b_pL"]])

            def st_SPEC(ln, f0):
                cmul_ic(ln, f0)

            def st_IS1(ln):
                k = ln["cur"]
                P1 = ln["P1"][k][:, :].rearrange("p (r c x) -> p r c x", r=2, c=4); P2 = ln["P2"][k][:, :].rearrange("p (r c x) -> p r c x", r=2, c=4)
                pB = ln["pL"][:, :].rearrange("p (c x) -> p c x", c=4)
                fns = []
                for cl in range(4):
                    fns.append(lambda e, cl=cl: e.matmul(pB[:, cl, :], lhsT=P1[:, 0, cl, :], rhs=G2A, start=True, stop=False))
                    fns.append(lambda e, cl=cl: e.matmul(pB[:, cl, :], lhsT=P2[:, 1, cl, :], rhs=G2NA, start=False, stop=False))
                    fns.append(lambda e, cl=cl: e.matmul(pB[:, cl, :], lhsT=P2[:, 0, cl, :], rhs=G2B, start=False, stop=False))
                    fns.append(lambda e, cl=cl: e.matmul(pB[:, cl, :], lhsT=P1[:, 1, cl, :], rhs=G2B, start=False, stop=True))
                P.group("pe", fns, reads=[ln["b_P1"][k], ln["b_P2"][k], b_tb], writes=[ln["b_pL"]])

            def st_ITW(ln):
                cmul_ci(ln)

            def st_IS2(ln):
                k = ln["cur"]
                P1 = ln["P1"][k][:, :].rearrange("p (c x) -> p c x", c=4); P2 = ln["P2"][k][:, :].rearrange("p (c x) -> p c x", c=4)
                n0, n3, n2, n1 = P1[:, :, 0:128], P1[:, :, 128:256], P2[:, :, 0:128], P2[:, :, 128:256]
                pY = ln["pL"][0:64, 0:512].rearrange("p (c x) -> p c x", c=4)
                P.group("pe", [lambda e: e.matmul(pY, lhsT=G1R, rhs=n0, start=True, stop=False),
                               lambda e: e.matmul(pY, lhsT=G1R, rhs=n1, start=False, stop=False),
                               lambda e: e.matmul(pY, lhsT=G1I, rhs=n3, start=False, stop=False),
                               lambda e: e.matmul(pY, lhsT=G1NI, rhs=n2, start=False, stop=True)],
                        reads=[ln["b_P1"][k], ln["b_P2"][k], b_tb], writes=[ln["b_pL"]])

            def st_gate(ln, cg, gin, b_gin, zout, b_zout):
                pY = ln["pL"][0:64, 0:512].rearrange("p (c x) -> p c x", c=4)
                P.op("dve", lambda e: e.tensor_tensor(out=zout, in0=pY, in1=gin, op=ALU.mult), reads=[ln["b_pL"], b_gin], writes=[b_zout])

            def sg_load(sg):
                us = sg % 2
                c0 = 16 * sg
                for k in range(3):
                    P.dma("pool", lambda e, k=k: e.dma_start(out=ut[us][k][:], in_=Uc[k * 512 + c0:k * 512 + c0 + 16, :].rearrange("c (a b) -> a c b", b=128)),
                          reads=[], writes=[b_ut[us][k]])

            def sg_kbatch(sg, bi):
                c0 = 16 * sg
                s = bi % 2
                pKv = pK[s][:, :].rearrange("p (n f) -> p n f", n=16)
                fns = []
                for nl in range(16):
                    n2 = bi * 16 + nl
                    fns.append(lambda e, nl=nl, n2=n2: e.matmul(pKv[0:64, nl, :].rearrange("p (o c) -> p o c", o=2),
                                                                lhsT=hdn2T[:, n2 * 128:n2 * 128 + 64], rhs=w3v[:, :, 0, c0:c0 + 16], start=True, stop=True))
                    fns.append(lambda e, nl=nl, n2=n2: e.matmul(pKv[64:128, nl, :].rearrange("p (o c) -> p o c", o=2),
                                                                lhsT=hdn2T[:, n2 * 128 + 64:n2 * 128 + 128], rhs=w3v[:, :, 1, c0:c0 + 16], start=True, stop=True))
                P.group("pe", fns, reads=[b_h2, b_w3], writes=[b_pK[s]])
                P.op("act", lambda e: e.copy(out=kraw[:, :, bi * 16:(bi + 1) * 16].rearrange("p f n -> p n f"), in_=pKv), reads=[b_pK[s]], writes=[b_kraw])

            def sg_window(sg):
                c0 = 16 * sg
                decv = dec[:, :, c0:c0 + 16]
                e1v = e1[:, :].rearrange("p (o c) -> p o c", o=2)
                wt4 = wtmp[:, :, :].rearrange("p (o c) n -> p o c n", o=2)

                def w0():
                    P.op("act", lambda e: e.activation(out=e1v, in_=decv, func=AF.Exp, scale=tf[:, 390:391]), reads=[b_dec, b_tf], writes=[b_e1])
                    P.op("pool", lambda e: e.tensor_tensor(out=wt4, in0=decv.unsqueeze(3).to_broadcast([128, 2, 16, 128]),
                                                           in1=tf[:, 528:656].unsqueeze(1).unsqueeze(1).to_broadcast([128, 2, 16, 128]), op=ALU.mult),
                         reads=[b_dec, b_tf], writes=[b_wtmp])

                def w1():
                    P.op("act", lambda e: e.activation(out=wtmp[:], in_=wtmp[:], func=AF.Exp), reads=[b_wtmp], writes=[b_wtmp])

                def w2():
                    P.op("pool", lambda e: e.tensor_tensor(out=wtmp[:], in0=wtmp[:], in1=e1[:, :].unsqueeze(2).to_broadcast([128, 32, 128]), op=ALU.mult),
                         reads=[b_wtmp, b_e1], writes=[b_wtmp])
                    P.op("pool", lambda e: e.tensor_scalar(out=r64[64:65, :], in0=kraw[64:65, :, 0], scalar1=1.05, scalar2=None, op0=ALU.mult),
                         reads=[b_kraw], writes=[b_r64])

                def w3():
                    P.op("dve", lambda e: e.scalar_tensor_tensor(out=kraw[:], in0=wtmp[:], scalar=0.05, in1=kraw[:], op0=ALU.add, op1=ALU.mult),
                         reads=[b_wtmp, b_kraw, b_r64], writes=[b_kraw])
                    P.op("pool", lambda e: e.tensor_copy(out=kraw[64:65, :, 0], in_=r64[64:65, :]), reads=[b_r64], writes=[b_kraw])

                def w4():
                    P.op("act", lambda e: e.activation(out=wtmp[:], in_=kraw[:], func=AF.Abs), reads=[b_kraw], writes=[b_wtmp])

                def w5():
                    P.op("dve", lambda e: e.tensor_reduce(out=ksum[:], in_=wtmp[:], axis=AX.X, op=ALU.add), reads=[b_wtmp], writes=[b_ksum])
                    P.group("pe", [lambda e: e.matmul(pN[:, 0:32], lhsT=tf[:, 656:784], rhs=ksum[:], start=True, stop=True)],
                            reads=[b_ksum, b_tf], writes=[b_pN])

                def w6():
                    P.op("dve", lambda e: e.reciprocal(out=ksum[:], in_=pN[:, 0:32]), reads=[b_pN], writes=[b_ksum])
                    P.op("pool", lambda e: e.memset(kraw[64:65, :, 0], 0.0), reads=[b_wtmp], writes=[b_kraw])

                def w7():
                    P.op("dve", lambda e: e.tensor_tensor(out=kcs[:], in0=kraw[:], in1=ksum[:, :].unsqueeze(2).to_broadcast([128, 32, 128]), op=ALU.mult),
                         reads=[b_kraw, b_ksum], writes=[b_kcs])
                    P.op("dve", lambda e: e.tensor_tensor(out=kcs[0:1, :, 0].rearrange("p (o c) -> p o c", o=2), in0=kcs[0:1, :, 0].rearrange("p (o c) -> p o c", o=2),
                                                          in1=skA[0:1, :].rearrange("p (o c) -> p o c", o=2)[:, :, c0:c0 + 16], op=ALU.add),
                         reads=[b_kcs, b_sk], writes=[b_kcs])
                    if debug == "p2" and sg == 0:
                        P.dma("sp", lambda e: e.dma_start(out=dbgkc[:, :, :], in_=kcs[:]), reads=[b_kcs])
                return [w0, w1, w2, w3, w4, w5, w6, w7]

            def sg_filtfft(sg):
                c0 = 16 * sg

                def filt_batch(f0s):
                    grp = [(lanes[li], f0) for li, f0 in enumerate(f0s)]
                    for ln, f0 in grp:
                        st_S1(ln, kcs[:, f0:f0 + 4, :], b_kcs, F1FULL)
                    for ln, f0 in grp:
                        st_TW(ln)
                    for ln, f0 in grp:
                        st_S2(ln)
                    for ln, f0 in grp:
                        pXr = ln["pL"][:, 0:512].rearrange("p (c x) -> p c x", c=4); pXi = ln["pL"][:, 512:1024].rearrange("p (c x) -> p c x", c=4)
                        P.op("act", lambda e, f0=f0, pXr=pXr: e.copy(out=Khr[:, f0:f0 + 4, :], in_=pXr), reads=[ln["b_pL"]], writes=[b_Kh])
                        P.op("act", lambda e, f0=f0, pXi=pXi: e.copy(out=Khi[:, f0:f0 + 4, :], in_=pXi), reads=[ln["b_pL"]], writes=[b_Kh])
                for f0s in ((0, 4, 8), (12, 16, 20), (24, 28)):
                    filt_batch(f0s)
                if debug == "p2" and sg == 0:
                    P.dma("sp", lambda e: e.dma_start(out=dbgkh[:, 0, :, :], in_=Khr[:]), reads=[b_Kh])
                    P.dma("sp", lambda e: e.dma_start(out=dbgkh[:, 1, :, :], in_=Khi[:]), reads=[b_Kh])

            def sg_conv_batch(sg, tasks, b_z1g, hooks=None):
                hk = (lambda k: hooks[k]()) if hooks else (lambda k: None)
                us = sg % 2
                grp = []
                for li, (o, cg) in enumerate(tasks):
                    if o == 0:
                        zin, b_zin = ut[us][0][:, 4 * cg:4 * cg + 4, :], b_ut[us][0]
                        gin, b_gin = ut[us][1][:, 4 * cg:4 * cg + 4, :], b_ut[us][1]
                        zout, b_zout = z1t[:, 4 * cg:4 * cg + 4, :], b_z1g[cg]
                    else:
                        zin, b_zin = z1t[:, 4 * cg:4 * cg + 4, :], b_z1g[cg]
                        gin, b_gin = ut[us][2][:, 4 * cg:4 * cg + 4, :], b_ut[us][2]
                        zout, b_zout = z2t[us][:, 4 * cg:4 * cg + 4, :], b_z2[us]
                    grp.append((lanes[li], o, cg, zin, b_zin, gin, b_gin, zout, b_zout))
                for (ln, o, cg, zin, b_zin, gin, b_gin, zout, b_zout) in grp:
                    st_S1(ln, zin, b_zin, F1C)
                hk(0)
                for g_ in grp:
                    st_TW(g_[0])
                hk(1)
                for g_ in grp:
                    st_S2(g_[0])
                hk(2)
                for g_ in grp:
                    st_SPEC(g_[0], g_[1] * 16 + 4 * g_[2])
                hk(3)
                for g_ in grp:
                    st_IS1(g_[0])
                hk(4)
                for g_ in grp:
                    st_ITW(g_[0])
                hk(5)
                for g_ in grp:
                    st_IS2(g_[0])
                hk(6)
                for (ln, o, cg, zin, b_zin, gin, b_gin, zout, b_zout) in grp:
                    st_gate(ln, cg, gin, b_gin, zout, b_zout)
                hk(7)

            def sg_store(sg):
                us = sg % 2
                c0 = 16 * sg
                P.dma("sp", lambda e: e.dma_start(out=Z2[c0:c0 + 16, :].rearrange("c (a b) -> a c b", b=128), in_=z2t[us][0:32, :, :]), reads=[b_z2[us]])

            cvs = [SB(st2, "cvs%d" % i, [128, 2048], BF16) for i in range(3)]; b_cvs = [Buf() for _ in range(3)]
            cv_tasks = [(ti, rb) for rb in range(ne // 128) for ti in range(3)]
            cv_state = {"i": 0}
            ew_src = (ew1, ew3, ew2)

            def cv():
                i = cv_state["i"]
                if i >= len(cv_tasks):
                    return
                cv_state["i"] = i + 1
                ti, rb = cv_tasks[i]
                s = i % 3
                P.dma("pool", lambda e: e.dma_start(out=cvs[s][:], in_=ew_src[ti][rb * 128:(rb + 1) * 128, :]), writes=[b_cvs[s]])
                ee, hh_ = rb // 2, rb % 2
                P.dma("sp", lambda e: e.dma_start(out=EWB[ti][ee * 128:(ee + 1) * 128, hh_ * 2048:(hh_ + 1) * 2048], in_=cvs[s][:]), reads=[b_cvs[s]])
            CONV_TASKS = (((0, 0), (0, 1), (0, 2)), ((0, 3), (1, 0), (1, 1)), ((1, 2), (1, 3)))
            nsg = 32 if debug != "p2" else int(DBG_NSG)
            b_z1g = [Buf() for _ in range(4)]
            sg_load(0)
            for bi in range(8):
                sg_kbatch(0, bi)
            for w_ in sg_window(0):
                w_()
            for sg in range(nsg):
                sg_filtfft(sg)
                nxt = sg + 1 < nsg
                if nxt:
                    sg_load(sg + 1)
                sg_conv_batch(sg, CONV_TASKS[0], b_z1g, hooks=[cv, cv, cv, cv, cv, cv, (lambda: None), (lambda: None)])
                if nxt:
                    for bi in range(0, 4):
                        sg_kbatch(sg + 1, bi)
                sg_conv_batch(sg, CONV_TASKS[1], b_z1g, hooks=[cv, cv, cv, cv, cv, cv, (lambda: None), (lambda: None)])
                if debug == "p2" and sg == 0:
                    P.dma("sp", lambda e: e.dma_start(out=dbgz1[:, :, :], in_=z1t[:]), reads=b_z1g)
                if nxt:
                    for bi in range(4, 8):
                        sg_kbatch(sg + 1, bi)
                sg_conv_batch(sg, CONV_TASKS[2], b_z1g, hooks=(sg_window(sg + 1) if nxt else None))
                sg_store(sg)
            while cv_state["i"] < len(cv_tasks):
                cv()
            P.barrier()
        if debug == "p2":
            return _finish(nc, P, out, gst)

        def layer_norm(st_tiles, r, b_r, g_b, b_b, b_gb, dst, b_dst, eng2="pool", lnexp=False):
            stats, mv, b_stat = st_tiles
            P.op("dve", lambda e: e.bn_stats(out=stats[:, 0, :], in_=r[:, 0:512]), reads=[b_r], writes=[b_stat])
            P.op("dve", lambda e: e.bn_stats(out=stats[:, 1, :], in_=r[:, 512:1024]), reads=[b_r], writes=[b_stat])
            P.op("dve", lambda e: e.bn_aggr(out=mv[:, 0:2], in_=stats[:].rearrange("p a b -> p (a b)")), reads=[b_stat], writes=[b_stat])
            P.op("dve", lambda e: e.tensor_scalar(out=mv[:, 2:3], in0=mv[:, 1:2], scalar1=LN_EPS, scalar2=None, op0=ALU.add), reads=[b_stat], writes=[b_stat])
            if lnexp:
                P.op("act", lambda e: e.activation(out=mv[:, 2:3], in_=mv[:, 2:3], func=AF.Ln), reads=[b_stat], writes=[b_stat])
                P.op("act", lambda e: e.activation(out=mv[:, 2:3], in_=mv[:, 2:3], func=AF.Exp, scale=-0.5), reads=[b_stat], writes=[b_stat])
            else:
                P.op("act", lambda e: e.activation(out=mv[:, 2:3], in_=mv[:, 2:3], func=AF.Sqrt), reads=[b_stat], writes=[b_stat])
                P.op("dve", lambda e: e.reciprocal(out=mv[:, 2:3], in_=mv[:, 2:3]), reads=[b_stat], writes=[b_stat])
            P.op("dve", lambda e: e.scalar_tensor_tensor(out=mv[:, 3:4], in0=mv[:, 0:1], scalar=-1.0, in1=mv[:, 2:3], op0=ALU.mult, op1=ALU.mult),
                 reads=[b_stat], writes=[b_stat])
            P.op("act", lambda e: e.activation(out=r[:], in_=r[:], func=AF.Identity, scale=mv[:, 2:3], bias=mv[:, 3:4]), reads=[b_r, b_stat], writes=[b_r])
            P.op(eng2, lambda e: e.tensor_tensor(out=r[:], in0=r[:], in1=g_b, op=ALU.mult), reads=[b_r, b_gb], writes=[b_r])
            P.op("dve", lambda e: e.tensor_tensor(out=dst, in0=r[:], in1=b_b, op=ALU.add), reads=[b_r, b_gb], writes=[b_dst])

        with ExitStack() as st34:
            dest = SB(st34, "dest", [128, 32, 2], I32); b_dest = Buf()
            wts = SB(st34, "wts", [128, 32, 2], F32); b_wts = Buf()
            widx = SB(st34, "widx", [128, 128, 2], I32); b_widx = Buf()
            with ExitStack() as st:
                wao = SB(st, "wao", [128, 4, D], BF16); who = SB(st, "who", [128, 4, D], BF16); wout = SB(st, "wout", [128, 8, D], BF16); b_w3p = Buf()

                def ldw(dst, srcw, nk):
                    for kc in range(nk):
                        P.dma("pool", lambda e, kc=kc: e.dma_start(out=dst[:, kc, :], in_=srcw[kc * 128:(kc + 1) * 128, :]), writes=[b_w3p])
                ldw(wao, w_attn_o, 4); ldw(who, w_hy_o, 4); ldw(wout, w_out, 8)
                bc = SB(st, "bc", [128, 5, D], F32); b_bc = Buf()
                for k, srcap in enumerate((mod_scr[0:1, 2048:3072], ln1g[0:1, :], ln1b[0:1, :], mod_scr[0:1, 4096:5120], mod_scr[0:1, 3072:4096])):
                    P.dma("sp", lambda e, k=k, srcap=srcap: e.dma_start(out=bc[:, k, :], in_=srcap.partition_broadcast(128)), writes=[b_bc])
                wrt = SB(st, "wrt", [128, 8, 72], F32); brb = SB(st, "brb", [128, 72], F32); b_wr = Buf()
                P.dma("sp", lambda e: e.dma_start(out=wrt[:], in_=wr[:, :].rearrange("(c p) n -> p c n", p=128)), writes=[b_wr])
                P.dma("sp", lambda e: e.dma_start(out=brb[:], in_=br[0:1, :].partition_broadcast(128)), writes=[b_wr])
                rcarry = SB(st, "rcarry", [128, 64], F32); b_rcarry = Buf()
                P.op("pool", lambda e: e.memset(rcarry[:], 0.0), writes=[b_rcarry])
                rnk = SB(st, "rnk", [128, 32, 2], F32); b_rnk = Buf()
                ohall = SB(st, "ohall", [128, 32, 2, 64], BF16); b_oh = Buf()
                atM = SB(st, "atTm", [128, 4, 512], BF16); z2T = SB(st, "z2T", [128, 4, 512], BF16)
                sga = SB(st, "sga", [128, 8, 512], BF16); sgh = SB(st, "sgh", [128, 8, 512], BF16)
                b_at = Buf(); b_z2T = Buf(); b_sga = Buf(); b_sgh = Buf()
                mrg = [SB(st, "mrg%d" % i, [128, 8, 512], BF16) for i in range(2)]; b_mrg = [Buf(), Buf()]
                t1 = [SB(st, "t1_%d" % i, [128, 512], F32) for i in range(2)]; t2 = [SB(st, "t2_%d" % i, [128, 512], F32) for i in range(2)]
                b_t1 = [Buf(), Buf()]; b_t2 = [Buf(), Buf()]
                xt = [SB(st, "xt%d" % i, [128, D], F32) for i in range(2)]; b_xt = [Buf(), Buf()]
                rr = [SB(st, "rr%d" % i, [128, D], F32) for i in range(2)]; b_rr = [Buf(), Buf()]
                x1s = [SB(st, "x1s%d" % i, [128, D], F32) for i in range(2)]; b_x1s = [Buf(), Buf()]
                h2 = SB(st, "h2", [128, D], F32); b_h2t = Buf()
                h2b = [SB(st, "h2b%d" % i, [128, D], BF16) for i in range(2)]; b_h2b = [Buf(), Buf()]
                h2T = SB(st, "h2T", [128, 8, 128], F32); b_h2T = Buf()
                stats = SB(st, "stats", [128, 2, 6], F32); mv = SB(st, "mv", [128, 4], F32); b_stat = Buf()
                lg = SB(st, "lg", [128, 72], F32); b_lg = Buf()
                sm = SB(st, "sm", [128, 64], F32); b_sm = Buf()
                elm = SB(st, "elm", [128, 64], F32); b_elm = Buf()
                m8 = SB(st, "m8", [128, 16], F32); b_m8 = Buf()
                ohs = SB(st, "ohs", [128, 64], BF16); b_ohs = Buf()
                basef = SB(st, "basef", [128, 64], F32); b_base = Buf()
                junk = SB(st, "junk", [128, 64], F32); b_junk = Buf()
                pMa = PS(st, "pMa", [128, 512]); pMh = PS(st, "pMh", [128, 512]); b_pMa = Buf(); b_pMh = Buf()
                pYt = PS(st, "pYt", [128, 1024]); b_pYt = Buf()
                pHT = PS(st, "pHT", [128, 1024]); b_pHT = Buf()
                pR = PS(st, "pR", [128, 512]); b_pR = Buf()
                pLg = PS(st, "pLg", [128, 512]); b_pLg = Buf()

                def route_tile(i, x1tile, b_x1tile, hs):
                    P.op("dve", lambda e: e.tensor_tensor(out=h2[:], in0=x1tile[:], in1=bc[:, 3, :], op=ALU.mult), reads=[b_x1tile, b_bc], writes=[b_h2t])
                    P.op("dve", lambda e: e.tensor_tensor(out=h2[:], in0=h2[:], in1=bc[:, 4, :], op=ALU.add), reads=[b_h2t, b_bc], writes=[b_h2t])
                    P.op("act", lambda e: e.copy(out=h2b[hs][:], in_=h2[:]), reads=[b_h2t], writes=[b_h2b[hs]])
                    P.dma("sp", lambda e: e.dma_start(out=H2[i * 128:(i + 1) * 128, :], in_=h2b[hs][:]), reads=[b_h2b[hs]])
                    pHTv = pHT[:, :].rearrange("p (c q) -> p c q", c=8)
                    P.group("pe", [lambda e, kc=kc: e.transpose(out=pHTv[:, kc, :], in_=h2[:, kc * 128:(kc + 1) * 128], identity=identf) for kc in range(8)],
                            reads=[b_h2t, b_tf], writes=[b_pHT])
                    P.op("act", lambda e: e.copy(out=h2T[:], in_=pHTv), reads=[b_pHT], writes=[b_h2T])
                    P.group("pe", [lambda e, kc=kc: e.matmul(pLg[:, 0:72], lhsT=h2T[:, kc, :], rhs=wrt[:, kc, :], start=(kc == 0), stop=(kc == 7)) for kc in range(8)],
                            reads=[b_h2T, b_wr], writes=[b_pLg])
                    P.op("dve", lambda e: e.tensor_tensor(out=lg[:], in0=pLg[:, 0:72], in1=brb[:], op=ALU.add), reads=[b_pLg, b_wr], writes=[b_lg])
                    P.op("dve", lambda e: e.max(out=m8[:, 0:8], in_=lg[:, 0:8]), reads=[b_lg], writes=[b_m8])
                    P.op("dve", lambda e: e.tensor_scalar(out=sm[:, 0:8], in0=lg[:, 0:8], scalar1=m8[:, 0:1], scalar2=None, op0=ALU.is_equal),
                         reads=[b_lg, b_m8], writes=[b_sm])
                    P.op("dve", lambda e: e.tensor_scalar(out=sm[:, 8:9], in0=m8[:, 0:1], scalar1=-1.0, scalar2=None, op0=ALU.mult), reads=[b_m8], writes=[b_sm])
                    P.op("act", lambda e: e.activation(out=sm[:, 16:24], in_=lg[:, 0:8], func=AF.Exp, bias=sm[:, 8:9], scale=1.0, accum_out=sm[:, 9:10]),
                         reads=[b_lg, b_sm], writes=[b_sm])
                    P.op("dve", lambda e: e.reciprocal(out=sm[:, 10:11], in_=sm[:, 9:10]), reads=[b_sm], writes=[b_sm])
                    P.op("dve", lambda e: e.tensor_scalar(out=sm[:, 24:32], in0=sm[:, 0:8], scalar1=1e30, scalar2=-1e30, op0=ALU.mult, op1=ALU.add),
                         reads=[b_sm], writes=[b_sm])
                    P.op("dve", lambda e: e.tensor_tensor(out=elm[:, :].rearrange("p (g e) -> p g e", g=8), in0=lg[:, 8:72].rearrange("p (g e) -> p g e", g=8),
                                                          in1=sm[:, 24:32].unsqueeze(2).to_broadcast([128, 8, 8]), op=ALU.add),
                         reads=[b_lg, b_sm], writes=[b_elm])
                    P.op("dve", lambda e: e.max(out=m8[:, 8:16], in_=elm[:]), reads=[b_elm], writes=[b_m8])
                    P.op("dve", lambda e: e.tensor_scalar(out=ohall[:, i, 0, :], in0=elm[:], scalar1=m8[:, 8:9], scalar2=None, op0=ALU.is_equal),
                         reads=[b_elm, b_m8], writes=[b_oh])
                    P.op("dve", lambda e: e.tensor_scalar(out=ohall[:, i, 1, :], in0=elm[:], scalar1=m8[:, 9:10], scalar2=None, op0=ALU.is_equal),
                         reads=[b_elm, b_m8], writes=[b_oh])
                    P.op("dve", lambda e: e.tensor_tensor(out=sm[:, 11:12], in0=m8[:, 9:10], in1=m8[:, 8:9], op=ALU.subtract), reads=[b_m8], writes=[b_sm])
                    P.op("act", lambda e: e.activation(out=sm[:, 12:13], in_=sm[:, 11:12], func=AF.Exp), reads=[b_sm], writes=[b_sm])
                    P.op("dve", lambda e: e.tensor_scalar(out=sm[:, 12:13], in0=sm[:, 12:13], scalar1=1.0, scalar2=None, op0=ALU.add), reads=[b_sm], writes=[b_sm])
                    P.op("dve", lambda e: e.reciprocal(out=sm[:, 13:14], in_=sm[:, 12:13]), reads=[b_sm], writes=[b_sm])
                    P.op("dve", lambda e: e.tensor_tensor(out=wts[:, i, 0:1], in0=sm[:, 13:14], in1=sm[:, 10:11], op=ALU.mult), reads=[b_sm], writes=[b_wts])
                    P.op("dve", lambda e: e.tensor_tensor(out=wts[:, i, 1:2], in0=sm[:, 10:11], in1=wts[:, i, 0:1], op=ALU.subtract), reads=[b_sm, b_wts], writes=[b_wts])
                    P.op("dve", lambda e: e.tensor_tensor(out=ohs[:], in0=ohall[:, i, 0, :], in1=ohall[:, i, 1, :], op=ALU.add), reads=[b_oh], writes=[b_ohs])
                    P.group("pe", [lambda e: e.matmul(pR[:, 0:64], lhsT=tb[:, CT_TRI:CT_TRI + 128], rhs=ohs[:], start=True, stop=True),
                                   lambda e: e.matmul(pR[:, 64:128], lhsT=tb[:, CT_ONES:CT_ONES + 128], rhs=ohs[:], start=True, stop=True)],
                            reads=[b_ohs, b_tb], writes=[b_pR])
                    P.op("dve", lambda e: e.tensor_tensor(out=basef[:], in0=pR[:, 0:64], in1=rcarry[:], op=ALU.add), reads=[b_pR, b_rcarry], writes=[b_base])
                    for k in range(2):
                        P.op("dve", lambda e, k=k: e.tensor_tensor(out=junk[:], in0=ohall[:, i, k, :], in1=basef[:], op=ALU.mult),
                             reads=[b_oh, b_base], writes=[b_junk])
                        P.op("dve", lambda e, k=k: e.tensor_reduce(out=rnk[:, i, k:k + 1], in_=junk[:], axis=AX.X, op=ALU.add),
                             reads=[b_junk], writes=[b_rnk])
                    P.op("dve", lambda e: e.tensor_tensor(out=rcarry[:], in0=rcarry[:], in1=pR[:, 64:128], op=ALU.add), reads=[b_pR, b_rcarry], writes=[b_rcarry])

                def merge_loads(s_i):
                    c0 = s_i * 512
                    P.dma("sp", lambda e: e.dma_start(out=atM[:], in_=AT[:, c0:c0 + 512].rearrange("(c p) q -> p c q", p=128)), writes=[b_at])
                    P.dma("sp", lambda e: e.dma_start(out=z2T[:], in_=Z2[:, c0:c0 + 512].rearrange("(c p) q -> p c q", p=128)), writes=[b_z2T])
                    P.dma("sp", lambda e: e.dma_start(out=sga[:], in_=Gs[0:1024, c0:c0 + 512].rearrange("(c p) q -> p c q", p=128)), writes=[b_sga])
                    P.dma("sp", lambda e: e.dma_start(out=sgh[:], in_=Gs[1024:2048, c0:c0 + 512].rearrange("(c p) q -> p c q", p=128)), writes=[b_sgh])

                def merge_compute(s_i):
                    ms = s_i % 2

                    def fchunk(fc):
                        ts = fc % 2
                        P.group("pe", [lambda e, kc=kc: e.matmul(pMa[:], lhsT=wao[:, kc, fc * 128:(fc + 1) * 128], rhs=atM[:, kc, :], start=(kc == 0), stop=(kc == 3))
                                       for kc in range(4)], reads=[b_w3p, b_at], writes=[b_pMa])
                        P.group("pe", [lambda e, kc=kc: e.matmul(pMh[:], lhsT=who[:, kc, fc * 128:(fc + 1) * 128], rhs=z2T[:, kc, :], start=(kc == 0), stop=(kc == 3))
                                       for kc in range(4)], reads=[b_w3p, b_z2T], writes=[b_pMh])
                        P.op("dve", lambda e: e.tensor_tensor(out=t1[ts][:], in0=pMa[:], in1=sga[:, fc, :], op=ALU.mult), reads=[b_pMa, b_sga], writes=[b_t1[ts]])
                        P.op("dve", lambda e: e.tensor_tensor(out=t2[ts][:], in0=pMh[:], in1=sgh[:, fc, :], op=ALU.mult), reads=[b_pMh, b_sgh], writes=[b_t2[ts]])
                        P.op("dve", lambda e: e.tensor_tensor(out=mrg[ms][:, fc, :], in0=t1[ts][:], in1=t2[ts][:], op=ALU.add),
                             reads=[b_t1[ts], b_t2[ts]], writes=[b_mrg[ms]])
                    for fc in range(8):
                        fchunk(fc)

                def tile_A(s_i, t):
                    ms = s_i % 2
                    i = s_i * 4 + t
                    xs_ = i % 2
                    P.dma("sp", lambda e: e.dma_start(out=xt[xs_][:], in_=xcat[i * 128:(i + 1) * 128, :]), writes=[b_xt[xs_]])
                    fns = []
                    for nh in range(2):
                        for kc in range(8):
                            fns.append(lambda e, nh=nh, kc=kc: e.matmul(pYt[:, nh * 512:(nh + 1) * 512], lhsT=mrg[ms][:, kc, t * 128:(t + 1) * 128],
                                                                      rhs=wout[:, kc, nh * 512:(nh + 1) * 512], start=(kc == 0), stop=(kc == 7)))
                    P.group("pe", fns, reads=[b_mrg[ms], b_w3p], writes=[b_pYt])
                    P.op("dve", lambda e: e.tensor_tensor(out=rr[xs_][:], in0=pYt[:], in1=bc[:, 0, :], op=ALU.mult), reads=[b_pYt, b_bc], writes=[b_rr[xs_]])
                    P.op("dve", lambda e: e.scalar_tensor_tensor(out=rr[xs_][:], in0=xt[xs_][:], scalar=DN_ALPHA, in1=rr[xs_][:], op0=ALU.mult, op1=ALU.add),
                         reads=[b_xt[xs_], b_rr[xs_]], writes=[b_rr[xs_]])
                    layer_norm((stats, mv, b_stat), rr[xs_], b_rr[xs_], bc[:, 1, :], bc[:, 2, :], b_bc, x1s[xs_][:], b_x1s[xs_], eng2="dve", lnexp=True)
                    P.dma("act", lambda e: e.dma_start(out=X1[i * 128:(i + 1) * 128, :], in_=x1s[xs_][:]), reads=[b_x1s[xs_]])

                def tile_B(s_i, t):
                    i = s_i * 4 + t
                    xs_ = i % 2
                    route_tile(i, x1s[xs_], b_x1s[xs_], xs_)

                merge_loads(0)
                merge_compute(0)
                for s_i in range(8):
                    nx = s_i + 1 < 8
                    if nx:
                        merge_loads(s_i + 1)
                    tile_A(s_i, 0)
                    tile_A(s_i, 1)
                    tile_B(s_i, 0)
                    if nx:
                        merge_compute(s_i + 1)
                    tile_A(s_i, 2)
                    tile_B(s_i, 1)
                    tile_A(s_i, 3)
                    tile_B(s_i, 2)
                    tile_B(s_i, 3)

                ci = SB(st, "cnt_i", [128, 64], I32); b_ci = Buf()
                psz = SB(st, "psz", [128, 64], F32); pends = SB(st, "pends", [128, 64], F32); poffs = SB(st, "poffs", [128, 64], F32)
                zer = SB(st, "zer", [128, 64], F32); b_pz = Buf()
                P.op("dve", lambda e: e.tensor_scalar(out=psz[:], in0=rcarry[:], scalar1=127.0, scalar2=None, op0=ALU.add), reads=[b_rcarry], writes=[b_pz])
                P.op("dve", lambda e: e.tensor_copy(out=ci[:], in_=psz[:]), reads=[b_pz], writes=[b_ci])
                P.op("dve", lambda e: e.tensor_scalar(out=ci[:], in0=ci[:], scalar1=7, scalar2=7, op0=ALU.arith_shift_right, op1=ALU.logical_shift_left),
                     reads=[b_ci], writes=[b_ci])
                P.op("dve", lambda e: e.tensor_copy(out=psz[:], in_=ci[:]), reads=[b_ci], writes=[b_pz])
                P.op("dve", lambda e: e.memset(zer[:], 0.0), writes=[b_pz])
                P.op("dve", lambda e: e.tensor_tensor_scan(out=pends[:], data0=psz[:], data1=zer[:], initial=0.0, op0=ALU.add, op1=ALU.add), reads=[b_pz], writes=[b_pz])
                P.op("dve", lambda e: e.tensor_tensor(out=poffs[:], in0=pends[:], in1=psz[:], op=ALU.subtract), reads=[b_pz], writes=[b_pz])
                big = SB(st, "big", [128, 64, 64], F32); b_big = Buf()
                dsf = SB(st, "dsf", [128, 64], F32); b_dsf = Buf()
                P.op("dve", lambda e: e.tensor_tensor(out=big[:], in0=ohall[:].rearrange("p i k e -> p (i k) e"),
                                                      in1=poffs[:, :].unsqueeze(1).to_broadcast([128, 64, 64]), op=ALU.mult), reads=[b_oh, b_pz], writes=[b_big])
                P.op("dve", lambda e: e.tensor_reduce(out=dsf[:], in_=big[:], axis=AX.X, op=ALU.add), reads=[b_big], writes=[b_dsf])
                P.op("dve", lambda e: e.tensor_tensor(out=dsf[:], in0=dsf[:], in1=rnk[:].rearrange("p i k -> p (i k)"), op=ALU.add), reads=[b_dsf, b_rnk], writes=[b_dsf])
                P.op("dve", lambda e: e.tensor_copy(out=dest[:].rearrange("p i k -> p (i k)"), in_=dsf[:]), reads=[b_dsf], writes=[b_dest])
                blke = SB(st, "blke", [128, 128], F32); b_blke = Buf()

                def blk_chunk(jc):
                    bigv = big[:, 0:32, :]
                    P.op("dve", lambda e: e.tensor_tensor(out=bigv, in0=pends[:, :].unsqueeze(1).to_broadcast([128, 32, 64]),
                                                          in1=tf[:, 400 + jc * 32:400 + (jc + 1) * 32].unsqueeze(2).to_broadcast([128, 32, 64]), op=ALU.is_le),
                         reads=[b_pz, b_tf, b_dsf], writes=[b_big])
                    P.op("dve", lambda e: e.tensor_reduce(out=blke[:, jc * 32:(jc + 1) * 32], in_=bigv, axis=AX.X, op=ALU.add), reads=[b_big], writes=[b_blke])
                for jc in range(4):
                    blk_chunk(jc)
                skipf = SB(st, "skipf", [128, 128], F32); b_skipf = Buf()
                P.op("dve", lambda e: e.memset(skipf[:, 0:3], 0.0), writes=[b_skipf])
                P.op("dve", lambda e: e.tensor_scalar(out=blke[:], in0=blke[:], scalar1=63.0, scalar2=None, op0=ALU.min), reads=[b_blke], writes=[b_blke])
                P.op("dve", lambda e: e.tensor_tensor(out=skipf[:, 3:128], in0=blke[:, 3:128], in1=blke[:, 0:125], op=ALU.is_equal), reads=[b_blke, b_skipf], writes=[b_skipf])
                P.op("dve", lambda e: e.tensor_scalar(out=blke[:], in0=blke[:], scalar1=128.0, scalar2=None, op0=ALU.mult), reads=[b_blke, b_skipf], writes=[b_blke])
                P.op("dve", lambda e: e.scalar_tensor_tensor(out=blke[:], in0=skipf[:], scalar=1000000.0, in1=blke[:], op0=ALU.mult, op1=ALU.add),
                     reads=[b_blke, b_skipf], writes=[b_blke])
                P.op("dve", lambda e: e.tensor_scalar(out=blke[:], in0=blke[:], scalar1=tf[:, 384:385], scalar2=None, op0=ALU.add), reads=[b_blke, b_tf], writes=[b_blke])
                P.op("dve", lambda e: e.tensor_copy(out=widx[:, :, 0], in_=blke[:]), reads=[b_blke], writes=[b_widx])
                P.op("dve", lambda e: e.tensor_scalar(out=blke[:], in0=blke[:], scalar1=128.0, scalar2=None, op0=ALU.add), reads=[b_blke, b_widx], writes=[b_blke])
                P.op("dve", lambda e: e.tensor_copy(out=widx[:, :, 1], in_=blke[:]), reads=[b_blke], writes=[b_widx])
                if debug in ("p3", "p4"):
                    dbgrt = dscr("dbgrt", [128, 64 + 64 + 256], F32)
                    dbt = SB(st, "dbt", [128, 384], F32); b_dbt = Buf()
                    P.op("dve", lambda e: e.tensor_copy(out=dbt[:, 0:64], in_=dest[:].rearrange("p i k -> p (i k)")), reads=[b_dest], writes=[b_dbt])
                    P.op("dve", lambda e: e.tensor_copy(out=dbt[:, 64:128], in_=wts[:].rearrange("p i k -> p (i k)")), reads=[b_wts], writes=[b_dbt])
                    P.op("dve", lambda e: e.tensor_copy(out=dbt[:, 128:384], in_=widx[:].rearrange("p j h -> p (j h)")), reads=[b_widx], writes=[b_dbt])
                    P.dma("sp", lambda e: e.dma_start(out=dbgrt[:, :], in_=dbt[:]), reads=[b_dbt])
                P.barrier()
            if debug == "p3":
                return _finish(nc, P, out, gst)

            with ExitStack() as st:
                w1s = [SB(st, "w1s%d" % i, [128, 2, 2048], BF16) for i in range(3)]
                w3s = [SB(st, "w3s%d" % i, [128, 2, 2048], BF16) for i in range(3)]
                w2s = [SB(st, "w2s%d" % i, [128, 2, 2048], BF16) for i in range(3)]
                b_w1s = [Buf() for _ in range(3)]; b_w3s = [Buf() for _ in range(3)]; b_w2s = [Buf() for _ in range(3)]
                xbt = [SB(st, "xbt%d" % i, [128, D], BF16) for i in range(3)]; b_xbt = [Buf() for _ in range(3)]
                xbT = [SB(st, "xbT%d" % i, [128, 8, 128], BF16) for i in range(2)]; b_xbT = [Buf(), Buf()]
                slu = SB(st, "slu", [128, 512], F32); b_slu = Buf()
                ggb = SB(st, "ggb", [128, 512], BF16); b_ggb = Buf()
                gTb = SB(st, "gTb", [128, 4, 128], BF16); b_gTb = Buf()
                ybt = [SB(st, "ybt%d" % i, [128, D], BF16) for i in range(2)]; b_ybt = [Buf(), Buf()]
                pXT = PS(st, "pXT", [128, 1024], BF16); b_pXT = Buf()
                pH1 = PS(st, "pE1", [128, 512]); pH3 = PS(st, "pE3", [128, 512]); b_pH1 = Buf(); b_pH3 = Buf()
                pGT = PS(st, "pGT", [128, 1024], BF16); b_pGT = Buf()
                pO2 = PS(st, "pE2", [128, 1024]); b_pO2 = Buf()

                _bcr = {}

                def _bc_reg(e):
                    if "r" not in _bcr:
                        _bcr["r"] = e.to_reg(ne // 2 - 1)
                    return _bcr["r"]

                NW = 3

                def stG(j):
                    s = j % NW
                    for (wsb, wdram, bw) in ((w1s, EWB[0], b_w1s), (w3s, EWB[1], b_w3s), (w2s, EWB[2], b_w2s)):
                        P.dma("pool", lambda e, wsb=wsb, wdram=wdram: e.indirect_dma_start(
                            out=wsb[s][:, :, :].rearrange("p h n -> p (h n)"), out_offset=None, in_=wdram[:, :],
                            in_offset=bass.IndirectOffsetOnAxis(ap=widx[:, j, 0:1], axis=0), bounds_check=_bc_reg(e), oob_is_err=False),
                            reads=[b_widx], writes=[bw[s]])

                def stGX(j):
                    x3 = j % 3
                    P.dma("sp", lambda e: e.dma_start(out=xbt[x3][:], in_=XB[j * 128:(j + 1) * 128, :]), writes=[b_xbt[x3]])

                def stAT(j):
                    x2 = j % 2
                    x3 = j % 3
                    pXTv = pXT[:, :].rearrange("p (c q) -> p c q", c=8)
                    P.group("pe", [lambda e, kc=kc: e.transpose(out=pXTv[:, kc, :], in_=xbt[x3][:, kc * 128:(kc + 1) * 128], identity=identb) for kc in range(8)],
                            reads=[b_xbt[x3], b_tb], writes=[b_pXT])
                    P.op("dve", lambda e: e.tensor_copy(out=xbT[x2][:], in_=pXTv), reads=[b_pXT], writes=[b_xbT[x2]])

                def stBM(j):
                    s = j % NW
                    x2 = j % 2
                    P.group("pe", [lambda e, kc=kc: e.matmul(pH1[:], lhsT=xbT[x2][:, kc, :], rhs=w1s[s][:, kc // 4, (kc % 4) * 512:(kc % 4 + 1) * 512],
                                                             start=(kc == 0), stop=(kc == 7)) for kc in range(8)],
                            reads=[b_xbT[x2], b_w1s[s]], writes=[b_pH1])
                    P.group("pe", [lambda e, kc=kc: e.matmul(pH3[:], lhsT=xbT[x2][:, kc, :], rhs=w3s[s][:, kc // 4, (kc % 4) * 512:(kc % 4 + 1) * 512],
                                                             start=(kc == 0), stop=(kc == 7)) for kc in range(8)],
                            reads=[b_xbT[x2], b_w3s[s]], writes=[b_pH3])
                    P.op("act", lambda e: e.activation(out=slu[:], in_=pH1[:], func=AF.Silu), reads=[b_pH1], writes=[b_slu])
                    P.op("dve", lambda e: e.tensor_tensor(out=ggb[:], in0=pH3[:], in1=slu[:], op=ALU.mult), reads=[b_pH3, b_slu], writes=[b_ggb])

                def stCT(j):
                    pGTv = pGT[:, 0:512].rearrange("p (c q) -> p c q", c=4)
                    P.group("pe", [lambda e, fc=fc: e.transpose(out=pGTv[:, fc, :], in_=ggb[:, fc * 128:(fc + 1) * 128], identity=identb) for fc in range(4)],
                            reads=[b_ggb, b_tb], writes=[b_pGT])
                    P.op("act", lambda e: e.copy(out=gTb[:], in_=pGTv), reads=[b_pGT], writes=[b_gTb])

                def stDM(j):
                    s = j % NW
                    y2 = j % 2
                    fns = []
                    for nh in range(2):
                        for fc in range(4):
                            fns.append(lambda e, nh=nh, fc=fc: e.matmul(pO2[:, nh * 512:(nh + 1) * 512], lhsT=gTb[:, fc, :],
                                                                      rhs=w2s[s][:, fc // 2, (fc % 2) * 1024 + nh * 512:(fc % 2) * 1024 + (nh + 1) * 512],
                                                                      start=(fc == 0), stop=(fc == 3)))
                    P.group("pe", fns, reads=[b_gTb, b_w2s[s]], writes=[b_pO2])
                    P.op("act", lambda e: e.copy(out=ybt[y2][:, 0:512], in_=pO2[:, 0:512]), reads=[b_pO2], writes=[b_ybt[y2]])
                    P.op("dve", lambda e: e.tensor_copy(out=ybt[y2][:, 512:1024], in_=pO2[:, 512:1024]), reads=[b_pO2], writes=[b_ybt[y2]])
                    P.dma("act", lambda e: e.dma_start(out=YB[j * 128:(j + 1) * 128, :], in_=ybt[y2][:]), reads=[b_ybt[y2]])

                for j in range(3):
                    stG(j)
                hrow = [SB(st, "hrow%d" % i, [128, D], BF16) for i in range(3)]; b_hrow = [Buf() for _ in range(3)]

                def scat(i):
                    s = i % 3
                    P.dma("sp", lambda e: e.dma_start(out=hrow[s][:], in_=H2[i * 128:(i + 1) * 128, :]), writes=[b_hrow[s]])
                    for k in range(2):
                        P.dma("pool", lambda e, k=k: e.indirect_dma_start(out=XB[:, :], out_offset=bass.IndirectOffsetOnAxis(ap=dest[:, i, k:k + 1], axis=0),
                                                                          in_=hrow[s][:], in_offset=None),
                              reads=[b_hrow[s], b_dest])
                for i in range(32):
                    scat(i)
                P.barrier()
                for j in range(3):
                    stGX(j)
                stAT(0); stAT(1); stBM(0)
                for j in range(NBLK):
                    stCT(j)
                    if j + 1 < NBLK:
                        stBM(j + 1)
                    if j + 2 < NBLK:
                        stAT(j + 2)
                    stDM(j)
                    if j + 3 < NBLK:
                        stG(j + 3)
                        stGX(j + 3)
                P.barrier()
            if debug == "p4":
                return _finish(nc, P, out, gst)

            with ExitStack() as st:
                bc2 = SB(st, "bc2", [128, 3, D], F32); b_bc2 = Buf()
                for k, srcap in enumerate((mod_scr[0:1, 5120:6144], ln2g[0:1, :], ln2b[0:1, :])):
                    P.dma("sp", lambda e, k=k, srcap=srcap: e.dma_start(out=bc2[:, k, :], in_=srcap.partition_broadcast(128)), writes=[b_bc2])
                NR = 3
                ra = [SB(st, "ra%d" % i, [128, D], F32) for i in range(NR)]; rb = [SB(st, "rb%d" % i, [128, D], BF16) for i in range(NR)]
                rab = [SB(st, "rab%d" % i, [128, D], BF16) for i in range(NR)]
                b_ra = [Buf() for _ in range(NR)]; b_rb = [Buf() for _ in range(NR)]
                x1c = [SB(st, "x1c%d" % i, [128, D], F32) for i in range(NR)]; b_x1c = [Buf() for _ in range(NR)]
                oc_ = [SB(st, "oc%d" % i, [128, D], F32) for i in range(2)]; b_oc = [Buf(), Buf()]
                stats2 = SB(st, "stats2", [128, 2, 6], F32); mv2 = SB(st, "mv2", [128, 4], F32); b_stat2 = Buf()

                def comb_load(i):
                    s = i % NR
                    P.dma("pool", lambda e: e.indirect_dma_start(out=rab[s][:], out_offset=None, in_=YB[:, :],
                                                                 in_offset=bass.IndirectOffsetOnAxis(ap=dest[:, i, 0:1], axis=0)), reads=[b_dest], writes=[b_ra[s]])
                    P.dma("pool", lambda e: e.indirect_dma_start(out=rb[s][:], out_offset=None, in_=YB[:, :],
                                                                 in_offset=bass.IndirectOffsetOnAxis(ap=dest[:, i, 1:2], axis=0)), reads=[b_dest], writes=[b_rb[s]])
                    P.dma("sp", lambda e: e.dma_start(out=x1c[s][:], in_=X1[i * 128:(i + 1) * 128, :]), writes=[b_x1c[s]])

                def comb(i):
                    s = i % NR
                    so = i % 2
                    P.op("act", lambda e: e.activation(out=ra[s][:], in_=rab[s][:], func=AF.Identity, scale=wts[:, i, 0:1]), reads=[b_ra[s], b_wts], writes=[b_ra[s]])
                    P.op("dve", lambda e: e.scalar_tensor_tensor(out=ra[s][:], in0=rb[s][:], scalar=wts[:, i, 1:2], in1=ra[s][:], op0=ALU.mult, op1=ALU.add),
                         reads=[b_rb[s], b_ra[s], b_wts], writes=[b_ra[s]])
                    P.op("dve", lambda e: e.tensor_tensor(out=ra[s][:], in0=ra[s][:], in1=bc2[:, 0, :], op=ALU.mult), reads=[b_ra[s], b_bc2], writes=[b_ra[s]])
                    P.op("dve", lambda e: e.scalar_tensor_tensor(out=ra[s][:], in0=x1c[s][:], scalar=DN_ALPHA, in1=ra[s][:], op0=ALU.mult, op1=ALU.add),
                         reads=[b_x1c[s], b_ra[s]], writes=[b_ra[s]])
                    layer_norm((stats2, mv2, b_stat2), ra[s], b_ra[s], bc2[:, 1, :], bc2[:, 2, :], b_bc2, oc_[so][:], b_oc[so], eng2="dve")
                    P.dma("sp", lambda e: e.dma_start(out=out[i * 128:(i + 1) * 128, :], in_=oc_[so][:]), reads=[b_oc[so]])
                comb_load(0); comb_load(1)
                for i in range(32):
                    if i + 2 < 32:
                        comb_load(i + 2)
                    comb(i)
    return _finish(nc, P, out, gst)


def _finish(nc, P, out, gst):
    P.barrier()
    P.emit()
    return nc


def make_in_maps(inp):
    f32 = np.float32
    x = np.asarray(inp["x"], f32)
    c = np.asarray(inp["c"], f32)
    perm = q_perm()
    w_in = np.ascontiguousarray(np.asarray(inp["w_in"], f32)[0][:, perm])
    conv_w = np.asarray(inp["conv_w"], f32)[0]
    conv_b = np.asarray(inp["conv_b"], f32)[0]
    cwl = np.ascontiguousarray(conv_w.reshape(3, 12, 128).transpose(2, 0, 1))
    cbl = np.ascontiguousarray(conv_b.reshape(12, 128).T)

    def ewl(w, kchunks):
        E, K, N = w.shape
        return np.ascontiguousarray(w.reshape(E, 2, kchunks, 128, N).transpose(0, 1, 3, 2, 4).reshape(E * 2 * 128, kchunks * N))

    ew1 = ewl(np.asarray(inp["exp_w1"], f32)[0], 4)
    ew3 = ewl(np.asarray(inp["exp_w3"], f32)[0], 4)
    ew2 = ewl(np.asarray(inp["exp_w2"], f32)[0], 2)
    wrr = np.ascontiguousarray(np.concatenate([np.asarray(inp["router_group_w"], f32)[0], np.asarray(inp["router_expert_w"], f32)[0]], axis=1))
    brr = np.ascontiguousarray(np.concatenate([np.asarray(inp["router_group_b"], f32)[0], np.asarray(inp["router_expert_b"], f32)[0]])[None, :])
    shared = {
        "w_ada": np.asarray(inp["w_ada"], f32)[0], "b_ada": np.asarray(inp["b_ada"], f32)[0][None, :],
        "w_in": w_in, "conv_w": cwl, "conv_b": cbl,
        "fw1": np.asarray(inp["filt_w1"], f32)[0], "fb1": np.asarray(inp["filt_b1"], f32)[0][:, None],
        "ff1": np.asarray(inp["filt_freq1"], f32)[0][:, None],
        "fw2": np.asarray(inp["filt_w2"], f32)[0], "fb2": np.asarray(inp["filt_b2"], f32)[0][:, None],
        "ff2": np.asarray(inp["filt_freq2"], f32)[0][:, None],
        "fw3": np.asarray(inp["filt_w3"], f32)[0], "fdec": np.asarray(inp["filt_decay"], f32)[0][None, :],
        "hskip": np.asarray(inp["hy_skip"], f32)[0].reshape(1, 1024),
        "w_hy_o": np.asarray(inp["w_hy_o"], f32)[0], "w_attn_o": np.asarray(inp["w_attn_o"], f32)[0],
        "sink": np.asarray(inp["attn_sink"], f32)[0][None, :], "w_out": np.asarray(inp["w_out"], f32)[0],
        "ln1g": np.asarray(inp["ln1_g"], f32)[0][None, :], "ln1b": np.asarray(inp["ln1_b"], f32)[0][None, :],
        "ln2g": np.asarray(inp["ln2_g"], f32)[0][None, :], "ln2b": np.asarray(inp["ln2_b"], f32)[0][None, :],
        "wr": wrr, "br": brr, "ew1": ew1, "ew3": ew3, "ew2": ew2,
    }
    bnd = np.zeros((33, 4), f32)
    bv = np.linspace(1e-4, 15.0, 16, dtype=f32)
    bnd[0, 0] = 0.0; bnd[0, 1] = 0.5
    bnd[1:17, 0] = bv / L; bnd[1:17, 1] = 0.25 + 0.5
    bnd[17:33, 0] = bv / L; bnd[17:33, 1] = 0.5 + 0.5
    shared["bands"] = bnd
    shared = {k: np.ascontiguousarray(v) for k, v in shared.items()}
    consts = [host_constants(0), host_constants(1)]
    maps = []
    for i in range(NCORES):
        b, half = i // 2, i % 2
        own = x[b, half * TOWN:(half + 1) * TOWN]
        oth = x[b, (1 - half) * TOWN:(2 - half) * TOWN]
        m = dict(shared)
        m["xcat"] = np.ascontiguousarray(np.concatenate([own, oth], axis=0))
        m["cb"] = np.ascontiguousarray(c[b].reshape(8, 128).T)
        m["tabs_b"], m["tabs_f"] = consts[half]
        maps.append(m)
    return maps


_NC_CACHE = {}


def kernel(**inputs):
    if "nc" not in _NC_CACHE:
        _NC_CACHE["nc"] = build_program()
    nc = _NC_CACHE["nc"]
    maps = make_in_maps(inputs)
    res = run_bass_kernel_spmd(nc, maps, core_ids=list(range(NCORES)))
    outp = np.zeros((4, SEQ, D), np.float32)
    for i in range(NCORES):
        b, half = i // 2, i % 2
        outp[b, half * TOWN:(half + 1) * TOWN] = res.results[i]["out"]
    return outp
```

```python
from contextlib import ExitStack
import math
import numpy as np
import ml_dtypes
import concourse.bass as bass
import concourse.mybir as mybir
from concourse.bass_utils import run_bass_kernel_spmd

F32 = mybir.dt.float32
BF16 = mybir.dt.bfloat16
I32 = mybir.dt.int32
U32 = mybir.dt.uint32
AF = mybir.ActivationFunctionType
ALU = mybir.AluOpType
AX = mybir.AxisListType

NCORES = 8
D = 1024
SEQ = 8192
TOWN = 4096
L = 8192
NFFT = 16384
DN_ALPHA = 2.0 ** 0.25
LN_EPS = 1e-5
DBG_NSG = 2
NBLK = 128
PROWS = NBLK * 128


class Buf:
    __slots__ = ("w", "r", "name")

    def __init__(self, name=""):
        self.w = None
        self.r = {}
        self.name = name


class _Eng:
    def __init__(self, name, hname, sem, self_sync):
        self.name = name
        self.hname = hname
        self.sem = sem
        self.cnt = 0
        self.known = {}
        self.ops = []
        self.self_sync = self_sync


class Prog:
    ENG = {"pe": "tensor", "act": "scalar", "dve": "vector", "pool": "gpsimd", "sp": "sync"}

    def __init__(self, nc, stack, n_dma_sems=8):
        self.nc = nc
        self.sems = {}
        self.engs = {}
        for k, h in self.ENG.items():
            self.sems["e_" + k] = stack.enter_context(nc.semaphore("s_" + k))
            self.engs[k] = _Eng(k, h, "e_" + k, self_sync=(k in ("act", "dve", "pool")))
        self.dq = {}
        for q in ("sp", "act", "pool"):
            lst = []
            for i in range(n_dma_sems):
                key = "d_%s%d" % (q, i)
                self.sems[key] = stack.enter_context(nc.semaphore(key))
                lst.append([key, 0])
            self.dq[q] = [lst, 0]

    def _collect(self, E, reads, writes):
        waits = {}

        def need(tok):
            if tok is None:
                return
            k, v = tok
            if waits.get(k, 0) < v:
                waits[k] = v

        for b in reads:
            need(b.w)
        for b in writes:
            need(b.w)
            for k, v in b.r.items():
                need((k, v))
        wl = []
        for k, v in waits.items():
            if k == E.sem and not E.self_sync:
                continue
            if E.known.get(k, 0) >= v:
                continue
            E.known[k] = v
            wl.append((k, v))
        return wl

    def _commit(self, tok, reads, writes):
        k, v = tok
        for b in reads:
            if b.r.get(k, 0) < v:
                b.r[k] = v
        for b in writes:
            b.w = tok
            b.r = {}

    def op(self, eng, fn, reads=(), writes=()):
        E = self.engs[eng]
        wl = self._collect(E, reads, writes)
        E.cnt += 1
        tok = (E.sem, E.cnt)
        E.ops.append((wl, fn, (E.sem, 1)))
        self._commit(tok, reads, writes)
        return tok

    def group(self, eng, fns, reads=(), writes=()):
        E = self.engs[eng]
        wl = self._collect(E, reads, writes)
        E.cnt += 1
        tok = (E.sem, E.cnt)
        n = len(fns)
        for i, fn in enumerate(fns):
            E.ops.append((wl if i == 0 else [], fn, (E.sem, 1) if i == n - 1 else None))
        self._commit(tok, reads, writes)
        return tok

    def dma(self, q, fn, reads=(), writes=()):
        E = self.engs[q]
        lst, idx = self.dq[q]
        slot = lst[idx % len(lst)]
        self.dq[q][1] = idx + 1
        key, val = slot
        wl = self._collect(E, reads, writes)
        if val > 0 and E.known.get(key, 0) < val:
            E.known[key] = val
            wl.append((key, val))
        val += 16
        slot[1] = val
        tok = (key, val)
        E.ops.append((wl, fn, (key, 16)))
        self._commit(tok, reads, writes)
        return tok

    def _all_tokens(self):
        toks = []
        for E in self.engs.values():
            if E.cnt:
                toks.append((E.sem, E.cnt))
        for q, (lst, _) in self.dq.items():
            for key, val in lst:
                if val:
                    toks.append((key, val))
        return toks

    def barrier(self):
        toks = self._all_tokens()
        for E in self.engs.values():
            wl = []
            for k, v in toks:
                if E.known.get(k, 0) >= v:
                    continue
                E.known[k] = v
                wl.append((k, v))
            if wl:
                E.ops.append((wl, None, None))

    def emit(self):
        nc = self.nc
        sems = self.sems
        with nc.Block() as block:
            for k, E in self.engs.items():
                def body(h, E=E):
                    for wl, fn, inc in E.ops:
                        for sk, v in wl:
                            h.wait_ge(sems[sk], v)
                        if fn is None:
                            continue
                        ins = fn(h)
                        if inc is not None:
                            ins.then_inc(sems[inc[0]], inc[1])
                getattr(block, E.hname)(body)


def _bf(a):
    return np.ascontiguousarray(a.astype(ml_dtypes.bfloat16))


def host_constants(half):
    a = np.arange(128)
    ang = 2.0 * np.pi * np.outer(a, a) / 128.0
    Fr = np.cos(ang)
    Fi = -np.sin(ang)
    rowpos = np.concatenate([np.arange(32), (32 if half == 0 else 96) + np.arange(32)])
    tb = np.zeros((128, NTB), np.float64)
    tb[:, 0:128] = Fr
    tb[:, 128:256] = Fi
    tb[0:64, 256:384] = Fr[rowpos, :]
    tb[0:64, 384:512] = Fi[rowpos, :]
    tb[:, 512:640] = Fr
    tb[:, 640:768] = Fi
    tb[:, 768:896] = -Fi
    tb[:, 896:1024] = Fr
    tb[:, 1024:1152] = -Fi
    tb[:, 1152:1280] = Fi
    tb[:, 1280:1408] = Fr
    tb[:, 1408:1472] = Fr[:, rowpos] / NFFT
    tb[:, 1472:1536] = Fi[:, rowpos] / NFFT
    tb[:, 1536:1664] = np.eye(128)
    tb[:, 1664:1792] = (a[:, None] < a[None, :]).astype(np.float64)
    tb[:, 1792:1920] = 1.0
    j = a[:, None]
    q = a[None, :]
    for g in range(2):
        for ty, off in enumerate((-128, 0, 128)):
            rel = j + off - q
            val = (np.abs(rel) <= 128)
            blk = np.zeros((128, 4, 128))
            for hh in range(4):
                h = g * 4 + hh
                slope = 2.0 ** (-8.0 * (h + 1) / 8.0)
                blk[:, hh, :] = np.exp(-slope * np.abs(rel)) * val
            c0 = 1920 + (g * 3 + ty) * 512
            tb[:, c0:c0 + 512] = blk.reshape(128, 512)
    tw = 2.0 * np.pi * np.outer(a, a) / NFFT
    tb[:, CT_TC1:CT_TC1 + 128] = np.cos(tw); tb[:, CT_TC1 + 128:CT_TC1 + 256] = np.cos(tw)
    tb[:, CT_TC2:CT_TC2 + 128] = -np.sin(tw); tb[:, CT_TC2 + 128:CT_TC2 + 256] = -np.sin(tw)
    tb[:, CT_F2NR:CT_F2NR + 128] = -Fr
    tb[:, CT_G2NA:CT_G2NA + 128] = -Fr
    tb[:, CT_G2NA + 128:CT_G2NA + 256] = Fi
    tb[:, CT_G1NI:CT_G1NI + 64] = -Fi[:, rowpos] / NFFT
    tf = np.zeros((128, NTF), np.float32)
    tf[:, 0:128] = np.cos(tw)
    tf[:, 128:256] = -np.sin(tw)
    tf[:, 256:384] = np.eye(128)
    tf[:, 384] = a
    tf[:, 385] = 1.0 - half
    tf[:, 386] = float(half)
    tf[:, 387] = float(half)
    tf[:, 388] = 1.0 - half
    tf[:, 400:528] = 128.0 * a[None, :]
    cwn = 1.0 / (L - 1)
    tf[:64, 389] = -cwn
    tf[64:, 389] = cwn
    tf[:64, 390] = 128.0 * a[:64]
    tf[64:, 390] = -(8192.0 - 128.0 * (a[64:] - 64))
    tf[:, 528:656] = a[None, :]
    tf[:, 656:784] = 1.0
    return _bf(tb), tf


CT_F1FULL, CT_F1C, CT_F2R, CT_F2I, CT_F2NI, CT_G2A, CT_G2B, CT_G1R, CT_G1I = 0, 256, 512, 640, 768, 896, 1152, 1408, 1472
CT_ID, CT_TRI, CT_ONES, CT_EM = 1536, 1664, 1792, 1920
CT_TC1 = 1920 + 6 * 512
CT_TC2 = CT_TC1 + 256
CT_F2NR = CT_TC2 + 256
CT_G2NA = CT_F2NR + 128
CT_G1NI = CT_G2NA + 256
NTB = CT_G1NI + 64
NTF = 784


def q_perm():
    cols = []
    for c in range(4):
        cols += list(range(c * 64, c * 64 + 64))
        cols += list(range((4 + c) * 64, (4 + c) * 64 + 64))
    return np.array(cols + list(range(512, 4352)))


def build_program(debug=None, lite=False):
    nc = bass.Bass("TRN2", target_bir_lowering=False)
    dbg = debug is not None

    def din(name, shape, dt=F32):
        return nc.dram_tensor(name, list(shape), dt, kind="ExternalInput").ap()

    DBG_OUT = {"p0": ["mod_scr"], "p1a": ["mod_scr", "Uc", "Gs", "dbgq", "dbgk", "dbgv"], "p1b": ["AT"],
               "p2": ["AT", "Z2", "dbgkc", "dbgz1", "dbgkh"], "p3": ["X1", "H2", "dbgrt"], "p4": ["XB", "YB", "dbgrt"]}

    def dscr(name, shape, dt=F32):
        isout = dbg and name in DBG_OUT.get(debug, [])
        return nc.dram_tensor(name, list(shape), dt, kind=("ExternalOutput" if isout else "Internal")).ap()

    xcat = din("xcat", [SEQ, D])
    cb = din("cb", [128, 8])
    w_ada = din("w_ada", [D, 6 * D])
    b_ada = din("b_ada", [1, 6 * D])
    w_in = din("w_in", [D, 4352])
    conv_w = din("conv_w", [128, 3, 12])
    conv_b = din("conv_b", [128, 12])
    fw1 = din("fw1", [33, 64]); fb1 = din("fb1", [64, 1]); ff1 = din("ff1", [64, 1])
    fw2 = din("fw2", [64, 64]); fb2 = din("fb2", [64, 1]); ff2 = din("ff2", [64, 1])
    fw3 = din("fw3", [64, 2048]); fdec = din("fdec", [1, 2048])
    hskip = din("hskip", [1, 1024])
    w_hy_o = din("w_hy_o", [512, D]); w_attn_o = din("w_attn_o", [512, D])
    sink = din("sink", [1, 8])
    w_out = din("w_out", [D, D])
    ln1g = din("ln1g", [1, D]); ln1b = din("ln1b", [1, D]); ln2g = din("ln2g", [1, D]); ln2b = din("ln2b", [1, D])
    wr = din("wr", [D, 72]); br = din("br", [1, 72])
    ne = 256 if lite else 64 * 2 * 128
    ew1 = din("ew1", [ne, 2048]); ew3 = din("ew3", [ne, 2048]); ew2 = din("ew2", [ne, 2048])
    tabs_b = din("tabs_b", [128, NTB], BF16)
    tabs_f = din("tabs_f", [128, NTF])
    bands = din("bands", [33, 4])
    out = nc.dram_tensor("out", [TOWN, D], F32, kind="ExternalOutput").ap()

    mod_scr = dscr("mod_scr", [1, 6 * D])
    Uc = dscr("Uc", [1536, SEQ])
    Gs = dscr("Gs", [2048, TOWN], BF16)
    AT = dscr("AT", [512, TOWN], BF16)
    Z2 = dscr("Z2", [512, TOWN], BF16)
    X1 = dscr("X1", [TOWN, D])
    H2 = dscr("H2", [TOWN, D], BF16)
    XB = dscr("XB", [PROWS, D], BF16)
    YB = dscr("YB", [PROWS, D], BF16)
    EWB = [dscr("EWB%d" % i, [ne // 2, 4096], BF16) for i in range(3)]
    dbg1a = (debug == "p1a")
    dbgq = dscr("dbgq", [128, 4, TOWN], BF16) if dbg1a else None
    dbgk = dscr("dbgk", [128, 34 * 128], BF16) if dbg1a else None
    dbgv = dscr("dbgv", [128, 34, 2, 65], BF16) if dbg1a else None
    dbgkc = dscr("dbgkc", [128, 32, 128], BF16) if debug == "p2" else None
    dbgz1 = dscr("dbgz1", [64, 16, 128], BF16) if debug == "p2" else None
    dbgkh = dscr("dbgkh", [128, 2, 32, 128], BF16) if debug == "p2" else None

    with ExitStack() as gst:
        P = Prog(nc, gst)

        def SB(st, name, shape, dt):
            return st.enter_context(nc.sbuf_tensor(name, list(shape), dt))

        def PS(st, name, shape, dt=F32):
            return st.enter_context(nc.psum_tensor(name, list(shape), dt))

        tb = SB(gst, "tb", [128, NTB], BF16); b_tb = Buf("tb")
        tf = SB(gst, "tf", [128, NTF], F32); b_tf = Buf("tf")
        P.dma("sp", lambda e: e.dma_start(out=tb[:], in_=tabs_b[:, :]), writes=[b_tb])
        P.dma("sp", lambda e: e.dma_start(out=tf[:], in_=tabs_f[:, :]), writes=[b_tf])
        identf = tf[:, 256:384]
        identb = tb[:, CT_ID:CT_ID + 128]
        mA, mB, nmA, nmB = tf[:, 385:386], tf[:, 386:387], tf[:, 387:388], tf[:, 388:389]
        m1 = SB(gst, "m1", [128, 16], F32); b_m1 = Buf()

        with ExitStack() as st:
            cbt = SB(st, "cbt", [128, 8], F32); b_cbt = Buf()
            sig = SB(st, "sig", [128, 8], F32)
            modrow = SB(st, "modrow", [1, 6 * D], F32); b_mod = Buf()
            badar = SB(st, "badar", [1, 6 * D], F32); b_bada = Buf()
            wa = [SB(st, "wa%d" % i, [128, 8, 512], BF16) for i in range(2)]
            cbb = SB(st, "cbb", [128, 8], BF16)
            b_wa = [Buf(), Buf()]
            pm = [PS(st, "pm%d" % i, [1, 512]) for i in range(2)]
            b_pm = [Buf(), Buf()]
            P.dma("act", lambda e: e.dma_start(out=cbt[:], in_=cb[:, :]), writes=[b_cbt])
            P.dma("act", lambda e: e.dma_start(out=badar[:], in_=b_ada[:, :]), writes=[b_bada])
            P.op("act", lambda e: e.activation(out=sig[:], in_=cbt[:], func=AF.Sigmoid), reads=[b_cbt], writes=[b_cbt])
            P.op("dve", lambda e: e.tensor_tensor(out=cbt[:], in0=cbt[:], in1=sig[:], op=ALU.mult), reads=[b_cbt], writes=[b_cbt])
            P.op("dve", lambda e: e.tensor_copy(out=cbb[:], in_=cbt[:]), reads=[b_cbt], writes=[b_cbt])
            for blk in range(12):
                s = blk % 2
                P.dma("pool", lambda e, s=s, blk=blk: e.dma_start(
                    out=wa[s][:], in_=w_ada[:, blk * 512:(blk + 1) * 512].rearrange("(c p) n -> p c n", p=128)),
                    writes=[b_wa[s]])
                P.group("pe", [lambda e, s=s, kc=kc: e.matmul(pm[s][:], lhsT=cbb[:, kc:kc + 1], rhs=wa[s][:, kc, :],
                                                               start=(kc == 0), stop=(kc == 7)) for kc in range(8)],
                        reads=[b_cbt, b_wa[s]], writes=[b_pm[s]])
                P.op("dve", lambda e, s=s, blk=blk: e.tensor_tensor(out=modrow[0:1, blk * 512:(blk + 1) * 512], in0=pm[s][:],
                                                                   in1=badar[0:1, blk * 512:(blk + 1) * 512], op=ALU.add),
                     reads=[b_pm[s], b_bada], writes=[b_mod])
            for off in (1024, 4096):
                P.op("dve", lambda e, off=off: e.tensor_scalar(out=modrow[0:1, off:off + 1024], in0=modrow[0:1, off:off + 1024],
                                                               scalar1=1.0, scalar2=None, op0=ALU.add), reads=[b_mod], writes=[b_mod])
            b_modscr = Buf()
            P.dma("sp", lambda e: e.dma_start(out=mod_scr[:, :], in_=modrow[:]), reads=[b_mod], writes=[b_modscr])
            pTm = PS(st, "pTm", [128, 16]); b_pTm = Buf()
            P.group("pe", [lambda e, c=c: e.transpose(out=pTm[:, c:c + 1], in_=modrow[0:1, c * 128:(c + 1) * 128],
                                                      identity=identf[0:1, 0:1]) for c in range(16)],
                    reads=[b_mod, b_tf], writes=[b_pTm])
            P.op("dve", lambda e: e.tensor_copy(out=m1[:], in_=pTm[:]), reads=[b_pTm], writes=[b_m1])
            P.barrier()

        if debug == "p0":
            return _finish(nc, P, out, gst)

        with ExitStack() as st1:
            qT = SB(st1, "qT", [128, 4, TOWN], BF16); b_qT = Buf()
            kT = SB(st1, "kT", [128, 34 * 128], BF16); b_kT = Buf()
            Va = SB(st1, "Va", [128, 34, 2, 65], BF16); b_Va = Buf()
            P.op("pool", lambda e: e.memset(Va[:], 1.0), writes=[b_Va])
            with ExitStack() as st:
                win = SB(st, "win", [128, 8, 4352], BF16); b_win = Buf()
                for kc in range(8):
                    for (c0, c1) in ((0, 2048), (2048, 4096), (4096, 4352)):
                        P.dma("pool", lambda e, kc=kc, c0=c0, c1=c1: e.dma_start(
                            out=win[:, kc, c0:c1], in_=w_in[kc * 128:(kc + 1) * 128, c0:c1]), writes=[b_win])
                cw = SB(st, "cw", [128, 3, 12], F32); cbias = SB(st, "cbias", [128, 12], F32); b_cw = Buf()
                P.dma("act", lambda e: e.dma_start(out=cw[:], in_=conv_w[:, :, :]), writes=[b_cw])
                P.dma("act", lambda e: e.dma_start(out=cbias[:], in_=conv_b[:, :]), writes=[b_cw])
                NXS = 3
                xs = [SB(st, "xs%d" % i, [128, D], F32) for i in range(NXS)]; b_xs = [Buf() for _ in range(NXS)]
                hT = [SB(st, "hT%d" % i, [128, 8, 512], BF16) for i in range(2)]; b_hT = [Buf(), Buf()]
                carry = SB(st, "carry", [128, 12, 2], F32); b_carry = Buf()
                NUB = 3
                ub = [SB(st, "ub%d" % i, [128, 514], F32) for i in range(NUB)]; b_ub = [Buf() for _ in range(NUB)]
                ua = [SB(st, "ua%d" % i, [128, 512], F32) for i in range(NUB)]; b_ua = [Buf() for _ in range(NUB)]
                gsb = [SB(st, "gsb%d" % i, [128, 512], BF16) for i in range(2)]; b_gsb = [Buf(), Buf()]
                fix = SB(st, "fix", [128, 2], F32); b_fix = Buf()
                pT = [PS(st, "pT%d" % i, [128, 8, 128]) for i in range(2)]; b_pT = [Buf(), Buf()]
                pO = [PS(st, "pO%d" % i, [128, 512]) for i in range(3)]; b_pO = [Buf() for _ in range(3)]
                pV = PS(st, "pV", [128, 128]); b_pV = Buf()
                cnt = {"x": 0, "pT": 0, "pO": 0, "ub": 0, "g": 0}
                b_Uc = Buf(); b_Gs = Buf()

                def load_transpose(tile_idx, hslot, col0, ncols=128):
                    xi = cnt["x"] % NXS; cnt["x"] += 1
                    P.dma("sp", lambda e: e.dma_start(out=xs[xi][:], in_=xcat[tile_idx * 128:(tile_idx + 1) * 128, :]),
                          writes=[b_xs[xi]])
                    pi = cnt["pT"] % 2; cnt["pT"] += 1
                    P.group("pe", [lambda e, kc=kc: e.transpose(out=pT[pi][:, kc, :], in_=xs[xi][:, kc * 128:(kc + 1) * 128],
                                                               identity=identf) for kc in range(8)],
                            reads=[b_xs[xi], b_tf], writes=[b_pT[pi]])
                    for kc in range(8):
                        P.op("act", lambda e, kc=kc: e.activation(out=hT[hslot][:, kc, col0:col0 + 128], in_=pT[pi][:, kc, :],
                                                                 func=AF.Identity, scale=m1[:, 8 + kc:9 + kc], bias=m1[:, kc:kc + 1]),
                             reads=[b_pT[pi], b_m1], writes=[b_hT[hslot]])

                def proj_chunk(hslot, oc, ncols=512):
                    pi = cnt["pO"] % 3; cnt["pO"] += 1
                    P.group("pe", [lambda e, kc=kc: e.matmul(pO[pi][:, 0:ncols], lhsT=win[:, kc, oc * 128:(oc + 1) * 128],
                                                             rhs=hT[hslot][:, kc, 0:ncols], start=(kc == 0), stop=(kc == 7))
                                   for kc in range(8)],
                            reads=[b_win, b_hT[hslot]], writes=[b_pO[pi]])
                    return pi

                def proj_v(hslot, t, vtile):
                    P.group("pe", [lambda e, kc=kc: e.matmul(pV[:], lhsT=hT[hslot][:, kc, t * 128:(t + 1) * 128],
                                                             rhs=win[:, kc, 640:768], start=(kc == 0), stop=(kc == 7))
                                   for kc in range(8)],
                            reads=[b_win, b_hT[hslot]], writes=[b_pV])
                    P.op("dve", lambda e: e.tensor_copy(out=Va[:, vtile, :, 0:64], in_=pV[:].rearrange("p (g d) -> p g d", g=2)),
                         reads=[b_pV], writes=[b_Va])

                load_transpose(63, 0, 0)
                for j in range(12):
                    pi = proj_chunk(0, 6 + j, ncols=128)
                    P.op("act", lambda e, j=j, pi=pi: e.copy(out=carry[:, j, :], in_=pO[pi][:, 126:128]),
                         reads=[b_pO[pi]], writes=[b_carry])

                def do_supertile(st_i):
                    hs = st_i % 2
                    own = st_i < 8
                    if st_i == 0:
                        for t in range(4):
                            load_transpose(st_i * 4 + t, hs, t * 128)
                    chunks = list(range(0, 5)) + list(range(6, 34)) if own else list(range(6, 18))
                    if st_i in (8, 15):
                        chunks = [4] + chunks
                    pre_at = {}
                    if st_i + 1 < 16:
                        step = max(1, (len(chunks) - 2) // 4)
                        for t in range(4):
                            pre_at[1 + t * step] = t
                    for ci_, oc in enumerate(chunks):
                        if ci_ in pre_at:
                            load_transpose((st_i + 1) * 4 + pre_at[ci_], 1 - hs, pre_at[ci_] * 128)
                        pi = proj_chunk(hs, oc)
                        if oc < 4:
                            P.op("dve", lambda e, oc=oc, pi=pi: e.tensor_copy(out=qT[:, oc, st_i * 512:(st_i + 1) * 512], in_=pO[pi][:]),
                                 reads=[b_pO[pi]], writes=[b_qT])
                        elif oc == 4:
                            if own:
                                P.op("dve", lambda e, pi=pi: e.tensor_copy(out=kT[:, 128 + st_i * 512:128 + (st_i + 1) * 512], in_=pO[pi][:]),
                                     reads=[b_pO[pi]], writes=[b_kT])
                            elif st_i == 8:
                                P.op("dve", lambda e, pi=pi: e.tensor_copy(out=kT[:, 33 * 128:34 * 128], in_=pO[pi][:, 0:128]),
                                     reads=[b_pO[pi]], writes=[b_kT])
                            else:
                                P.op("dve", lambda e, pi=pi: e.tensor_copy(out=kT[:, 0:128], in_=pO[pi][:, 384:512]),
                                     reads=[b_pO[pi]], writes=[b_kT])
                        elif oc < 18:
                            j = oc - 6
                            ui = cnt["ub"] % NUB; cnt["ub"] += 1
                            P.op("act", lambda e, pi=pi, ui=ui: e.copy(out=ub[ui][:, 2:514], in_=pO[pi][:]),
                                 reads=[b_pO[pi]], writes=[b_ub[ui]])
                            P.op("act", lambda e, j=j, ui=ui: e.copy(out=ub[ui][:, 0:2], in_=carry[:, j, :]),
                                 reads=[b_carry], writes=[b_ub[ui]])
                            P.op("act", lambda e, j=j, ui=ui: e.copy(out=carry[:, j, :], in_=ub[ui][:, 512:514]),
                                 reads=[b_ub[ui]], writes=[b_carry])
                            P.op("act", lambda e, j=j, ui=ui: e.activation(out=ua[ui][:], in_=ub[ui][:, 1:513], func=AF.Identity,
                                                                          scale=cw[:, 1, j:j + 1], bias=cbias[:, j:j + 1]),
                                 reads=[b_ub[ui], b_cw], writes=[b_ua[ui]])
                            P.op("dve", lambda e, j=j, ui=ui: e.scalar_tensor_tensor(out=ua[ui][:], in0=ub[ui][:, 0:512], scalar=cw[:, 0, j:j + 1],
                                                                                    in1=ua[ui][:], op0=ALU.mult, op1=ALU.add),
                                 reads=[b_ub[ui], b_cw], writes=[b_ua[ui]])
                            P.op("dve", lambda e, j=j, ui=ui: e.scalar_tensor_tensor(out=ua[ui][:], in0=ub[ui][:, 2:514], scalar=cw[:, 2, j:j + 1],
                                                                                    in1=ua[ui][:], op0=ALU.mult, op1=ALU.add),
                                 reads=[b_ub[ui], b_cw], writes=[b_ua[ui]])
                            if st_i in (0, 8):
                                nm = nmB if st_i == 0 else nmA
                                P.op("dve", lambda e, j=j, ui=ui, nm=nm: e.tensor_scalar(out=fix[:, 0:1], in0=ub[ui][:, 2:3], scalar1=cw[:, 2, j:j + 1],
                                                                                        scalar2=nm, op0=ALU.mult, op1=ALU.mult),
                                     reads=[b_ub[ui], b_cw, b_tf], writes=[b_fix])
                                P.op("dve", lambda e, j=j, ui=ui, nm=nm: e.tensor_scalar(out=fix[:, 1:2], in0=ub[ui][:, 1:2], scalar1=cw[:, 0, j:j + 1],
                                                                                        scalar2=nm, op0=ALU.mult, op1=ALU.mult),
                                     reads=[b_ub[ui], b_cw, b_tf], writes=[b_fix])
                                P.op("dve", lambda e, ui=ui: e.tensor_tensor(out=ua[ui][:, 0:2], in0=ua[ui][:, 0:2], in1=fix[:, 0:2], op=ALU.subtract),
                                     reads=[b_fix], writes=[b_ua[ui]])
                            s0 = st_i * 512
                            r0 = j * 128
                            P.dma("pool", lambda e, ui=ui, r0=r0, s0=s0: e.dma_start(out=Uc[r0:r0 + 128, (s0 - 1) % SEQ:(s0 - 1) % SEQ + 1], in_=ua[ui][:, 0:1], allow_slow_non_contiguous=True),
                                  reads=[b_ua[ui]])
                            P.dma("pool", lambda e, ui=ui, r0=r0, s0=s0: e.dma_start(out=Uc[r0:r0 + 128, s0:s0 + 511], in_=ua[ui][:, 1:512]),
                                  reads=[b_ua[ui]])
                        else:
                            gi = cnt["g"] % 2; cnt["g"] += 1
                            P.op("act", lambda e, pi=pi, gi=gi: e.activation(out=gsb[gi][:], in_=pO[pi][:], func=AF.Sigmoid),
                                 reads=[b_pO[pi]], writes=[b_gsb[gi]])
                            r0 = (oc - 18) * 128
                            P.dma("pool", lambda e, gi=gi, r0=r0: e.dma_start(out=Gs[r0:r0 + 128, st_i * 512:(st_i + 1) * 512], in_=gsb[gi][:]),
                                  reads=[b_gsb[gi]])
                    if own:
                        for t in range(4):
                            proj_v(hs, t, 1 + st_i * 4 + t)
                    elif st_i == 8:
                        proj_v(hs, 0, 33)
                    elif st_i == 15:
                        proj_v(hs, 3, 0)
                for st_i in range(16):
                    do_supertile(st_i)
                if dbg1a:
                    P.dma("sp", lambda e: e.dma_start(out=dbgq[:, :, :], in_=qT[:]), reads=[b_qT])
                    P.dma("sp", lambda e: e.dma_start(out=dbgk[:, :], in_=kT[:]), reads=[b_kT])
                    P.dma("sp", lambda e: e.dma_start(out=dbgv[:, :, :, :], in_=Va[:]), reads=[b_Va])
                P.barrier()
            if debug == "p1a":
                return _finish(nc, P, out, gst)

            with ExitStack() as st:
                esink = SB(st, "esink", [128, 8], F32); b_es = Buf()
                P.dma("sp", lambda e: e.dma_start(out=esink[:], in_=sink[0:1, :].partition_broadcast(128)), writes=[b_es])
                P.op("act", lambda e: e.activation(out=esink[:], in_=esink[:], func=AF.Exp), reads=[b_es], writes=[b_es])
                emJ = SB(st, "emJ", [128, 2, 2, 512], BF16); b_emJ = Buf()
                for g in range(2):
                    P.op("dve", lambda e, g=g: e.tensor_scalar(out=emJ[:, g, 0, :], in0=tb[:, CT_EM + (g * 3 + 0) * 512:CT_EM + (g * 3 + 1) * 512],
                                                               scalar1=mB, scalar2=None, op0=ALU.mult), reads=[b_tb, b_tf], writes=[b_emJ])
                    P.op("dve", lambda e, g=g: e.tensor_scalar(out=emJ[:, g, 1, :], in0=tb[:, CT_EM + (g * 3 + 2) * 512:CT_EM + (g * 3 + 3) * 512],
                                                               scalar1=mA, scalar2=None, op0=ALU.mult), reads=[b_tb, b_tf], writes=[b_emJ])
                pS = [PS(st, "pS%d" % i, [128, 512]) for i in range(3)]; b_pS = [Buf() for _ in range(3)]
                pPVb = [[PS(st, "pPV%d_%d" % (s, g), [128, 512]) for g in range(2)] for s in range(2)]
                pPV = [[pPVb[s][g][:, 0:260].rearrange("p (h d) -> p h d", h=4) for g in range(2)] for s in range(2)]
                b_pPV = [[Buf(), Buf()], [Buf(), Buf()]]
                pTab = PS(st, "pTa", [128, 1024], BF16); b_pTa = Buf()
                pTa = pTab[:, 0:512].rearrange("p (c q) -> p c q", c=4)
                pex = [SB(st, "pex%d" % i, [128, 512], BF16) for i in range(3)]; b_pex = [Buf() for _ in range(3)]
                pmk = [[[SB(st, "pmk%d_%d_%d" % (s, g, kb), [128, 512], BF16) for kb in range(3)] for g in range(2)] for s in range(2)]
                b_pmk = [[[Buf() for kb in range(3)] for g in range(2)] for s in range(2)]
                den = SB(st, "den", [128, 2, 4], F32); b_den = Buf()
                att = [SB(st, "att%d" % i, [128, 8, 64], BF16) for i in range(2)]; b_att = [Buf(), Buf()]
                atT = [SB(st, "atT%d" % i, [128, 4, 128], BF16) for i in range(2)]; b_atT = [Buf(), Buf()]
                cn = {"s": 0}

                def attn_part1(i):
                    s2 = i % 2
                    for g in range(2):
                        for kb in range(3):
                            si = cn["s"] % 3; cn["s"] += 1
                            P.group("pe", [lambda e, g=g, kb=kb, si=si: e.matmul(pS[si][:].rearrange("p (h q) -> p h q", h=4),
                                                              lhsT=kT[64 * g:64 * g + 64, (i + kb) * 128:(i + kb + 1) * 128],
                                                              rhs=qT[64 * g:64 * g + 64, :, i * 128:(i + 1) * 128], start=True, stop=True)],
                                    reads=[b_kT, b_qT], writes=[b_pS[si]])
                            P.op("act", lambda e, si=si: e.activation(out=pex[si][:], in_=pS[si][:], func=AF.Exp, scale=0.125),
                                 reads=[b_pS[si]], writes=[b_pex[si]])
                            if kb == 0 and i == 0:
                                em = emJ[:, g, 0, :]
                            elif kb == 2 and i == 31:
                                em = emJ[:, g, 1, :]
                            else:
                                em = tb[:, CT_EM + (g * 3 + kb) * 512:CT_EM + (g * 3 + kb + 1) * 512]
                            eng = "dve"
                            P.op(eng, lambda e, em=em, g=g, kb=kb, si=si: e.tensor_tensor(out=pmk[s2][g][kb][:], in0=pex[si][:], in1=em, op=ALU.mult),
                                 reads=[b_pex[si], b_tb, b_emJ], writes=[b_pmk[s2][g][kb]])
                def attn_part2(i):
                    s2 = i % 2
                    for g in range(2):
                        fns = []
                        for hh in range(4):
                            for kb in range(3):
                                fns.append(lambda e, hh=hh, kb=kb, g=g: e.matmul(pPV[s2][g][:, hh, :], lhsT=pmk[s2][g][kb][:, hh * 128:(hh + 1) * 128],
                                                                          rhs=Va[:, i + kb, g, :], start=(kb == 0), stop=(kb == 2)))
                        P.group("pe", fns, reads=[b_pmk[s2][g][0], b_pmk[s2][g][1], b_pmk[s2][g][2], b_Va], writes=[b_pPV[s2][g]])
                        P.op("dve", lambda e, g=g: e.tensor_tensor(out=den[:, g, :], in0=pPV[s2][g][:, :, 64], in1=esink[:, 4 * g:4 * g + 4], op=ALU.add),
                             reads=[b_pPV[s2][g], b_es], writes=[b_den])
                        P.op("dve", lambda e, g=g: e.reciprocal(out=den[:, g, :], in_=den[:, g, :]), reads=[b_den], writes=[b_den])
                        P.op("dve", lambda e, g=g: e.tensor_tensor(out=att[s2][:, 4 * g:4 * g + 4, :], in0=pPV[s2][g][:, :, 0:64],
                                                                   in1=den[:, g, :].unsqueeze(2).to_broadcast([128, 4, 64]), op=ALU.mult),
                             reads=[b_pPV[s2][g], b_den], writes=[b_att[s2]])
                    P.group("pe", [lambda e, c=c: e.transpose(out=pTa[:, c, :], in_=att[s2][:].rearrange("p h d -> p (h d)")[:, c * 128:(c + 1) * 128],
                                                               identity=identb) for c in range(4)],
                            reads=[b_att[s2], b_tb], writes=[b_pTa])
                    P.op("act", lambda e: e.copy(out=atT[s2][:], in_=pTa), reads=[b_pTa], writes=[b_atT[s2]])
                    P.dma("sp", lambda e: e.dma_start(out=AT[:, i * 128:(i + 1) * 128].rearrange("(c p) q -> p c q", p=128), in_=atT[s2][:]),
                          reads=[b_atT[s2]])

                attn_part1(0)
                for i in range(32):
                    if i + 1 < 32:
                        attn_part1(i + 1)
                    attn_part2(i)
                P.barrier()
        if debug == "p1b":
            return _finish(nc, P, out, gst)

        TWO_PI = 2.0 * math.pi
        with ExitStack() as st2:
            hdn2T = SB(st2, "hdn2T", [64, NFFT], BF16); b_h2 = Buf()
            w3b = SB(st2, "w3b", [64, 2048], BF16); b_w3 = Buf()
            P.dma("pool", lambda e: e.dma_start(out=w3b[:, :], in_=fw3[:, :]), writes=[b_w3])
            dec = SB(st2, "dec", [128, 2, 512], F32); b_dec = Buf()

            def load_dec(o):
                P.dma("sp", lambda e: e.dma_start(out=dec[0:64, o, :], in_=fdec[0:1, o * 1024:o * 1024 + 512].partition_broadcast(64)), writes=[b_dec])
                P.dma("sp", lambda e: e.dma_start(out=dec[64:128, o, :], in_=fdec[0:1, o * 1024 + 512:(o + 1) * 1024].partition_broadcast(64)), writes=[b_dec])
            load_dec(0); load_dec(1)
            P.op("act", lambda e: e.activation(out=dec[:], in_=dec[:], func=AF.Abs), reads=[b_dec], writes=[b_dec])
            P.op("dve", lambda e: e.tensor_scalar(out=dec[:], in0=dec[:], scalar1=tf[:, 389:390], scalar2=None, op0=ALU.mult),
                 reads=[b_dec, b_tf], writes=[b_dec])
            skA = SB(st2, "skA", [128, 1024], F32); b_sk = Buf()
            P.dma("sp", lambda e: e.dma_start(out=skA[:], in_=hskip[0:1, :].partition_broadcast(128)), writes=[b_sk])

            with ExitStack() as st:
                w1t = SB(st, "w1t", [33, 64], F32); w2t = SB(st, "w2t", [64, 64], F32); b_fw = Buf()
                fsc = SB(st, "fsc", [64, 8], F32); b_fsc = Buf()
                bnd = SB(st, "bnd", [33, 4], F32)
                P.dma("sp", lambda e: e.dma_start(out=w1t[:], in_=fw1[:, :]), writes=[b_fw])
                P.dma("sp", lambda e: e.dma_start(out=w2t[:], in_=fw2[:, :]), writes=[b_fw])
                P.dma("sp", lambda e: e.dma_start(out=bnd[:], in_=bands[:, :]), writes=[b_fw])
                for ci, srcap in enumerate((ff1, fb1, ff2, fb2)):
                    P.dma("sp", lambda e, ci=ci, srcap=srcap: e.dma_start(out=fsc[:, ci:ci + 1], in_=srcap[:, :]), writes=[b_fsc])
                for (a, b, o1, o2) in ((0, 1, 4, 5), (2, 3, 6, 7)):
                    P.op("dve", lambda e, a=a, b=b, o2=o2: e.tensor_tensor(out=fsc[:, o2:o2 + 1], in0=fsc[:, a:a + 1], in1=fsc[:, b:b + 1], op=ALU.mult),
                         reads=[b_fsc], writes=[b_fsc])
                    P.op("dve", lambda e, o2=o2: e.tensor_scalar(out=fsc[:, o2:o2 + 1], in0=fsc[:, o2:o2 + 1], scalar1=1.0 / TWO_PI, scalar2=8.5,
                                                                 op0=ALU.mult, op1=ALU.add), reads=[b_fsc], writes=[b_fsc])
                    P.op("dve", lambda e, a=a, o1=o1: e.tensor_scalar(out=fsc[:, o1:o1 + 1], in0=fsc[:, a:a + 1], scalar1=1.0 / TWO_PI, scalar2=None,
                                                                      op0=ALU.mult), reads=[b_fsc], writes=[b_fsc])
                idx = SB(st, "idx", [33, 16, 128], I32); b_idx = Buf()
                idxf = SB(st, "idxf", [33, 2048], F32); b_idxf = Buf()
                uu = SB(st, "uu", [64, 2048], F32); b_uu = Buf()
                ki = SB(st, "ki", [64, 2048], I32); b_ki = Buf()
                zT = SB(st, "zT", [33, 2048], F32); b_zT = Buf()
                h1 = SB(st, "h1", [64, 512], F32); b_h1 = Buf()
                pH = [PS(st, "pH%d" % i, [64, 512]) for i in range(2)]; b_pH = [Buf(), Buf()]

                def sin_reduce(np_, ncols, src, b_src, sc_mul, sc_add, dst, b_dst, extra_reads=()):
                    P.op("dve", lambda e: e.tensor_scalar(out=uu[0:np_, 0:ncols], in0=src, scalar1=sc_mul, scalar2=sc_add, op0=ALU.mult, op1=ALU.add),
                         reads=[b_src] + list(extra_reads), writes=[b_uu])
                    P.op("dve", lambda e: e.tensor_copy(out=ki[0:np_, 0:ncols], in_=uu[0:np_, 0:ncols]), reads=[b_uu], writes=[b_ki])
                    P.op("dve", lambda e: e.tensor_tensor(out=uu[0:np_, 0:ncols], in0=uu[0:np_, 0:ncols], in1=ki[0:np_, 0:ncols], op=ALU.subtract),
                         reads=[b_uu, b_ki], writes=[b_uu])
                    P.op("dve", lambda e: e.scalar_tensor_tensor(out=uu[0:np_, 0:ncols], in0=uu[0:np_, 0:ncols], scalar=0.0, in1=uu[0:np_, 0:ncols],
                                                                 op0=ALU.is_lt, op1=ALU.add), reads=[b_uu], writes=[b_uu])
                    P.op("act", lambda e: e.activation(out=dst, in_=uu[0:np_, 0:ncols], func=AF.Sin, bias=-math.pi, scale=TWO_PI),
                         reads=[b_uu], writes=[b_dst])

                def mlp_chunk(c):
                    P.op("pool", lambda e: e.iota(idx[:, :, 0:64], pattern=[[1, 16], [128, 64]], base=16 * c, channel_multiplier=0), writes=[b_idx])
                    P.op("pool", lambda e: e.iota(idx[:, :, 64:128], pattern=[[-1, 16], [-128, 64]], base=8192 - 16 * c, channel_multiplier=0), writes=[b_idx])
                    P.op("dve", lambda e: e.tensor_single_scalar(out=idx[:, :, 64:128], in_=idx[:, :, 64:128], scalar=8191, op=ALU.bitwise_and),
                         reads=[b_idx], writes=[b_idx])
                    P.op("dve", lambda e: e.tensor_copy(out=idxf[:], in_=idx[:].rearrange("p a b -> p (a b)")), reads=[b_idx], writes=[b_idxf])
                    sin_reduce(33, 2048, idxf[:], b_idxf, bnd[:, 0:1], bnd[:, 1:2], zT[:], b_zT, extra_reads=[b_fw])
                    P.op("dve", lambda e: e.tensor_scalar(out=zT[0:1, :], in0=idxf[0:1, :], scalar1=1.0 / (L - 1), scalar2=None, op0=ALU.mult),
                         reads=[b_idxf], writes=[b_zT])

                    def quarter(q):
                        s = q % 2
                        P.group("pe", [lambda e: e.matmul(pH[s][:], lhsT=w1t[:], rhs=zT[:, q * 512:(q + 1) * 512], start=True, stop=True)],
                                reads=[b_fw, b_zT], writes=[b_pH[s]])
                        sin_reduce(64, 512, pH[s][:], b_pH[s], fsc[:, 4:5], fsc[:, 5:6], h1[:], b_h1, extra_reads=[b_fsc])
                        P.group("pe", [lambda e: e.matmul(pH[s][:], lhsT=w2t[:], rhs=h1[:], start=True, stop=True)],
                                reads=[b_fw, b_h1], writes=[b_pH[s]])
                        sin_reduce(64, 512, pH[s][:], b_pH[s], fsc[:, 6:7], fsc[:, 7:8], hdn2T[:, c * 2048 + q * 512:c * 2048 + (q + 1) * 512], b_h2,
                                   extra_reads=[b_fsc])
                    for q in range(4):
                        quarter(q)
                for c in range(8):
                    mlp_chunk(c)
                P.barrier()

            kraw = SB(st2, "kraw", [128, 32, 128], F32); b_kraw = Buf()
            wtmp = SB(st2, "wtmp", [128, 32, 128], F32); b_wtmp = Buf()
            kcs = SB(st2, "kcs", [128, 32, 128], BF16); b_kcs = Buf()
            Khr = SB(st2, "Khr", [128, 32, 128], BF16); Khi = SB(st2, "Khi", [128, 32, 128], BF16); b_Kh = Buf()
            e1 = SB(st2, "e1", [128, 32], F32); b_e1 = Buf()
            ksum = SB(st2, "ksum", [128, 32], F32); b_ksum = Buf()
            r64 = SB(st2, "r64", [128, 32], F32); b_r64 = Buf()
            ut = [[SB(st2, "ut%d_%d" % (s, k), [64, 16, 128], BF16) for k in range(3)] for s in range(2)]
            b_ut = [[Buf() for k in range(3)] for s in range(2)]
            z1t = SB(st2, "z1t", [64, 16, 128], BF16); b_z1 = Buf()
            z2t = [SB(st2, "z2t%d" % s, [64, 16, 128], BF16) for s in range(2)]; b_z2 = [Buf(), Buf()]
            pK = [PS(st2, "pK%d" % i, [128, 512]) for i in range(2)]; b_pK = [Buf(), Buf()]
            pN = pK[0]; b_pN = b_pK[0]
            NL = 3
            lanes = []
            for li in range(NL):
                ln = {"pL": PS(st2, "pL%d" % li, [128, 1024]), "b_pL": Buf()}
                ln["S"] = SB(st2, "S%d" % li, [128, 1024], BF16); ln["b_S"] = Buf()
                ln["P1"] = [SB(st2, "P1_%d_%d" % (li, k), [128, 1024], BF16) for k in range(2)]; ln["b_P1"] = [Buf(), Buf()]
                ln["P2"] = [SB(st2, "P2_%d_%d" % (li, k), [128, 1024], BF16) for k in range(2)]; ln["b_P2"] = [Buf(), Buf()]
                ln["pk"] = 0
                lanes.append(ln)
            TC1 = tb[:, CT_TC1:CT_TC1 + 256].unsqueeze(1).to_broadcast([128, 4, 256])
            TC2 = tb[:, CT_TC2:CT_TC2 + 256].unsqueeze(1).to_broadcast([128, 4, 256])
            F1FULL = tb[:, CT_F1FULL:CT_F1FULL + 256]
            F1C = tb[0:64, CT_F1C:CT_F1C + 256]
            F2R = tb[:, CT_F2R:CT_F2R + 128]; F2I = tb[:, CT_F2I:CT_F2I + 128]; F2NI = tb[:, CT_F2NI:CT_F2NI + 128]
            G2A = tb[:, CT_G2A:CT_G2A + 256]; G2B = tb[:, CT_G2B:CT_G2B + 256]
            G1R = tb[:, CT_G1R:CT_G1R + 64]; G1I = tb[:, CT_G1I:CT_G1I + 64]
            w3v = w3b[:, :].rearrange("p (o d c) -> p o d c", o=2, d=2)

            F2NR = tb[:, CT_F2NR:CT_F2NR + 128]; G2NA = tb[:, CT_G2NA:CT_G2NA + 256]; G1NI = tb[:, CT_G1NI:CT_G1NI + 64]

            def cmul_ci(ln):
                k = ln["pk"]; ln["pk"] = 1 - k; ln["cur"] = k
                S4 = ln["S"][:, :].rearrange("p (c x) -> p c x", c=4)
                P1 = ln["P1"][k][:, :].rearrange("p (c x) -> p c x", c=4); P2 = ln["P2"][k][:, :].rearrange("p (c x) -> p c x", c=4)
                P.op("act", lambda e: e.copy(out=ln["S"][:, :], in_=ln["pL"][:, :]), reads=[ln["b_pL"]], writes=[ln["b_S"]])
                P.op("dve", lambda e: e.tensor_tensor(out=P1, in0=S4, in1=TC1, op=ALU.mult), reads=[ln["b_S"], b_tb], writes=[ln["b_P1"][k]])
                P.op("dve", lambda e: e.tensor_tensor(out=P2, in0=S4, in1=TC2, op=ALU.mult), reads=[ln["b_S"], b_tb], writes=[ln["b_P2"][k]])

            def cmul_ic(ln, f0):
                k = ln["pk"]; ln["pk"] = 1 - k; ln["cur"] = k
                S4 = ln["S"][:, :].rearrange("p (r c x) -> p r c x", r=2, c=4)
                P1 = ln["P1"][k][:, :].rearrange("p (r c x) -> p r c x", r=2, c=4); P2 = ln["P2"][k][:, :].rearrange("p (r c x) -> p r c x", r=2, c=4)
                kr = Khr[:, f0:f0 + 4, :].unsqueeze(1).to_broadcast([128, 2, 4, 128]); ki_ = Khi[:, f0:f0 + 4, :].unsqueeze(1).to_broadcast([128, 2, 4, 128])
                P.op("act", lambda e: e.copy(out=ln["S"][:, :], in_=ln["pL"][:, :]), reads=[ln["b_pL"]], writes=[ln["b_S"]])
                P.op("dve", lambda e: e.tensor_tensor(out=P1, in0=S4, in1=kr, op=ALU.mult), reads=[ln["b_S"], b_Kh], writes=[ln["b_P1"][k]])
                P.op("dve", lambda e: e.tensor_tensor(out=P2, in0=S4, in1=ki_, op=ALU.mult), reads=[ln["b_S"], b_Kh], writes=[ln["b_P2"][k]])

            def st_S1(ln, src, b_src, rhs):
                pA = ln["pL"][:, :].rearrange("p (c x) -> p c x", c=4)
                P.group("pe", [lambda e, cl=cl: e.matmul(pA[:, cl, :], lhsT=src[:, cl, :], rhs=rhs, start=True, stop=True) for cl in range(4)],
                        reads=[b_src, b_tb], writes=[ln["b_pL"]])

            def st_TW(ln):
                cmul_ci(ln)

            def st_S2(ln):
                k = ln["cur"]
                P1 = ln["P1"][k][:, :].rearrange("p (c x) -> p c x", c=4); P2 = ln["P2"][k][:, :].rearrange("p (c x) -> p c x", c=4)
                m0, m3, m2, m1 = P1[:, :, 0:128], P1[:, :, 128:256], P2[:, :, 0:128], P2[:, :, 128:256]
                pXr = ln["pL"][:, 0:512].rearrange("p (c x) -> p c x", c=4); pXi = ln["pL"][:, 512:1024].rearrange("p (c x) -> p c x", c=4)
                P.group("pe", [lambda e: e.matmul(pXr, lhsT=F2R, rhs=m0, start=True, stop=False),
                               lambda e: e.matmul(pXr, lhsT=F2NR, rhs=m1, start=False, stop=False),
                               lambda e: e.matmul(pXr, lhsT=F2NI, rhs=m2, start=False, stop=False),
                               lambda e: e.matmul(pXr, lhsT=F2NI, rhs=m3, start=False, stop=True),
                               lambda e: e.matmul(pXi, lhsT=F2R, rhs=m2, start=True, stop=False),
                               lambda e: e.matmul(pXi, lhsT=F2R, rhs=m3, start=False, stop=False),
                               lambda e: e.matmul(pXi, lhsT=F2I, rhs=m0, start=False, stop=False),
                               lambda e: e.matmul(pXi, lhsT=F2NI, rhs=m1, start=False, stop=True)],
                        reads=[ln["b_P1"][k], ln["b_P2"][k], b_tb], writes=[ln["b_pL"]])

            def st_SPEC(ln, f0):
                cmul_ic(ln, f0)

            def st_IS1(ln):
                k = ln["cur"]
                P1 = ln["P1"][k][:, :].rearrange("p (r c x) -> p r c x", r=2, c=4); P2 = ln["P2"][k][:, :].rearrange("p (r c x) -> p r c x", r=2, c=4)
                pB = ln["pL"][:, :].rearrange("p (c x) -> p c x", c=4)
                fns = []
                for cl in range(4):
                    fns.append(lambda e, cl=cl: e.matmul(pB[:, cl, :], lhsT=P1[:, 0, cl, :], rhs=G2A, start=True, stop=False))
                    fns.append(lambda e, cl=cl: e.matmul(pB[:, cl, :], lhsT=P2[:, 1, cl, :], rhs=G2NA, start=False, stop=False))
                    fns.append(lambda e, cl=cl: e.matmul(pB[:, cl, :], lhsT=P2[:, 0, cl, :], rhs=G2B, start=False, stop=False))
                    fns.append(lambda e, cl=cl: e.matmul(pB[:, cl, :], lhsT=P1[:, 1, cl, :], rhs=G2B, start=False, stop=True))
                P.group("pe", fns, reads=[ln["b_P1"][k], ln["b_P2"][k], b_tb], writes=[ln["b_pL"]])

            def st_ITW(ln):
                cmul_ci(ln)

            def st_IS2(ln):
                k = ln["cur"]
                P1 = ln["P1"][k][:, :].rearrange("p (c x) -> p c x", c=4); P2 = ln["P2"][k][:, :].rearrange("p (c x) -> p c x", c=4)
                n0, n3, n2, n1 = P1[:, :, 0:128], P1[:, :, 128:256], P2[:, :, 0:128], P2[:, :, 128:256]
                pY = ln["pL"][0:64, 0:512].rearrange("p (c x) -> p c x", c=4)
                P.group("pe", [lambda e: e.matmul(pY, lhsT=G1R, rhs=n0, start=True, stop=False),
                               lambda e: e.matmul(pY, lhsT=G1R, rhs=n1, start=False, stop=False),
                               lambda e: e.matmul(pY, lhsT=G1I, rhs=n3, start=False, stop=False),
                               lambda e: e.matmul(pY, lhsT=G1NI, rhs=n2, start=False, stop=True)],
                        reads=[ln["b_P1"][k], ln["b_P2"][k], b_tb], writes=[ln["b_pL"]])

            def st_gate(ln, cg, gin, b_gin, zout, b_zout):
                pY = ln["pL"][0:64, 0:512].rearrange("p (c x) -> p c x", c=4)
                P.op("dve", lambda e: e.tensor_tensor(out=zout, in0=pY, in1=gin, op=ALU.mult), reads=[ln["b_pL"], b_gin], writes=[b_zout])

            def sg_load(sg):
                us = sg % 2
                c0 = 16 * sg
                for k in range(3):
                    P.dma("pool", lambda e, k=k: e.dma_start(out=ut[us][k][:], in_=Uc[k * 512 + c0:k * 512 + c0 + 16, :].rearrange("c (a b) -> a c b", b=128)),
                          reads=[], writes=[b_ut[us][k]])

            def sg_kbatch(sg, bi):
                c0 = 16 * sg
                s = bi % 2
                pKv = pK[s][:, :].rearrange("p (n f) -> p n f", n=16)
                fns = []
                for nl in range(16):
                    n2 = bi * 16 + nl
                    fns.append(lambda e, nl=nl, n2=n2: e.matmul(pKv[0:64, nl, :].rearrange("p (o c) -> p o c", o=2),
                                                                lhsT=hdn2T[:, n2 * 128:n2 * 128 + 64], rhs=w3v[:, :, 0, c0:c0 + 16], start=True, stop=True))
                    fns.append(lambda e, nl=nl, n2=n2: e.matmul(pKv[64:128, nl, :].rearrange("p (o c) -> p o c", o=2),
                                                                lhsT=hdn2T[:, n2 * 128 + 64:n2 * 128 + 128], rhs=w3v[:, :, 1, c0:c0 + 16], start=True, stop=True))
                P.group("pe", fns, reads=[b_h2, b_w3], writes=[b_pK[s]])
                P.op("act", lambda e: e.copy(out=kraw[:, :, bi * 16:(bi + 1) * 16].rearrange("p f n -> p n f"), in_=pKv), reads=[b_pK[s]], writes=[b_kraw])

            def sg_window(sg):
                c0 = 16 * sg
                decv = dec[:, :, c0:c0 + 16]
                e1v = e1[:, :].rearrange("p (o c) -> p o c", o=2)
                wt4 = wtmp[:, :, :].rearrange("p (o c) n -> p o c n", o=2)

                def w0():
                    P.op("act", lambda e: e.activation(out=e1v, in_=decv, func=AF.Exp, scale=tf[:, 390:391]), reads=[b_dec, b_tf], writes=[b_e1])
                    P.op("pool", lambda e: e.tensor_tensor(out=wt4, in0=decv.unsqueeze(3).to_broadcast([128, 2, 16, 128]),
                                                           in1=tf[:, 528:656].unsqueeze(1).unsqueeze(1).to_broadcast([128, 2, 16, 128]), op=ALU.mult),
                         reads=[b_dec, b_tf], writes=[b_wtmp])

                def w1():
                    P.op("act", lambda e: e.activation(out=wtmp[:], in_=wtmp[:], func=AF.Exp), reads=[b_wtmp], writes=[b_wtmp])

                def w2():
                    P.op("pool", lambda e: e.tensor_tensor(out=wtmp[:], in0=wtmp[:], in1=e1[:, :].unsqueeze(2).to_broadcast([128, 32, 128]), op=ALU.mult),
                         reads=[b_wtmp, b_e1], writes=[b_wtmp])
                    P.op("pool", lambda e: e.tensor_scalar(out=r64[64:65, :], in0=kraw[64:65, :, 0], scalar1=1.05, scalar2=None, op0=ALU.mult),
                         reads=[b_kraw], writes=[b_r64])

                def w3():
                    P.op("dve", lambda e: e.scalar_tensor_tensor(out=kraw[:], in0=wtmp[:], scalar=0.05, in1=kraw[:], op0=ALU.add, op1=ALU.mult),
                         reads=[b_wtmp, b_kraw, b_r64], writes=[b_kraw])
                    P.op("pool", lambda e: e.tensor_copy(out=kraw[64:65, :, 0], in_=r64[64:65, :]), reads=[b_r64], writes=[b_kraw])

                def w4():
                    P.op("act", lambda e: e.activation(out=wtmp[:], in_=kraw[:], func=AF.Abs), reads=[b_kraw], writes=[b_wtmp])

                def w5():
                    P.op("dve", lambda e: e.tensor_reduce(out=ksum[:], in_=wtmp[:], axis=AX.X, op=ALU.add), reads=[b_wtmp], writes=[b_ksum])
                    P.group("pe", [lambda e: e.matmul(pN[:, 0:32], lhsT=tf[:, 656:784], rhs=ksum[:], start=True, stop=True)],
                            reads=[b_ksum, b_tf], writes=[b_pN])

                def w6():
                    P.op("dve", lambda e: e.reciprocal(out=ksum[:], in_=pN[:, 0:32]), reads=[b_pN], writes=[b_ksum])
                    P.op("pool", lambda e: e.memset(kraw[64:65, :, 0], 0.0), reads=[b_wtmp], writes=[b_kraw])

                def w7():
                    P.op("dve", lambda e: e.tensor_tensor(out=kcs[:], in0=kraw[:], in1=ksum[:, :].unsqueeze(2).to_broadcast([128, 32, 128]), op=ALU.mult),
                         reads=[b_kraw, b_ksum], writes=[b_kcs])
                    P.op("dve", lambda e: e.tensor_tensor(out=kcs[0:1, :, 0].rearrange("p (o c) -> p o c", o=2), in0=kcs[0:1, :, 0].rearrange("p (o c) -> p o c", o=2),
                                                          in1=skA[0:1, :].rearrange("p (o c) -> p o c", o=2)[:, :, c0:c0 + 16], op=ALU.add),
                         reads=[b_kcs, b_sk], writes=[b_kcs])
                    if debug == "p2" and sg == 0:
                        P.dma("sp", lambda e: e.dma_start(out=dbgkc[:, :, :], in_=kcs[:]), reads=[b_kcs])
                return [w0, w1, w2, w3, w4, w5, w6, w7]

            def sg_filtfft(sg):
                c0 = 16 * sg

                def filt_batch(f0s):
                    grp = [(lanes[li], f0) for li, f0 in enumerate(f0s)]
                    for ln, f0 in grp:
                        st_S1(ln, kcs[:, f0:f0 + 4, :], b_kcs, F1FULL)
                    for ln, f0 in grp:
                        st_TW(ln)
                    for ln, f0 in grp:
                        st_S2(ln)
                    for ln, f0 in grp:
                        pXr = ln["pL"][:, 0:512].rearrange("p (c x) -> p c x", c=4); pXi = ln["pL"][:, 512:1024].rearrange("p (c x) -> p c x", c=4)
                        P.op("act", lambda e, f0=f0, pXr=pXr: e.copy(out=Khr[:, f0:f0 + 4, :], in_=pXr), reads=[ln["b_pL"]], writes=[b_Kh])
                        P.op("act", lambda e, f0=f0, pXi=pXi: e.copy(out=Khi[:, f0:f0 + 4, :], in_=pXi), reads=[ln["b_pL"]], writes=[b_Kh])
                for f0s in ((0, 4, 8), (12, 16, 20), (24, 28)):
                    filt_batch(f0s)
                if debug == "p2" and sg == 0:
                    P.dma("sp", lambda e: e.dma_start(out=dbgkh[:, 0, :, :], in_=Khr[:]), reads=[b_Kh])
                    P.dma("sp", lambda e: e.dma_start(out=dbgkh[:, 1, :, :], in_=Khi[:]), reads=[b_Kh])

            def sg_conv_batch(sg, tasks, b_z1g, hooks=None):
                hk = (lambda k: hooks[k]()) if hooks else (lambda k: None)
                us = sg % 2
                grp = []
                for li, (o, cg) in enumerate(tasks):
                    if o == 0:
                        zin, b_zin = ut[us][0][:, 4 * cg:4 * cg + 4, :], b_ut[us][0]
                        gin, b_gin = ut[us][1][:, 4 * cg:4 * cg + 4, :], b_ut[us][1]
                        zout, b_zout = z1t[:, 4 * cg:4 * cg + 4, :], b_z1g[cg]
                    else:
                        zin, b_zin = z1t[:, 4 * cg:4 * cg + 4, :], b_z1g[cg]
                        gin, b_gin = ut[us][2][:, 4 * cg:4 * cg + 4, :], b_ut[us][2]
                        zout, b_zout = z2t[us][:, 4 * cg:4 * cg + 4, :], b_z2[us]
                    grp.append((lanes[li], o, cg, zin, b_zin, gin, b_gin, zout, b_zout))
                for (ln, o, cg, zin, b_zin, gin, b_gin, zout, b_zout) in grp:
                    st_S1(ln, zin, b_zin, F1C)
                hk(0)
                for g_ in grp:
                    st_TW(g_[0])
                hk(1)
                for g_ in grp:
                    st_S2(g_[0])
                hk(2)
                for g_ in grp:
                    st_SPEC(g_[0], g_[1] * 16 + 4 * g_[2])
                hk(3)
                for g_ in grp:
                    st_IS1(g_[0])
                hk(4)
                for g_ in grp:
                    st_ITW(g_[0])
                hk(5)
                for g_ in grp:
                    st_IS2(g_[0])
                hk(6)
                for (ln, o, cg, zin, b_zin, gin, b_gin, zout, b_zout) in grp:
                    st_gate(ln, cg, gin, b_gin, zout, b_zout)
                hk(7)

            def sg_store(sg):
                us = sg % 2
                c0 = 16 * sg
                P.dma("sp", lambda e: e.dma_start(out=Z2[c0:c0 + 16, :].rearrange("c (a b) -> a c b", b=128), in_=z2t[us][0:32, :, :]), reads=[b_z2[us]])

            cvs = [SB(st2, "cvs%d" % i, [128, 2048], BF16) for i in range(3)]; b_cvs = [Buf() for _ in range(3)]
            cv_tasks = [(ti, rb) for rb in range(ne // 128) for ti in range(3)]
            cv_state = {"i": 0}
            ew_src = (ew1, ew3, ew2)

            def cv():
                i = cv_state["i"]
                if i >= len(cv_tasks):
                    return
                cv_state["i"] = i + 1
                ti, rb = cv_tasks[i]
                s = i % 3
                P.dma("pool", lambda e: e.dma_start(out=cvs[s][:], in_=ew_src[ti][rb * 128:(rb + 1) * 128, :]), writes=[b_cvs[s]])
                ee, hh_ = rb // 2, rb % 2
                P.dma("sp", lambda e: e.dma_start(out=EWB[ti][ee * 128:(ee + 1) * 128, hh_ * 2048:(hh_ + 1) * 2048], in_=cvs[s][:]), reads=[b_cvs[s]])
            CONV_TASKS = (((0, 0), (0, 1), (0, 2)), ((0, 3), (1, 0), (1, 1)), ((1, 2), (1, 3)))
            nsg = 32 if debug != "p2" else int(DBG_NSG)
            b_z1g = [Buf() for _ in range(4)]
            sg_load(0)
            for bi in range(8):
                sg_kbatch(0, bi)
            for w_ in sg_window(0):
                w_()
            for sg in range(nsg):
                sg_filtfft(sg)
                nxt = sg + 1 < nsg
                if nxt:
                    sg_load(sg + 1)
                cvh = [cv, cv, cv, cv, cv, cv, (lambda: None), (lambda: None)]
                sg_conv_batch(sg, CONV_TASKS[0], b_z1g, hooks=cvh)
                if nxt:
                    for bi in range(8):
                        sg_kbatch(sg + 1, bi)
                sg_conv_batch(sg, CONV_TASKS[1], b_z1g, hooks=(sg_window(sg + 1) if nxt else None))
                if debug == "p2" and sg == 0:
                    P.dma("sp", lambda e: e.dma_start(out=dbgz1[:, :, :], in_=z1t[:]), reads=b_z1g)
                sg_conv_batch(sg, CONV_TASKS[2], b_z1g, hooks=cvh)
                sg_store(sg)
            while cv_state["i"] < len(cv_tasks):
                cv()
            P.barrier()
        if debug == "p2":
            return _finish(nc, P, out, gst)

        def layer_norm(st_tiles, r, b_r, g_b, b_b, b_gb, dst, b_dst, eng2="pool", lnexp=False):
            stats, mv, b_stat = st_tiles
            P.op("dve", lambda e: e.bn_stats(out=stats[:, 0, :], in_=r[:, 0:512]), reads=[b_r], writes=[b_stat])
            P.op("dve", lambda e: e.bn_stats(out=stats[:, 1, :], in_=r[:, 512:1024]), reads=[b_r], writes=[b_stat])
            P.op("dve", lambda e: e.bn_aggr(out=mv[:, 0:2], in_=stats[:].rearrange("p a b -> p (a b)")), reads=[b_stat], writes=[b_stat])
            P.op("dve", lambda e: e.tensor_scalar(out=mv[:, 2:3], in0=mv[:, 1:2], scalar1=LN_EPS, scalar2=None, op0=ALU.add), reads=[b_stat], writes=[b_stat])
            if lnexp:
                P.op("act", lambda e: e.activation(out=mv[:, 2:3], in_=mv[:, 2:3], func=AF.Ln), reads=[b_stat], writes=[b_stat])
                P.op("act", lambda e: e.activation(out=mv[:, 2:3], in_=mv[:, 2:3], func=AF.Exp, scale=-0.5), reads=[b_stat], writes=[b_stat])
            else:
                P.op("act", lambda e: e.activation(out=mv[:, 2:3], in_=mv[:, 2:3], func=AF.Sqrt), reads=[b_stat], writes=[b_stat])
                P.op("dve", lambda e: e.reciprocal(out=mv[:, 2:3], in_=mv[:, 2:3]), reads=[b_stat], writes=[b_stat])
            P.op("dve", lambda e: e.scalar_tensor_tensor(out=mv[:, 3:4], in0=mv[:, 0:1], scalar=-1.0, in1=mv[:, 2:3], op0=ALU.mult, op1=ALU.mult),
                 reads=[b_stat], writes=[b_stat])
            P.op("act", lambda e: e.activation(out=r[:], in_=r[:], func=AF.Identity, scale=mv[:, 2:3], bias=mv[:, 3:4]), reads=[b_r, b_stat], writes=[b_r])
            P.op(eng2, lambda e: e.tensor_tensor(out=r[:], in0=r[:], in1=g_b, op=ALU.mult), reads=[b_r, b_gb], writes=[b_r])
            P.op("dve", lambda e: e.tensor_tensor(out=dst, in0=r[:], in1=b_b, op=ALU.add), reads=[b_r, b_gb], writes=[b_dst])

        with ExitStack() as st34:
            dest = SB(st34, "dest", [128, 32, 2], I32); b_dest = Buf()
            wts = SB(st34, "wts", [128, 32, 2], F32); b_wts = Buf()
            widx = SB(st34, "widx", [128, 128, 2], I32); b_widx = Buf()
            with ExitStack() as st:
                wao = SB(st, "wao", [128, 4, D], BF16); who = SB(st, "who", [128, 4, D], BF16); wout = SB(st, "wout", [128, 8, D], BF16); b_w3p = Buf()

                def ldw(dst, srcw, nk):
                    for kc in range(nk):
                        P.dma("pool", lambda e, kc=kc: e.dma_start(out=dst[:, kc, :], in_=srcw[kc * 128:(kc + 1) * 128, :]), writes=[b_w3p])
                ldw(wao, w_attn_o, 4); ldw(who, w_hy_o, 4); ldw(wout, w_out, 8)
                bc = SB(st, "bc", [128, 5, D], F32); b_bc = Buf()
                for k, srcap in enumerate((mod_scr[0:1, 2048:3072], ln1g[0:1, :], ln1b[0:1, :], mod_scr[0:1, 4096:5120], mod_scr[0:1, 3072:4096])):
                    P.dma("sp", lambda e, k=k, srcap=srcap: e.dma_start(out=bc[:, k, :], in_=srcap.partition_broadcast(128)), writes=[b_bc])
                wrt = SB(st, "wrt", [128, 8, 72], F32); brb = SB(st, "brb", [128, 72], F32); b_wr = Buf()
                P.dma("sp", lambda e: e.dma_start(out=wrt[:], in_=wr[:, :].rearrange("(c p) n -> p c n", p=128)), writes=[b_wr])
                P.dma("sp", lambda e: e.dma_start(out=brb[:], in_=br[0:1, :].partition_broadcast(128)), writes=[b_wr])
                rcarry = SB(st, "rcarry", [128, 64], F32); b_rcarry = Buf()
                P.op("pool", lambda e: e.memset(rcarry[:], 0.0), writes=[b_rcarry])
                rnk = SB(st, "rnk", [128, 32, 2], F32); b_rnk = Buf()
                ohall = SB(st, "ohall", [128, 32, 2, 64], BF16); b_oh = Buf()
                atM = SB(st, "atTm", [128, 4, 512], BF16); z2T = SB(st, "z2T", [128, 4, 512], BF16)
                sga = SB(st, "sga", [128, 8, 512], BF16); sgh = SB(st, "sgh", [128, 8, 512], BF16)
                b_at = Buf(); b_z2T = Buf(); b_sga = Buf(); b_sgh = Buf()
                mrg = [SB(st, "mrg%d" % i, [128, 8, 512], BF16) for i in range(2)]; b_mrg = [Buf(), Buf()]
                t1 = [SB(st, "t1_%d" % i, [128, 512], F32) for i in range(2)]; t2 = [SB(st, "t2_%d" % i, [128, 512], F32) for i in range(2)]
                b_t1 = [Buf(), Buf()]; b_t2 = [Buf(), Buf()]
                xt = [SB(st, "xt%d" % i, [128, D], F32) for i in range(2)]; b_xt = [Buf(), Buf()]
                rr = [SB(st, "rr%d" % i, [128, D], F32) for i in range(2)]; b_rr = [Buf(), Buf()]
                x1s = [SB(st, "x1s%d" % i, [128, D], F32) for i in range(2)]; b_x1s = [Buf(), Buf()]
                h2 = SB(st, "h2", [128, D], F32); b_h2t = Buf()
                h2b = [SB(st, "h2b%d" % i, [128, D], BF16) for i in range(2)]; b_h2b = [Buf(), Buf()]
                h2T = SB(st, "h2T", [128, 8, 128], F32); b_h2T = Buf()
                stats = SB(st, "stats", [128, 2, 6], F32); mv = SB(st, "mv", [128, 4], F32); b_stat = Buf()
                lg = SB(st, "lg", [128, 72], F32); b_lg = Buf()
                sm = SB(st, "sm", [128, 64], F32); b_sm = Buf()
                elm = SB(st, "elm", [128, 64], F32); b_elm = Buf()
                m8 = SB(st, "m8", [128, 16], F32); b_m8 = Buf()
                ohs = SB(st, "ohs", [128, 64], BF16); b_ohs = Buf()
                basef = SB(st, "basef", [128, 64], F32); b_base = Buf()
                junk = SB(st, "junk", [128, 64], F32); b_junk = Buf()
                pMa = PS(st, "pMa", [128, 512]); pMh = PS(st, "pMh", [128, 512]); b_pMa = Buf(); b_pMh = Buf()
                pYt = PS(st, "pYt", [128, 1024]); b_pYt = Buf()
                pHT = PS(st, "pHT", [128, 1024]); b_pHT = Buf()
                pR = PS(st, "pR", [128, 512]); b_pR = Buf()
                pLg = PS(st, "pLg", [128, 512]); b_pLg = Buf()

                def route_tile(i, x1tile, b_x1tile, hs):
                    P.op("dve", lambda e: e.tensor_tensor(out=h2[:], in0=x1tile[:], in1=bc[:, 3, :], op=ALU.mult), reads=[b_x1tile, b_bc], writes=[b_h2t])
                    P.op("dve", lambda e: e.tensor_tensor(out=h2[:], in0=h2[:], in1=bc[:, 4, :], op=ALU.add), reads=[b_h2t, b_bc], writes=[b_h2t])
                    P.op("act", lambda e: e.copy(out=h2b[hs][:], in_=h2[:]), reads=[b_h2t], writes=[b_h2b[hs]])
                    P.dma("sp", lambda e: e.dma_start(out=H2[i * 128:(i + 1) * 128, :], in_=h2b[hs][:]), reads=[b_h2b[hs]])
                    pHTv = pHT[:, :].rearrange("p (c q) -> p c q", c=8)
                    P.group("pe", [lambda e, kc=kc: e.transpose(out=pHTv[:, kc, :], in_=h2[:, kc * 128:(kc + 1) * 128], identity=identf) for kc in range(8)],
                            reads=[b_h2t, b_tf], writes=[b_pHT])
                    P.op("act", lambda e: e.copy(out=h2T[:], in_=pHTv), reads=[b_pHT], writes=[b_h2T])
                    P.group("pe", [lambda e, kc=kc: e.matmul(pLg[:, 0:72], lhsT=h2T[:, kc, :], rhs=wrt[:, kc, :], start=(kc == 0), stop=(kc == 7)) for kc in range(8)],
                            reads=[b_h2T, b_wr], writes=[b_pLg])
                    P.op("dve", lambda e: e.tensor_tensor(out=lg[:], in0=pLg[:, 0:72], in1=brb[:], op=ALU.add), reads=[b_pLg, b_wr], writes=[b_lg])
                    P.op("dve", lambda e: e.max(out=m8[:, 0:8], in_=lg[:, 0:8]), reads=[b_lg], writes=[b_m8])
                    P.op("dve", lambda e: e.tensor_scalar(out=sm[:, 0:8], in0=lg[:, 0:8], scalar1=m8[:, 0:1], scalar2=None, op0=ALU.is_equal),
                         reads=[b_lg, b_m8], writes=[b_sm])
                    P.op("dve", lambda e: e.tensor_scalar(out=sm[:, 8:9], in0=m8[:, 0:1], scalar1=-1.0, scalar2=None, op0=ALU.mult), reads=[b_m8], writes=[b_sm])
                    P.op("act", lambda e: e.activation(out=sm[:, 16:24], in_=lg[:, 0:8], func=AF.Exp, bias=sm[:, 8:9], scale=1.0, accum_out=sm[:, 9:10]),
                         reads=[b_lg, b_sm], writes=[b_sm])
                    P.op("dve", lambda e: e.reciprocal(out=sm[:, 10:11], in_=sm[:, 9:10]), reads=[b_sm], writes=[b_sm])
                    P.op("dve", lambda e: e.tensor_scalar(out=sm[:, 24:32], in0=sm[:, 0:8], scalar1=1e30, scalar2=-1e30, op0=ALU.mult, op1=ALU.add),
                         reads=[b_sm], writes=[b_sm])
                    P.op("dve", lambda e: e.tensor_tensor(out=elm[:, :].rearrange("p (g e) -> p g e", g=8), in0=lg[:, 8:72].rearrange("p (g e) -> p g e", g=8),
                                                          in1=sm[:, 24:32].unsqueeze(2).to_broadcast([128, 8, 8]), op=ALU.add),
                         reads=[b_lg, b_sm], writes=[b_elm])
                    P.op("dve", lambda e: e.max(out=m8[:, 8:16], in_=elm[:]), reads=[b_elm], writes=[b_m8])
                    P.op("dve", lambda e: e.tensor_scalar(out=ohall[:, i, 0, :], in0=elm[:], scalar1=m8[:, 8:9], scalar2=None, op0=ALU.is_equal),
                         reads=[b_elm, b_m8], writes=[b_oh])
                    P.op("dve", lambda e: e.tensor_scalar(out=ohall[:, i, 1, :], in0=elm[:], scalar1=m8[:, 9:10], scalar2=None, op0=ALU.is_equal),
                         reads=[b_elm, b_m8], writes=[b_oh])
                    P.op("dve", lambda e: e.tensor_tensor(out=sm[:, 11:12], in0=m8[:, 9:10], in1=m8[:, 8:9], op=ALU.subtract), reads=[b_m8], writes=[b_sm])
                    P.op("act", lambda e: e.activation(out=sm[:, 12:13], in_=sm[:, 11:12], func=AF.Exp), reads=[b_sm], writes=[b_sm])
                    P.op("dve", lambda e: e.tensor_scalar(out=sm[:, 12:13], in0=sm[:, 12:13], scalar1=1.0, scalar2=None, op0=ALU.add), reads=[b_sm], writes=[b_sm])
                    P.op("dve", lambda e: e.reciprocal(out=sm[:, 13:14], in_=sm[:, 12:13]), reads=[b_sm], writes=[b_sm])
                    P.op("dve", lambda e: e.tensor_tensor(out=wts[:, i, 0:1], in0=sm[:, 13:14], in1=sm[:, 10:11], op=ALU.mult), reads=[b_sm], writes=[b_wts])
                    P.op("dve", lambda e: e.tensor_tensor(out=wts[:, i, 1:2], in0=sm[:, 10:11], in1=wts[:, i, 0:1], op=ALU.subtract), reads=[b_sm, b_wts], writes=[b_wts])
                    P.op("dve", lambda e: e.tensor_tensor(out=ohs[:], in0=ohall[:, i, 0, :], in1=ohall[:, i, 1, :], op=ALU.add), reads=[b_oh], writes=[b_ohs])
                    P.group("pe", [lambda e: e.matmul(pR[:, 0:64], lhsT=tb[:, CT_TRI:CT_TRI + 128], rhs=ohs[:], start=True, stop=True),
                                   lambda e: e.matmul(pR[:, 64:128], lhsT=tb[:, CT_ONES:CT_ONES + 128], rhs=ohs[:], start=True, stop=True)],
                            reads=[b_ohs, b_tb], writes=[b_pR])
                    P.op("dve", lambda e: e.tensor_tensor(out=basef[:], in0=pR[:, 0:64], in1=rcarry[:], op=ALU.add), reads=[b_pR, b_rcarry], writes=[b_base])
                    for k in range(2):
                        P.op("dve", lambda e, k=k: e.tensor_tensor(out=junk[:], in0=ohall[:, i, k, :], in1=basef[:], op=ALU.mult),
                             reads=[b_oh, b_base], writes=[b_junk])
                        P.op("dve", lambda e, k=k: e.tensor_reduce(out=rnk[:, i, k:k + 1], in_=junk[:], axis=AX.X, op=ALU.add),
                             reads=[b_junk], writes=[b_rnk])
                    P.op("dve", lambda e: e.tensor_tensor(out=rcarry[:], in0=rcarry[:], in1=pR[:, 64:128], op=ALU.add), reads=[b_pR, b_rcarry], writes=[b_rcarry])

                def merge_loads(s_i):
                    c0 = s_i * 512
                    P.dma("sp", lambda e: e.dma_start(out=atM[:], in_=AT[:, c0:c0 + 512].rearrange("(c p) q -> p c q", p=128)), writes=[b_at])
                    P.dma("sp", lambda e: e.dma_start(out=z2T[:], in_=Z2[:, c0:c0 + 512].rearrange("(c p) q -> p c q", p=128)), writes=[b_z2T])
                    P.dma("sp", lambda e: e.dma_start(out=sga[:], in_=Gs[0:1024, c0:c0 + 512].rearrange("(c p) q -> p c q", p=128)), writes=[b_sga])
                    P.dma("sp", lambda e: e.dma_start(out=sgh[:], in_=Gs[1024:2048, c0:c0 + 512].rearrange("(c p) q -> p c q", p=128)), writes=[b_sgh])

                def merge_compute(s_i):
                    ms = s_i % 2

                    def fchunk(fc):
                        ts = fc % 2
                        P.group("pe", [lambda e, kc=kc: e.matmul(pMa[:], lhsT=wao[:, kc, fc * 128:(fc + 1) * 128], rhs=atM[:, kc, :], start=(kc == 0), stop=(kc == 3))
                                       for kc in range(4)], reads=[b_w3p, b_at], writes=[b_pMa])
                        P.group("pe", [lambda e, kc=kc: e.matmul(pMh[:], lhsT=who[:, kc, fc * 128:(fc + 1) * 128], rhs=z2T[:, kc, :], start=(kc == 0), stop=(kc == 3))
                                       for kc in range(4)], reads=[b_w3p, b_z2T], writes=[b_pMh])
                        P.op("dve", lambda e: e.tensor_tensor(out=t1[ts][:], in0=pMa[:], in1=sga[:, fc, :], op=ALU.mult), reads=[b_pMa, b_sga], writes=[b_t1[ts]])
                        P.op("dve", lambda e: e.tensor_tensor(out=t2[ts][:], in0=pMh[:], in1=sgh[:, fc, :], op=ALU.mult), reads=[b_pMh, b_sgh], writes=[b_t2[ts]])
                        P.op("dve", lambda e: e.tensor_tensor(out=mrg[ms][:, fc, :], in0=t1[ts][:], in1=t2[ts][:], op=ALU.add),
                             reads=[b_t1[ts], b_t2[ts]], writes=[b_mrg[ms]])
                    for fc in range(8):
                        fchunk(fc)

                def tile_A(s_i, t):
                    ms = s_i % 2
                    i = s_i * 4 + t
                    xs_ = i % 2
                    P.dma("sp", lambda e: e.dma_start(out=xt[xs_][:], in_=xcat[i * 128:(i + 1) * 128, :]), writes=[b_xt[xs_]])
                    fns = []
                    for nh in range(2):
                        for kc in range(8):
                            fns.append(lambda e, nh=nh, kc=kc: e.matmul(pYt[:, nh * 512:(nh + 1) * 512], lhsT=mrg[ms][:, kc, t * 128:(t + 1) * 128],
                                                                      rhs=wout[:, kc, nh * 512:(nh + 1) * 512], start=(kc == 0), stop=(kc == 7)))
                    P.group("pe", fns, reads=[b_mrg[ms], b_w3p], writes=[b_pYt])
                    P.op("dve", lambda e: e.tensor_tensor(out=rr[xs_][:], in0=pYt[:], in1=bc[:, 0, :], op=ALU.mult), reads=[b_pYt, b_bc], writes=[b_rr[xs_]])
                    P.op("dve", lambda e: e.scalar_tensor_tensor(out=rr[xs_][:], in0=xt[xs_][:], scalar=DN_ALPHA, in1=rr[xs_][:], op0=ALU.mult, op1=ALU.add),
                         reads=[b_xt[xs_], b_rr[xs_]], writes=[b_rr[xs_]])
                    layer_norm((stats, mv, b_stat), rr[xs_], b_rr[xs_], bc[:, 1, :], bc[:, 2, :], b_bc, x1s[xs_][:], b_x1s[xs_], eng2="dve", lnexp=True)
                    P.dma("act", lambda e: e.dma_start(out=X1[i * 128:(i + 1) * 128, :], in_=x1s[xs_][:]), reads=[b_x1s[xs_]])

                def tile_B(s_i, t):
                    i = s_i * 4 + t
                    xs_ = i % 2
                    route_tile(i, x1s[xs_], b_x1s[xs_], xs_)

                merge_loads(0)
                merge_compute(0)
                for s_i in range(8):
                    nx = s_i + 1 < 8
                    if nx:
                        merge_loads(s_i + 1)
                    tile_A(s_i, 0)
                    tile_A(s_i, 1)
                    tile_B(s_i, 0)
                    if nx:
                        merge_compute(s_i + 1)
                    tile_A(s_i, 2)
                    tile_B(s_i, 1)
                    tile_A(s_i, 3)
                    tile_B(s_i, 2)
                    tile_B(s_i, 3)

                ci = SB(st, "cnt_i", [128, 64], I32); b_ci = Buf()
                psz = SB(st, "psz", [128, 64], F32); pends = SB(st, "pends", [128, 64], F32); poffs = SB(st, "poffs", [128, 64], F32)
                zer = SB(st, "zer", [128, 64], F32); b_pz = Buf()
                P.op("dve", lambda e: e.tensor_scalar(out=psz[:], in0=rcarry[:], scalar1=127.0, scalar2=None, op0=ALU.add), reads=[b_rcarry], writes=[b_pz])
                P.op("dve", lambda e: e.tensor_copy(out=ci[:], in_=psz[:]), reads=[b_pz], writes=[b_ci])
                P.op("dve", lambda e: e.tensor_scalar(out=ci[:], in0=ci[:], scalar1=7, scalar2=7, op0=ALU.arith_shift_right, op1=ALU.logical_shift_left),
                     reads=[b_ci], writes=[b_ci])
                P.op("dve", lambda e: e.tensor_copy(out=psz[:], in_=ci[:]), reads=[b_ci], writes=[b_pz])
                P.op("dve", lambda e: e.memset(zer[:], 0.0), writes=[b_pz])
                P.op("dve", lambda e: e.tensor_tensor_scan(out=pends[:], data0=psz[:], data1=zer[:], initial=0.0, op0=ALU.add, op1=ALU.add), reads=[b_pz], writes=[b_pz])
                P.op("dve", lambda e: e.tensor_tensor(out=poffs[:], in0=pends[:], in1=psz[:], op=ALU.subtract), reads=[b_pz], writes=[b_pz])
                big = SB(st, "big", [128, 64, 64], F32); b_big = Buf()
                dsf = SB(st, "dsf", [128, 64], F32); b_dsf = Buf()
                P.op("dve", lambda e: e.tensor_tensor(out=big[:], in0=ohall[:].rearrange("p i k e -> p (i k) e"),
                                                      in1=poffs[:, :].unsqueeze(1).to_broadcast([128, 64, 64]), op=ALU.mult), reads=[b_oh, b_pz], writes=[b_big])
                P.op("dve", lambda e: e.tensor_reduce(out=dsf[:], in_=big[:], axis=AX.X, op=ALU.add), reads=[b_big], writes=[b_dsf])
                P.op("dve", lambda e: e.tensor_tensor(out=dsf[:], in0=dsf[:], in1=rnk[:].rearrange("p i k -> p (i k)"), op=ALU.add), reads=[b_dsf, b_rnk], writes=[b_dsf])
                P.op("dve", lambda e: e.tensor_copy(out=dest[:].rearrange("p i k -> p (i k)"), in_=dsf[:]), reads=[b_dsf], writes=[b_dest])
                blke = SB(st, "blke", [128, 128], F32); b_blke = Buf()

                def blk_chunk(jc):
                    bigv = big[:, 0:32, :]
                    P.op("dve", lambda e: e.tensor_tensor(out=bigv, in0=pends[:, :].unsqueeze(1).to_broadcast([128, 32, 64]),
                                                          in1=tf[:, 400 + jc * 32:400 + (jc + 1) * 32].unsqueeze(2).to_broadcast([128, 32, 64]), op=ALU.is_le),
                         reads=[b_pz, b_tf, b_dsf], writes=[b_big])
                    P.op("dve", lambda e: e.tensor_reduce(out=blke[:, jc * 32:(jc + 1) * 32], in_=bigv, axis=AX.X, op=ALU.add), reads=[b_big], writes=[b_blke])
                for jc in range(4):
                    blk_chunk(jc)
                skipf = SB(st, "skipf", [128, 128], F32); b_skipf = Buf()
                P.op("dve", lambda e: e.memset(skipf[:, 0:3], 0.0), writes=[b_skipf])
                P.op("dve", lambda e: e.tensor_scalar(out=blke[:], in0=blke[:], scalar1=63.0, scalar2=None, op0=ALU.min), reads=[b_blke], writes=[b_blke])
                P.op("dve", lambda e: e.tensor_tensor(out=skipf[:, 3:128], in0=blke[:, 3:128], in1=blke[:, 0:125], op=ALU.is_equal), reads=[b_blke, b_skipf], writes=[b_skipf])
                P.op("dve", lambda e: e.tensor_scalar(out=blke[:], in0=blke[:], scalar1=128.0, scalar2=None, op0=ALU.mult), reads=[b_blke, b_skipf], writes=[b_blke])
                P.op("dve", lambda e: e.scalar_tensor_tensor(out=blke[:], in0=skipf[:], scalar=1000000.0, in1=blke[:], op0=ALU.mult, op1=ALU.add),
                     reads=[b_blke, b_skipf], writes=[b_blke])
                P.op("dve", lambda e: e.tensor_scalar(out=blke[:], in0=blke[:], scalar1=tf[:, 384:385], scalar2=None, op0=ALU.add), reads=[b_blke, b_tf], writes=[b_blke])
                P.op("dve", lambda e: e.tensor_copy(out=widx[:, :, 0], in_=blke[:]), reads=[b_blke], writes=[b_widx])
                P.op("dve", lambda e: e.tensor_scalar(out=blke[:], in0=blke[:], scalar1=128.0, scalar2=None, op0=ALU.add), reads=[b_blke, b_widx], writes=[b_blke])
                P.op("dve", lambda e: e.tensor_copy(out=widx[:, :, 1], in_=blke[:]), reads=[b_blke], writes=[b_widx])
                if debug in ("p3", "p4"):
                    dbgrt = dscr("dbgrt", [128, 64 + 64 + 256], F32)
                    dbt = SB(st, "dbt", [128, 384], F32); b_dbt = Buf()
                    P.op("dve", lambda e: e.tensor_copy(out=dbt[:, 0:64], in_=dest[:].rearrange("p i k -> p (i k)")), reads=[b_dest], writes=[b_dbt])
                    P.op("dve", lambda e: e.tensor_copy(out=dbt[:, 64:128], in_=wts[:].rearrange("p i k -> p (i k)")), reads=[b_wts], writes=[b_dbt])
                    P.op("dve", lambda e: e.tensor_copy(out=dbt[:, 128:384], in_=widx[:].rearrange("p j h -> p (j h)")), reads=[b_widx], writes=[b_dbt])
                    P.dma("sp", lambda e: e.dma_start(out=dbgrt[:, :], in_=dbt[:]), reads=[b_dbt])
                P.barrier()
            if debug == "p3":
                return _finish(nc, P, out, gst)

            with ExitStack() as st:
                hrow = [SB(st, "hrow%d" % i, [128, D], BF16) for i in range(3)]; b_hrow = [Buf() for _ in range(3)]

                def scat(i):
                    s = i % 3
                    P.dma("sp", lambda e: e.dma_start(out=hrow[s][:], in_=H2[i * 128:(i + 1) * 128, :]), writes=[b_hrow[s]])
                    for k in range(2):
                        P.dma("pool", lambda e, k=k: e.indirect_dma_start(out=XB[:, :], out_offset=bass.IndirectOffsetOnAxis(ap=dest[:, i, k:k + 1], axis=0),
                                                                          in_=hrow[s][:], in_offset=None),
                              reads=[b_hrow[s], b_dest])
                for i in range(32):
                    scat(i)
                P.barrier()

            with ExitStack() as st:
                w1s = [SB(st, "w1s%d" % i, [128, 2, 2048], BF16) for i in range(3)]
                w3s = [SB(st, "w3s%d" % i, [128, 2, 2048], BF16) for i in range(3)]
                w2s = [SB(st, "w2s%d" % i, [128, 2, 2048], BF16) for i in range(3)]
                b_w1s = [Buf() for _ in range(3)]; b_w3s = [Buf() for _ in range(3)]; b_w2s = [Buf() for _ in range(3)]
                xbt = [SB(st, "xbt%d" % i, [128, D], BF16) for i in range(3)]; b_xbt = [Buf() for _ in range(3)]
                xbT = [SB(st, "xbT%d" % i, [128, 8, 128], BF16) for i in range(2)]; b_xbT = [Buf(), Buf()]
                slu = SB(st, "slu", [128, 512], F32); b_slu = Buf()
                ggb = SB(st, "ggb", [128, 512], BF16); b_ggb = Buf()
                gTb = SB(st, "gTb", [128, 4, 128], BF16); b_gTb = Buf()
                ybt = [SB(st, "ybt%d" % i, [128, D], BF16) for i in range(2)]; b_ybt = [Buf(), Buf()]
                pXT = PS(st, "pXT", [128, 1024], BF16); b_pXT = Buf()
                pH1 = PS(st, "pE1", [128, 512]); pH3 = PS(st, "pE3", [128, 512]); b_pH1 = Buf(); b_pH3 = Buf()
                pGT = PS(st, "pGT", [128, 1024], BF16); b_pGT = Buf()
                pO2 = PS(st, "pE2", [128, 1024]); b_pO2 = Buf()

                _bcr = {}

                def _bc_reg(e):
                    if "r" not in _bcr:
                        _bcr["r"] = e.to_reg(ne // 2 - 1)
                    return _bcr["r"]

                NW = 3

                def stG(j):
                    s = j % NW
                    for (wsb, wdram, bw) in ((w1s, EWB[0], b_w1s), (w3s, EWB[1], b_w3s), (w2s, EWB[2], b_w2s)):
                        P.dma("pool", lambda e, wsb=wsb, wdram=wdram: e.indirect_dma_start(
                            out=wsb[s][:, :, :].rearrange("p h n -> p (h n)"), out_offset=None, in_=wdram[:, :],
                            in_offset=bass.IndirectOffsetOnAxis(ap=widx[:, j, 0:1], axis=0), bounds_check=_bc_reg(e), oob_is_err=False),
                            reads=[b_widx], writes=[bw[s]])
                    x3 = j % 3
                    P.dma("sp", lambda e: e.dma_start(out=xbt[x3][:], in_=XB[j * 128:(j + 1) * 128, :]), writes=[b_xbt[x3]])

                def stAT(j):
                    x2 = j % 2
                    x3 = j % 3
                    pXTv = pXT[:, :].rearrange("p (c q) -> p c q", c=8)
                    P.group("pe", [lambda e, kc=kc: e.transpose(out=pXTv[:, kc, :], in_=xbt[x3][:, kc * 128:(kc + 1) * 128], identity=identb) for kc in range(8)],
                            reads=[b_xbt[x3], b_tb], writes=[b_pXT])
                    P.op("dve", lambda e: e.tensor_copy(out=xbT[x2][:], in_=pXTv), reads=[b_pXT], writes=[b_xbT[x2]])

                def stBM(j):
                    s = j % NW
                    x2 = j % 2
                    P.group("pe", [lambda e, kc=kc: e.matmul(pH1[:], lhsT=xbT[x2][:, kc, :], rhs=w1s[s][:, kc // 4, (kc % 4) * 512:(kc % 4 + 1) * 512],
                                                             start=(kc == 0), stop=(kc == 7)) for kc in range(8)],
                            reads=[b_xbT[x2], b_w1s[s]], writes=[b_pH1])
                    P.group("pe", [lambda e, kc=kc: e.matmul(pH3[:], lhsT=xbT[x2][:, kc, :], rhs=w3s[s][:, kc // 4, (kc % 4) * 512:(kc % 4 + 1) * 512],
                                                             start=(kc == 0), stop=(kc == 7)) for kc in range(8)],
                            reads=[b_xbT[x2], b_w3s[s]], writes=[b_pH3])
                    P.op("act", lambda e: e.activation(out=slu[:], in_=pH1[:], func=AF.Silu), reads=[b_pH1], writes=[b_slu])
                    P.op("dve", lambda e: e.tensor_tensor(out=ggb[:], in0=pH3[:], in1=slu[:], op=ALU.mult), reads=[b_pH3, b_slu], writes=[b_ggb])

                def stCT(j):
                    pGTv = pGT[:, 0:512].rearrange("p (c q) -> p c q", c=4)
                    P.group("pe", [lambda e, fc=fc: e.transpose(out=pGTv[:, fc, :], in_=ggb[:, fc * 128:(fc + 1) * 128], identity=identb) for fc in range(4)],
                            reads=[b_ggb, b_tb], writes=[b_pGT])
                    P.op("act", lambda e: e.copy(out=gTb[:], in_=pGTv), reads=[b_pGT], writes=[b_gTb])

                def stDM(j):
                    s = j % NW
                    y2 = j % 2
                    fns = []
                    for nh in range(2):
                        for fc in range(4):
                            fns.append(lambda e, nh=nh, fc=fc: e.matmul(pO2[:, nh * 512:(nh + 1) * 512], lhsT=gTb[:, fc, :],
                                                                      rhs=w2s[s][:, fc // 2, (fc % 2) * 1024 + nh * 512:(fc % 2) * 1024 + (nh + 1) * 512],
                                                                      start=(fc == 0), stop=(fc == 3)))
                    P.group("pe", fns, reads=[b_gTb, b_w2s[s]], writes=[b_pO2])
                    P.op("act", lambda e: e.copy(out=ybt[y2][:, 0:512], in_=pO2[:, 0:512]), reads=[b_pO2], writes=[b_ybt[y2]])
                    P.op("dve", lambda e: e.tensor_copy(out=ybt[y2][:, 512:1024], in_=pO2[:, 512:1024]), reads=[b_pO2], writes=[b_ybt[y2]])
                    P.dma("act", lambda e: e.dma_start(out=YB[j * 128:(j + 1) * 128, :], in_=ybt[y2][:]), reads=[b_ybt[y2]])

                for j in range(3):
                    stG(j)
                stAT(0); stAT(1); stBM(0)
                for j in range(NBLK):
                    stCT(j)
                    if j + 1 < NBLK:
                        stBM(j + 1)
                    if j + 2 < NBLK:
                        stAT(j + 2)
                    stDM(j)
                    if j + 3 < NBLK:
                        stG(j + 3)
                P.barrier()
            if debug == "p4":
                return _finish(nc, P, out, gst)

            with ExitStack() as st:
                bc2 = SB(st, "bc2", [128, 3, D], F32); b_bc2 = Buf()
                for k, srcap in enumerate((mod_scr[0:1, 5120:6144], ln2g[0:1, :], ln2b[0:1, :])):
                    P.dma("sp", lambda e, k=k, srcap=srcap: e.dma_start(out=bc2[:, k, :], in_=srcap.partition_broadcast(128)), writes=[b_bc2])
                NR = 3
                ra = [SB(st, "ra%d" % i, [128, D], F32) for i in range(NR)]; rb = [SB(st, "rb%d" % i, [128, D], BF16) for i in range(NR)]
                rab = [SB(st, "rab%d" % i, [128, D], BF16) for i in range(NR)]
                b_ra = [Buf() for _ in range(NR)]; b_rb = [Buf() for _ in range(NR)]
                x1c = [SB(st, "x1c%d" % i, [128, D], F32) for i in range(NR)]; b_x1c = [Buf() for _ in range(NR)]
                oc_ = [SB(st, "oc%d" % i, [128, D], F32) for i in range(2)]; b_oc = [Buf(), Buf()]
                stats2 = SB(st, "stats2", [128, 2, 6], F32); mv2 = SB(st, "mv2", [128, 4], F32); b_stat2 = Buf()

                def comb_load(i):
                    s = i % NR
                    P.dma("pool", lambda e: e.indirect_dma_start(out=rab[s][:], out_offset=None, in_=YB[:, :],
                                                                 in_offset=bass.IndirectOffsetOnAxis(ap=dest[:, i, 0:1], axis=0)), reads=[b_dest], writes=[b_ra[s]])
                    P.dma("pool", lambda e: e.indirect_dma_start(out=rb[s][:], out_offset=None, in_=YB[:, :],
                                                                 in_offset=bass.IndirectOffsetOnAxis(ap=dest[:, i, 1:2], axis=0)), reads=[b_dest], writes=[b_rb[s]])
                    P.dma("sp", lambda e: e.dma_start(out=x1c[s][:], in_=X1[i * 128:(i + 1) * 128, :]), writes=[b_x1c[s]])

                def comb(i):
                    s = i % NR
                    so = i % 2
                    P.op("act", lambda e: e.activation(out=ra[s][:], in_=rab[s][:], func=AF.Identity, scale=wts[:, i, 0:1]), reads=[b_ra[s], b_wts], writes=[b_ra[s]])
                    P.op("dve", lambda e: e.scalar_tensor_tensor(out=ra[s][:], in0=rb[s][:], scalar=wts[:, i, 1:2], in1=ra[s][:], op0=ALU.mult, op1=ALU.add),
                         reads=[b_rb[s], b_ra[s], b_wts], writes=[b_ra[s]])
                    P.op("dve", lambda e: e.tensor_tensor(out=ra[s][:], in0=ra[s][:], in1=bc2[:, 0, :], op=ALU.mult), reads=[b_ra[s], b_bc2], writes=[b_ra[s]])
                    P.op("dve", lambda e: e.scalar_tensor_tensor(out=ra[s][:], in0=x1c[s][:], scalar=DN_ALPHA, in1=ra[s][:], op0=ALU.mult, op1=ALU.add),
                         reads=[b_x1c[s], b_ra[s]], writes=[b_ra[s]])
                    layer_norm((stats2, mv2, b_stat2), ra[s], b_ra[s], bc2[:, 1, :], bc2[:, 2, :], b_bc2, oc_[so][:], b_oc[so], eng2="dve")
                    P.dma("sp", lambda e: e.dma_start(out=out[i * 128:(i + 1) * 128, :], in_=oc_[so][:]), reads=[b_oc[so]])
                comb_load(0); comb_load(1)
                for i in range(32):
                    if i + 2 < 32:
                        comb_load(i + 2)
                    comb(i)
    return _finish(nc, P, out, gst)


def _finish(nc, P, out, gst):
    P.barrier()
    P.emit()
    return nc


def make_in_maps(inp):
    f32 = np.float32
    x = np.asarray(inp["x"], f32)
    c = np.asarray(inp["c"], f32)
    perm = q_perm()
    w_in = np.ascontiguousarray(np.asarray(inp["w_in"], f32)[0][:, perm])
    conv_w = np.asarray(inp["conv_w"], f32)[0]
    conv_b = np.asarray(inp["conv_b"], f32)[0]
    cwl = np.ascontiguousarray(conv_w.reshape(3, 12, 128).transpose(2, 0, 1))
    cbl = np.ascontiguousarray(conv_b.reshape(12, 128).T)

    def ewl(w, kchunks):
        E, K, N = w.shape
        return np.ascontiguousarray(w.reshape(E, 2, kchunks, 128, N).transpose(0, 1, 3, 2, 4).reshape(E * 2 * 128, kchunks * N))

    ew1 = ewl(np.asarray(inp["exp_w1"], f32)[0], 4)
    ew3 = ewl(np.asarray(inp["exp_w3"], f32)[0], 4)
    ew2 = ewl(np.asarray(inp["exp_w2"], f32)[0], 2)
    wrr = np.ascontiguousarray(np.concatenate([np.asarray(inp["router_group_w"], f32)[0], np.asarray(inp["router_expert_w"], f32)[0]], axis=1))
    brr = np.ascontiguousarray(np.concatenate([np.asarray(inp["router_group_b"], f32)[0], np.asarray(inp["router_expert_b"], f32)[0]])[None, :])
    shared = {
        "w_ada": np.asarray(inp["w_ada"], f32)[0], "b_ada": np.asarray(inp["b_ada"], f32)[0][None, :],
        "w_in": w_in, "conv_w": cwl, "conv_b": cbl,
        "fw1": np.asarray(inp["filt_w1"], f32)[0], "fb1": np.asarray(inp["filt_b1"], f32)[0][:, None],
        "ff1": np.asarray(inp["filt_freq1"], f32)[0][:, None],
        "fw2": np.asarray(inp["filt_w2"], f32)[0], "fb2": np.asarray(inp["filt_b2"], f32)[0][:, None],
        "ff2": np.asarray(inp["filt_freq2"], f32)[0][:, None],
        "fw3": np.asarray(inp["filt_w3"], f32)[0], "fdec": np.asarray(inp["filt_decay"], f32)[0][None, :],
        "hskip": np.asarray(inp["hy_skip"], f32)[0].reshape(1, 1024),
        "w_hy_o": np.asarray(inp["w_hy_o"], f32)[0], "w_attn_o": np.asarray(inp["w_attn_o"], f32)[0],
        "sink": np.asarray(inp["attn_sink"], f32)[0][None, :], "w_out": np.asarray(inp["w_out"], f32)[0],
        "ln1g": np.asarray(inp["ln1_g"], f32)[0][None, :], "ln1b": np.asarray(inp["ln1_b"], f32)[0][None, :],
        "ln2g": np.asarray(inp["ln2_g"], f32)[0][None, :], "ln2b": np.asarray(inp["ln2_b"], f32)[0][None, :],
        "wr": wrr, "br": brr, "ew1": ew1, "ew3": ew3, "ew2": ew2,
    }
    bnd = np.zeros((33, 4), f32)
    bv = np.linspace(1e-4, 15.0, 16, dtype=f32)
    bnd[0, 0] = 0.0; bnd[0, 1] = 0.5
    bnd[1:17, 0] = bv / L; bnd[1:17, 1] = 0.25 + 0.5
    bnd[17:33, 0] = bv / L; bnd[17:33, 1] = 0.5 + 0.5
    shared["bands"] = bnd
    shared = {k: np.ascontiguousarray(v) for k, v in shared.items()}
    consts = [host_constants(0), host_constants(1)]
    maps = []
    for i in range(NCORES):
        b, half = i // 2, i % 2
        own = x[b, half * TOWN:(half + 1) * TOWN]
        oth = x[b, (1 - half) * TOWN:(2 - half) * TOWN]
        m = dict(shared)
        m["xcat"] = np.ascontiguousarray(np.concatenate([own, oth], axis=0))
        m["cb"] = np.ascontiguousarray(c[b].reshape(8, 128).T)
        m["tabs_b"], m["tabs_f"] = consts[half]
        maps.append(m)
    return maps


_NC_CACHE = {}


def kernel(**inputs):
    if "nc" not in _NC_CACHE:
        _NC_CACHE["nc"] = build_program()
    nc = _NC_CACHE["nc"]
    maps = make_in_maps(inputs)
    res = run_bass_kernel_spmd(nc, maps, core_ids=list(range(NCORES)))
    outp = np.zeros((4, SEQ, D), np.float32)
    for i in range(NCORES):
        b, half = i // 2, i % 2
        outp[b, half * TOWN:(half + 1) * TOWN] = res.results[i]["out"]
    return outp
```
